# Optimizing a Trainium2 kernel written in Bass

```python
import math
import jax
import jax.numpy as jnp
from jax import lax
import numpy as np

D_MODEL = 1024
BATCH = 16
SEQ = 2048
DEPTH = 4

GRID_W = 64
CTX_LEN = 256
N_MIXERS = 4
ALPHA = (2 * DEPTH) ** 0.25
BETA = (8 * DEPTH) ** -0.25
LN_EPS = 1e-5
ROPE_BASE = 10000.0

GDN_HEADS = 8
GDN_DK = D_MODEL // GDN_HEADS
GDN_DV = D_MODEL // GDN_HEADS
GDN_CONV = 3
GDN_CHUNK = 64
GDN_IN = 2 * GDN_HEADS * GDN_DK + 2 * GDN_HEADS * GDN_DV + 4 * GDN_HEADS

MLSTM_HEADS = 4
MLSTM_DQK = D_MODEL // (2 * MLSTM_HEADS)
MLSTM_DV = D_MODEL // MLSTM_HEADS
MLSTM_CHUNK = 64
MLSTM_IN = 2 * MLSTM_HEADS * MLSTM_DQK + 2 * MLSTM_HEADS * MLSTM_DV + 4 * MLSTM_HEADS

RWKV_HEAD = 64
RWKV_HEADS = D_MODEL // RWKV_HEAD
RWKV_DECAY_LORA = 64
RWKV_AAA_LORA = 64
RWKV_GATE_LORA = 128
RWKV_GN_EPS = 64e-5

NA_HEADS = 16
NA_DH = D_MODEL // NA_HEADS
NA_WIN_ROWS = 8
NA_WIN_COLS = 16
NA_COL_BLOCK = 16
NA_BAND = NA_COL_BLOCK + NA_WIN_COLS

N_EXPERTS = 16
N_GROUPS = 4
TOP_K = 2
D_EXPERT = 512

kernel_name = 'hybrid_flow_backbone_gdn_mlstm_rwkv7_natten_moe'

F32 = jnp.float32


def _heads(t, n_heads):
    b, t_len, hd = t.shape
    return t.reshape(b, t_len, n_heads, hd // n_heads).transpose(0, 2, 1, 3)


def _merge(t):
    b, h, t_len, d = t.shape
    return t.transpose(0, 2, 1, 3).reshape(b, t_len, h * d)


def layer_norm(x, g, b):
    xf = x.astype(F32)
    mu = jnp.mean(xf, axis=-1, keepdims=True)
    var = jnp.mean(jnp.square(xf - mu), axis=-1, keepdims=True)
    return ((xf - mu) * lax.rsqrt(var + LN_EPS) * g.astype(F32) + b.astype(F32)).astype(x.dtype)


def head_norm(t, eps):
    mu = jnp.mean(t, axis=-1, keepdims=True)
    var = jnp.mean(jnp.square(t - mu), axis=-1, keepdims=True)
    return (t - mu) * lax.rsqrt(var + eps)


def head_rms(t, eps=1e-6):
    return t * lax.rsqrt(jnp.mean(t * t, axis=-1, keepdims=True) + eps)


def l2norm(t, eps=1e-6):
    return t * lax.rsqrt(jnp.sum(t * t, axis=-1, keepdims=True) + eps)


def axial_rope(t):
    t_len, dh = t.shape[-2], t.shape[-1]
    half = dh // 2
    quarter = half // 2
    pos = jnp.arange(t_len)
    row = (pos // GRID_W).astype(F32)
    col = (pos % GRID_W).astype(F32)
    inv_freq = ROPE_BASE ** (-jnp.arange(quarter, dtype=F32) / quarter)

    def rot(xa, p):
        ang = p[:, None] * inv_freq[None, :]
        cos, sin = jnp.cos(ang), jnp.sin(ang)
        x1, x2 = xa[..., :quarter], xa[..., quarter:]
        return jnp.concatenate([x1 * cos - x2 * sin, x1 * sin + x2 * cos], axis=-1)

    return jnp.concatenate([rot(t[..., :half], row), rot(t[..., half:], col)], axis=-1)


def centred_dwconv(x, w):
    k = w.shape[0]
    return lax.conv_general_dilated(
        x, w[:, None, :].astype(x.dtype), window_strides=(1,), padding=[(k // 2, k // 2)],
        dimension_numbers=('NWC', 'WIO', 'NWC'), feature_group_count=x.shape[-1])


def centred_token_shift(x):
    xp = jnp.pad(x, ((0, 0), (1, 1), (0, 0)))
    return 0.5 * (xp[:, :-2] + xp[:, 2:])


def _flip_t(t, axis):
    return jnp.flip(t, axis=axis)


def gdn_chunked(q, k, v, log_alpha, beta, s0):
    b, h, t_len, dk = q.shape
    dv = v.shape[-1]
    c = GDN_CHUNK
    n = t_len // c
    ch = lambda t: t.reshape(b, h, n, c, *t.shape[3:])
    q, k, v, log_alpha, beta = map(ch, (q, k, v, log_alpha, beta))
    incl = jnp.tril(jnp.ones((c, c), dtype=bool))
    strict = jnp.tril(jnp.ones((c, c), dtype=bool), -1)
    g = jnp.cumsum(log_alpha, axis=-1)
    gamma = jnp.exp(jnp.where(incl, g[..., :, None] - g[..., None, :], -jnp.inf))
    a_mat = jnp.where(strict, beta[..., :, None] * jnp.einsum('bhnid,bhnjd->bhnij', k, k) * gamma, 0.0)
    rhs = jnp.concatenate([beta[..., None] * v, (beta * jnp.exp(g))[..., None] * k], axis=-1)
    sol = lax.linalg.triangular_solve(a_mat, rhs, left_side=True, lower=True, unit_diagonal=True)
    u, w = sol[..., :dv], sol[..., dv:]
    p_mat = jnp.where(incl, jnp.einsum('bhnid,bhnjd->bhnij', q, k) * gamma, 0.0)
    q_dec = q * jnp.exp(g)[..., None]
    k_dec = k * jnp.exp(g[..., -1:] - g)[..., None]
    g_end = jnp.exp(g[..., -1])

    def step(s, xs):
        u_c, w_c, p_c, qd_c, kd_c, ge_c = xs
        delta = u_c - jnp.einsum('bhck,bhkv->bhcv', w_c, s)
        o = jnp.einsum('bhck,bhkv->bhcv', qd_c, s) + jnp.einsum('bhij,bhjv->bhiv', p_c, delta)
        s = ge_c[..., None, None] * s + jnp.einsum('bhck,bhcv->bhkv', kd_c, delta)
        return s, o

    xs = tuple(jnp.moveaxis(t, 2, 0) for t in (u, w, p_mat, q_dec, k_dec, g_end))
    s, o = lax.scan(step, s0, xs)
    return jnp.moveaxis(o, 0, 2).reshape(b, h, t_len, dv), s


def gdn_mixer(h_lat, h_ctx, w_in, conv_w, a_log, dt_bias, norm_g, w_out):
    wq = GDN_HEADS * GDN_DK
    wv = GDN_HEADS * GDN_DV

    def prep(h, rope):
        b, t_len, _ = h.shape
        z = h @ w_in
        qkv = jax.nn.silu(centred_dwconv(z[..., :2 * wq + wv], conv_w)).astype(F32)
        q = l2norm(_heads(qkv[..., :wq], GDN_HEADS))
        k = l2norm(_heads(qkv[..., wq:2 * wq], GDN_HEADS))
        v = _heads(qkv[..., 2 * wq:], GDN_HEADS)
        if rope:
            q, k = axial_rope(q), axial_rope(k)
        gate = z[..., 2 * wq + wv:2 * wq + 2 * wv]
        ab = z[..., 2 * wq + 2 * wv:].astype(F32).reshape(b, t_len, 2, 2, GDN_HEADS).transpose(2, 3, 0, 4, 1)
        log_alpha = -jnp.exp(a_log.astype(F32))[:, None, :, None] * jax.nn.softplus(
            ab[:, 0] + dt_bias.astype(F32)[:, None, :, None])
        beta = jax.nn.sigmoid(ab[:, 1])
        return q * GDN_DK ** -0.5, k, v, gate, log_alpha, beta

    qc, kc, vc, gc, lac, bc = prep(h_ctx, False)
    ql, kl, vl, gl, lal, bl = prep(h_lat, True)
    s0 = jnp.zeros((h_lat.shape[0], GDN_HEADS, GDN_DK, GDN_DV), F32)
    fl = lambda t: _flip_t(t, 2)
    oc_f, sc_f = gdn_chunked(qc, kc, vc, lac[0], bc[0], s0)
    ol_f, _ = gdn_chunked(ql, kl, vl, lal[0], bl[0], sc_f)
    oc_b, sc_b = gdn_chunked(fl(qc), fl(kc), fl(vc), fl(lac[1]), fl(bc[1]), s0)
    ol_b, _ = gdn_chunked(fl(ql), fl(kl), fl(vl), fl(lal[1]), fl(bl[1]), sc_b)

    def out(o, gate, like):
        y = _merge(head_rms(o) * norm_g.astype(F32)) * jax.nn.silu(gate.astype(F32))
        return y.astype(like.dtype) @ w_out

    return out(ol_f + fl(ol_b), gl, h_lat), out(oc_f + fl(oc_b), gc, h_ctx)


def mlstm_chunked(q, k, v, i_pre, log_f, state):
    b, h, t_len, dqk = q.shape
    c = MLSTM_CHUNK
    n = t_len // c
    ch = lambda t: t.reshape(b, h, n, c, *t.shape[3:])
    q, k, v, i_pre, log_f = map(ch, (q, k, v, i_pre, log_f))
    incl = jnp.tril(jnp.ones((c, c), dtype=bool))
    bcum = jnp.cumsum(log_f, axis=-1)
    log_d = jnp.where(incl, bcum[..., :, None] - bcum[..., None, :] + i_pre[..., None, :], -jnp.inf)
    m_intra = jnp.max(log_d, axis=-1)
    qk = jnp.einsum('bhnid,bhnjd->bhnij', q, k)
    log_end = bcum[..., -1:] - bcum + i_pre
    m_end = jnp.max(log_end, axis=-1)
    b_last = bcum[..., -1]

    def step(carry, xs):
        c_st, n_st, m_st = carry
        q_c, k_c, v_c, b_c, ld_c, mi_c, qk_c, le_c, me_c, bl_c = xs
        m_row = jnp.maximum(b_c + m_st[..., None], mi_c)
        w_intra = jnp.exp(ld_c - m_row[..., None]) * qk_c
        w_state = jnp.exp(b_c + m_st[..., None] - m_row)
        num = w_state[..., None] * jnp.einsum('bhck,bhkv->bhcv', q_c, c_st) + jnp.einsum('bhij,bhjv->bhiv', w_intra, v_c)
        den = w_state * jnp.einsum('bhck,bhk->bhc', q_c, n_st) + jnp.sum(w_intra, axis=-1)
        h_c = num / jnp.maximum(jnp.abs(den), jnp.exp(-m_row))[..., None]
        m_new = jnp.maximum(bl_c + m_st, me_c)
        decay = jnp.exp(bl_c + m_st - m_new)
        k_w = k_c * jnp.exp(le_c - m_new[..., None])[..., None]
        c_st = decay[..., None, None] * c_st + jnp.einsum('bhck,bhcv->bhkv', k_w, v_c)
        n_st = decay[..., None] * n_st + jnp.sum(k_w, axis=2)
        return (c_st, n_st, m_new), h_c

    xs = tuple(jnp.moveaxis(t, 2, 0) for t in (q, k, v, bcum, log_d, m_intra, qk, log_end, m_end, b_last))
    state, hs = lax.scan(step, state, xs)
    return jnp.moveaxis(hs, 0, 2).reshape(b, h, t_len, v.shape[-1]), state


def mlstm_mixer(h_lat, h_ctx, w_in, gate_b, norm_g, w_out):
    wq = MLSTM_HEADS * MLSTM_DQK
    wv = MLSTM_HEADS * MLSTM_DV

    def prep(h, rope):
        b, t_len, _ = h.shape
        z = h @ w_in
        q = _heads(z[..., :wq].astype(F32), MLSTM_HEADS)
        k = _heads(z[..., wq:2 * wq].astype(F32), MLSTM_HEADS)
        v = _heads(z[..., 2 * wq:2 * wq + wv].astype(F32), MLSTM_HEADS)
        o_gate = z[..., 2 * wq + wv:2 * wq + 2 * wv]
        gates = z[..., 2 * wq + 2 * wv:].astype(F32).reshape(b, t_len, 2, 2, MLSTM_HEADS).transpose(2, 3, 0, 4, 1)
        gates = gates + gate_b.astype(F32)[:, :, None, :, None]
        if rope:
            q, k = axial_rope(q), axial_rope(k)
        return q * MLSTM_DQK ** -0.5, k, v, o_gate, gates[:, 0], jax.nn.log_sigmoid(gates[:, 1])

    qc, kc, vc, oc, ic, fc = prep(h_ctx, False)
    ql, kl, vl, ol, il, fl_g = prep(h_lat, True)
    b = h_lat.shape[0]
    s0 = (jnp.zeros((b, MLSTM_HEADS, MLSTM_DQK, MLSTM_DV), F32),
          jnp.zeros((b, MLSTM_HEADS, MLSTM_DQK), F32),
          jnp.zeros((b, MLSTM_HEADS), F32))
    fl = lambda t: _flip_t(t, 2)
    hc_f, sc_f = mlstm_chunked(qc, kc, vc, ic[0], fc[0], s0)
    hl_f, _ = mlstm_chunked(ql, kl, vl, il[0], fl_g[0], sc_f)
    hc_b, sc_b = mlstm_chunked(fl(qc), fl(kc), fl(vc), fl(ic[1]), fl(fc[1]), s0)
    hl_b, _ = mlstm_chunked(fl(ql), fl(kl), fl(vl), fl(il[1]), fl(fl_g[1]), sc_b)

    def out(hsum, o_gate, like):
        y = _merge(head_norm(hsum, 1e-6)) * norm_g.astype(F32) * jax.nn.sigmoid(o_gate.astype(F32))
        return y.astype(like.dtype) @ w_out

    return out(hl_f + fl(hl_b), ol, h_lat), out(hc_f + fl(hc_b), oc, h_ctx)


def rwkv7_scan(r, log_w, k, v, kk, a, s0):
    def step(s, xs):
        r_t, lw_t, k_t, v_t, kk_t, a_t = xs
        sa = jnp.einsum('bhvk,bhk->bhv', s, -kk_t)
        s = (s * jnp.exp(lw_t)[:, :, None, :] + sa[..., None] * (kk_t * a_t)[:, :, None, :]
             + v_t[..., None] * k_t[:, :, None, :])
        return s, jnp.einsum('bhvk,bhk->bhv', s, r_t)

    xs = tuple(jnp.moveaxis(t, 1, 0) for t in (r, log_w, k, v, kk, a))
    s, y = lax.scan(step, s0, xs)
    return jnp.moveaxis(y, 0, 1), s


def rwkv_mixer(h_lat, h_ctx, mu, w_rkv, w0, w1, w2, a0, a1, a2, g1, g2, k_k, k_a, r_k, lnx_g, lnx_b, w_out):
    nh, hd = RWKV_HEADS, RWKV_HEAD

    def prep(h):
        b, t_len, _ = h.shape
        heads = lambda t: t.astype(F32).reshape(b, t_len, nh, hd)
        dx = centred_token_shift(h) - h
        xr, xw, xk, xv, xa, xg = [h + dx * mu[j] for j in range(6)]
        rkv = jnp.einsum('sbtd,sde->sbte', jnp.stack([xr, xk, xv]), w_rkv)
        r, k, v = heads(rkv[0]), heads(rkv[1]), heads(rkv[2])
        g = (jax.nn.sigmoid(xg @ g1) @ g2).astype(F32)
        kk = l2norm(k * k_k.astype(F32).reshape(nh, hd))
        dirs = []
        for d in range(2):
            w_raw = -jax.nn.softplus(-(w0[d] + jnp.tanh(xw @ w1[d]) @ w2[d])) - 0.5
            a = heads(jax.nn.sigmoid(a0[d] + (xa @ a1[d]) @ a2[d]))
            k_d = k * (1 + (a - 1) * k_a.astype(F32).reshape(nh, hd))
            dirs.append((-jnp.exp(heads(w_raw)), a, k_d))
        return r, v, kk, g, dirs

    def run(r, v, kk, dparams, s0, reverse):
        log_w, a, k_d = dparams
        if reverse:
            fl = lambda t: _flip_t(t, 1)
            y, s = rwkv7_scan(fl(r), fl(log_w), fl(k_d), fl(v), fl(kk), fl(a), s0)
            return fl(y), s
        return rwkv7_scan(r, log_w, k_d, v, kk, a, s0)

    rc, vc, kkc, gc, dc = prep(h_ctx)
    rl, vl, kkl, gl, dl = prep(h_lat)
    s0 = jnp.zeros((h_lat.shape[0], nh, hd, hd), F32)
    yc_f, sc_f = run(rc, vc, kkc, dc[0], s0, False)
    yl_f, _ = run(rl, vl, kkl, dl[0], sc_f, False)
    yc_b, sc_b = run(rc, vc, kkc, dc[1], s0, True)
    yl_b, _ = run(rl, vl, kkl, dl[1], sc_b, True)

    def out(y, r, v, g, dirs, like):
        b, t_len = y.shape[0], y.shape[1]
        yn = head_norm(y, RWKV_GN_EPS)
        bonus = sum(jnp.sum(r * k_d * r_k.astype(F32), axis=-1, keepdims=True) * v for (_, _, k_d) in dirs)
        yo = (yn.reshape(b, t_len, nh * hd) * lnx_g.astype(F32) + lnx_b.astype(F32)
              + bonus.reshape(b, t_len, nh * hd)) * g
        return yo.astype(like.dtype) @ w_out

    return out(yl_f + yl_b, rl, vl, gl, dl, h_lat), out(yc_f + yc_b, rc, vc, gc, dc, h_ctx)


def na_mixer(h_lat, h_ctx, w_in, rpb, w_out, need_ctx_out):
    nh, dh = NA_HEADS, NA_DH
    b, t_len, d_model = h_lat.shape
    rows = t_len // GRID_W
    wr = min(NA_WIN_ROWS, rows)
    n_cb = GRID_W // NA_COL_BLOCK
    scale = dh ** -0.5
    ql, kl, vl = (_heads(t, nh) for t in jnp.split(h_lat @ w_in, 3, axis=-1))
    kc, vc = (_heads(t, nh) for t in jnp.split(h_ctx @ w_in[:, d_model:], 2, axis=-1))
    grid = lambda t: t.reshape(b, nh, rows, GRID_W, dh)
    qg, kg, vg = grid(ql), grid(kl), grid(vl)
    cols = np.arange(GRID_W).reshape(n_cb, NA_COL_BLOCK)
    win_c0 = np.clip(cols - NA_WIN_COLS // 2, 0, GRID_W - NA_WIN_COLS)
    band_c0 = np.clip(np.arange(n_cb) * NA_COL_BLOCK - NA_WIN_COLS // 2, 0, GRID_W - NA_BAND)
    band_cols = band_c0[:, None] + np.arange(NA_BAND)
    col_mask = ((band_cols[:, None, :] >= win_c0[..., None])
                & (band_cols[:, None, :] < win_c0[..., None] + NA_WIN_COLS))
    dc_idx = np.clip(band_cols[:, None, :] - cols[..., None] + NA_WIN_COLS - 1, 0, 2 * NA_WIN_COLS - 2)
    rpb_c = rpb[:, :, dc_idx]

    def row_fn(r):
        r0 = jnp.clip(r - NA_WIN_ROWS // 2, 0, rows - wr)
        kb = lax.dynamic_slice_in_dim(kg, r0, wr, axis=2)[:, :, :, band_cols]
        vb = lax.dynamic_slice_in_dim(vg, r0, wr, axis=2)[:, :, :, band_cols]
        qr = lax.dynamic_index_in_dim(qg, r, axis=2, keepdims=False).reshape(b, nh, n_cb, NA_COL_BLOCK, dh)
        s_lat = jnp.einsum('bhjqd,bhijkd->bhjqik', qr, kb).astype(F32) * scale
        dr_idx = r0 + jnp.arange(wr) - r + NA_WIN_ROWS - 1
        bias = jnp.take(rpb_c, dr_idx, axis=1).transpose(0, 2, 3, 1, 4).astype(F32)
        s_lat = jnp.where(col_mask[:, :, None, :], s_lat + bias, -jnp.inf).reshape(b, nh, n_cb, NA_COL_BLOCK, wr * NA_BAND)
        s_ctx = jnp.einsum('bhjqd,bhkd->bhjqk', qr, kc).astype(F32) * scale
        pr = jax.nn.softmax(jnp.concatenate([s_lat, s_ctx], axis=-1), axis=-1).astype(vg.dtype)
        p_lat = pr[..., :wr * NA_BAND].reshape(b, nh, n_cb, NA_COL_BLOCK, wr, NA_BAND)
        o = (jnp.einsum('bhjqik,bhijkd->bhjqd', p_lat, vb)
             + jnp.einsum('bhjqk,bhkd->bhjqd', pr[..., wr * NA_BAND:], vc))
        return o.reshape(b, nh, GRID_W, dh)

    o_rows = lax.map(row_fn, jnp.arange(rows))
    o_lat = o_rows.transpose(1, 2, 0, 3, 4).reshape(b, nh, t_len, dh)
    out_lat = _merge(o_lat) @ w_out
    if not need_ctx_out:
        return out_lat, None
    qc = _heads(h_ctx @ w_in[:, :d_model], nh)
    pc = jax.nn.softmax(jnp.einsum('bhqd,bhkd->bhqk', qc, kc).astype(F32) * scale, axis=-1).astype(vc.dtype)
    return out_lat, _merge(jnp.einsum('bhqk,bhkd->bhqd', pc, vc)) @ w_out


def moe(h, router_w, router_b, w1, w3, w2):
    hf = h.astype(F32)
    probs = jax.nn.softmax(hf @ router_w.astype(F32), axis=-1)
    sel = probs + router_b.astype(F32)
    per_group = N_EXPERTS // N_GROUPS
    group_score = jnp.sum(lax.top_k(sel.reshape(*sel.shape[:-1], N_GROUPS, per_group), TOP_K)[0], axis=-1)
    best = jnp.argmax(group_score, axis=-1)
    in_group = (jnp.arange(N_EXPERTS) // per_group) == best[..., None]
    _, idx = lax.top_k(jnp.where(in_group, sel, -jnp.inf), TOP_K)
    w_sel = jnp.take_along_axis(probs, idx, axis=-1)
    w_sel = w_sel / jnp.sum(w_sel, axis=-1, keepdims=True)
    combine = jnp.sum(jax.nn.one_hot(idx, N_EXPERTS, dtype=F32) * w_sel[..., None], axis=-2)
    out = jnp.zeros_like(hf)
    for e in range(N_EXPERTS):
        hid = jax.nn.silu(h @ w1[e]) * (h @ w3[e])
        out = out + combine[..., e:e + 1] * (hid @ w2[e]).astype(F32)
    return out.astype(h.dtype)


def setup_inputs(seed: int = 0) -> dict:
    key = jax.random.key(seed)
    ks = iter(jax.random.split(key, 64))
    d = D_MODEL

    def nrm(shape, scale):
        return jax.random.normal(next(ks), shape, F32) * scale

    def unif(shape, lo, hi):
        return jax.random.uniform(next(ks), shape, F32, lo, hi)

    gdn_dt = jnp.exp(unif((2, GDN_HEADS), math.log(1e-3), math.log(1e-1)))
    return {
        'x': nrm((BATCH, SEQ, d), 1.0),
        'c': nrm((BATCH, d), 1.0),
        'ctx': nrm((BATCH, CTX_LEN, d), 1.0),
        'c_ctx': nrm((d,), 1.0),
        'ada_w': nrm((DEPTH, d, 6 * d), 0.5 * d ** -0.5),
        'ada_b': nrm((DEPTH, 6 * d), 0.02),
        'ln_g': 1.0 + nrm((DEPTH, 2, d), 0.05),
        'ln_b': nrm((DEPTH, 2, d), 0.02),
        'router_w': nrm((d, N_EXPERTS), d ** -0.5),
        'router_b': nrm((N_EXPERTS,), 0.01),
        'moe_w1': nrm((DEPTH, N_EXPERTS, d, D_EXPERT), d ** -0.5),
        'moe_w3': nrm((DEPTH, N_EXPERTS, d, D_EXPERT), d ** -0.5),
        'moe_w2': nrm((DEPTH, N_EXPERTS, D_EXPERT, d), D_EXPERT ** -0.5 * BETA),
        'gdn_w_in': nrm((d, GDN_IN), d ** -0.5),
        'gdn_conv': nrm((GDN_CONV, 2 * GDN_HEADS * GDN_DK + GDN_HEADS * GDN_DV), GDN_CONV ** -0.5),
        'gdn_a_log': jnp.log(unif((2, GDN_HEADS), 1.0, 16.0)),
        'gdn_dt_bias': gdn_dt + jnp.log(-jnp.expm1(-gdn_dt)),
        'gdn_norm_g': 1.0 + nrm((GDN_DV,), 0.05),
        'gdn_w_out': nrm((GDN_HEADS * GDN_DV, d), (GDN_HEADS * GDN_DV) ** -0.5 * BETA),
        'mlstm_w_in': nrm((d, MLSTM_IN), d ** -0.5),
        'mlstm_gate_b': jnp.stack([nrm((2, MLSTM_HEADS), 0.5), unif((2, MLSTM_HEADS), 3.0, 6.0)], axis=1),
        'mlstm_norm_g': 1.0 + nrm((MLSTM_HEADS * MLSTM_DV,), 0.05),
        'mlstm_w_out': nrm((MLSTM_HEADS * MLSTM_DV, d), (MLSTM_HEADS * MLSTM_DV) ** -0.5 * BETA),
        'rwkv_mu': unif((6, d), 0.0, 1.0),
        'rwkv_w_rkv': nrm((3, d, d), d ** -0.5),
        'rwkv_w0': unif((2, d), -6.0, 1.0),
        'rwkv_w1': nrm((2, d, RWKV_DECAY_LORA), d ** -0.5),
        'rwkv_w2': nrm((2, RWKV_DECAY_LORA, d), 0.5 * RWKV_DECAY_LORA ** -0.5),
        'rwkv_a0': nrm((2, d), 0.1),
        'rwkv_a1': nrm((2, d, RWKV_AAA_LORA), d ** -0.5),
        'rwkv_a2': nrm((2, RWKV_AAA_LORA, d), RWKV_AAA_LORA ** -0.5),
        'rwkv_g1': nrm((d, RWKV_GATE_LORA), d ** -0.5),
        'rwkv_g2': nrm((RWKV_GATE_LORA, d), RWKV_GATE_LORA ** -0.5),
        'rwkv_k_k': 0.85 + nrm((d,), 0.05),
        'rwkv_k_a': 1.0 + nrm((d,), 0.05),
        'rwkv_r_k': nrm((RWKV_HEADS, RWKV_HEAD), 0.1),
        'rwkv_lnx_g': 1.0 + nrm((d,), 0.05),
        'rwkv_lnx_b': nrm((d,), 0.02),
        'rwkv_w_out': nrm((d, d), d ** -0.5 * BETA),
        'na_w_in': nrm((d, 3 * d), d ** -0.5),
        'na_rpb': nrm((NA_HEADS, 2 * NA_WIN_ROWS - 1, 2 * NA_WIN_COLS - 1), 0.5),
        'na_w_out': nrm((d, d), d ** -0.5 * BETA),
    }


def reference(x, c, ctx, c_ctx, ada_w, ada_b, ln_g, ln_b, router_w, router_b,
              moe_w1, moe_w3, moe_w2,
              gdn_w_in, gdn_conv, gdn_a_log, gdn_dt_bias, gdn_norm_g, gdn_w_out,
              mlstm_w_in, mlstm_gate_b, mlstm_norm_g, mlstm_w_out,
              rwkv_mu, rwkv_w_rkv, rwkv_w0, rwkv_w1, rwkv_w2, rwkv_a0, rwkv_a1, rwkv_a2,
              rwkv_g1, rwkv_g2, rwkv_k_k, rwkv_k_a, rwkv_r_k, rwkv_lnx_g, rwkv_lnx_b, rwkv_w_out,
              na_w_in, na_rpb, na_w_out):
    for i in range(DEPTH):
        last = i == DEPTH - 1
        kind = i % N_MIXERS
        mod_lat = jax.nn.silu(c) @ ada_w[i] + ada_b[i]
        mod_ctx = jax.nn.silu(c_ctx) @ ada_w[i] + ada_b[i]
        sh1, sc1, g1, sh2, sc2, g2 = jnp.split(mod_lat[:, None, :], 6, axis=-1)
        csh1, csc1, cg1, csh2, csc2, cg2 = jnp.split(mod_ctx, 6, axis=-1)
        h_lat = x * (1 + sc1) + sh1
        h_ctx = ctx * (1 + csc1) + csh1
        if kind == 0:
            o_lat, o_ctx = gdn_mixer(h_lat, h_ctx, gdn_w_in, gdn_conv, gdn_a_log, gdn_dt_bias, gdn_norm_g, gdn_w_out)
        elif kind == 1:
            o_lat, o_ctx = mlstm_mixer(h_lat, h_ctx, mlstm_w_in, mlstm_gate_b, mlstm_norm_g, mlstm_w_out)
        elif kind == 2:
            o_lat, o_ctx = rwkv_mixer(h_lat, h_ctx, rwkv_mu, rwkv_w_rkv, rwkv_w0, rwkv_w1, rwkv_w2,
                                      rwkv_a0, rwkv_a1, rwkv_a2, rwkv_g1, rwkv_g2, rwkv_k_k, rwkv_k_a,
                                      rwkv_r_k, rwkv_lnx_g, rwkv_lnx_b, rwkv_w_out)
        else:
            o_lat, o_ctx = na_mixer(h_lat, h_ctx, na_w_in, na_rpb, na_w_out, not last)
        x = layer_norm(ALPHA * x + g1 * o_lat, ln_g[i, 0], ln_b[i, 0])
        f_lat = moe(x * (1 + sc2) + sh2, router_w, router_b, moe_w1[i], moe_w3[i], moe_w2[i])
        x = layer_norm(ALPHA * x + g2 * f_lat, ln_g[i, 1], ln_b[i, 1])
        if not last:
            ctx = layer_norm(ALPHA * ctx + cg1 * o_ctx, ln_g[i, 0], ln_b[i, 0])
            f_ctx = moe(ctx * (1 + csc2) + csh2, router_w, router_b, moe_w1[i], moe_w3[i], moe_w2[i])
            ctx = layer_norm(ALPHA * ctx + cg2 * f_ctx, ln_g[i, 1], ln_b[i, 1])
    return x
```

```python
import numpy as np
import concourse.bass as bass
import concourse.mybir as mybir
from concourse.bass_utils import run_bass_kernel_spmd

F32 = mybir.dt.float32
BF16 = mybir.dt.bfloat16
AF = mybir.ActivationFunctionType
ALU = mybir.AluOpType
AX = mybir.AxisListType

D = 1024
DEPTH = 4
NT = 4608
TT = 512
NTILE = NT // TT
NE = 16
DE = 512
ALPHA = float((2 * DEPTH) ** 0.25)
LN_EPS = 1e-5
NCORES = 8


class View:
    __slots__ = ("ap", "key")

    def __init__(self, ap, key):
        self.ap = ap
        self.key = key

    def bc(self, shape):
        return View(self.ap.to_broadcast(list(shape)), self.key)


class Buf:
    def __init__(self, name, t):
        self.name = name
        self.t = t

    def __getitem__(self, idx):
        return View(self.t[idx], (self.name, None))

    def k(self, sub):
        return _SubBuf(self, sub)


class _SubBuf:
    def __init__(self, buf, sub):
        self.buf = buf
        self.sub = sub

    def __getitem__(self, idx):
        return View(self.buf.t[idx], (self.buf.name, self.sub))


class Op:
    __slots__ = ("eng", "fn", "deps", "is_dma", "id", "inc", "ticket", "dsem", "dval", "is_mm")


ENGS = ("pe", "act", "dve", "pool", "sp")
NDSEM = 14


class Prog:
    def __init__(self, nc):
        self.nc = nc
        self.ops = []
        self.state = {}
        self.scopes = [[]]
        self.pending = {}
        self.last = {}
        self.open_dmas = set()

    def _enter(self, cm):
        t = cm.__enter__()
        self.scopes[-1].append(cm)
        return t

    def sb(self, name, shape, dt):
        self.uid = getattr(self, "uid", 0) + 1
        nm = "S%d_%s" % (self.uid, name)
        return Buf(nm, self._enter(self.nc.sbuf_tensor(nm, list(shape), dt)))

    def ps(self, name, shape=(128, 512), dt=F32):
        self.uid = getattr(self, "uid", 0) + 1
        nm = "P_%d_%s" % (self.uid, name)
        return Buf(nm, self._enter(self.nc.psum_tensor(nm, list(shape), dt)))

    def dram(self, name, shape, dt, kind="Internal"):
        return Buf(name, self.nc.dram_tensor(name, list(shape), dt, kind=kind).ap())

    def push(self):
        self.scopes.append([])

    def pop(self):
        for cm in reversed(self.scopes.pop()):
            cm.__exit__(None, None, None)
        bar = set(self.last.values()) | set(self.open_dmas)
        self.open_dmas = set()
        self.pending = {e: set(bar) | self.pending.get(e, set()) for e in ENGS}

    def _deps(self, key, is_write):
        name, sub = key
        st = self.state.get(name)
        if not st:
            return set()
        subs = list(st.keys()) if sub is None else [s for s in (sub, None) if s in st]
        deps = set()
        for s in subs:
            w, rs = st[s]
            if w is not None:
                deps.add(w)
            if is_write:
                deps.update(rs)
        return deps

    def _record(self, key, is_write, opid):
        name, sub = key
        st = self.state.setdefault(name, {})
        if is_write:
            if sub is None:
                st.clear()
            st[sub] = [opid, []]
        else:
            if sub not in st:
                st[sub] = [None, []]
            st[sub][1].append(opid)

    def add(self, eng, fn, writes, reads, is_dma=False, is_mm=False):
        op = Op()
        op.eng, op.fn, op.is_dma, op.is_mm = eng, fn, is_dma, is_mm
        op.inc, op.ticket, op.dsem, op.dval = False, 0, None, 0
        op.deps = self.pending.pop(eng, set())
        op.id = len(self.ops)
        reads = self._vs(*reads)
        writes = self._vs(*writes)
        for v in reads:
            dr = self._deps(v.key, False)
            op.deps |= dr
            if v.key[0].startswith("P_"):
                for x in self._deps(v.key, True) - dr:
                    if self.ops[x].eng != eng:
                        op.deps.add(x)
        for v in writes:
            op.deps |= self._deps(v.key, True)
        for v in reads:
            self._record(v.key, False, op.id)
        for v in writes:
            self._record(v.key, True, op.id)
        self.ops.append(op)
        if is_dma:
            self.open_dmas.add(op.id)
        else:
            self.last[eng] = op.id
        return op

    @staticmethod
    def _a(x):
        return x.ap if isinstance(x, View) else x

    @staticmethod
    def _vs(*xs):
        return [x for x in xs if isinstance(x, View)]

    def mm(self, out, lhsT, rhs, start=True, stop=True, **kw):
        a = self._a
        return self.add("pe", lambda e: e.matmul(a(out), a(lhsT), a(rhs), start=start, stop=stop, **kw),
                        [out], [lhsT, rhs], is_mm=True)

    def transpose(self, out, in_, ident):
        a = self._a
        return self.add("pe", lambda e: e.transpose(a(out), a(in_), a(ident)), [out], [in_, ident], is_mm=True)

    def act(self, out, in_, func, bias=0.0, scale=1.0, accum_out=None):
        a = self._a
        kw = {}
        if accum_out is not None:
            kw["accum_out"] = a(accum_out)
        return self.add("act", lambda e: e.activation(out=a(out), in_=a(in_), func=func, bias=a(bias), scale=a(scale), **kw),
                        self._vs(out, accum_out), self._vs(in_, bias, scale))

    def copy(self, eng, out, in_):
        a = self._a
        if eng == "act":
            return self.add(eng, lambda e: e.copy(out=a(out), in_=a(in_)), [out], [in_])
        return self.add(eng, lambda e: e.tensor_copy(out=a(out), in_=a(in_)), [out], [in_])

    def tt(self, eng, out, in0, in1, op):
        a = self._a
        return self.add(eng, lambda e: e.tensor_tensor(out=a(out), in0=a(in0), in1=a(in1), op=op), [out], [in0, in1])

    def ts(self, eng, out, in0, s1, s2=None, op0=ALU.mult, op1=None, accum_out=None):
        a = self._a
        kw = {}
        if op1 is not None:
            kw["op1"] = op1
        if accum_out is not None:
            kw["accum_out"] = a(accum_out)
        return self.add(eng, lambda e: e.tensor_scalar(out=a(out), in0=a(in0), scalar1=a(s1), scalar2=a(s2), op0=op0, **kw),
                        self._vs(out, accum_out), self._vs(in0, s1, s2))

    def stt(self, eng, out, in0, scalar, in1, op0, op1):
        a = self._a
        eng = "dve"
        return self.add(eng, lambda e: e.scalar_tensor_tensor(out=a(out), in0=a(in0), scalar=a(scalar), in1=a(in1), op0=op0, op1=op1),
                        [out], self._vs(in0, scalar, in1))

    def memset(self, eng, out, val):
        a = self._a
        return self.add(eng, lambda e: e.memset(a(out), val), [out], [])

    def reduce(self, eng, out, in_, op, axis=AX.X):
        a = self._a
        return self.add(eng, lambda e: e.tensor_reduce(out=a(out), in_=a(in_), axis=axis, op=op), [out], [in_])

    def recip(self, out, in_):
        a = self._a
        return self.add("dve", lambda e: e.reciprocal(out=a(out), in_=a(in_)), [out], [in_])

    def dma(self, q, out, in_, **kw):
        a = self._a
        return self.add(q, lambda e: e.dma_start(out=a(out), in_=a(in_), **kw), [out], [in_], is_dma=True)

    def emit(self):
        nc = self.nc
        ops = self.ops

        def pe_chain(op, dop):
            return dop.eng == "pe" and op.eng == "pe" and op.is_mm and dop.is_mm

        for op in ops:
            for d in op.deps:
                dop = ops[d]
                if dop.is_dma or pe_chain(op, dop):
                    continue
                dop.inc = True
        semctx = []

        def newsem(name):
            cm = nc.semaphore(name)
            s = cm.__enter__()
            semctx.append(cm)
            return s

        esem = {e: newsem("s_" + e) for e in ENGS}
        dq = ("sp", "pool", "act")
        dsems = {q: [newsem("d_%s_%d" % (q, i)) for i in range(NDSEM)] for q in dq}
        dcount = {q: [0] * NDSEM for q in dq}
        drr = {q: 0 for q in dq}
        tick = {e: 0 for e in ENGS}
        per_eng = {e: [] for e in ENGS}
        waits = {}
        seen = {e: {} for e in ENGS}
        for op in ops:
            w = []
            sn = seen[op.eng]

            def want(sem, val, key):
                if sn.get(key, 0) >= val:
                    return
                sn[key] = val
                w.append((sem, val))

            if op.is_dma:
                q = op.eng
                i = drr[q]
                drr[q] = (i + 1) % NDSEM
                if dcount[q][i] > 0:
                    want(dsems[q][i], dcount[q][i] * 16, ("d", q, i))
                dcount[q][i] += 1
                op.dsem = (q, i)
                op.dval = dcount[q][i] * 16
            for d in sorted(op.deps):
                dop = ops[d]
                if dop.is_dma:
                    q, i = dop.dsem
                    want(dsems[q][i], dop.dval, ("d", q, i))
                elif not pe_chain(op, dop):
                    want(esem[dop.eng], dop.ticket, ("e", dop.eng))
            if (not op.is_dma) and op.inc:
                tick[op.eng] += 1
                op.ticket = tick[op.eng]
            waits[op.id] = w
            per_eng[op.eng].append(op)
        self.n_waits = sum(len(w) for w in waits.values())
        final = []
        for q in dq:
            for i in range(NDSEM):
                if dcount[q][i] > 0:
                    final.append((dsems[q][i], dcount[q][i] * 16))

        def run_engine(e, name):
            for op in per_eng[name]:
                for (sem, val) in waits[op.id]:
                    e.wait_ge(sem, val)
                ins = op.fn(e)
                if op.is_dma:
                    q, i = op.dsem
                    ins.then_inc(dsems[q][i], 16)
                elif op.inc:
                    ins.then_inc(esem[name], 1)
            if name == "sp":
                for (sem, val) in final:
                    e.wait_ge(sem, val)

        with nc.Block() as block:
            @block.sync
            def _(e):
                run_engine(e, "sp")

            @block.tensor
            def _(e):
                run_engine(e, "pe")

            @block.scalar
            def _(e):
                run_engine(e, "act")

            @block.vector
            def _(e):
                run_engine(e, "dve")

            @block.gpsimd
            def _(e):
                run_engine(e, "pool")
        for cm in reversed(semctx):
            cm.__exit__(None, None, None)
        while self.scopes:
            for cm in reversed(self.scopes.pop()):
                cm.__exit__(None, None, None)


def seg_of_tile(t):
    return 2 if t == 0 else (0 if t <= 4 else 1)


def modcol(i, grp, kc, seg):
    return (i * 48 + grp * 8 + kc) * 3 + seg


class Ctx:
    pass


def phase_consts(P, C, IN):
    C.ones = P.sb("ones", [128, 128], F32)
    C.ident = P.sb("ident", [128, 128], F32)
    C.identb = P.sb("identb", [128, 128], BF16)
    C.mod = P.sb("mod", [128, DEPTH * 48 * 3], F32)
    C.lng = P.sb("lng", [128, DEPTH * 2 * 8], F32)
    C.lnb = P.sb("lnb", [128, DEPTH * 2 * 8], F32)
    C.rw = P.sb("rw", [128, 8, NE], F32)
    C.rb = P.sb("rb", [128, NE], F32)
    C.selE = P.sb("selE", [NE, NE * 128], F32)
    C.eps = P.sb("epsc", [128, 1], F32)
    P.memset("dve", C.ones[:], 1.0)
    P.memset("dve", C.eps[:], LN_EPS)
    P.dma("sp", C.ident[:], IN("ident")[:, :])
    P.copy("dve", C.identb[:], C.ident[:])
    P.dma("sp", C.lng[:], IN("lng")[:, :])
    P.dma("sp", C.lnb[:], IN("lnb")[:, :])
    P.dma("sp", C.rw[:], IN("router_w").t.rearrange("(kc p) e -> p kc e", p=128))
    P.dma("sp", C.rb[:], IN("rb")[:, :])
    P.dma("sp", C.selE[:], IN("selE")[:, :])


def phase_mod(P, C, IN, layers):
    P.push()
    cT = P.sb("cT", [128, 8, 3], F32)
    cv = P.sb("cv", [128, 8, 3], F32)
    adab = P.sb("adab", [128, DEPTH * 48 * 3], F32)
    aw = [P.sb("aw%d" % j, [128, 8, 512], F32) for j in range(2)]
    pm = [P.ps("pm%d" % j) for j in range(2)]
    P.dma("sp", cT[:], IN("cT")[:, :, :])
    P.dma("sp", adab[:], IN("adab")[:, :])
    P.act(cv[:], cT[:], AF.Silu)
    n = 0
    for i in layers:
        awv = IN("ada_w_%d" % i, [D, 6 * D]).t.rearrange("(kc p) n -> p kc n", p=128)
        for p in range(12):
            a = aw[n % 2]
            ps = pm[n % 2]
            n += 1
            P.dma("sp" if n % 2 else "pool", a[:], awv[:, :, p * 512:(p + 1) * 512])
            for f in range(4):
                for kc in range(8):
                    P.mm(ps[:, f * 3:(f + 1) * 3], a[:, kc, f * 128:(f + 1) * 128], cv[:, kc, :], start=(kc == 0), stop=(kc == 7))
            c0 = (i * 48 + p * 4) * 3
            P.tt("dve", C.mod[:, c0:c0 + 12], ps[:, 0:12], adab[:, c0:c0 + 12], ALU.add)
        for grp in (1, 4):
            c0 = (i * 48 + grp * 8) * 3
            P.ts("dve", C.mod[:, c0:c0 + 24], C.mod[:, c0:c0 + 24], 1.0, None, op0=ALU.add)
    P.pop()


def ln_tile(P, C, L, y, out, i, which, ps_a, ps_b):
    mean, rstd, sq = L.mean, L.rstd, L.sq
    for kc in range(8):
        P.mm(ps_a[:], C.ones[:], y[:, kc, :], start=(kc == 0), stop=(kc == 7))
    P.act(mean[:], ps_a[:], AF.Identity, scale=1.0 / D)
    for kc in range(8):
        P.tt("pool" if kc % 2 else "dve", y[:, kc, :], y[:, kc, :], mean[:], ALU.subtract)
        P.act(sq[:, kc, :], y[:, kc, :], AF.Square)
    for kc in range(8):
        P.mm(ps_b[:], C.ones[:], sq[:, kc, :], start=(kc == 0), stop=(kc == 7))
    P.act(rstd[:], ps_b[:], AF.Sqrt, bias=C.eps[:], scale=1.0 / D)
    P.recip(rstd[:], rstd[:])
    for kc in range(8):
        col = (i * 2 + which) * 8 + kc
        P.stt("dve", sq[:, kc, :], y[:, kc, :], C.lng[:, col:col + 1], rstd[:], ALU.mult, ALU.mult)
        P.act(out[:, kc, :], sq[:, kc, :], AF.Identity, bias=C.lnb[:, col:col + 1])


def phase_wprep(P, C, IN, i):
    P.push()
    st = [P.sb("wp_f%d" % j, [128, 8, 512], F32) for j in range(3)]
    sb = [P.sb("wp_b%d" % j, [128, 8, 512], BF16) for j in range(3)]
    n = 0
    engs = ("pool", "act", "dve")
    for e in range(NE):
        for (src, dst, pat, isw2) in ((IN("moe_w1_%d" % i, [NE, D, DE]), C.W1B, "(kc p) n -> p kc n", False),
                                      (IN("moe_w3_%d" % i, [NE, D, DE]), C.W3B, "(kc p) n -> p kc n", False),
                                      (IN("moe_w2_%d" % i, [NE, DE, D]), C.W2B, "", True)):
            j = n % 3
            n += 1
            if isw2:
                sv = src.t[e].rearrange("(kc p) (h n) -> p kc h n", p=128, h=2)
                dv = dst.t[e].rearrange("(kc p) (h n) -> p kc h n", p=128, h=2)
                stv = View(st[j].t[:].rearrange("p (kc h) n -> p kc h n", h=2), st[j][:].key)
                sbv = View(sb[j].t[:].rearrange("p (kc h) n -> p kc h n", h=2), sb[j][:].key)
            else:
                sv = src.t[e].rearrange(pat, p=128)
                dv = dst.t[e].rearrange(pat, p=128)
                stv = st[j][:]
                sbv = sb[j][:]
            P.dma("sp", stv, sv)
            P.copy(engs[j], sb[j][:], st[j][:])
            P.dma("pool", View(dv, (dst.name, e)), sbv)
    P.pop()


def phase_moe(P, C, IN, i, tiles):
    P.push()
    L = Ctx()
    xt = P.sb("m_x", [128, 8, TT], F32)
    h2f = P.sb("m_h2f", [128, 8, TT], F32)
    h2b = P.sb("m_h2b", [128, 8, TT], BF16)
    hid = P.sb("m_hid", [128, NE * 4, TT], BF16)
    wst = [P.sb("m_w%d" % j, [128, 2, 8, 512], BF16) for j in range(2)]
    w2st = [P.sb("m_w2%d" % j, [128, 4, 512], BF16) for j in range(3)]
    L.mean = P.sb("m_mean", [128, TT], F32)
    L.rstd = P.sb("m_rstd", [128, TT], F32)
    L.sq = h2f
    cbs = [P.sb("m_cb%d" % j, [128, TT], F32) for j in range(2)]
    gS = [P.sb("m_g%d" % j, [128, TT], F32) for j in range(3)]
    uS = [P.sb("m_u%d" % j, [128, TT], F32) for j in range(3)]
    combT = P.sb("m_combT", [NE, TT], F32)
    R = [dict((nm, P.sb("r_%s%d" % (nm, j), [128, w], F32)) for nm, w in
              (("lg", 16), ("e", 16), ("pr", 16), ("sel", 16), ("eq", 16), ("s2", 16), ("msk", 16), ("pw", 16), ("cmb", 16),
               ("mx", 1), ("se", 1), ("m1", 4), ("m2", 4), ("gs", 4), ("gm", 1), ("ing", 4), ("sw", 1))) for j in range(2)]
    pb = [P.ps("m_ps%d" % j) for j in range(8)]
    rr = 0
    for t in tiles:
        seg = seg_of_tile(t)
        tok = slice(t * TT, (t + 1) * TT)
        P.dma("sp", xt[:], View(C.XS.t[:, :, tok].rearrange("c p n -> p c n"), ("XS", t)))
        for kc in range(8):
            c_sc = modcol(i, 4, kc, seg)
            c_sh = modcol(i, 3, kc, seg)
            P.ts("dve" if kc % 2 else "pool", h2f[:, kc, :], xt[:, kc, :], C.mod[:, c_sc:c_sc + 1], C.mod[:, c_sh:c_sh + 1], op0=ALU.mult, op1=ALU.add)
            P.copy("act", h2b[:, kc, :], h2f[:, kc, :])
        for s in range(4):
            r = R[rr % 2]
            rr += 1
            pl = pb[6 + (s % 2)]
            for kc in range(8):
                P.mm(pl[:, 0:16], h2f[:, kc, s * 128:(s + 1) * 128], C.rw[:, kc, :], start=(kc == 0), stop=(kc == 7))
            P.copy("act", r["lg"][:], pl[:, 0:16])
            P.reduce("dve", r["mx"][:], r["lg"][:], ALU.max)
            P.ts("dve", r["mx"][:], r["mx"][:], -1.0, None, op0=ALU.mult)
            P.act(r["e"][:], r["lg"][:], AF.Exp, bias=r["mx"][:], accum_out=r["se"][:])
            P.recip(r["se"][:], r["se"][:])
            P.ts("dve", r["pr"][:], r["e"][:], r["se"][:], None, op0=ALU.mult)
            P.tt("dve", r["sel"][:], r["pr"][:], C.rb[:], ALU.add)
            P.reduce("dve", r["m1"][:], View(r["sel"].t[:].rearrange("p (g k) -> p g k", k=4), r["sel"][:].key), ALU.max)
            for g in range(4):
                P.ts("dve", r["eq"][:, g * 4:(g + 1) * 4], r["sel"][:, g * 4:(g + 1) * 4], r["m1"][:, g:g + 1], None, op0=ALU.is_equal)
            P.stt("dve", r["s2"][:], r["eq"][:], -1e9, r["sel"][:], ALU.mult, ALU.add)
            P.reduce("dve", r["m2"][:], View(r["s2"].t[:].rearrange("p (g k) -> p g k", k=4), r["s2"][:].key), ALU.max)
            P.tt("dve", r["gs"][:], r["m1"][:], r["m2"][:], ALU.add)
            P.reduce("dve", r["gm"][:], r["gs"][:], ALU.max)
            P.ts("dve", r["ing"][:], r["gs"][:], r["gm"][:], None, op0=ALU.is_equal)
            for g in range(4):
                P.ts("dve", r["msk"][:, g * 4:(g + 1) * 4], r["sel"][:, g * 4:(g + 1) * 4], r["m2"][:, g:g + 1], r["ing"][:, g:g + 1],
                     op0=ALU.is_ge, op1=ALU.mult)
            P.tt("dve", r["pw"][:], r["pr"][:], r["msk"][:], ALU.mult)
            P.reduce("dve", r["sw"][:], r["pw"][:], ALU.add)
            P.recip(r["sw"][:], r["sw"][:])
            P.ts("dve", r["cmb"][:], r["pw"][:], r["sw"][:], None, op0=ALU.mult)
            P.transpose(pl[0:16, 128:256], r["cmb"][:], C.ident[:])
            P.copy("act", combT[:, s * 128:(s + 1) * 128], pl[0:16, 128:256])
        n = 0
        for e in range(NE):
            w = wst[e % 2]
            P.dma("sp", w[:, 0], View(C.W1B.t[e].rearrange("(kc p) n -> p kc n", p=128), ("W1B", e)))
            P.dma("pool", w[:, 1], View(C.W3B.t[e].rearrange("(kc p) n -> p kc n", p=128), ("W3B", e)))
            cb = cbs[e % 2]
            pc = pb[6 + (e % 2)]
            P.mm(pc[:], C.selE[:, e * 128:(e + 1) * 128], combT[:], start=True, stop=True)
            P.copy("act", cb[:], pc[:])
            for fc in range(4):
                p1 = pb[(n % 3) * 2]
                p3 = pb[(n % 3) * 2 + 1]
                g = gS[n % 3]
                u = uS[n % 3]
                n += 1
                for kc in range(8):
                    P.mm(p1[:], w[:, 0, kc, fc * 128:(fc + 1) * 128], h2b[:, kc, :], start=(kc == 0), stop=(kc == 7))
                for kc in range(8):
                    P.mm(p3[:], w[:, 1, kc, fc * 128:(fc + 1) * 128], h2b[:, kc, :], start=(kc == 0), stop=(kc == 7))
                P.act(g[:], p1[:], AF.Silu)
                P.tt("dve", u[:], g[:], p3[:], ALU.mult)
                P.tt("pool", hid[:, e * 4 + fc, :], u[:], cb[:], ALU.mult)
        n = 0
        for half in range(2):
            for e in range(NE):
                w2 = w2st[n % 3]
                n += 1
                P.dma("sp" if n % 2 else "pool", w2[:],
                      View(C.W2B.t[e].rearrange("(fc p) n -> p fc n", p=128)[:, :, half * 512:(half + 1) * 512], ("W2B", e)))
                for fc in range(4):
                    for o in range(4):
                        P.mm(pb[half * 4 + o][:], w2[:, fc, o * 128:(o + 1) * 128], hid[:, e * 4 + fc, :],
                             start=(e == 0 and fc == 0), stop=(e == NE - 1 and fc == 3))
            for o in range(4):
                oc = half * 4 + o
                cg = modcol(i, 5, oc, seg)
                P.act(h2f[:, oc, :], pb[half * 4 + o][:], AF.Identity, scale=C.mod[:, cg:cg + 1])
                P.stt("dve", xt[:, oc, :], xt[:, oc, :], ALPHA, h2f[:, oc, :], ALU.mult, ALU.add)
        ln_tile(P, C, L, xt, xt, i, 1, pb[0], pb[1])
        P.dma("sp", View(C.XS.t[:, :, tok].rearrange("c p n -> p c n"), ("XS", t)), xt[:])
    P.pop()


CH = 64


def chunk_plan(b, d):
    ctx0 = 256 * b
    lat0 = 512 + 2048 * b
    blocks = [(ctx0, 4)] + [(lat0 + 512 * k, 8) for k in range(4)]
    if d == 0:
        return [(t0, list(range(n))) for (t0, n) in blocks]
    return [(blocks[0][0], [3, 2, 1, 0])] + [(t0, list(range(n - 1, -1, -1))) for (t0, n) in reversed(blocks[1:])]


def load_w_bf16(P, dst, src_ap, ncols, stg, engs=("pool", "act", "dve"), ctr=[0]):
    sv = src_ap.rearrange("(kc p) n -> p kc n", p=128)
    c0 = 0
    while c0 < ncols:
        w = min(512, ncols - c0)
        j = ctr[0] % len(stg)
        ctr[0] += 1
        P.dma("sp" if j % 2 else "pool", stg[j][:, :, 0:w], sv[:, :, c0:c0 + w])
        P.copy(engs[j % 3], dst[:, :, c0:c0 + w], stg[j][:, :, 0:w])
        c0 += w


def mixer_consts(P, C, IN):
    C.cm = P.sb("cm", [64, 4 * 64], F32)
    P.dma("sp", C.cm[:], IN("cmats", [64, 256])[:, :])
    C.ones64 = P.sb("ones64", [64, 128], F32)
    P.memset("dve", C.ones64[:], 1.0)


def cmat(C, name, d):
    idx = {"U": 0, "UT": 1, "S": 2, "ST": 3}[name]
    if d == 1:
        idx = idx ^ 1
    return C.cm[:, idx * 64:(idx + 1) * 64]


def load_xh(P, C, i, t, xt, hb, halo=False):
    seg = seg_of_tile(t)
    tok = slice(t * TT, (t + 1) * TT)
    P.dma("sp", xt[:], View(C.XS.t[:, :, tok].rearrange("c p n -> p c n"), ("XS", t)))
    for kc in range(8):
        c_sc = modcol(i, 1, kc, seg)
        c_sh = modcol(i, 0, kc, seg)
        P.ts("dve" if kc % 2 else "pool", hb[:, kc, :], xt[:, kc, :], C.mod[:, c_sc:c_sc + 1], C.mod[:, c_sh:c_sh + 1], op0=ALU.mult, op1=ALU.add)


def out_proj_ln(P, C, L, i, t, xt, yT, Wo, pb):
    seg = seg_of_tile(t)
    tok = slice(t * TT, (t + 1) * TT)
    for oc in range(8):
        ps = pb[2 + oc % 4]
        for kc in range(8):
            P.mm(ps[:], Wo[:, kc, oc * 128:(oc + 1) * 128], yT[:, kc, :], start=(kc == 0), stop=(kc == 7))
        cg = modcol(i, 2, oc, seg)
        P.act(L.sq[:, oc, :], ps[:], AF.Identity, scale=C.mod[:, cg:cg + 1])
        P.stt("dve", xt[:, oc, :], xt[:, oc, :], ALPHA, L.sq[:, oc, :], ALU.mult, ALU.add)
    ln_tile(P, C, L, xt, xt, i, 0, pb[0], pb[1])
    P.dma("sp", View(C.XS.t[:, :, tok].rearrange("c p n -> p c n"), ("XS", t)), xt[:])


def rope_evac(P, C, R, ps, dst, t, scale, n):
    y = R.y[n % 2]
    if t == 0:
        P.act(dst, ps[:], AF.Identity, scale=scale)
        return
    pos = ((t - 1) % 4) * TT
    P.act(y[:], ps[:], AF.Identity, scale=scale)
    pr = R.pr[n % 2]
    P.mm(pr[:], R.rotT[:], y[:], start=True, stop=True)
    y1 = R.y1[n % 2]
    P.tt("pool", y1[:], y[:], R.cos[:, pos:pos + TT], ALU.mult)
    y2 = R.y2[n % 2]
    P.tt("dve", y2[:], pr[:], R.sin[:, pos:pos + TT], ALU.mult)
    P.tt("pool", dst, y1[:], y2[:], ALU.add)


def rope_setup(P, C, IN, pbanks):
    R = Ctx()
    R.cos = P.sb("ropecos", [128, 2048], F32)
    R.sin = P.sb("ropesin", [128, 2048], F32)
    R.rotT = P.sb("rotT", [128, 128], F32)
    P.dma("sp", R.cos[:], IN("ropecos", [128, 2048])[:, :])
    P.dma("pool", R.sin[:], IN("ropesin", [128, 2048])[:, :])
    P.dma("sp", R.rotT[:], IN("rotT", [128, 128])[:, :])
    R.y = [P.sb("rp_y%d" % j, [128, TT], F32) for j in range(2)]
    R.y1 = [P.sb("rp_y1%d" % j, [128, TT], F32) for j in range(2)]
    R.y2 = [P.sb("rp_y2%d" % j, [128, TT], F32) for j in range(2)]
    R.pr = pbanks
    return R


ML_H = 4
ML_DV = 256
ML_VW = ML_DV + 1


def phase_mlstm(P, C, IN, i, tiles):
    QK = P.dram("ml_QK", [8, 128, NT], BF16)
    OG = P.dram("ml_OG", [8, 128, NT], BF16)
    KT = P.dram("ml_KT", [NT, 512], BF16)
    VT = P.dram("ml_VT", [NT, ML_H * ML_VW], BF16)
    GT = P.dram("ml_GT", [NT, 16], F32)
    OD = [P.dram("ml_OD%d" % d, [NT, 1024], F32) for d in range(2)]
    w_in = IN("mlstm_w_in", [D, 3088])
    P.push()
    pb = [P.ps("ml_ps%d" % j) for j in range(6)]
    pbt = [P.ps("ml_pt%d" % j, [128, 1024], BF16) for j in range(2)]
    W = P.sb("ml_W", [128, 8, 3088], BF16)
    stg = [P.sb("ml_stg%d" % j, [128, 8, 512], F32) for j in range(2)]
    load_w_bf16(P, W, w_in.t, 3088, stg)
    R = rope_setup(P, C, IN, pb[4:6])
    gb = P.sb("ml_gb", [128, 16], F32)
    P.dma("sp", gb[:], IN("ml_gateb", [128, 16])[:, :])
    xt = P.sb("ml_x", [128, 8, TT], F32)
    hb = P.sb("ml_hb", [128, 8, TT], BF16)
    qk = P.sb("ml_qk", [128, 8, TT], BF16)
    og = P.sb("ml_og", [128, 8, TT], BF16)
    kt = P.sb("ml_kt", [128, 4, 512], BF16)
    vt = P.sb("ml_vt", [128, 4, ML_H * ML_VW], BF16)
    gt = P.sb("ml_gt", [128, 4, 16], F32)
    ge = P.sb("ml_ge", [128, 4, 16], F32)
    P.memset("dve", vt[:], 1.0)
    n = 0
    for t in tiles:
        tok = slice(t * TT, (t + 1) * TT)
        load_xh(P, C, i, t, xt, hb)
        for oc in range(8):
            ps = pb[n % 4]
            for kc in range(8):
                P.mm(ps[:], W[:, kc, oc * 128:(oc + 1) * 128], hb[:, kc, :], start=(kc == 0), stop=(kc == 7))
            rope_evac(P, C, R, ps, qk[:, oc, :], t, (128.0 ** -0.5) if oc < 4 else 1.0, n)
            n += 1
            if oc >= 4:
                for s in range(4):
                    pt = pbt[s % 2]
                    P.transpose(pt[:, 0:128], qk[:, oc, s * 128:(s + 1) * 128], C.identb[:])
                    P.copy("act" if s % 2 else "dve", kt[:, s, (oc - 4) * 128:(oc - 3) * 128], pt[:, 0:128])
        for c in range(8):
            ps = pb[n % 4]
            n += 1
            for kc in range(8):
                P.mm(ps[:], W[:, kc, 2048 + c * 128:2048 + (c + 1) * 128], hb[:, kc, :], start=(kc == 0), stop=(kc == 7))
            P.act(og[:, c, :], ps[:], AF.Sigmoid)
        for s in range(4):
            for blk in range(2):
                ps = pb[n % 4]
                n += 1
                for kc in range(8):
                    P.mm(ps[:], hb[:, kc, s * 128:(s + 1) * 128], W[:, kc, 1024 + blk * 512:1024 + (blk + 1) * 512], start=(kc == 0), stop=(kc == 7))
                for hh in range(2):
                    h = blk * 2 + hh
                    P.copy("act" if hh else "dve", vt[:, s, h * ML_VW:h * ML_VW + ML_DV], ps[:, hh * 256:(hh + 1) * 256])
            ps = pb[n % 4]
            n += 1
            for kc in range(8):
                P.mm(ps[:, 0:16], hb[:, kc, s * 128:(s + 1) * 128], W[:, kc, 3072:3088], start=(kc == 0), stop=(kc == 7))
            P.tt("dve", gt[:, s, :], ps[:, 0:16], gb[:], ALU.add)
        P.act(ge[:], gt[:], AF.Exp, scale=-1.0)
        P.act(ge[:], ge[:], AF.Ln, bias=1.0)
        for d in range(2):
            P.ts("dve", gt[:, :, d * 8 + 4:d * 8 + 8], ge[:, :, d * 8 + 4:d * 8 + 8], -1.0, None, op0=ALU.mult)
        P.dma("sp", View(QK.t[:, :, tok].rearrange("c p n -> p c n"), ("ml_QK", t)), qk[:])
        P.dma("pool", View(OG.t[:, :, tok].rearrange("c p n -> p c n"), ("ml_OG", t)), og[:])
        P.dma("sp", View(KT.t[tok, :].rearrange("(s p) f -> p s f", p=128), ("ml_KT", t)), kt[:])
        P.dma("pool", View(VT.t[tok, :].rearrange("(s p) f -> p s f", p=128), ("ml_VT", t)), vt[:])
        P.dma("sp", View(GT.t[tok, :].rearrange("(s p) f -> p s f", p=128), ("ml_GT", t)), gt[:])
    P.pop()
    P.push()
    pb = [P.ps("m2_ps%d" % j) for j in range(8)]
    qkb = [P.sb("m2_qk%d" % j, [128, 8, TT], BF16) for j in range(2)]
    ktb = [P.sb("m2_kt%d" % j, [64, 8, 512], BF16) for j in range(2)]
    vtb = [P.sb("m2_vt%d" % j, [64, 8, ML_H * ML_VW], BF16) for j in range(2)]
    gtb = [P.sb("m2_gt%d" % j, [64, 8, 16], F32) for j in range(2)]
    S = [P.sb("m2_S%d" % j, [128, ML_H, ML_VW], F32) for j in range(4)]
    Sb = [P.sb("m2_Sb%d" % j, [128, ML_H, ML_VW], BF16) for j in range(4)]
    NB = 3
    eg = [P.sb("m2_eg%d" % j, [64, 8], F32) for j in range(NB)]
    ege = [P.sb("m2_ege%d" % j, [128, 4], F32) for j in range(NB)]
    e1la = [P.sb("m2_e1la%d" % j, [64, 64], F32) for j in range(NB)]
    gm = [P.sb("m2_gm%d" % j, [64, 64], F32) for j in range(NB)]
    ptb = [P.sb("m2_pt%d" % j, [64, 64], BF16) for j in range(NB)]
    kd = [P.sb("m2_kd%d" % j, [64, 128], BF16) for j in range(NB)]
    o1 = [P.sb("m2_o1%d" % j, [64, ML_VW], F32) for j in range(NB)]
    den = [P.sb("m2_den%d" % j, [64, 1], F32) for j in range(NB)]
    ob = [P.sb("m2_ob%d" % j, [64, 1024], F32) for j in range(2)]
    nblk = 0
    n = 0
    nch = 0
    chains = [(b, d) for b in range(2) for d in range(2)]
    plans = {bd: chunk_plan(*bd) for bd in chains}
    for ci, bd in enumerate(chains):
        P.memset("pool", S[ci][:], 0.0)
        P.memset("pool", Sb[ci][:], 0.0)
    for step in range(5):
        for ci, (b, d) in enumerate(chains):
            t0, clist = plans[(b, d)][step]
            ntok = 64 * len(clist)
            j = nblk % 2
            nblk += 1
            tk = ("blk", t0)
            P.dma("sp", qkb[j][:, :, 0:ntok], View(QK.t[:, :, t0:t0 + ntok].rearrange("c p n -> p c n"), ("ml_QK", None)))
            P.dma("pool", ktb[j][:, 0:len(clist), :], View(KT.t[t0:t0 + ntok, :].rearrange("(c p) f -> p c f", p=64), ("ml_KT", None)))
            P.dma("sp", vtb[j][:, 0:len(clist), :], View(VT.t[t0:t0 + ntok, :].rearrange("(c p) f -> p c f", p=64), ("ml_VT", None)))
            P.dma("pool", gtb[j][:, 0:len(clist), :], View(GT.t[t0:t0 + ntok, :].rearrange("(c p) f -> p c f", p=64), ("ml_GT", None)))
            for c in clist:
                cs = slice(c * 64, (c + 1) * 64)
                la = gtb[j][:, c, d * 8 + 4:d * 8 + 8]
                ip = gtb[j][:, c, d * 8:d * 8 + 4]
                m = nch % NB
                nch += 1
                pg = pb[6 + nch % 2]
                P.mm(pg[0:64, 0:4], cmat(C, "U", d), la, start=True, stop=True)
                P.mm(pg[0:64, 4:8], cmat(C, "ST", d), la, start=True, stop=True)
                P.mm(pg[:, 8:12], C.ones64[:], la, start=True, stop=True)
                P.tt("dve", eg[m][:, 4:8], pg[0:64, 4:8], ip, ALU.add)
                P.act(eg[m][:, 4:8], eg[m][:, 4:8], AF.Exp)
                P.act(eg[m][:, 0:4], pg[0:64, 0:4], AF.Exp)
                P.act(ege[m][:], pg[:, 8:12], AF.Exp)
                obuf = ob[nch % 2]
                for h in range(ML_H):
                    u = n % NB
                    n += 1
                    P.ts("dve", e1la[u][:], cmat(C, "ST", d), la[:, h:h + 1] if False else gtb[j][:, c, d * 8 + 4 + h:d * 8 + 5 + h], None, op0=ALU.mult)
                    pl = pb[(n % 3) * 2]
                    P.mm(pl[0:64, 0:64], e1la[u][:], cmat(C, "U", d), start=True, stop=True)
                    P.act(gm[u][:], pl[0:64, 0:64], AF.Exp, bias=gtb[j][:, c, d * 8 + h:d * 8 + h + 1])
                    P.tt("pool", gm[u][:], gm[u][:], cmat(C, "U", d), ALU.mult)
                    P.mm(pl[0:64, 64:128], qkb[j][:, 4 + h, cs], qkb[j][:, h, cs], start=True, stop=True)
                    P.tt("dve", ptb[u][:], pl[0:64, 64:128], gm[u][:], ALU.mult)
                    P.ts("pool", kd[u][:], ktb[j][:, c, h * 128:(h + 1) * 128], eg[m][:, 4 + h:5 + h], None, op0=ALU.mult)
                    vh = vtb[j][:, c, h * ML_VW:(h + 1) * ML_VW]
                    po = pb[(n % 3) * 2 + 1]
                    P.mm(po[0:64, 0:ML_VW], qkb[j][:, h, cs], Sb[ci][:, h, :], start=True, stop=True)
                    P.act(o1[u][:], po[0:64, 0:ML_VW], AF.Identity, scale=eg[m][:, h:h + 1])
                    P.mm(pl[0:64, 128:128 + ML_VW], ptb[u][:], vh, start=True, stop=True)
                    P.tt("dve", o1[u][:], o1[u][:], pl[0:64, 128:128 + ML_VW], ALU.add)
                    P.act(den[u][:], o1[u][:, ML_DV:ML_VW], AF.Abs)
                    P.ts("dve", den[u][:], den[u][:], 1.0, None, op0=ALU.max)
                    P.recip(den[u][:], den[u][:])
                    P.ts("pool", obuf[:, h * ML_DV:(h + 1) * ML_DV], o1[u][:, 0:ML_DV], den[u][:], None, op0=ALU.mult)
                    P.mm(po[:, 0:ML_VW], kd[u][:], vh, start=True, stop=True)
                    P.stt("dve", S[ci][:, h, :], S[ci][:, h, :], ege[m][:, h:h + 1], po[:, 0:ML_VW], ALU.mult, ALU.add)
                    P.copy("act", Sb[ci][:, h, :], S[ci][:, h, :])
                tk0 = t0 + c * 64
                P.dma("sp" if nch % 2 else "pool", View(OD[d].t[tk0:tk0 + 64, :], ("ml_OD%d" % d, None)), obuf[:])
    P.pop()
    P.push()
    pb = [P.ps("m3_ps%d" % j) for j in range(8)]
    L = Ctx()
    L.mean = P.sb("m3_mean", [128, TT], F32)
    L.rstd = P.sb("m3_rstd", [128, TT], F32)
    L.sq = P.sb("m3_sq", [128, 8, TT], F32)
    Wo = P.sb("m3_Wo", [128, 8, 1024], BF16)
    stg = [P.sb("m3_stg%d" % j, [128, 8, 512], F32) for j in range(2)]
    load_w_bf16(P, Wo, IN("mlstm_w_out", [D, D]).t, 1024, stg)
    ng = P.sb("m3_ng", [128, 8], F32)
    P.dma("sp", ng[:], IN("ml_normg", [128, 8])[:, :])
    xt = P.sb("m3_x", [128, 8, TT], F32)
    yT = P.sb("m3_yT", [128, 8, TT], BF16)
    ogt = P.sb("m3_og", [128, 8, TT], BF16)
    oa = [P.sb("m3_oa%d" % j, [128, 1024], F32) for j in range(2)]
    obb = [P.sb("m3_ob%d" % j, [128, 1024], F32) for j in range(2)]
    st = [P.sb("m3_st%d" % j, [128, 8], F32) for j in range(2)]
    n = 0
    for t in tiles:
        seg = seg_of_tile(t)
        tok = slice(t * TT, (t + 1) * TT)
        P.dma("sp", xt[:], View(C.XS.t[:, :, tok].rearrange("c p n -> p c n"), ("XS", t)))
        P.dma("pool", ogt[:], View(OG.t[:, :, tok].rearrange("c p n -> p c n"), ("ml_OG", None)))
        for s in range(4):
            a = oa[s % 2]
            bb = obb[s % 2]
            sv = st[s % 2]
            r0 = t * TT + s * 128
            P.dma("sp", a[:], View(OD[0].t[r0:r0 + 128, :], ("ml_OD0", None)))
            P.dma("pool", bb[:], View(OD[1].t[r0:r0 + 128, :], ("ml_OD1", None)))
            P.tt("pool", a[:], a[:], bb[:], ALU.add)
            P.reduce("dve", sv[:, 0:4], View(a.t[:].rearrange("p (h k) -> p h k", k=ML_DV), a[:].key), ALU.add)
            P.ts("dve", sv[:, 0:4], sv[:, 0:4], -1.0 / ML_DV, None, op0=ALU.mult)
            for h in range(ML_H):
                hs = slice(h * ML_DV, (h + 1) * ML_DV)
                P.ts("dve" if h % 2 else "pool", a[:, hs], a[:, hs], sv[:, h:h + 1], None, op0=ALU.add)
                P.act(bb[:, hs], a[:, hs], AF.Square, accum_out=sv[:, 4 + h:5 + h])
            P.ts("dve", sv[:, 4:8], sv[:, 4:8], 1.0 / ML_DV, 1e-6, op0=ALU.mult, op1=ALU.add)
            P.act(sv[:, 4:8], sv[:, 4:8], AF.Sqrt)
            P.recip(sv[:, 4:8], sv[:, 4:8])
            for h in range(ML_H):
                hs = slice(h * ML_DV, (h + 1) * ML_DV)
                P.ts("dve" if h % 2 else "pool", a[:, hs], a[:, hs], sv[:, 4 + h:5 + h], None, op0=ALU.mult)
            for c in range(8):
                pt = pb[2 + n % 6]
                n += 1
                P.transpose(pt[:, 0:128], a[:, c * 128:(c + 1) * 128], C.ident[:])
                P.stt("dve", yT[:, c, s * 128:(s + 1) * 128], pt[:, 0:128], ng[:, c:c + 1], ogt[:, c, s * 128:(s + 1) * 128], ALU.mult, ALU.mult)
        out_proj_ln(P, C, L, i, t, xt, yT, Wo, pb)
    P.pop()


GD_H = 8
SEQS = [(0, 256, False), (256, 256, False), (512, 2048, True), (2560, 2048, True)]


def inv_unit_lower(P, C, Bm, W, pbX, pbY, pbP):
    X = [Bm] + W.x
    Y = W.y
    Pm = W.p
    P.tt("pool", Pm[:], Y[0][:], C.ident[0:64, 0:64], ALU.add)
    for k in range(1, 6):
        cx = ((k - 1) % 2) * 64
        P.mm(pbX[0:64, cx:cx + 64], Y[k - 1][:], X[k - 1][:], start=True, stop=True)
        if k < 5:
            P.mm(pbY[0:64, cx:cx + 64], X[k - 1][:], Y[k - 1][:], start=True, stop=True)
        P.copy("act", X[k][:], pbX[0:64, cx:cx + 64])
        if k < 5:
            P.copy("dve", Y[k][:], pbY[0:64, cx:cx + 64])
        P.mm(pbP[0:64, cx:cx + 64], X[k][:], Pm[:], start=True, stop=True)
        P.tt("dve", Pm[:], Pm[:], pbP[0:64, cx:cx + 64], ALU.add)
    return Pm


def phase_gdn(P, C, IN, i, tiles):
    ZR = P.dram("gd_ZR", [24, 128, NT], F32)
    GG = P.dram("gd_GG", [8, 128, NT], BF16)
    QK = P.dram("gd_QK", [16, 128, NT], BF16)
    KT = P.dram("gd_KT", [NT, 1024], BF16)
    VT = P.dram("gd_VT", [NT, 1024], BF16)
    GT = P.dram("gd_GT", [NT, 32], F32)
    OD = [P.dram("gd_OD%d" % d, [NT, 1024], F32) for d in range(2)]
    P.push()
    pb = [P.ps("g1_ps%d" % j) for j in range(6)]
    W = P.sb("g1_W", [128, 8, 4128], BF16)
    stg = [P.sb("g1_stg%d" % j, [128, 8, 512], F32) for j in range(2)]
    load_w_bf16(P, W, IN("gdn_w_in", [D, 4128]).t, 4128, stg)
    dtb = P.sb("g1_dtb", [128, 32], F32)
    nea = P.sb("g1_nea", [128, 32], F32)
    P.dma("sp", dtb[:], IN("gd_dtb", [128, 32])[:, :])
    P.dma("sp", nea[:], IN("gd_alog", [128, 32])[:, :])
    P.act(nea[:], nea[:], AF.Exp)
    P.ts("dve", nea[:], nea[:], -1.0, None, op0=ALU.mult)
    xt = P.sb("g1_x", [128, 8, TT], F32)
    hb = P.sb("g1_hb", [128, 8, TT], BF16)
    zt = [P.sb("g1_z%d" % j, [128, 4, TT], F32) for j in range(2)]
    gg = P.sb("g1_gg", [128, 8, TT], BF16)
    gt = P.sb("g1_gt", [128, 4, 32], F32)
    ge = P.sb("g1_ge", [128, 4, 32], F32)
    n = 0
    for t in tiles:
        tok = slice(t * TT, (t + 1) * TT)
        load_xh(P, C, i, t, xt, hb)
        for g4 in range(6):
            z = zt[g4 % 2]
            for cc in range(4):
                oc = g4 * 4 + cc
                ps = pb[n % 4]
                n += 1
                for kc in range(8):
                    P.mm(ps[:], W[:, kc, oc * 128:(oc + 1) * 128], hb[:, kc, :], start=(kc == 0), stop=(kc == 7))
                P.copy("act" if cc % 2 else "dve", z[:, cc, :], ps[:])
            P.dma("sp" if g4 % 2 else "pool", View(ZR.t[g4 * 4:(g4 + 1) * 4, :, tok].rearrange("c p n -> p c n"), ("gd_ZR", t)), z[:])
        for c in range(8):
            ps = pb[n % 4]
            n += 1
            for kc in range(8):
                P.mm(ps[:], W[:, kc, 3072 + c * 128:3072 + (c + 1) * 128], hb[:, kc, :], start=(kc == 0), stop=(kc == 7))
            P.act(gg[:, c, :], ps[:], AF.Silu)
        P.dma("pool", View(GG.t[:, :, tok].rearrange("c p n -> p c n"), ("gd_GG", t)), gg[:])
        for s in range(4):
            ps = pb[4 + s % 2]
            for kc in range(8):
                P.mm(ps[:, 0:32], hb[:, kc, s * 128:(s + 1) * 128], W[:, kc, 4096:4128], start=(kc == 0), stop=(kc == 7))
            P.tt("dve", gt[:, s, :], ps[:, 0:32], dtb[:], ALU.add)
        P.act(ge[:], gt[:], AF.Exp)
        P.act(ge[:], ge[:], AF.Ln, bias=1.0)
        P.act(gt[:], gt[:], AF.Sigmoid)
        for d in range(2):
            for s in range(4):
                P.tt("dve", gt[:, s, d * 16:d * 16 + 8], ge[:, s, d * 16:d * 16 + 8], nea[:, d * 16:d * 16 + 8], ALU.mult)
        P.dma("sp", View(GT.t[tok, :].rearrange("(s p) f -> p s f", p=128), ("gd_GT", t)), gt[:])
    P.pop()
    P.push()
    pb = [P.ps("g1b_ps%d" % j) for j in range(6)]
    pbt = [P.ps("g1b_pt%d" % j, [128, 1024], BF16) for j in range(2)]
    R = rope_setup(P, C, IN, pb[4:6])
    cw = P.sb("g1b_cw", [128, 24 * 3], F32)
    P.dma("sp", cw[:], IN("gd_conv", [128, 72])[:, :])
    zr = [P.sb("g1b_z%d" % j, [128, 2048], F32) for j in range(2)]
    yy = [P.sb("g1b_y%d" % j, [128, 2048], F32) for j in range(2)]
    sq = P.sb("g1b_sq", [128, 512], F32)
    rn = P.sb("g1b_rn", [128, 512], F32)
    yb = [P.sb("g1b_yb%d" % j, [128, 2048], BF16) for j in range(2)]
    tm = [P.sb("g1b_tm%d" % j, [128, 16, 128], BF16) for j in range(2)]
    epsq = P.sb("g1b_epsq", [128, 1], F32)
    epsk = P.sb("g1b_epsk", [128, 1], F32)
    P.memset("dve", epsq[:], 1e-6 * 128.0)
    P.memset("dve", epsk[:], 1e-6)
    n = 0
    for c in range(24):
        for (t0, ns, is_lat) in SEQS:
            j = n % 2
            n += 1
            z = zr[j]
            y = yy[j]
            P.dma("sp" if n % 2 else "pool", z[:, 0:ns], View(ZR.t[c, :, t0:t0 + ns], ("gd_ZR", None)))
            P.ts("dve", y[:, 0:ns], z[:, 0:ns], cw[:, c * 3 + 1:c * 3 + 2], None, op0=ALU.mult)
            P.stt("pool", y[:, 1:ns], z[:, 0:ns - 1], cw[:, c * 3:c * 3 + 1], y[:, 1:ns], ALU.mult, ALU.add)
            P.stt("dve", y[:, 0:ns - 1], z[:, 1:ns], cw[:, c * 3 + 2:c * 3 + 3], y[:, 0:ns - 1], ALU.mult, ALU.add)
            P.act(y[:, 0:ns], y[:, 0:ns], AF.Silu)
            ybj = yb[j]
            if c < 16:
                for s0 in range(0, ns, 512):
                    w = min(512, ns - s0)
                    sl = slice(s0, s0 + w)
                    P.act(sq[:, 0:w], y[:, sl], AF.Square)
                    ps = pb[n % 2]
                    P.mm(ps[:, 0:w], C.ones[:], sq[:, 0:w], start=True, stop=True)
                    if c < 8:
                        P.act(rn[:, 0:w], ps[:, 0:w], AF.Sqrt, bias=epsq[:], scale=128.0)
                    else:
                        P.act(rn[:, 0:w], ps[:, 0:w], AF.Sqrt, bias=epsk[:], scale=1.0)
                    P.recip(rn[:, 0:w], rn[:, 0:w])
                    if is_lat:
                        P.tt("dve", y[:, sl], y[:, sl], rn[:, 0:w], ALU.mult)
                        pr = pb[2 + (s0 // 512) % 2]
                        P.mm(pr[:, 0:w], R.rotT[:], y[:, sl], start=True, stop=True)
                        P.tt("pool", sq[:, 0:w], y[:, sl], R.cos[:, sl], ALU.mult)
                        P.tt("dve", rn[:, 0:w], pr[:, 0:w], R.sin[:, sl], ALU.mult)
                        P.tt("pool", ybj[:, sl], sq[:, 0:w], rn[:, 0:w], ALU.add)
                    else:
                        P.tt("dve", ybj[:, sl], y[:, sl], rn[:, 0:w], ALU.mult)
                P.dma("sp", View(QK.t[c, :, t0:t0 + ns], ("gd_QK", None)), ybj[:, 0:ns])
            else:
                P.copy("pool", ybj[:, 0:ns], y[:, 0:ns])
            if c >= 8:
                tmj = tm[j]
                for s in range(ns // 128):
                    pt = pbt[s % 2]
                    P.transpose(pt[:, 0:128], ybj[:, s * 128:(s + 1) * 128], C.identb[:])
                    P.copy("act" if s % 2 else "dve", tmj[:, s, :], pt[:, 0:128])
                dst = KT if c < 16 else VT
                hh = (c - 8) % 8
                P.dma("pool", View(dst.t[t0:t0 + ns, hh * 128:(hh + 1) * 128].rearrange("(s p) f -> p s f", p=128), (dst.name, None)),
                      tmj[:, 0:ns // 128, :])
    P.pop()
    P.push()
    pb = [P.ps("g2_ps%d" % j) for j in range(8)]
    qkb = [P.sb("g2_qk%d" % j, [128, 16, TT], BF16) for j in range(2)]
    ktb = [P.sb("g2_kt%d" % j, [64, 8, 1024], BF16) for j in range(2)]
    vtb = [P.sb("g2_vt%d" % j, [64, 8, 1024], BF16) for j in range(2)]
    gtb = [P.sb("g2_gt%d" % j, [64, 8, 32], F32) for j in range(2)]
    S = [P.sb("g2_S%d" % j, [128, GD_H, 128], F32) for j in range(4)]
    Sb = [P.sb("g2_Sb%d" % j, [128, GD_H, 128], BF16) for j in range(4)]
    NB = 2
    eg = [P.sb("g2_eg%d" % j, [64, 32], F32) for j in range(NB)]
    ege = [P.sb("g2_ege%d" % j, [128, 8], F32) for j in range(NB)]
    ob = [P.sb("g2_ob%d" % j, [64, 1024], F32) for j in range(2)]

    def tset(j):
        T = Ctx()
        f = lambda nm, shp=(64, 64), dt=F32: P.sb("g2_%s%d" % (nm, j), list(shp), dt)
        T.ula, T.sla, T.gi, T.gj, T.A = f("ula"), f("sla"), f("gi"), f("gj"), f("A")
        T.x = [f("x%d" % k) for k in range(1, 6)]
        T.y = [f("y%d" % k) for k in range(0, 5)]
        T.p = f("p")
        T.ttb = f("ttb", dt=BF16)
        T.ptb = f("ptb", dt=BF16)
        T.bv = f("bv", (64, 128), BF16)
        T.bk = f("bk", (64, 128), BF16)
        T.kd = f("kd", (64, 128), BF16)
        T.u0 = f("u0", (64, 128))
        T.wx = f("wx", (128, 64), BF16)
        T.dl = f("dl", (64, 128), BF16)
        T.o1 = f("o1", (64, 128))
        return T

    TS = [tset(j) for j in range(2)]
    chains = [(b, d) for b in range(2) for d in range(2)]
    plans = {bd: chunk_plan(*bd) for bd in chains}
    for ci in range(4):
        P.memset("pool", S[ci][:], 0.0)
        P.memset("pool", Sb[ci][:], 0.0)
    nblk = 0
    n = 0
    nch = 0
    for step in range(5):
        for ci, (b, d) in enumerate(chains):
            t0, clist = plans[(b, d)][step]
            ntok = 64 * len(clist)
            nc_ = len(clist)
            j = nblk % 2
            nblk += 1
            P.dma("sp", qkb[j][:, :, 0:ntok], View(QK.t[:, :, t0:t0 + ntok].rearrange("c p n -> p c n"), ("gd_QK", None)))
            P.dma("pool", ktb[j][:, 0:nc_, :], View(KT.t[t0:t0 + ntok, :].rearrange("(c p) f -> p c f", p=64), ("gd_KT", None)))
            P.dma("sp", vtb[j][:, 0:nc_, :], View(VT.t[t0:t0 + ntok, :].rearrange("(c p) f -> p c f", p=64), ("gd_VT", None)))
            P.dma("pool", gtb[j][:, 0:nc_, :], View(GT.t[t0:t0 + ntok, :].rearrange("(c p) f -> p c f", p=64), ("gd_GT", None)))
            for c in clist:
                cs = slice(c * 64, (c + 1) * 64)
                la = gtb[j][:, c, d * 16:d * 16 + 8]
                be = gtb[j][:, c, d * 16 + 8:d * 16 + 16]
                m = nch % NB
                nch += 1
                pg = pb[3 + 4 * (nch % 2)]
                P.mm(pg[0:64, 448:456], cmat(C, "U", d), la, start=True, stop=True)
                P.mm(pg[0:64, 456:464], cmat(C, "ST", d), la, start=True, stop=True)
                P.mm(pg[:, 464:472], C.ones64[:], la, start=True, stop=True)
                P.act(eg[m][:, 0:16], pg[0:64, 448:464], AF.Exp)
                P.act(ege[m][:], pg[:, 464:472], AF.Exp)
                P.tt("dve", eg[m][:, 16:24], eg[m][:, 0:8], be, ALU.mult)
                P.ts("dve", eg[m][:, 24:32], be, -1.0, None, op0=ALU.mult)
                obuf = ob[nch % 2]
                for h in range(GD_H):
                    T = TS[n % 2]
                    b0, b1, b2, b3 = [pb[4 * (n % 2) + q] for q in range(4)]
                    n += 1
                    lah = gtb[j][:, c, d * 16 + h:d * 16 + h + 1]
                    kT = qkb[j][:, 8 + h, cs]
                    qT = qkb[j][:, h, cs]
                    P.ts("dve", T.ula[:], cmat(C, "U", d), lah, None, op0=ALU.mult)
                    P.ts("pool", T.sla[:], cmat(C, "ST", d), lah, None, op0=ALU.mult)
                    P.mm(b0[0:64, 0:64], T.ula[:], cmat(C, "ST", d), start=True, stop=True)
                    P.mm(b0[0:64, 64:128], T.sla[:], cmat(C, "U", d), start=True, stop=True)
                    P.mm(b0[0:64, 128:192], kT, kT, start=True, stop=True)
                    P.mm(b0[0:64, 192:256], kT, qT, start=True, stop=True)
                    P.act(T.gi[:], b0[0:64, 0:64], AF.Exp)
                    P.act(T.gj[:], b0[0:64, 64:128], AF.Exp)
                    P.tt("pool", T.gi[:], T.gi[:], cmat(C, "ST", d), ALU.mult)
                    P.tt("pool", T.gj[:], T.gj[:], cmat(C, "U", d), ALU.mult)
                    P.stt("dve", T.A[:], b0[0:64, 128:192], eg[m][:, 24 + h:25 + h], T.gi[:], ALU.mult, ALU.mult)
                    P.tt("dve", T.ptb[:], b0[0:64, 192:256], T.gj[:], ALU.mult)
                    P.transpose(b0[0:64, 256:320], T.A[:], C.ident[0:64, 0:64])
                    P.copy("act", T.y[0][:], b0[0:64, 256:320])
                    Tm = inv_unit_lower(P, C, T.A, T, b1, b2, b3)
                    P.copy("act", T.ttb[:], Tm[:])
                    P.ts("pool", T.bv[:], vtb[j][:, c, h * 128:(h + 1) * 128], gtb[j][:, c, d * 16 + 8 + h:d * 16 + 9 + h], None, op0=ALU.mult)
                    P.ts("pool", T.bk[:], ktb[j][:, c, h * 128:(h + 1) * 128], eg[m][:, 16 + h:17 + h], None, op0=ALU.mult)
                    P.ts("pool", T.kd[:], ktb[j][:, c, h * 128:(h + 1) * 128], eg[m][:, 8 + h:9 + h], None, op0=ALU.mult)
                    P.mm(b0[0:64, 320:448], T.ttb[:], T.bv[:], start=True, stop=True)
                    P.copy("dve", T.u0[:], b0[0:64, 320:448])
                    P.mm(b1[:, 128:192], T.bk[:], T.ttb[:], start=True, stop=True)
                    P.act(T.wx[:], b1[:, 128:192], AF.Identity, scale=-1.0)
                    P.mm(b2[0:64, 128:256], T.wx[:], Sb[ci][:, h, :], start=True, stop=True)
                    P.tt("dve", T.dl[:], b2[0:64, 128:256], T.u0[:], ALU.add)
                    P.mm(b3[0:64, 128:256], qT, Sb[ci][:, h, :], start=True, stop=True)
                    P.act(T.o1[:], b3[0:64, 128:256], AF.Identity, scale=eg[m][:, h:h + 1])
                    P.mm(b1[0:64, 256:384], T.ptb[:], T.dl[:], start=True, stop=True)
                    P.tt("dve", obuf[:, h * 128:(h + 1) * 128], T.o1[:], b1[0:64, 256:384], ALU.add)
                    P.mm(b2[:, 256:384], T.kd[:], T.dl[:], start=True, stop=True)
                    P.stt("dve", S[ci][:, h, :], S[ci][:, h, :], ege[m][:, h:h + 1], b2[:, 256:384], ALU.mult, ALU.add)
                    P.copy("act", Sb[ci][:, h, :], S[ci][:, h, :])
                tk0 = t0 + c * 64
                P.dma("sp" if nch % 2 else "pool", View(OD[d].t[tk0:tk0 + 64, :], ("gd_OD%d" % d, None)), obuf[:])
    P.pop()
    P.push()
    pb = [P.ps("g3_ps%d" % j) for j in range(8)]
    L = Ctx()
    L.mean = P.sb("g3_mean", [128, TT], F32)
    L.rstd = P.sb("g3_rstd", [128, TT], F32)
    L.sq = P.sb("g3_sq", [128, 8, TT], F32)
    Wo = P.sb("g3_Wo", [128, 8, 1024], BF16)
    stg = [P.sb("g3_stg%d" % j, [128, 8, 512], F32) for j in range(2)]
    load_w_bf16(P, Wo, IN("gdn_w_out", [D, D]).t, 1024, stg)
    ng = P.sb("g3_ng", [128, 1], F32)
    P.dma("sp", ng[:], IN("gd_normg", [128, 1])[:, :])
    xt = P.sb("g3_x", [128, 8, TT], F32)
    yT = P.sb("g3_yT", [128, 8, TT], BF16)
    ggt = P.sb("g3_gg", [128, 8, TT], BF16)
    oa = [P.sb("g3_oa%d" % j, [128, 1024], F32) for j in range(2)]
    obb = [P.sb("g3_ob%d" % j, [128, 1024], F32) for j in range(2)]
    st = [P.sb("g3_st%d" % j, [128, 8], F32) for j in range(2)]
    n = 0
    for t in tiles:
        tok = slice(t * TT, (t + 1) * TT)
        P.dma("sp", xt[:], View(C.XS.t[:, :, tok].rearrange("c p n -> p c n"), ("XS", t)))
        P.dma("pool", ggt[:], View(GG.t[:, :, tok].rearrange("c p n -> p c n"), ("gd_GG", None)))
        for s in range(4):
            a = oa[s % 2]
            bb = obb[s % 2]
            sv = st[s % 2]
            r0 = t * TT + s * 128
            P.dma("sp", a[:], View(OD[0].t[r0:r0 + 128, :], ("gd_OD0", None)))
            P.dma("pool", bb[:], View(OD[1].t[r0:r0 + 128, :], ("gd_OD1", None)))
            P.tt("pool", a[:], a[:], bb[:], ALU.add)
            for h in range(GD_H):
                hs = slice(h * 128, (h + 1) * 128)
                P.act(bb[:, hs], a[:, hs], AF.Square, accum_out=sv[:, h:h + 1])
            P.ts("dve", sv[:], sv[:], 1.0 / 128, 1e-6, op0=ALU.mult, op1=ALU.add)
            P.act(sv[:], sv[:], AF.Sqrt)
            P.recip(sv[:], sv[:])
            for h in range(GD_H):
                hs = slice(h * 128, (h + 1) * 128)
                P.ts("dve" if h % 2 else "pool", a[:, hs], a[:, hs], sv[:, h:h + 1], None, op0=ALU.mult)
            for c in range(8):
                pt = pb[2 + n % 6]
                n += 1
                P.transpose(pt[:, 0:128], a[:, c * 128:(c + 1) * 128], C.ident[:])
                P.stt("dve", yT[:, c, s * 128:(s + 1) * 128], pt[:, 0:128], ng[:, 0:1], ggt[:, c, s * 128:(s + 1) * 128], ALU.mult, ALU.mult)
        out_proj_ln(P, C, L, i, t, xt, yT, Wo, pb)
    P.pop()


RW_H = 16
RW_DEBUG = 0
RW_CUT = 0
HT = 128


def dma_heads_out(P, X, u0, n, src):
    for half in range(2):
        dv = X.t.rearrange("(c two) p n -> two c p n", two=2)[half][:, :, u0:u0 + n].rearrange("c p n -> p c n")
        sv = View(src.ap[half * 64:(half + 1) * 64], src.key)
        P.dma("sp" if half else "pool", View(dv, (X.name, None)), sv)


def phase_rwkv(P, C, IN, i, tiles):
    RF = P.dram("rw_RF", [16, 64, NT], F32)
    KK = P.dram("rw_KK", [16, 64, NT], F32)
    KD = [P.dram("rw_KD%d" % d, [16, 64, NT], F32) for d in range(2)]
    AT = [P.dram("rw_AT%d" % d, [16, 64, NT], F32) for d in range(2)]
    LW = [P.dram("rw_LW%d" % d, [NT, 1024], F32) for d in range(2)]
    VT = P.dram("rw_VT", [NT, 1024], BF16)
    GG = P.dram("rw_GG", [8, 128, NT], BF16)
    BN = P.dram("rw_BN", [8, 128, NT], F32)
    OD = [P.dram("rw_OD%d" % d, [NT, 1024], F32) for d in range(2)]
    P.push()
    pb = [P.ps("r1_ps%d" % j) for j in range(8)]
    Wr = [P.sb("r1_W%d" % j, [128, 8, 1024], BF16) for j in range(3)]
    g1w = P.sb("r1_g1w", [128, 8, 128], BF16)
    a1w = [P.sb("r1_a1w%d" % d, [128, 8, 64], BF16) for d in range(2)]
    w1w = [P.sb("r1_w1w%d" % d, [128, 8, 64], BF16) for d in range(2)]
    g2w = P.sb("r1_g2w", [128, 1024], BF16)
    a2w = [P.sb("r1_a2w%d" % d, [64, 1024], BF16) for d in range(2)]
    w2w = [P.sb("r1_w2w%d" % d, [64, 1024], BF16) for d in range(2)]
    P.push()
    stg = [P.sb("r1_stg%d" % j, [128, 8, 512], F32) for j in range(2)]
    for j in range(3):
        load_w_bf16(P, Wr[j], IN("rwkv_w_rkv", [3, D, D]).t[j], 1024, stg)
    load_w_bf16(P, g1w, IN("rwkv_g1", [D, 128]).t, 128, stg)
    for d in range(2):
        load_w_bf16(P, a1w[d], IN("rwkv_a1", [2, D, 64]).t[d], 64, stg)
        load_w_bf16(P, w1w[d], IN("rwkv_w1", [2, D, 64]).t[d], 64, stg)
    sflat = stg[0].t[:].rearrange("p a b -> p (a b)")
    skey = stg[0][:].key
    P.dma("sp", View(sflat[:, 0:1024], skey), IN("rwkv_g2", [128, D])[:, :])
    P.copy("dve", g2w[:], View(sflat[:, 0:1024], skey))
    for d in range(2):
        P.dma("sp", View(sflat[0:64, 0:1024], skey), IN("rwkv_a2", [2, 64, D])[d])
        P.copy("dve", a2w[d][:], View(sflat[0:64, 0:1024], skey))
        P.dma("sp", View(sflat[0:64, 0:1024], skey), IN("rwkv_w2", [2, 64, D])[d])
        P.copy("dve", w2w[d][:], View(sflat[0:64, 0:1024], skey))
    P.pop()
    w0r = P.sb("r1_w0", [128, 2, 1024], F32)
    P.dma("sp", w0r[:], IN("rw_w0", [128, 2, 1024])[:, :, :])
    cst = P.sb("r1_cst", [128, 13 * 8], F32)
    P.dma("sp", cst[:], IN("rw_cst", [128, 104])[:, :])
    P.ts("dve", cst[:, 64:72], cst[:, 56:64], -1.0, 1.0, op0=ALU.mult, op1=ALU.add)
    bones = P.sb("r1_bones", [128, 128], F32)
    P.dma("sp", bones[:], IN("rw_bones", [128, 128])[:, :])
    eps6 = P.sb("r1_eps6", [128, 1], F32)
    P.memset("dve", eps6[:], 1e-6)
    mhalf = P.sb("r1_mhalf", [128, 1], F32)
    P.memset("dve", mhalf[:], -0.5)
    HL = 64
    HW = HT + 2 * HL
    hx = P.sb("r1_hx", [128, 8, HW], F32)
    dx = P.sb("r1_dx", [128, 8, HT], F32)
    xm = [P.sb("r1_xm%d" % j, [128, 8, HT], BF16) for j in range(2)]
    kf = P.sb("r1_kf", [128, 8, HT], F32)
    vf = P.sb("r1_vf", [128, 8, HT], F32)
    rr = P.sb("r1_rr", [128, 8, HT], F32)
    kkf = P.sb("r1_kkf", [128, 8, HT], F32)
    of = [P.sb("r1_of%d" % j, [128, 8, HT], F32) for j in range(2)]
    ogb = P.sb("r1_ogb", [128, 8, HT], BF16)
    vtt = P.sb("r1_vtt", [128, HT // 128, 1024], BF16)
    lwt = [P.sb("r1_lwt%d" % j, [128, max(HT // 128, 1), 1024], F32) for j in range(2)]
    tmp = [P.sb("r1_tmp%d" % j, [128, 512], F32) for j in range(4)]
    tl = [P.sb("r1_tl%d" % j, [128, HT], BF16) for j in range(2)]
    n = 0
    nm = 0

    def mix(j):
        nonlocal nm
        x = xm[nm % 2]
        nm += 1
        for kc in range(8):
            P.stt("dve", x[:, kc, :], dx[:, kc, :], cst[:, j * 8 + kc:j * 8 + kc + 1], hx[:, kc, HL:HL + HT], ALU.mult, ALU.add)
        return x

    def proj_fm(x, Wt, oc, ncols=128, krows=128, kchunks=8):
        nonlocal n
        ps = pb[n % 6]
        n += 1
        for kc in range(kchunks):
            P.mm(ps[0:ncols, 0:HT], Wt[:, kc, oc * 128:oc * 128 + ncols], x[:, kc, :], start=(kc == 0), stop=(kc == kchunks - 1))
        return ps

    units = []
    for t in tiles:
        units += [(t, hf) for hf in range(TT // HT)]
    for (t, hf) in units:
        seg = seg_of_tile(t)
        u0 = t * TT + hf * HT
        sq0, sqn = [(a, b) for (a, b, _) in SEQS if a <= u0 < a + b][0]
        has_l, has_r = u0 > sq0, u0 + HT < sq0 + sqn
        P.dma("sp", hx[:, :, HL:HL + HT], View(C.XS.t[:, :, u0:u0 + HT].rearrange("c p n -> p c n"), ("XS", t)))
        if has_l:
            P.dma("pool", hx[:, :, 0:HL], View(C.XS.t[:, :, u0 - HL:u0].rearrange("c p n -> p c n"), ("XS", (u0 - 1) // TT)))
        if has_r:
            P.dma("pool", hx[:, :, HL + HT:HW], View(C.XS.t[:, :, u0 + HT:u0 + HT + HL].rearrange("c p n -> p c n"), ("XS", (u0 + HT) // TT)))
        for kc in range(8):
            c_sc = modcol(i, 1, kc, seg)
            c_sh = modcol(i, 0, kc, seg)
            P.ts("dve" if kc % 2 else "pool", hx[:, kc, HL - 1:HL + HT + 1], hx[:, kc, HL - 1:HL + HT + 1], C.mod[:, c_sc:c_sc + 1], C.mod[:, c_sh:c_sh + 1], op0=ALU.mult, op1=ALU.add)
        if not has_l:
            P.memset("pool", hx[:, :, HL - 1:HL], 0.0)
        if not has_r:
            P.memset("pool", hx[:, :, HL + HT:HL + HT + 1], 0.0)
        for kc in range(8):
            P.tt("pool", dx[:, kc, :], hx[:, kc, HL - 1:HL - 1 + HT], hx[:, kc, HL + 1:HL + 1 + HT], ALU.add)
            P.stt("dve", dx[:, kc, :], dx[:, kc, :], 0.5, hx[:, kc, HL:HL + HT], ALU.mult, ALU.subtract)
        x = mix(0)
        o = of[0]
        for c in range(8):
            ps = proj_fm(x, Wr[0], c)
            P.copy("act", o[:, c, :], ps[:, 0:HT])
            P.ts("pool", rr[:, c, :], o[:, c, :], cst[:, 72 + c:73 + c], None, op0=ALU.mult)
        dma_heads_out(P, RF, u0, HT, o[:])
        x = mix(2)
        o = of[1]
        for c in range(8):
            ps = proj_fm(x, Wr[1], c)
            P.copy("act", kf[:, c, :], ps[:, 0:HT])
            t1 = tmp[c % 2]
            P.ts("pool", t1[:, 0:HT], kf[:, c, :], cst[:, 48 + c:49 + c], None, op0=ALU.mult)
            t2 = tmp[2 + c % 2]
            P.act(t2[:, 0:HT], t1[:, 0:HT], AF.Square)
            pq = pb[6 + c % 2]
            P.mm(pq[:, 0:HT], bones[:], t2[:, 0:HT], start=True, stop=True)
            P.act(t2[:, 0:HT], pq[:, 0:HT], AF.Sqrt, bias=eps6[:])
            P.recip(t2[:, 0:HT], t2[:, 0:HT])
            P.tt("pool", kkf[:, c, :], t1[:, 0:HT], t2[:, 0:HT], ALU.mult)
        dma_heads_out(P, KK, u0, HT, kkf[:])
        x = mix(3)
        for c in range(8):
            ps = proj_fm(x, Wr[2], c)
            P.copy("act", vf[:, c, :], ps[:, 0:HT])
        for s in range(HT // 128):
            for blk in range(2):
                ps = pb[n % 6]
                n += 1
                for kc in range(8):
                    P.mm(ps[:], x[:, kc, s * 128:(s + 1) * 128], Wr[2][:, kc, blk * 512:(blk + 1) * 512], start=(kc == 0), stop=(kc == 7))
                P.copy("act" if blk else "dve", vtt[:, s, blk * 512:(blk + 1) * 512], ps[:])
        P.dma("sp", View(VT.t[u0:u0 + HT, :].rearrange("(s p) f -> p s f", p=128), ("rw_VT", None)), vtt[:])
        x = mix(5)
        ps = proj_fm(x, g1w, 0)
        P.act(tl[0][:], ps[:, 0:HT], AF.Sigmoid)
        for c in range(8):
            ps = pb[n % 6]
            n += 1
            P.mm(ps[:, 0:HT], g2w[:, c * 128:(c + 1) * 128], tl[0][:], start=True, stop=True)
            P.copy("act", ogb[:, c, :], ps[:, 0:HT])
        P.dma("pool", View(GG.t[:, :, u0:u0 + HT].rearrange("c p n -> p c n"), ("rw_GG", None)), ogb[:])
        xa = mix(4)
        kdo = [of[0], of[1]]
        for d in range(2):
            ps = proj_fm(xa, a1w[d], 0, ncols=64)
            P.copy("act", tl[d][0:64, :], ps[0:64, 0:HT])
        ato = lwt
        atv = [View(lwt[d].t[:].rearrange("p a b -> p (a b)")[:, 0:8 * HT].rearrange("p (c n) -> p c n", c=8), lwt[d][:].key) for d in range(2)]
        for c in range(8):
            pbn = pb[6 + c % 2]
            for d in range(2):
                ps = pb[n % 6]
                n += 1
                P.mm(ps[:, 0:HT], a2w[d][:, c * 128:(c + 1) * 128], tl[d][0:64, :], start=True, stop=True)
                af = tmp[d]
                P.act(af[:, 0:HT], ps[:, 0:HT], AF.Sigmoid, bias=cst[:, 80 + d * 8 + c:81 + d * 8 + c])
                P.tt("pool", View(atv[d].ap[:, c, :], atv[d].key), af[:, 0:HT], kkf[:, c, :], ALU.mult)
                P.ts("dve", af[:, 0:HT], af[:, 0:HT], cst[:, 56 + c:57 + c], cst[:, 64 + c:65 + c], op0=ALU.mult, op1=ALU.add)
                P.tt("dve", kdo[d][:, c, :], kf[:, c, :], af[:, 0:HT], ALU.mult)
                t2 = tmp[2 + d]
                P.tt("pool", t2[:, 0:HT], rr[:, c, :], kdo[d][:, c, :], ALU.mult)
                P.mm(pbn[:, 0:HT], bones[:], t2[:, 0:HT], start=(d == 0), stop=(d == 1))
            P.tt("dve", vf[:, c, :], vf[:, c, :], pbn[:, 0:HT], ALU.mult)
        for d in range(2):
            dma_heads_out(P, KD[d], u0, HT, kdo[d][:])
            dma_heads_out(P, AT[d], u0, HT, atv[d])
        P.dma("sp", View(BN.t[:, :, u0:u0 + HT].rearrange("c p n -> p c n"), ("rw_BN", None)), vf[:])
        xw = mix(1)
        for d in range(2):
            ps = proj_fm(xw, w1w[d], 0, ncols=64)
            P.act(tl[d][0:64, :], ps[0:64, 0:HT], AF.Tanh)
        for d in range(2):
            lo = lwt[d]
            for s in range(HT // 128):
                for blk in range(2):
                    ps = pb[n % 6]
                    n += 1
                    P.mm(ps[:], tl[d][0:64, s * 128:(s + 1) * 128], w2w[d][:, blk * 512:(blk + 1) * 512], start=True, stop=True)
                    sl = slice(blk * 512, (blk + 1) * 512)
                    tq = tmp[(s * 2 + blk) % 4]
                    P.tt("dve", tq[:], ps[:], w0r[:, d, sl], ALU.add)
                    P.act(tq[:], tq[:], AF.Exp, scale=-1.0)
                    P.act(tq[:], tq[:], AF.Ln, bias=1.0)
                    P.act(tq[:], tq[:], AF.Exp, scale=-1.0, bias=mhalf[:])
                    P.ts("pool", lo[:, s, sl], tq[:], -1.0, None, op0=ALU.mult)
            P.dma("sp" if d else "pool", View(LW[d].t[u0:u0 + HT, :].rearrange("(s p) f -> p s f", p=128), ("rw_LW%d" % d, None)), lo[:])
    P.pop()
    if RW_DEBUG == 1:
        return
    P.push()
    pb = [P.ps("r2_ps%d" % j) for j in range(6)]
    pbt = [P.ps("r2_pt%d" % j, [128, 1024], BF16) for j in range(2)]
    HG = 4
    fmb = [[P.sb("r2_fm%d_%d" % (k, j), [64, HG, TT], F32) for k in range(4)] for j in range(2)]
    lwb = [P.sb("r2_lw%d" % j, [64, 8, HG * 64], F32) for j in range(2)]
    vtb = [P.sb("r2_vt%d" % j, [64, 8, HG * 64], BF16) for j in range(2)]
    S = P.sb("r2_S", [64, 64, 64], F32)
    Sb = P.sb("r2_Sb", [64, 64, 64], BF16)
    for q in range(8):
        P.memset("pool", S[:, q * 8:(q + 1) * 8, :], 0.0)
        P.memset("dve", Sb[:, q * 8:(q + 1) * 8, :], 0.0)
    ob = [P.sb("r2_ob%d" % j, [64, 8, HG * 64], F32) for j in range(2)]

    def tset(j):
        T = Ctx()
        f = lambda nm, shp=(64, 64), dt=F32: P.sb("r2_%s%d" % (nm, j), list(shp), dt)
        T.cls, T.ecl, T.ece, T.encl, T.dec, T.A = f("cls"), f("ecl"), f("ece"), f("encl"), f("dec"), f("A")
        T.sc = f("sc", (64, 4, 64), BF16)
        T.kdec = f("kdec")
        T.adec = f("adec")
        T.kdT = f("kdT", dt=BF16)
        T.nadT = f("nadT", dt=BF16)
        T.g2t = f("g2t", dt=BF16)
        T.g3t = f("g3t", dt=BF16)
        T.ng4t = f("ng4t", dt=BF16)
        T.x = [f("x%d" % k) for k in range(1, 6)]
        T.y = [f("y%d" % k) for k in range(0, 5)]
        T.p = f("p")
        T.ttb = f("ttb", dt=BF16)
        T.g2v = f("g2v")
        T.zz = f("zz", dt=BF16)
        T.ub = f("ub", dt=BF16)
        return T

    TS = [tset(j) for j in range(2)]
    chains = [(b, d) for b in range(2) for d in range(2)]
    plans = {bd: chunk_plan(*bd) for bd in chains}
    nblk = 0
    n = 0
    for step in range(5 if RW_DEBUG < 3 else 1):
        for ci, (b, d) in enumerate(chains):
            t0, clist = plans[(b, d)][step]
            ntok = 64 * len(clist)
            nc_ = len(clist)
            last = 63 if d == 0 else 0
            for hg in range(RW_H // HG):
                j = nblk % 2
                nblk += 1
                hs = slice(hg * HG, (hg + 1) * HG)
                for k, src in enumerate((RF, KK, KD[d], AT[d])):
                    P.dma("sp" if k % 2 else "pool", fmb[j][k][:, :, 0:ntok], View(src.t[hs, :, t0:t0 + ntok].rearrange("h p n -> p h n"), (src.name, None)))
                P.dma("sp", lwb[j][:, 0:nc_, :], View(LW[d].t[t0:t0 + ntok, hg * 256:(hg + 1) * 256].rearrange("(c p) f -> p c f", p=64), ("rw_LW%d" % d, None)))
                P.dma("pool", vtb[j][:, 0:nc_, :], View(VT.t[t0:t0 + ntok, hg * 256:(hg + 1) * 256].rearrange("(c p) f -> p c f", p=64), ("rw_VT", None)))
                obuf = ob[j]
                for c in clist:
                    if RW_CUT == 9:
                        continue
                    cs = slice(c * 64, (c + 1) * 64)
                    for hh in range(HG):
                        h = hg * HG + hh
                        si = ci * 16 + h
                        T = TS[n % 2]
                        b0, b1, b2 = [pb[3 * (n % 2) + q] for q in range(3)]
                        b3 = b2
                        pt = pbt[n % 2]
                        n += 1
                        lw = lwb[j][:, c, hh * 64:(hh + 1) * 64]
                        vh = vtb[j][:, c, hh * 64:(hh + 1) * 64]
                        rF, kkF, kdF, atF = [fmb[j][k][:, hh, cs] for k in range(4)]
                        P.mm(b0[0:64, 0:64], lw, cmat(C, "U", d), start=True, stop=True)
                        P.mm(b0[0:64, 64:128], lw, cmat(C, "S", d), start=True, stop=True)
                        P.copy("dve", T.cls[:], b0[0:64, 0:64])
                        if RW_CUT == 6:
                            continue
                        P.act(T.ecl[:], b0[0:64, 0:64], AF.Exp)
                        P.act(T.ece[:], b0[0:64, 64:128], AF.Exp)
                        P.act(T.encl[:], b0[0:64, 0:64], AF.Exp, scale=-1.0)
                        P.act(T.dec[:], T.cls[:], AF.Exp, scale=-1.0, bias=T.cls[:, last:last + 1])
                        if RW_CUT == 7:
                            continue
                        P.tt("pool", T.sc[:, 0, :], kkF, T.ece[:], ALU.mult)
                        P.tt("dve", T.sc[:, 1, :], rF, T.ecl[:], ALU.mult)
                        P.tt("pool", T.sc[:, 2, :], kdF, T.encl[:], ALU.mult)
                        P.tt("dve", T.sc[:, 3, :], atF, T.encl[:], ALU.mult)
                        P.tt("pool", T.kdec[:], kdF, T.dec[:], ALU.mult)
                        P.tt("pool", T.adec[:], atF, T.dec[:], ALU.mult)
                        if RW_CUT == 8:
                            continue
                        P.transpose(b1[0:64, 256:320], T.kdec[:], C.ident[0:64, 0:64])
                        P.transpose(b1[0:64, 320:384], T.adec[:], C.ident[0:64, 0:64])
                        P.copy("act", T.kdT[:], b1[0:64, 256:320])
                        P.act(T.nadT[:], b1[0:64, 320:384], AF.Identity, scale=-1.0)
                        if RW_CUT == 1:
                            continue
                        bt, rt, kt_, at_ = [T.sc[:, q, :] for q in range(4)]
                        br = View(T.sc.t[:, 0:2, :].rearrange("p a b -> p (a b)"), T.sc[:].key)
                        P.mm(b0[0:64, 128:192], bt, at_, start=True, stop=True)
                        P.mm(b0[0:64, 192:320], kt_, br, start=True, stop=True)
                        P.mm(b0[0:64, 320:448], at_, br, start=True, stop=True)
                        P.stt("dve", T.A[:], b0[0:64, 128:192], -1.0, cmat(C, "ST", d), ALU.mult, ALU.mult)
                        P.stt("dve", T.y[0][:], b0[0:64, 320:384], -1.0, cmat(C, "S", d), ALU.mult, ALU.mult)
                        P.tt("dve", T.g2t[:], b0[0:64, 192:256], cmat(C, "S", d), ALU.mult)
                        P.tt("dve", T.g3t[:], b0[0:64, 256:320], cmat(C, "U", d), ALU.mult)
                        P.stt("dve", T.ng4t[:], b0[0:64, 384:448], -1.0, cmat(C, "U", d), ALU.mult, ALU.mult)
                        if RW_CUT == 2:
                            continue
                        Tm = inv_unit_lower(P, C, T.A, T, b1, b2, b3)
                        P.copy("act", T.ttb[:], Tm[:])
                        if RW_CUT == 3:
                            continue
                        P.mm(b1[0:64, 128:192], T.g2t[:], vh, start=True, stop=True)
                        P.copy("act", T.g2v[:], b1[0:64, 128:192])
                        Sh = Sb[:, si, :]
                        P.mm(b2[0:64, 128:192], bt, Sh, start=True, stop=True)
                        P.tt("dve", T.zz[:], b2[0:64, 128:192], T.g2v[:], ALU.add)
                        P.mm(b2[0:64, 192:256], T.ttb[:], T.zz[:], start=True, stop=True)
                        P.copy("act", T.ub[:], b2[0:64, 192:256])
                        P.mm(b3[0:64, 384:448], rt, Sh, start=True, stop=False)
                        P.mm(b3[0:64, 384:448], T.g3t[:], vh, start=False, stop=False)
                        P.mm(b3[0:64, 384:448], T.ng4t[:], T.ub[:], start=False, stop=True)
                        P.copy("act", obuf[:, c, hh * 64:(hh + 1) * 64], b3[0:64, 384:448])
                        P.mm(b1[0:64, 192:256], T.kdT[:], vh, start=True, stop=False)
                        P.mm(b1[0:64, 192:256], T.nadT[:], T.ub[:], start=False, stop=True)
                        P.stt("dve", S[:, si, :], S[:, si, :], T.ecl[:, last:last + 1], b1[0:64, 192:256], ALU.mult, ALU.add)
                        P.copy("act", Sb[:, si, :], S[:, si, :])
                P.dma("sp", View(OD[d].t[t0:t0 + ntok, hg * 256:(hg + 1) * 256].rearrange("(c p) f -> p c f", p=64), ("rw_OD%d" % d, None)),
                      obuf[:, 0:nc_, :])
    P.pop()
    if RW_DEBUG in (2, 4):
        return
    P.push()
    pb = [P.ps("r3_ps%d" % j) for j in range(8)]
    L = Ctx()
    L.mean = P.sb("r3_mean", [128, TT], F32)
    L.rstd = P.sb("r3_rstd", [128, TT], F32)
    L.sq = P.sb("r3_sq", [128, 8, TT], F32)
    Wo = P.sb("r3_Wo", [128, 8, 1024], BF16)
    stg = [P.sb("r3_stg%d" % j, [128, 8, 512], F32) for j in range(2)]
    load_w_bf16(P, Wo, IN("rwkv_w_out", [D, D]).t, 1024, stg)
    lx = P.sb("r3_lx", [128, 16], F32)
    P.dma("sp", lx[:], IN("rw_lnx", [128, 16])[:, :])
    xt = P.sb("r3_x", [128, 8, TT], F32)
    yT = P.sb("r3_yT", [128, 8, TT], BF16)
    ggt = P.sb("r3_gg", [128, 8, TT], BF16)
    bnt = P.sb("r3_bn", [128, 8, TT], F32)
    oa = [P.sb("r3_oa%d" % j, [128, 1024], F32) for j in range(2)]
    obb = [P.sb("r3_ob%d" % j, [128, 1024], F32) for j in range(2)]
    st = [P.sb("r3_st%d" % j, [128, 32], F32) for j in range(2)]
    yf = [P.sb("r3_yf%d" % j, [128, 128], F32) for j in range(2)]
    n = 0
    for t in tiles:
        tok = slice(t * TT, (t + 1) * TT)
        P.dma("sp", xt[:], View(C.XS.t[:, :, tok].rearrange("c p n -> p c n"), ("XS", t)))
        P.dma("pool", ggt[:], View(GG.t[:, :, tok].rearrange("c p n -> p c n"), ("rw_GG", None)))
        P.dma("sp", bnt[:], View(BN.t[:, :, tok].rearrange("c p n -> p c n"), ("rw_BN", None)))
        for s in range(4):
            a = oa[s % 2]
            bb = obb[s % 2]
            sv = st[s % 2]
            r0 = t * TT + s * 128
            P.dma("sp", a[:], View(OD[0].t[r0:r0 + 128, :], ("rw_OD0", None)))
            P.dma("pool", bb[:], View(OD[1].t[r0:r0 + 128, :], ("rw_OD1", None)))
            P.tt("pool", a[:], a[:], bb[:], ALU.add)
            P.reduce("dve", sv[:, 0:16], View(a.t[:].rearrange("p (h k) -> p h k", k=64), a[:].key), ALU.add)
            P.ts("dve", sv[:, 0:16], sv[:, 0:16], -1.0 / 64, None, op0=ALU.mult)
            for h in range(RW_H):
                hs = slice(h * 64, (h + 1) * 64)
                P.ts("dve" if h % 2 else "pool", a[:, hs], a[:, hs], sv[:, h:h + 1], None, op0=ALU.add)
                P.act(bb[:, hs], a[:, hs], AF.Square, accum_out=sv[:, 16 + h:17 + h])
            P.ts("dve", sv[:, 16:32], sv[:, 16:32], 1.0 / 64, 64e-5, op0=ALU.mult, op1=ALU.add)
            P.act(sv[:, 16:32], sv[:, 16:32], AF.Sqrt)
            P.recip(sv[:, 16:32], sv[:, 16:32])
            for h in range(RW_H):
                hs = slice(h * 64, (h + 1) * 64)
                P.ts("dve" if h % 2 else "pool", a[:, hs], a[:, hs], sv[:, 16 + h:17 + h], None, op0=ALU.mult)
            for c in range(8):
                pt = pb[2 + n % 6]
                y = yf[n % 2]
                n += 1
                sl = slice(s * 128, (s + 1) * 128)
                P.transpose(pt[:, 0:128], a[:, c * 128:(c + 1) * 128], C.ident[:])
                P.act(y[:], pt[:, 0:128], AF.Identity, scale=lx[:, c:c + 1], bias=lx[:, 8 + c:9 + c])
                P.tt("pool", y[:], y[:], bnt[:, c, sl], ALU.add)
                P.tt("dve", yT[:, c, sl], y[:], ggt[:, c, sl], ALU.mult)
        out_proj_ln(P, C, L, i, t, xt, yT, Wo, pb)
    P.pop()


NA_H = 16


def na_geom(r):
    R0 = min(max(r - 4, 0), 23)
    ty = 0 if r == 0 else 1 if r == 2 else 3 if r == 28 else 4 if r == 30 else 2
    return ty, R0


def dma_heads_out_b(P, X, u0, n, src):
    for half in range(2):
        dv = X.t.rearrange("(c two) p n -> two c p n", two=2)[half][:, :, u0:u0 + n].rearrange("c p n -> p c n")
        sv = View(src.ap[half * 64:(half + 1) * 64], src.key)
        P.dma("sp" if half else "pool", View(dv, (X.name, None)), sv)


def phase_na(P, C, IN, i, tiles):
    QF = P.dram("na_QF", [16, 64, NT], BF16)
    KF = P.dram("na_KF", [16, 64, NT], BF16)
    VT = P.dram("na_VT", [NT, 1024], BF16)
    OT = P.dram("na_OT", [NT, 1024], F32)
    P.push()
    pb = [P.ps("n1_ps%d" % j) for j in range(6)]
    W = P.sb("n1_W", [128, 8, 3072], BF16)
    P.push()
    stg = [P.sb("n1_stg%d" % j, [128, 8, 512], F32) for j in range(2)]
    load_w_bf16(P, W, IN("na_w_in", [D, 3072]).t, 3072, stg)
    P.pop()
    xt = P.sb("n1_x", [128, 8, TT], F32)
    hb = P.sb("n1_hb", [128, 8, TT], BF16)
    qf = P.sb("n1_qf", [128, 8, TT], BF16)
    kfb = P.sb("n1_kf", [128, 8, TT], BF16)
    vt = P.sb("n1_vt", [128, 4, 1024], BF16)
    n = 0
    for t in tiles:
        tok = slice(t * TT, (t + 1) * TT)
        load_xh(P, C, i, t, xt, hb)
        for c in range(16):
            if c < 8 and t == 0:
                continue
            ps = pb[n % 6]
            n += 1
            for kc in range(8):
                P.mm(ps[:], W[:, kc, c * 128:(c + 1) * 128], hb[:, kc, :], start=(kc == 0), stop=(kc == 7))
            if c < 8:
                P.act(qf[:, c, :], ps[:], AF.Identity, scale=0.125)
            else:
                P.copy("dve", kfb[:, c - 8, :], ps[:])
        if t > 0:
            dma_heads_out_b(P, QF, t * TT, TT, qf[:])
        dma_heads_out_b(P, KF, t * TT, TT, kfb[:])
        for s in range(4):
            for blk in range(2):
                ps = pb[n % 6]
                n += 1
                for kc in range(8):
                    P.mm(ps[:], hb[:, kc, s * 128:(s + 1) * 128], W[:, kc, 2048 + blk * 512:2048 + (blk + 1) * 512], start=(kc == 0), stop=(kc == 7))
                P.copy("act" if blk else "dve", vt[:, s, blk * 512:(blk + 1) * 512], ps[:])
        P.dma("sp", View(VT.t[tok, :].rearrange("(s p) f -> p s f", p=128), ("na_VT", None)), vt[:])
    P.pop()
    P.push()
    ps1 = [P.ps("n2_s1%d" % j) for j in range(2)]
    ps2 = [P.ps("n2_s2%d" % j) for j in range(2)]
    pst = [P.ps("n2_pt%d" % j, [128, 1024], BF16) for j in range(2)]
    pso = [P.ps("n2_po%d" % j) for j in range(2)]
    bias = [P.sb("n2_bias%d" % j, [128, 5, 576], F32) for j in range(2)]
    qT = [P.sb("n2_q%d" % j, [64, 2048], BF16) for j in range(2)]
    kT = [P.sb("n2_k%d" % j, [64, 2304], BF16) for j in range(2)]
    va = [P.sb("n2_va%d" % j, [128, 16, 64], BF16) for j in range(2)]
    vb = [P.sb("n2_vb%d" % j, [128, 16, 64], BF16) for j in range(2)]
    vc = [P.sb("n2_vc%d" % j, [128, 2, 64], BF16) for j in range(2)]
    Sm = [P.sb("n2_S%d" % j, [128, 832], F32) for j in range(2)]
    Pm = [P.sb("n2_P%d" % j, [128, 896], BF16) for j in range(2)]
    PT = [P.sb("n2_PT%d" % j, [128, 7, 128], BF16) for j in range(2)]
    sm = [P.sb("n2_sm%d" % j, [128, 4], F32) for j in range(2)]
    oo = [P.sb("n2_o%d" % j, [128, 64], F32) for j in range(2)]
    for j in range(2):
        P.memset("pool", vb[j][:], 0.0)
    bsrc = IN("na_bias", [NA_H, 128, 5, 576])
    n = 0
    nhb = 0
    for h in range(NA_H):
        bj = bias[h % 2]
        P.dma("sp", bj[:], bsrc[h])
        for b in range(2):
            j = nhb % 2
            nhb += 1
            lat0 = 512 + 2048 * b
            ctx0 = 256 * b
            P.dma("sp", qT[j][:], View(QF.t[h, :, lat0:lat0 + 2048], ("na_QF", None)))
            P.dma("pool", kT[j][:, 0:2048], View(KF.t[h, :, lat0:lat0 + 2048], ("na_KF", None)))
            P.dma("pool", kT[j][:, 2048:2304], View(KF.t[h, :, ctx0:ctx0 + 256], ("na_KF", None)))
            P.dma("sp", va[j][:], View(VT.t[lat0:lat0 + 2048, h * 64:(h + 1) * 64].rearrange("(t p) f -> p t f", p=128), ("na_VT", None)))
            P.dma("pool", vb[j][:, 0:15, :], View(VT.t[lat0 + 64:lat0 + 64 + 1920, h * 64:(h + 1) * 64].rearrange("(t p) f -> p t f", p=128), ("na_VT", None)))
            P.dma("sp", vb[j][0:64, 15, :], View(VT.t[lat0 + 1984:lat0 + 2048, h * 64:(h + 1) * 64], ("na_VT", None)))
            P.dma("sp", vc[j][:], View(VT.t[ctx0:ctx0 + 256, h * 64:(h + 1) * 64].rearrange("(t p) f -> p t f", p=128), ("na_VT", None)))
            for r in range(0, 32, 2):
                ty, R0 = na_geom(r)
                u = n % 2
                n += 1
                q = qT[j][:, r * 64:r * 64 + 128]
                k0 = R0 * 64
                p1, p2 = ps1[u], ps2[u]
                P.mm(p1[:], q, kT[j][:, k0:k0 + 512], start=True, stop=True)
                P.mm(p2[:, 0:64], q, kT[j][:, k0 + 512:k0 + 576], start=True, stop=True)
                P.mm(p2[:, 64:320], q, kT[j][:, 2048:2304], start=True, stop=True)
                S = Sm[u]
                P.copy("act", S[:, 0:256], p2[:, 64:320])
                P.tt("dve", S[:, 256:768], p1[:], bj[:, ty, 0:512], ALU.add)
                P.tt("dve", S[:, 768:832], p2[:, 0:64], bj[:, ty, 512:576], ALU.add)
                st = sm[u]
                P.reduce("dve", st[:, 0:1], S[:], ALU.max)
                P.ts("dve", st[:, 0:1], st[:, 0:1], -1.0, None, op0=ALU.mult)
                Pb = Pm[u]
                P.act(Pb[:, 0:832], S[:], AF.Exp, bias=st[:, 0:1], accum_out=st[:, 1:2])
                P.recip(st[:, 2:3], st[:, 1:2])
                pt = pst[u]
                ptr = PT[u]
                for kt in range(7):
                    w = 128 if kt < 6 else 64
                    P.transpose(pt[0:w, kt * 128:(kt + 1) * 128], Pb[:, kt * 128:kt * 128 + w], C.identb[:])
                P.copy("act", ptr[:, 0:3, :], View(pt.t[:, 0:384].rearrange("p (a b) -> p a b", b=128), pt[:].key))
                P.copy("dve", ptr[:, 3:6, :], View(pt.t[:, 384:768].rearrange("p (a b) -> p a b", b=128), pt[:].key))
                P.copy("act", ptr[0:64, 6, :], pt[0:64, 768:896])
                po = pso[u]
                vx = va[j] if R0 % 2 == 0 else vb[j]
                t0 = R0 // 2
                P.mm(po[:, 0:64], ptr[:, 0, :], vc[j][:, 0, :], start=True, stop=False)
                P.mm(po[:, 0:64], ptr[:, 1, :], vc[j][:, 1, :], start=False, stop=False)
                for kt in range(4):
                    P.mm(po[:, 0:64], ptr[:, 2 + kt, :], vx[:, t0 + kt, :], start=False, stop=False)
                P.mm(po[:, 0:64], ptr[0:64, 6, :], vx[0:64, t0 + 4, :], start=False, stop=True)
                o = oo[u]
                P.act(o[:], po[:, 0:64], AF.Identity, scale=st[:, 2:3])
                q0 = lat0 + r * 64
                P.dma("sp" if n % 2 else "pool", View(OT.t[q0:q0 + 128, h * 64:(h + 1) * 64], ("na_OT", None)), o[:])
    P.pop()
    P.push()
    pb = [P.ps("n3_ps%d" % j) for j in range(8)]
    L = Ctx()
    L.mean = P.sb("n3_mean", [128, TT], F32)
    L.rstd = P.sb("n3_rstd", [128, TT], F32)
    L.sq = P.sb("n3_sq", [128, 8, TT], F32)
    Wo = P.sb("n3_Wo", [128, 8, 1024], BF16)
    stg = [P.sb("n3_stg%d" % j, [128, 8, 512], F32) for j in range(2)]
    load_w_bf16(P, Wo, IN("na_w_out", [D, D]).t, 1024, stg)
    xt = P.sb("n3_x", [128, 8, TT], F32)
    yT = P.sb("n3_yT", [128, 8, TT], BF16)
    oa = [P.sb("n3_oa%d" % j, [128, 1024], F32) for j in range(2)]
    n = 0
    for t in tiles:
        if t == 0:
            continue
        tok = slice(t * TT, (t + 1) * TT)
        P.dma("sp", xt[:], View(C.XS.t[:, :, tok].rearrange("c p n -> p c n"), ("XS", t)))
        for s in range(4):
            a = oa[s % 2]
            r0 = t * TT + s * 128
            P.dma("sp" if s % 2 else "pool", a[:], View(OT.t[r0:r0 + 128, :], ("na_OT", None)))
            for c in range(8):
                pt = pb[2 + n % 6]
                n += 1
                P.transpose(pt[:, 0:128], a[:, c * 128:(c + 1) * 128], C.ident[:])
                P.copy("act" if c % 2 else "dve", yT[:, c, s * 128:(s + 1) * 128], pt[:, 0:128])
        out_proj_ln(P, C, L, i, t, xt, yT, Wo, pb)
    P.pop()


class Inputs:
    def __init__(self, P):
        self.P = P
        self.d = {}

    def __call__(self, name, shape=None, dt=F32):
        if name not in self.d:
            self.d[name] = self.P.dram(name, SHAPES[name] if shape is None else shape, dt, kind="ExternalInput")
        return self.d[name]


SHAPES = {
    "xT": [8, 128, NT], "cT": [128, 8, 3], "adab": [128, DEPTH * 48 * 3], "lng": [128, DEPTH * 2 * 8], "lnb": [128, DEPTH * 2 * 8],
    "ident": [128, 128], "selE": [NE, NE * 128], "rb": [128, NE], "router_w": [D, NE],
}


def build(stages, tiles=None):
    nc = bass.Bass("TRN2", target_bir_lowering=False)
    P = Prog(nc)
    C = Ctx()
    IN = Inputs(P)
    OUT = P.dram("out", [8, 128, NT], F32, kind="ExternalOutput")
    C.XS = P.dram("XS", [8, 128, NT], F32)
    C.W1B = P.dram("W1B", [NE, D, DE], BF16)
    C.W3B = P.dram("W3B", [NE, D, DE], BF16)
    C.W2B = P.dram("W2B", [NE, DE, D], BF16)
    tiles = list(range(NTILE)) if tiles is None else tiles
    for c in range(8):
        P.dma("sp" if c % 2 else "pool", C.XS[c], IN("xT")[c])
    phase_consts(P, C, IN)
    mixer_consts(P, C, IN)
    layers = sorted(set(i for (_, i) in stages))
    phase_mod(P, C, IN, layers)
    for (kind, i) in stages:
        if kind == "moe":
            phase_wprep(P, C, IN, i)
            phase_moe(P, C, IN, i, tiles if i < DEPTH - 1 else [t for t in tiles if t > 0])
        elif kind == "mixer":
            if i % 4 == 0:
                phase_gdn(P, C, IN, i, tiles)
            elif i % 4 == 1:
                phase_mlstm(P, C, IN, i, tiles)
            elif i % 4 == 2:
                phase_rwkv(P, C, IN, i, tiles)
            else:
                phase_na(P, C, IN, i, tiles)
    P.pop()
    P.push()
    for c in range(8):
        P.dma("sp" if c % 2 else "pool", OUT[c], C.XS[c])
    P.emit()
    return nc, P, IN


def host_inputs(inputs, core):
    f = np.float32
    b0, b1 = 2 * core, 2 * core + 1
    x, ctx = inputs["x"], inputs["ctx"]
    tok = np.concatenate([ctx[b0], ctx[b1], x[b0], x[b1]], axis=0)
    m = {}
    m["xT"] = np.ascontiguousarray(tok.T.reshape(8, 128, NT)).astype(f)
    c3 = np.stack([inputs["c"][b0], inputs["c"][b1], inputs["c_ctx"]], axis=0)
    m["cT"] = np.ascontiguousarray(c3.reshape(3, 8, 128).transpose(2, 1, 0)).astype(f)
    return m


def rowrep(v, n=128):
    v = np.asarray(v, np.float32).reshape(1, -1)
    return np.ascontiguousarray(np.broadcast_to(v, (n, v.shape[1])))


def fm(v):
    v = np.asarray(v, np.float32)
    return np.ascontiguousarray(v.reshape(-1, 128).T)


def host_shared(inputs, names):
    f = np.float32
    m = {}
    ab = inputs["ada_b"].reshape(DEPTH, 48, 128).transpose(2, 0, 1)
    m["adab"] = np.ascontiguousarray(np.repeat(ab[..., None], 3, axis=-1).reshape(128, -1)).astype(f)
    m["lng"] = np.ascontiguousarray(inputs["ln_g"].reshape(DEPTH, 2, 8, 128).transpose(3, 0, 1, 2).reshape(128, -1)).astype(f)
    m["lnb"] = np.ascontiguousarray(inputs["ln_b"].reshape(DEPTH, 2, 8, 128).transpose(3, 0, 1, 2).reshape(128, -1)).astype(f)
    m["ident"] = np.eye(128, dtype=f)
    sel = np.zeros((NE, NE, 128), f)
    for e in range(NE):
        sel[e, e, :] = 1.0
    m["selE"] = sel.reshape(NE, NE * 128)
    m["rb"] = rowrep(inputs["router_b"])
    m["router_w"] = inputs["router_w"]
    for i in range(DEPTH):
        m["ada_w_%d" % i] = inputs["ada_w"][i]
        m["moe_w1_%d" % i] = inputs["moe_w1"][i]
        m["moe_w3_%d" % i] = inputs["moe_w3"][i]
        m["moe_w2_%d" % i] = inputs["moe_w2"][i]
    a = np.arange(64)
    Uf = (a[:, None] <= a[None, :]).astype(f)
    Sf = (a[:, None] < a[None, :]).astype(f)
    m["cmats"] = np.concatenate([Uf, Uf.T, Sf, Sf.T], axis=1)
    p = np.arange(128)
    inv = (10000.0 ** (-(p % 32).astype(np.float64) / 32.0))
    t = np.arange(2048)
    posv = np.where((p // 64)[:, None] == 0, (t // 64)[None, :], (t % 64)[None, :]).astype(np.float64)
    ang = posv * inv[:, None]
    m["ropecos"] = np.cos(ang).astype(f)
    m["ropesin"] = np.sin(ang).astype(f)
    R = np.zeros((128, 128), f)
    for mm_ in range(128):
        if (mm_ % 64) < 32:
            R[mm_, mm_ + 32] = -1.0
        else:
            R[mm_, mm_ - 32] = 1.0
    m["rotT"] = np.ascontiguousarray(R.T)
    m["mlstm_w_in"] = inputs["mlstm_w_in"]
    m["mlstm_w_out"] = inputs["mlstm_w_out"]
    m["ml_gateb"] = rowrep(inputs["mlstm_gate_b"].reshape(-1))
    m["ml_normg"] = fm(inputs["mlstm_norm_g"])
    m["gdn_w_in"] = inputs["gdn_w_in"]
    m["gdn_w_out"] = inputs["gdn_w_out"]
    dtb = np.zeros((2, 16), f)
    dtb[:, 0:8] = inputs["gdn_dt_bias"]
    m["gd_dtb"] = rowrep(dtb.reshape(-1))
    al = np.zeros((2, 16), f)
    al[:, 0:8] = inputs["gdn_a_log"]
    m["gd_alog"] = rowrep(al.reshape(-1))
    m["gd_conv"] = np.ascontiguousarray(inputs["gdn_conv"].T.reshape(24, 128, 3).transpose(1, 0, 2).reshape(128, 72))
    m["gd_normg"] = np.asarray(inputs["gdn_norm_g"], f).reshape(128, 1)
    for k in ("rwkv_w_rkv", "rwkv_g1", "rwkv_a1", "rwkv_w1", "rwkv_g2", "rwkv_a2", "rwkv_w2", "rwkv_w_out"):
        m[k] = inputs[k]
    m["rw_w0"] = np.ascontiguousarray(np.broadcast_to(np.asarray(inputs["rwkv_w0"], f)[None], (128, 2, 1024)))
    cst = [fm(inputs["rwkv_mu"][j]) for j in range(6)]
    cst += [fm(inputs["rwkv_k_k"]), fm(inputs["rwkv_k_a"]), np.zeros((128, 8), f), fm(inputs["rwkv_r_k"].reshape(-1))]
    cst += [fm(inputs["rwkv_a0"][0]), fm(inputs["rwkv_a0"][1]), np.zeros((128, 8), f)]
    m["rw_cst"] = np.concatenate(cst, axis=1)
    bo = np.zeros((128, 128), f)
    bo[0:64, 0:64] = 1.0
    bo[64:128, 64:128] = 1.0
    m["rw_bones"] = bo
    m["rw_lnx"] = np.concatenate([fm(inputs["rwkv_lnx_g"]), fm(inputs["rwkv_lnx_b"])], axis=1)
    m["na_w_in"] = inputs["na_w_in"]
    m["na_w_out"] = inputs["na_w_out"]
    if "na_bias" in names:
        rpb = np.asarray(inputs["na_rpb"], f)
        bt = np.full((NA_H, 128, 5, 576), -30000.0, f)
        w = np.arange(64)
        c0 = np.clip(w - 8, 0, 48)
        for ty, (r, R0, r00, r01) in enumerate([(0, 0, 0, 0), (2, 0, 0, 0), (6, 2, 2, 3), (28, 23, 24, 24), (30, 23, 24, 24)]):
            for dq, r0q in ((0, r00), (1, r01)):
                qrow = r + dq
                for kr in range(9):
                    krow = R0 + kr
                    if not (r0q <= krow < r0q + 8):
                        continue
                    dr = krow - qrow + 7
                    wp = np.arange(64)
                    valid = (wp[None, :] >= c0[:, None]) & (wp[None, :] < c0[:, None] + 16)
                    dc = np.clip(wp[None, :] - w[:, None] + 15, 0, 30)
                    vals = rpb[:, dr, :][:, dc]
                    blk = np.where(valid[None], vals, f(-30000.0))
                    bt[:, dq * 64:(dq + 1) * 64, ty, kr * 64:(kr + 1) * 64] = blk
        m["na_bias"] = bt
    return {k: np.ascontiguousarray(np.asarray(v, f)) for k, v in m.items() if k in names}


def unpack_out(res_core):
    o = res_core["out"].reshape(D, NT).T
    return o[512:512 + 2048], o[512 + 2048:], o[0:256], o[256:512]


ALL_STAGES = [(k, i) for i in range(DEPTH) for k in ("mixer", "moe")]


def kernel(**inputs):
    inputs = {k: np.asarray(v) for k, v in inputs.items()}
    nc, _, IN = build(ALL_STAGES)
    shared = host_shared(inputs, set(IN.d.keys()))
    in_maps = []
    for core in range(NCORES):
        m = dict(shared)
        m.update(host_inputs(inputs, core))
        in_maps.append(m)
    res = run_bass_kernel_spmd(nc, in_maps, core_ids=list(range(NCORES)))
    out = np.zeros((16, 2048, D), np.float32)
    for core in range(NCORES):
        l0, l1, _, _ = unpack_out(res.results[core])
        out[2 * core] = l0
        out[2 * core + 1] = l1
    return out
```

```python
import numpy as np
import concourse.bass as bass
import concourse.mybir as mybir
from concourse.bass_utils import run_bass_kernel_spmd

F32 = mybir.dt.float32
BF16 = mybir.dt.bfloat16
AF = mybir.ActivationFunctionType
ALU = mybir.AluOpType
AX = mybir.AxisListType

D = 1024
DEPTH = 4
NT = 4608
TT = 512
NTILE = NT // TT
NE = 16
DE = 512
ALPHA = float((2 * DEPTH) ** 0.25)
LN_EPS = 1e-5
NCORES = 8


class View:
    __slots__ = ("ap", "key")

    def __init__(self, ap, key):
        self.ap = ap
        self.key = key

    def bc(self, shape):
        return View(self.ap.to_broadcast(list(shape)), self.key)


class Buf:
    def __init__(self, name, t):
        self.name = name
        self.t = t

    def __getitem__(self, idx):
        return View(self.t[idx], (self.name, None))

    def k(self, sub):
        return _SubBuf(self, sub)


class _SubBuf:
    def __init__(self, buf, sub):
        self.buf = buf
        self.sub = sub

    def __getitem__(self, idx):
        return View(self.buf.t[idx], (self.buf.name, self.sub))


class Op:
    __slots__ = ("eng", "fn", "deps", "is_dma", "id", "inc", "ticket", "dsem", "dval", "is_mm")


ENGS = ("pe", "act", "dve", "pool", "sp")
NDSEM = 14


class Prog:
    def __init__(self, nc):
        self.nc = nc
        self.ops = []
        self.state = {}
        self.scopes = [[]]
        self.pending = {}
        self.last = {}
        self.open_dmas = set()

    def _enter(self, cm):
        t = cm.__enter__()
        self.scopes[-1].append(cm)
        return t

    def sb(self, name, shape, dt):
        self.uid = getattr(self, "uid", 0) + 1
        nm = "S%d_%s" % (self.uid, name)
        return Buf(nm, self._enter(self.nc.sbuf_tensor(nm, list(shape), dt)))

    def ps(self, name, shape=(128, 512), dt=F32):
        self.uid = getattr(self, "uid", 0) + 1
        nm = "P_%d_%s" % (self.uid, name)
        return Buf(nm, self._enter(self.nc.psum_tensor(nm, list(shape), dt)))

    def dram(self, name, shape, dt, kind="Internal"):
        return Buf(name, self.nc.dram_tensor(name, list(shape), dt, kind=kind).ap())

    def push(self):
        self.scopes.append([])

    def pop(self):
        for cm in reversed(self.scopes.pop()):
            cm.__exit__(None, None, None)
        bar = set(self.last.values()) | set(self.open_dmas)
        self.open_dmas = set()
        self.pending = {e: set(bar) | self.pending.get(e, set()) for e in ENGS}

    def _deps(self, key, is_write):
        name, sub = key
        st = self.state.get(name)
        if not st:
            return set()
        subs = list(st.keys()) if sub is None else [s for s in (sub, None) if s in st]
        deps = set()
        for s in subs:
            w, rs = st[s]
            if w is not None:
                deps.add(w)
            if is_write:
                deps.update(rs)
        return deps

    def _record(self, key, is_write, opid):
        name, sub = key
        st = self.state.setdefault(name, {})
        if is_write:
            if sub is None:
                st.clear()
            st[sub] = [opid, []]
        else:
            if sub not in st:
                st[sub] = [None, []]
            st[sub][1].append(opid)

    def add(self, eng, fn, writes, reads, is_dma=False, is_mm=False):
        op = Op()
        op.eng, op.fn, op.is_dma, op.is_mm = eng, fn, is_dma, is_mm
        op.inc, op.ticket, op.dsem, op.dval = False, 0, None, 0
        op.deps = self.pending.pop(eng, set())
        op.id = len(self.ops)
        reads = self._vs(*reads)
        writes = self._vs(*writes)
        for v in reads:
            dr = self._deps(v.key, False)
            op.deps |= dr
            if v.key[0].startswith("P_"):
                for x in self._deps(v.key, True) - dr:
                    if self.ops[x].eng != eng:
                        op.deps.add(x)
        for v in writes:
            op.deps |= self._deps(v.key, True)
        for v in reads:
            self._record(v.key, False, op.id)
        for v in writes:
            self._record(v.key, True, op.id)
        self.ops.append(op)
        if is_dma:
            self.open_dmas.add(op.id)
        else:
            self.last[eng] = op.id
        return op

    @staticmethod
    def _a(x):
        return x.ap if isinstance(x, View) else x

    @staticmethod
    def _vs(*xs):
        return [x for x in xs if isinstance(x, View)]

    def mm(self, out, lhsT, rhs, start=True, stop=True, **kw):
        a = self._a
        return self.add("pe", lambda e: e.matmul(a(out), a(lhsT), a(rhs), start=start, stop=stop, **kw),
                        [out], [lhsT, rhs], is_mm=True)

    def transpose(self, out, in_, ident):
        a = self._a
        return self.add("pe", lambda e: e.transpose(a(out), a(in_), a(ident)), [out], [in_, ident], is_mm=True)

    def act(self, out, in_, func, bias=0.0, scale=1.0, accum_out=None):
        a = self._a
        kw = {}
        if accum_out is not None:
            kw["accum_out"] = a(accum_out)
        return self.add("act", lambda e: e.activation(out=a(out), in_=a(in_), func=func, bias=a(bias), scale=a(scale), **kw),
                        self._vs(out, accum_out), self._vs(in_, bias, scale))

    def copy(self, eng, out, in_):
        a = self._a
        if eng == "act":
            return self.add(eng, lambda e: e.copy(out=a(out), in_=a(in_)), [out], [in_])
        return self.add(eng, lambda e: e.tensor_copy(out=a(out), in_=a(in_)), [out], [in_])

    def tt(self, eng, out, in0, in1, op):
        a = self._a
        return self.add(eng, lambda e: e.tensor_tensor(out=a(out), in0=a(in0), in1=a(in1), op=op), [out], [in0, in1])

    def ts(self, eng, out, in0, s1, s2=None, op0=ALU.mult, op1=None, accum_out=None):
        a = self._a
        kw = {}
        if op1 is not None:
            kw["op1"] = op1
        if accum_out is not None:
            kw["accum_out"] = a(accum_out)
        return self.add(eng, lambda e: e.tensor_scalar(out=a(out), in0=a(in0), scalar1=a(s1), scalar2=a(s2), op0=op0, **kw),
                        self._vs(out, accum_out), self._vs(in0, s1, s2))

    def stt(self, eng, out, in0, scalar, in1, op0, op1):
        a = self._a
        eng = "dve"
        return self.add(eng, lambda e: e.scalar_tensor_tensor(out=a(out), in0=a(in0), scalar=a(scalar), in1=a(in1), op0=op0, op1=op1),
                        [out], self._vs(in0, scalar, in1))

    def memset(self, eng, out, val):
        a = self._a
        return self.add(eng, lambda e: e.memset(a(out), val), [out], [])

    def reduce(self, eng, out, in_, op, axis=AX.X):
        a = self._a
        return self.add(eng, lambda e: e.tensor_reduce(out=a(out), in_=a(in_), axis=axis, op=op), [out], [in_])

    def recip(self, out, in_):
        a = self._a
        return self.add("dve", lambda e: e.reciprocal(out=a(out), in_=a(in_)), [out], [in_])

    def dma(self, q, out, in_, **kw):
        a = self._a
        return self.add(q, lambda e: e.dma_start(out=a(out), in_=a(in_), **kw), [out], [in_], is_dma=True)

    def emit(self):
        nc = self.nc
        ops = self.ops

        def pe_chain(op, dop):
            return dop.eng == "pe" and op.eng == "pe" and op.is_mm and dop.is_mm

        for op in ops:
            for d in op.deps:
                dop = ops[d]
                if dop.is_dma or pe_chain(op, dop):
                    continue
                dop.inc = True
        semctx = []

        def newsem(name):
            cm = nc.semaphore(name)
            s = cm.__enter__()
            semctx.append(cm)
            return s

        esem = {e: newsem("s_" + e) for e in ENGS}
        dq = ("sp", "pool", "act")
        dsems = {q: [newsem("d_%s_%d" % (q, i)) for i in range(NDSEM)] for q in dq}
        dcount = {q: [0] * NDSEM for q in dq}
        drr = {q: 0 for q in dq}
        tick = {e: 0 for e in ENGS}
        per_eng = {e: [] for e in ENGS}
        waits = {}
        seen = {e: {} for e in ENGS}
        for op in ops:
            w = []
            sn = seen[op.eng]

            def want(sem, val, key):
                if sn.get(key, 0) >= val:
                    return
                sn[key] = val
                w.append((sem, val))

            if op.is_dma:
                q = op.eng
                i = drr[q]
                drr[q] = (i + 1) % NDSEM
                if dcount[q][i] > 0:
                    want(dsems[q][i], dcount[q][i] * 16, ("d", q, i))
                dcount[q][i] += 1
                op.dsem = (q, i)
                op.dval = dcount[q][i] * 16
            for d in sorted(op.deps):
                dop = ops[d]
                if dop.is_dma:
                    q, i = dop.dsem
                    want(dsems[q][i], dop.dval, ("d", q, i))
                elif not pe_chain(op, dop):
                    want(esem[dop.eng], dop.ticket, ("e", dop.eng))
            if (not op.is_dma) and op.inc:
                tick[op.eng] += 1
                op.ticket = tick[op.eng]
            waits[op.id] = w
            per_eng[op.eng].append(op)
        self.n_waits = sum(len(w) for w in waits.values())
        final = []
        for q in dq:
            for i in range(NDSEM):
                if dcount[q][i] > 0:
                    final.append((dsems[q][i], dcount[q][i] * 16))

        def run_engine(e, name):
            for op in per_eng[name]:
                for (sem, val) in waits[op.id]:
                    e.wait_ge(sem, val)
                ins = op.fn(e)
                if op.is_dma:
                    q, i = op.dsem
                    ins.then_inc(dsems[q][i], 16)
                elif op.inc:
                    ins.then_inc(esem[name], 1)
            if name == "sp":
                for (sem, val) in final:
                    e.wait_ge(sem, val)

        with nc.Block() as block:
            @block.sync
            def _(e):
                run_engine(e, "sp")

            @block.tensor
            def _(e):
                run_engine(e, "pe")

            @block.scalar
            def _(e):
                run_engine(e, "act")

            @block.vector
            def _(e):
                run_engine(e, "dve")

            @block.gpsimd
            def _(e):
                run_engine(e, "pool")
        for cm in reversed(semctx):
            cm.__exit__(None, None, None)
        while self.scopes:
            for cm in reversed(self.scopes.pop()):
                cm.__exit__(None, None, None)


def seg_of_tile(t):
    return 2 if t == 0 else (0 if t <= 4 else 1)


def modcol(i, grp, kc, seg):
    return (i * 48 + grp * 8 + kc) * 3 + seg


class Ctx:
    pass


def phase_consts(P, C, IN):
    C.ones = P.sb("ones", [128, 128], F32)
    C.ident = P.sb("ident", [128, 128], F32)
    C.identb = P.sb("identb", [128, 128], BF16)
    C.mod = P.sb("mod", [128, DEPTH * 48 * 3], F32)
    C.lng = P.sb("lng", [128, DEPTH * 2 * 8], F32)
    C.lnb = P.sb("lnb", [128, DEPTH * 2 * 8], F32)
    C.rw = P.sb("rw", [128, 8, NE], F32)
    C.rb = P.sb("rb", [128, NE], F32)
    C.selE = P.sb("selE", [NE, NE * 128], F32)
    C.eps = P.sb("epsc", [128, 1], F32)
    P.memset("dve", C.ones[:], 1.0)
    P.memset("dve", C.eps[:], LN_EPS)
    P.dma("sp", C.ident[:], IN("ident")[:, :])
    P.copy("dve", C.identb[:], C.ident[:])
    P.dma("sp", C.lng[:], IN("lng")[:, :])
    P.dma("sp", C.lnb[:], IN("lnb")[:, :])
    P.dma("sp", C.rw[:], IN("router_w").t.rearrange("(kc p) e -> p kc e", p=128))
    P.dma("sp", C.rb[:], IN("rb")[:, :])
    P.dma("sp", C.selE[:], IN("selE")[:, :])


def phase_mod(P, C, IN, layers):
    P.push()
    cT = P.sb("cT", [128, 8, 3], F32)
    cv = P.sb("cv", [128, 8, 3], F32)
    adab = P.sb("adab", [128, DEPTH * 48 * 3], F32)
    aw = [P.sb("aw%d" % j, [128, 8, 512], F32) for j in range(2)]
    pm = [P.ps("pm%d" % j) for j in range(2)]
    P.dma("sp", cT[:], IN("cT")[:, :, :])
    P.dma("sp", adab[:], IN("adab")[:, :])
    P.act(cv[:], cT[:], AF.Silu)
    n = 0
    for i in layers:
        awv = IN("ada_w_%d" % i, [D, 6 * D]).t.rearrange("(kc p) n -> p kc n", p=128)
        for p in range(12):
            a = aw[n % 2]
            ps = pm[n % 2]
            n += 1
            P.dma("sp" if n % 2 else "pool", a[:], awv[:, :, p * 512:(p + 1) * 512])
            for f in range(4):
                for kc in range(8):
                    P.mm(ps[:, f * 3:(f + 1) * 3], a[:, kc, f * 128:(f + 1) * 128], cv[:, kc, :], start=(kc == 0), stop=(kc == 7))
            c0 = (i * 48 + p * 4) * 3
            P.tt("dve", C.mod[:, c0:c0 + 12], ps[:, 0:12], adab[:, c0:c0 + 12], ALU.add)
        for grp in (1, 4):
            c0 = (i * 48 + grp * 8) * 3
            P.ts("dve", C.mod[:, c0:c0 + 24], C.mod[:, c0:c0 + 24], 1.0, None, op0=ALU.add)
    P.pop()


def ln_tile(P, C, L, y, out, i, which, ps_a, ps_b):
    mean, rstd, sq = L.mean, L.rstd, L.sq
    for kc in range(8):
        P.mm(ps_a[:], C.ones[:], y[:, kc, :], start=(kc == 0), stop=(kc == 7))
    P.act(mean[:], ps_a[:], AF.Identity, scale=1.0 / D)
    for kc in range(8):
        P.tt("pool" if kc % 2 else "dve", y[:, kc, :], y[:, kc, :], mean[:], ALU.subtract)
        P.act(sq[:, kc, :], y[:, kc, :], AF.Square)
    for kc in range(8):
        P.mm(ps_b[:], C.ones[:], sq[:, kc, :], start=(kc == 0), stop=(kc == 7))
    P.act(rstd[:], ps_b[:], AF.Sqrt, bias=C.eps[:], scale=1.0 / D)
    P.recip(rstd[:], rstd[:])
    for kc in range(8):
        col = (i * 2 + which) * 8 + kc
        P.stt("dve", sq[:, kc, :], y[:, kc, :], C.lng[:, col:col + 1], rstd[:], ALU.mult, ALU.mult)
        P.act(out[:, kc, :], sq[:, kc, :], AF.Identity, bias=C.lnb[:, col:col + 1])


def phase_wprep(P, C, IN, i):
    P.push()
    st = [P.sb("wp_f%d" % j, [128, 8, 512], F32) for j in range(3)]
    sb = [P.sb("wp_b%d" % j, [128, 8, 512], BF16) for j in range(3)]
    n = 0
    engs = ("pool", "act", "dve")
    for e in range(NE):
        for (src, dst, pat, isw2) in ((IN("moe_w1_%d" % i, [NE, D, DE]), C.W1B, "(kc p) n -> p kc n", False),
                                      (IN("moe_w3_%d" % i, [NE, D, DE]), C.W3B, "(kc p) n -> p kc n", False),
                                      (IN("moe_w2_%d" % i, [NE, DE, D]), C.W2B, "", True)):
            j = n % 3
            n += 1
            if isw2:
                sv = src.t[e].rearrange("(kc p) (h n) -> p kc h n", p=128, h=2)
                stv = View(st[j].t[:].rearrange("p (kc h) n -> p kc h n", h=2), st[j][:].key)
                P.dma("sp", stv, sv)
                P.copy(engs[j], sb[j][:], st[j][:])
                for half in range(2):
                    dv = dst.t[e, half].rearrange("p (kc n) -> p kc n", kc=4)
                    sbv = View(sb[j].t[:].rearrange("p (kc h) n -> p kc h n", h=2)[:, :, half, :], sb[j][:].key)
                    P.dma("pool", View(dv, (dst.name, e)), sbv)
            else:
                sv = src.t[e].rearrange(pat, p=128)
                dv = dst.t[e].rearrange("p (kc n) -> p kc n", kc=8)
                P.dma("sp", st[j][:], sv)
                P.copy(engs[j], sb[j][:], st[j][:])
                P.dma("pool", View(dv, (dst.name, e)), sb[j][:])
    P.pop()


def phase_moe(P, C, IN, i, tiles):
    P.push()
    L = Ctx()
    xt = P.sb("m_x", [128, 8, TT], F32)
    h2f = P.sb("m_h2f", [128, 8, TT], F32)
    h2b = P.sb("m_h2b", [128, 8, TT], BF16)
    hid = P.sb("m_hid", [128, NE * 4, TT], BF16)
    wst = [P.sb("m_w%d" % j, [128, 2, 8, 512], BF16) for j in range(2)]
    w2st = [P.sb("m_w2%d" % j, [128, 4, 512], BF16) for j in range(3)]
    L.mean = P.sb("m_mean", [128, TT], F32)
    L.rstd = P.sb("m_rstd", [128, TT], F32)
    L.sq = h2f
    cbs = [P.sb("m_cb%d" % j, [128, TT], F32) for j in range(2)]
    gS = [P.sb("m_g%d" % j, [128, TT], F32) for j in range(3)]
    uS = [P.sb("m_u%d" % j, [128, TT], F32) for j in range(3)]
    combT = P.sb("m_combT", [NE, TT], F32)
    R = [dict((nm, P.sb("r_%s%d" % (nm, j), [128, w], F32)) for nm, w in
              (("lg", 16), ("e", 16), ("pr", 16), ("sel", 16), ("eq", 16), ("s2", 16), ("msk", 16), ("pw", 16), ("cmb", 16),
               ("mx", 1), ("se", 1), ("m1", 4), ("m2", 4), ("gs", 4), ("gm", 1), ("ing", 4), ("sw", 1))) for j in range(2)]
    pb = [P.ps("m_ps%d" % j) for j in range(8)]
    rr = 0
    for t in tiles:
        seg = seg_of_tile(t)
        tok = slice(t * TT, (t + 1) * TT)
        P.dma("sp", xt[:], View(C.XS.t[:, :, tok].rearrange("c p n -> p c n"), ("XS", t)))
        for kc in range(8):
            c_sc = modcol(i, 4, kc, seg)
            c_sh = modcol(i, 3, kc, seg)
            P.ts("dve" if kc % 2 else "pool", h2f[:, kc, :], xt[:, kc, :], C.mod[:, c_sc:c_sc + 1], C.mod[:, c_sh:c_sh + 1], op0=ALU.mult, op1=ALU.add)
            P.copy("act", h2b[:, kc, :], h2f[:, kc, :])
        for s in range(4):
            r = R[rr % 2]
            rr += 1
            pl = pb[6 + (s % 2)]
            for kc in range(8):
                P.mm(pl[:, 0:16], h2f[:, kc, s * 128:(s + 1) * 128], C.rw[:, kc, :], start=(kc == 0), stop=(kc == 7))
            P.copy("act", r["lg"][:], pl[:, 0:16])
            P.reduce("dve", r["mx"][:], r["lg"][:], ALU.max)
            P.ts("dve", r["mx"][:], r["mx"][:], -1.0, None, op0=ALU.mult)
            P.act(r["e"][:], r["lg"][:], AF.Exp, bias=r["mx"][:], accum_out=r["se"][:])
            P.recip(r["se"][:], r["se"][:])
            P.ts("dve", r["pr"][:], r["e"][:], r["se"][:], None, op0=ALU.mult)
            P.tt("dve", r["sel"][:], r["pr"][:], C.rb[:], ALU.add)
            P.reduce("dve", r["m1"][:], View(r["sel"].t[:].rearrange("p (g k) -> p g k", k=4), r["sel"][:].key), ALU.max)
            for g in range(4):
                P.ts("dve", r["eq"][:, g * 4:(g + 1) * 4], r["sel"][:, g * 4:(g + 1) * 4], r["m1"][:, g:g + 1], None, op0=ALU.is_equal)
            P.stt("dve", r["s2"][:], r["eq"][:], -1e9, r["sel"][:], ALU.mult, ALU.add)
            P.reduce("dve", r["m2"][:], View(r["s2"].t[:].rearrange("p (g k) -> p g k", k=4), r["s2"][:].key), ALU.max)
            P.tt("dve", r["gs"][:], r["m1"][:], r["m2"][:], ALU.add)
            P.reduce("dve", r["gm"][:], r["gs"][:], ALU.max)
            P.ts("dve", r["ing"][:], r["gs"][:], r["gm"][:], None, op0=ALU.is_equal)
            for g in range(4):
                P.ts("dve", r["msk"][:, g * 4:(g + 1) * 4], r["sel"][:, g * 4:(g + 1) * 4], r["m2"][:, g:g + 1], r["ing"][:, g:g + 1],
                     op0=ALU.is_ge, op1=ALU.mult)
            P.tt("dve", r["pw"][:], r["pr"][:], r["msk"][:], ALU.mult)
            P.reduce("dve", r["sw"][:], r["pw"][:], ALU.add)
            P.recip(r["sw"][:], r["sw"][:])
            P.ts("dve", r["cmb"][:], r["pw"][:], r["sw"][:], None, op0=ALU.mult)
            P.transpose(pl[0:16, 128:256], r["cmb"][:], C.ident[:])
            P.copy("act", combT[:, s * 128:(s + 1) * 128], pl[0:16, 128:256])
        n = 0
        for e in range(NE):
            w = wst[e % 2]
            P.dma("sp", w[:, 0], View(C.W1B.t[e].rearrange("p (kc n) -> p kc n", kc=8), ("W1B", e)))
            P.dma("pool", w[:, 1], View(C.W3B.t[e].rearrange("p (kc n) -> p kc n", kc=8), ("W3B", e)))
            cb = cbs[e % 2]
            pc = pb[6 + (e % 2)]
            P.mm(pc[:], C.selE[:, e * 128:(e + 1) * 128], combT[:], start=True, stop=True)
            P.copy("act", cb[:], pc[:])
            for fc in range(4):
                p1 = pb[(n % 3) * 2]
                p3 = pb[(n % 3) * 2 + 1]
                g = gS[n % 3]
                u = uS[n % 3]
                n += 1
                for kc in range(8):
                    P.mm(p1[:], w[:, 0, kc, fc * 128:(fc + 1) * 128], h2b[:, kc, :], start=(kc == 0), stop=(kc == 7))
                for kc in range(8):
                    P.mm(p3[:], w[:, 1, kc, fc * 128:(fc + 1) * 128], h2b[:, kc, :], start=(kc == 0), stop=(kc == 7))
                P.act(g[:], p1[:], AF.Silu)
                P.tt("dve", u[:], g[:], p3[:], ALU.mult)
                P.tt("pool", hid[:, e * 4 + fc, :], u[:], cb[:], ALU.mult)
        n = 0
        for half in range(2):
            for e in range(NE):
                w2 = w2st[n % 3]
                n += 1
                P.dma("sp" if n % 2 else "pool", w2[:],
                      View(C.W2B.t[e, half].rearrange("p (fc n) -> p fc n", fc=4), ("W2B", e)))
                for fc in range(4):
                    for o in range(4):
                        P.mm(pb[half * 4 + o][:], w2[:, fc, o * 128:(o + 1) * 128], hid[:, e * 4 + fc, :],
                             start=(e == 0 and fc == 0), stop=(e == NE - 1 and fc == 3))
            for o in range(4):
                oc = half * 4 + o
                cg = modcol(i, 5, oc, seg)
                P.act(h2f[:, oc, :], pb[half * 4 + o][:], AF.Identity, scale=C.mod[:, cg:cg + 1])
                P.stt("dve", xt[:, oc, :], xt[:, oc, :], ALPHA, h2f[:, oc, :], ALU.mult, ALU.add)
        ln_tile(P, C, L, xt, xt, i, 1, pb[0], pb[1])
        P.dma("sp", View(C.XS.t[:, :, tok].rearrange("c p n -> p c n"), ("XS", t)), xt[:])
    P.pop()


CH = 64


def chunk_plan(b, d):
    ctx0 = 256 * b
    lat0 = 512 + 2048 * b
    blocks = [(ctx0, 4)] + [(lat0 + 512 * k, 8) for k in range(4)]
    if d == 0:
        return [(t0, list(range(n))) for (t0, n) in blocks]
    return [(blocks[0][0], [3, 2, 1, 0])] + [(t0, list(range(n - 1, -1, -1))) for (t0, n) in reversed(blocks[1:])]


def load_w_bf16(P, dst, src_ap, ncols, stg, engs=("pool", "act", "dve"), ctr=[0]):
    sv = src_ap.rearrange("(kc p) n -> p kc n", p=128)
    c0 = 0
    while c0 < ncols:
        w = min(512, ncols - c0)
        j = ctr[0] % len(stg)
        ctr[0] += 1
        P.dma("sp" if j % 2 else "pool", stg[j][:, :, 0:w], sv[:, :, c0:c0 + w])
        P.copy(engs[j % 3], dst[:, :, c0:c0 + w], stg[j][:, :, 0:w])
        c0 += w


def mixer_consts(P, C, IN):
    C.cm = P.sb("cm", [64, 4 * 64], F32)
    P.dma("sp", C.cm[:], IN("cmats", [64, 256])[:, :])
    C.ones64 = P.sb("ones64", [64, 128], F32)
    P.memset("dve", C.ones64[:], 1.0)


def cmat(C, name, d):
    idx = {"U": 0, "UT": 1, "S": 2, "ST": 3}[name]
    if d == 1:
        idx = idx ^ 1
    return C.cm[:, idx * 64:(idx + 1) * 64]


def load_xh(P, C, i, t, xt, hb, halo=False):
    seg = seg_of_tile(t)
    tok = slice(t * TT, (t + 1) * TT)
    P.dma("sp", xt[:], View(C.XS.t[:, :, tok].rearrange("c p n -> p c n"), ("XS", t)))
    for kc in range(8):
        c_sc = modcol(i, 1, kc, seg)
        c_sh = modcol(i, 0, kc, seg)
        P.ts("dve" if kc % 2 else "pool", hb[:, kc, :], xt[:, kc, :], C.mod[:, c_sc:c_sc + 1], C.mod[:, c_sh:c_sh + 1], op0=ALU.mult, op1=ALU.add)


def out_proj_ln(P, C, L, i, t, xt, yT, Wo, pb):
    seg = seg_of_tile(t)
    tok = slice(t * TT, (t + 1) * TT)
    for oc in range(8):
        ps = pb[2 + oc % 4]
        for kc in range(8):
            P.mm(ps[:], Wo[:, kc, oc * 128:(oc + 1) * 128], yT[:, kc, :], start=(kc == 0), stop=(kc == 7))
        cg = modcol(i, 2, oc, seg)
        P.act(L.sq[:, oc, :], ps[:], AF.Identity, scale=C.mod[:, cg:cg + 1])
        P.stt("dve", xt[:, oc, :], xt[:, oc, :], ALPHA, L.sq[:, oc, :], ALU.mult, ALU.add)
    ln_tile(P, C, L, xt, xt, i, 0, pb[0], pb[1])
    P.dma("sp", View(C.XS.t[:, :, tok].rearrange("c p n -> p c n"), ("XS", t)), xt[:])


def rope_evac(P, C, R, ps, dst, t, scale, n):
    y = R.y[n % 2]
    if t == 0:
        P.act(dst, ps[:], AF.Identity, scale=scale)
        return
    pos = ((t - 1) % 4) * TT
    P.act(y[:], ps[:], AF.Identity, scale=scale)
    pr = R.pr[n % 2]
    P.mm(pr[:], R.rotT[:], y[:], start=True, stop=True)
    y1 = R.y1[n % 2]
    P.tt("pool", y1[:], y[:], R.cos[:, pos:pos + TT], ALU.mult)
    y2 = R.y2[n % 2]
    P.tt("dve", y2[:], pr[:], R.sin[:, pos:pos + TT], ALU.mult)
    P.tt("pool", dst, y1[:], y2[:], ALU.add)


def rope_setup(P, C, IN, pbanks):
    R = Ctx()
    R.cos = P.sb("ropecos", [128, 2048], F32)
    R.sin = P.sb("ropesin", [128, 2048], F32)
    R.rotT = P.sb("rotT", [128, 128], F32)
    P.dma("sp", R.cos[:], IN("ropecos", [128, 2048])[:, :])
    P.dma("pool", R.sin[:], IN("ropesin", [128, 2048])[:, :])
    P.dma("sp", R.rotT[:], IN("rotT", [128, 128])[:, :])
    R.y = [P.sb("rp_y%d" % j, [128, TT], F32) for j in range(2)]
    R.y1 = [P.sb("rp_y1%d" % j, [128, TT], F32) for j in range(2)]
    R.y2 = [P.sb("rp_y2%d" % j, [128, TT], F32) for j in range(2)]
    R.pr = pbanks
    return R


ML_H = 4
ML_DV = 256
ML_VW = ML_DV + 1


def phase_mlstm(P, C, IN, i, tiles):
    QK = P.dram("ml_QK", [8, 128, NT], BF16)
    OG = P.dram("ml_OG", [8, 128, NT], BF16)
    KT = P.dram("ml_KT", [NT, 512], BF16)
    VT = P.dram("ml_VT", [NT, ML_H * ML_VW], BF16)
    GT = P.dram("ml_GT", [NT, 16], F32)
    OD = [P.dram("ml_OD%d" % d, [NT, 1024], F32) for d in range(2)]
    w_in = IN("mlstm_w_in", [D, 3088])
    P.push()
    pb = [P.ps("ml_ps%d" % j) for j in range(6)]
    pbt = [P.ps("ml_pt%d" % j, [128, 1024], BF16) for j in range(2)]
    W = P.sb("ml_W", [128, 8, 3088], BF16)
    stg = [P.sb("ml_stg%d" % j, [128, 8, 512], F32) for j in range(2)]
    load_w_bf16(P, W, w_in.t, 3088, stg)
    R = rope_setup(P, C, IN, pb[4:6])
    gb = P.sb("ml_gb", [128, 16], F32)
    P.dma("sp", gb[:], IN("ml_gateb", [128, 16])[:, :])
    xt = P.sb("ml_x", [128, 8, TT], F32)
    hb = P.sb("ml_hb", [128, 8, TT], BF16)
    qk = P.sb("ml_qk", [128, 8, TT], BF16)
    og = P.sb("ml_og", [128, 8, TT], BF16)
    kt = P.sb("ml_kt", [128, 4, 512], BF16)
    vt = P.sb("ml_vt", [128, 4, ML_H * ML_VW], BF16)
    gt = P.sb("ml_gt", [128, 4, 16], F32)
    ge = P.sb("ml_ge", [128, 4, 16], F32)
    P.memset("dve", vt[:], 1.0)
    n = 0
    for t in tiles:
        tok = slice(t * TT, (t + 1) * TT)
        load_xh(P, C, i, t, xt, hb)
        for oc in range(8):
            ps = pb[n % 4]
            for kc in range(8):
                P.mm(ps[:], W[:, kc, oc * 128:(oc + 1) * 128], hb[:, kc, :], start=(kc == 0), stop=(kc == 7))
            rope_evac(P, C, R, ps, qk[:, oc, :], t, (128.0 ** -0.5) if oc < 4 else 1.0, n)
            n += 1
            if oc >= 4:
                for s in range(4):
                    pt = pbt[s % 2]
                    P.transpose(pt[:, 0:128], qk[:, oc, s * 128:(s + 1) * 128], C.identb[:])
                    P.copy("act" if s % 2 else "dve", kt[:, s, (oc - 4) * 128:(oc - 3) * 128], pt[:, 0:128])
        for c in range(8):
            ps = pb[n % 4]
            n += 1
            for kc in range(8):
                P.mm(ps[:], W[:, kc, 2048 + c * 128:2048 + (c + 1) * 128], hb[:, kc, :], start=(kc == 0), stop=(kc == 7))
            P.act(og[:, c, :], ps[:], AF.Sigmoid)
        for s in range(4):
            for blk in range(2):
                ps = pb[n % 4]
                n += 1
                for kc in range(8):
                    P.mm(ps[:], hb[:, kc, s * 128:(s + 1) * 128], W[:, kc, 1024 + blk * 512:1024 + (blk + 1) * 512], start=(kc == 0), stop=(kc == 7))
                for hh in range(2):
                    h = blk * 2 + hh
                    P.copy("act" if hh else "dve", vt[:, s, h * ML_VW:h * ML_VW + ML_DV], ps[:, hh * 256:(hh + 1) * 256])
            ps = pb[n % 4]
            n += 1
            for kc in range(8):
                P.mm(ps[:, 0:16], hb[:, kc, s * 128:(s + 1) * 128], W[:, kc, 3072:3088], start=(kc == 0), stop=(kc == 7))
            P.tt("dve", gt[:, s, :], ps[:, 0:16], gb[:], ALU.add)
        P.act(ge[:], gt[:], AF.Exp, scale=-1.0)
        P.act(ge[:], ge[:], AF.Ln, bias=1.0)
        for d in range(2):
            P.ts("dve", gt[:, :, d * 8 + 4:d * 8 + 8], ge[:, :, d * 8 + 4:d * 8 + 8], -1.0, None, op0=ALU.mult)
        P.dma("sp", View(QK.t[:, :, tok].rearrange("c p n -> p c n"), ("ml_QK", t)), qk[:])
        P.dma("pool", View(OG.t[:, :, tok].rearrange("c p n -> p c n"), ("ml_OG", t)), og[:])
        P.dma("sp", View(KT.t[tok, :].rearrange("(s p) f -> p s f", p=128), ("ml_KT", t)), kt[:])
        P.dma("pool", View(VT.t[tok, :].rearrange("(s p) f -> p s f", p=128), ("ml_VT", t)), vt[:])
        P.dma("sp", View(GT.t[tok, :].rearrange("(s p) f -> p s f", p=128), ("ml_GT", t)), gt[:])
    P.pop()
    P.push()
    pb = [P.ps("m2_ps%d" % j) for j in range(8)]
    qkb = [P.sb("m2_qk%d" % j, [128, 8, TT], BF16) for j in range(2)]
    ktb = [P.sb("m2_kt%d" % j, [64, 8, 512], BF16) for j in range(2)]
    vtb = [P.sb("m2_vt%d" % j, [64, 8, ML_H * ML_VW], BF16) for j in range(2)]
    gtb = [P.sb("m2_gt%d" % j, [64, 8, 16], F32) for j in range(2)]
    S = [P.sb("m2_S%d" % j, [128, ML_H, ML_VW], F32) for j in range(4)]
    Sb = [P.sb("m2_Sb%d" % j, [128, ML_H, ML_VW], BF16) for j in range(4)]
    NB = 3
    eg = [P.sb("m2_eg%d" % j, [64, 8], F32) for j in range(NB)]
    ege = [P.sb("m2_ege%d" % j, [128, 4], F32) for j in range(NB)]
    e1la = [P.sb("m2_e1la%d" % j, [64, 64], F32) for j in range(NB)]
    gm = [P.sb("m2_gm%d" % j, [64, 64], F32) for j in range(NB)]
    ptb = [P.sb("m2_pt%d" % j, [64, 64], BF16) for j in range(NB)]
    kd = [P.sb("m2_kd%d" % j, [64, 128], BF16) for j in range(NB)]
    o1 = [P.sb("m2_o1%d" % j, [64, ML_VW], F32) for j in range(NB)]
    den = [P.sb("m2_den%d" % j, [64, 1], F32) for j in range(NB)]
    ob = [P.sb("m2_ob%d" % j, [64, 1024], F32) for j in range(2)]
    nblk = 0
    n = 0
    nch = 0
    chains = [(b, d) for b in range(2) for d in range(2)]
    plans = {bd: chunk_plan(*bd) for bd in chains}
    for ci, bd in enumerate(chains):
        P.memset("pool", S[ci][:], 0.0)
        P.memset("pool", Sb[ci][:], 0.0)
    for step in range(5):
        for ci, (b, d) in enumerate(chains):
            t0, clist = plans[(b, d)][step]
            ntok = 64 * len(clist)
            j = nblk % 2
            nblk += 1
            tk = ("blk", t0)
            P.dma("sp", qkb[j][:, :, 0:ntok], View(QK.t[:, :, t0:t0 + ntok].rearrange("c p n -> p c n"), ("ml_QK", None)))
            P.dma("pool", ktb[j][:, 0:len(clist), :], View(KT.t[t0:t0 + ntok, :].rearrange("(c p) f -> p c f", p=64), ("ml_KT", None)))
            P.dma("sp", vtb[j][:, 0:len(clist), :], View(VT.t[t0:t0 + ntok, :].rearrange("(c p) f -> p c f", p=64), ("ml_VT", None)))
            P.dma("pool", gtb[j][:, 0:len(clist), :], View(GT.t[t0:t0 + ntok, :].rearrange("(c p) f -> p c f", p=64), ("ml_GT", None)))
            for c in clist:
                cs = slice(c * 64, (c + 1) * 64)
                la = gtb[j][:, c, d * 8 + 4:d * 8 + 8]
                ip = gtb[j][:, c, d * 8:d * 8 + 4]
                m = nch % NB
                nch += 1
                pg = pb[6 + nch % 2]
                P.mm(pg[0:64, 0:4], cmat(C, "U", d), la, start=True, stop=True)
                P.mm(pg[0:64, 4:8], cmat(C, "ST", d), la, start=True, stop=True)
                P.mm(pg[:, 8:12], C.ones64[:], la, start=True, stop=True)
                P.tt("dve", eg[m][:, 4:8], pg[0:64, 4:8], ip, ALU.add)
                P.act(eg[m][:, 4:8], eg[m][:, 4:8], AF.Exp)
                P.act(eg[m][:, 0:4], pg[0:64, 0:4], AF.Exp)
                P.act(ege[m][:], pg[:, 8:12], AF.Exp)
                obuf = ob[nch % 2]
                for h in range(ML_H):
                    u = n % NB
                    n += 1
                    P.ts("dve", e1la[u][:], cmat(C, "ST", d), la[:, h:h + 1] if False else gtb[j][:, c, d * 8 + 4 + h:d * 8 + 5 + h], None, op0=ALU.mult)
                    pl = pb[(n % 3) * 2]
                    P.mm(pl[0:64, 0:64], e1la[u][:], cmat(C, "U", d), start=True, stop=True)
                    P.act(gm[u][:], pl[0:64, 0:64], AF.Exp, bias=gtb[j][:, c, d * 8 + h:d * 8 + h + 1])
                    P.tt("pool", gm[u][:], gm[u][:], cmat(C, "U", d), ALU.mult)
                    P.mm(pl[0:64, 64:128], qkb[j][:, 4 + h, cs], qkb[j][:, h, cs], start=True, stop=True)
                    P.tt("dve", ptb[u][:], pl[0:64, 64:128], gm[u][:], ALU.mult)
                    P.ts("pool", kd[u][:], ktb[j][:, c, h * 128:(h + 1) * 128], eg[m][:, 4 + h:5 + h], None, op0=ALU.mult)
                    vh = vtb[j][:, c, h * ML_VW:(h + 1) * ML_VW]
                    po = pb[(n % 3) * 2 + 1]
                    P.mm(po[0:64, 0:ML_VW], qkb[j][:, h, cs], Sb[ci][:, h, :], start=True, stop=True)
                    P.act(o1[u][:], po[0:64, 0:ML_VW], AF.Identity, scale=eg[m][:, h:h + 1])
                    P.mm(pl[0:64, 128:128 + ML_VW], ptb[u][:], vh, start=True, stop=True)
                    P.tt("dve", o1[u][:], o1[u][:], pl[0:64, 128:128 + ML_VW], ALU.add)
                    P.act(den[u][:], o1[u][:, ML_DV:ML_VW], AF.Abs)
                    P.ts("dve", den[u][:], den[u][:], 1.0, None, op0=ALU.max)
                    P.recip(den[u][:], den[u][:])
                    P.ts("pool", obuf[:, h * ML_DV:(h + 1) * ML_DV], o1[u][:, 0:ML_DV], den[u][:], None, op0=ALU.mult)
                    P.mm(po[:, 0:ML_VW], kd[u][:], vh, start=True, stop=True)
                    P.stt("dve", S[ci][:, h, :], S[ci][:, h, :], ege[m][:, h:h + 1], po[:, 0:ML_VW], ALU.mult, ALU.add)
                    P.copy("act", Sb[ci][:, h, :], S[ci][:, h, :])
                tk0 = t0 + c * 64
                P.dma("sp" if nch % 2 else "pool", View(OD[d].t[tk0:tk0 + 64, :], ("ml_OD%d" % d, None)), obuf[:])
    P.pop()
    P.push()
    pb = [P.ps("m3_ps%d" % j) for j in range(8)]
    L = Ctx()
    L.mean = P.sb("m3_mean", [128, TT], F32)
    L.rstd = P.sb("m3_rstd", [128, TT], F32)
    L.sq = P.sb("m3_sq", [128, 8, TT], F32)
    Wo = P.sb("m3_Wo", [128, 8, 1024], BF16)
    stg = [P.sb("m3_stg%d" % j, [128, 8, 512], F32) for j in range(2)]
    load_w_bf16(P, Wo, IN("mlstm_w_out", [D, D]).t, 1024, stg)
    ng = P.sb("m3_ng", [128, 8], F32)
    P.dma("sp", ng[:], IN("ml_normg", [128, 8])[:, :])
    xt = P.sb("m3_x", [128, 8, TT], F32)
    yT = P.sb("m3_yT", [128, 8, TT], BF16)
    ogt = P.sb("m3_og", [128, 8, TT], BF16)
    oa = [P.sb("m3_oa%d" % j, [128, 1024], F32) for j in range(2)]
    obb = [P.sb("m3_ob%d" % j, [128, 1024], F32) for j in range(2)]
    st = [P.sb("m3_st%d" % j, [128, 8], F32) for j in range(2)]
    n = 0
    for t in tiles:
        seg = seg_of_tile(t)
        tok = slice(t * TT, (t + 1) * TT)
        P.dma("sp", xt[:], View(C.XS.t[:, :, tok].rearrange("c p n -> p c n"), ("XS", t)))
        P.dma("pool", ogt[:], View(OG.t[:, :, tok].rearrange("c p n -> p c n"), ("ml_OG", None)))
        for s in range(4):
            a = oa[s % 2]
            bb = obb[s % 2]
            sv = st[s % 2]
            r0 = t * TT + s * 128
            P.dma("sp", a[:], View(OD[0].t[r0:r0 + 128, :], ("ml_OD0", None)))
            P.dma("pool", bb[:], View(OD[1].t[r0:r0 + 128, :], ("ml_OD1", None)))
            P.tt("pool", a[:], a[:], bb[:], ALU.add)
            P.reduce("dve", sv[:, 0:4], View(a.t[:].rearrange("p (h k) -> p h k", k=ML_DV), a[:].key), ALU.add)
            P.ts("dve", sv[:, 0:4], sv[:, 0:4], -1.0 / ML_DV, None, op0=ALU.mult)
            for h in range(ML_H):
                hs = slice(h * ML_DV, (h + 1) * ML_DV)
                P.ts("dve" if h % 2 else "pool", a[:, hs], a[:, hs], sv[:, h:h + 1], None, op0=ALU.add)
                P.act(bb[:, hs], a[:, hs], AF.Square, accum_out=sv[:, 4 + h:5 + h])
            P.ts("dve", sv[:, 4:8], sv[:, 4:8], 1.0 / ML_DV, 1e-6, op0=ALU.mult, op1=ALU.add)
            P.act(sv[:, 4:8], sv[:, 4:8], AF.Sqrt)
            P.recip(sv[:, 4:8], sv[:, 4:8])
            for h in range(ML_H):
                hs = slice(h * ML_DV, (h + 1) * ML_DV)
                P.ts("dve" if h % 2 else "pool", a[:, hs], a[:, hs], sv[:, 4 + h:5 + h], None, op0=ALU.mult)
            for c in range(8):
                pt = pb[2 + n % 6]
                n += 1
                P.transpose(pt[:, 0:128], a[:, c * 128:(c + 1) * 128], C.ident[:])
                P.stt("dve", yT[:, c, s * 128:(s + 1) * 128], pt[:, 0:128], ng[:, c:c + 1], ogt[:, c, s * 128:(s + 1) * 128], ALU.mult, ALU.mult)
        out_proj_ln(P, C, L, i, t, xt, yT, Wo, pb)
    P.pop()


GD_H = 8
SEQS = [(0, 256, False), (256, 256, False), (512, 2048, True), (2560, 2048, True)]


def inv_unit_lower(P, C, Bm, W, pbX, pbY, pbP):
    X = [Bm] + W.x
    Y = W.y
    Pm = W.p
    P.tt("pool", Pm[:], Y[0][:], C.ident[0:64, 0:64], ALU.add)
    for k in range(1, 6):
        cx = ((k - 1) % 2) * 64
        P.mm(pbX[0:64, cx:cx + 64], Y[k - 1][:], X[k - 1][:], start=True, stop=True)
        if k < 5:
            P.mm(pbY[0:64, cx:cx + 64], X[k - 1][:], Y[k - 1][:], start=True, stop=True)
        P.copy("act", X[k][:], pbX[0:64, cx:cx + 64])
        if k < 5:
            P.copy("dve", Y[k][:], pbY[0:64, cx:cx + 64])
        P.mm(pbP[0:64, cx:cx + 64], X[k][:], Pm[:], start=True, stop=True)
        P.tt("dve", Pm[:], Pm[:], pbP[0:64, cx:cx + 64], ALU.add)
    return Pm


def phase_gdn(P, C, IN, i, tiles):
    ZR = P.dram("gd_ZR", [24, 128, NT], F32)
    GG = P.dram("gd_GG", [8, 128, NT], BF16)
    QK = P.dram("gd_QK", [16, 128, NT], BF16)
    KT = P.dram("gd_KT", [NT, 1024], BF16)
    VT = P.dram("gd_VT", [NT, 1024], BF16)
    GT = P.dram("gd_GT", [NT, 32], F32)
    OD = [P.dram("gd_OD%d" % d, [NT, 1024], F32) for d in range(2)]
    P.push()
    pb = [P.ps("g1_ps%d" % j) for j in range(6)]
    W = P.sb("g1_W", [128, 8, 4128], BF16)
    stg = [P.sb("g1_stg%d" % j, [128, 8, 512], F32) for j in range(2)]
    load_w_bf16(P, W, IN("gdn_w_in", [D, 4128]).t, 4128, stg)
    dtb = P.sb("g1_dtb", [128, 32], F32)
    nea = P.sb("g1_nea", [128, 32], F32)
    P.dma("sp", dtb[:], IN("gd_dtb", [128, 32])[:, :])
    P.dma("sp", nea[:], IN("gd_alog", [128, 32])[:, :])
    P.act(nea[:], nea[:], AF.Exp)
    P.ts("dve", nea[:], nea[:], -1.0, None, op0=ALU.mult)
    xt = P.sb("g1_x", [128, 8, TT], F32)
    hb = P.sb("g1_hb", [128, 8, TT], BF16)
    zt = [P.sb("g1_z%d" % j, [128, 4, TT], F32) for j in range(2)]
    gg = P.sb("g1_gg", [128, 8, TT], BF16)
    gt = P.sb("g1_gt", [128, 4, 32], F32)
    ge = P.sb("g1_ge", [128, 4, 32], F32)
    n = 0
    for t in tiles:
        tok = slice(t * TT, (t + 1) * TT)
        load_xh(P, C, i, t, xt, hb)
        for g4 in range(6):
            z = zt[g4 % 2]
            for cc in range(4):
                oc = g4 * 4 + cc
                ps = pb[n % 4]
                n += 1
                for kc in range(8):
                    P.mm(ps[:], W[:, kc, oc * 128:(oc + 1) * 128], hb[:, kc, :], start=(kc == 0), stop=(kc == 7))
                P.copy("act" if cc % 2 else "dve", z[:, cc, :], ps[:])
            P.dma("sp" if g4 % 2 else "pool", View(ZR.t[g4 * 4:(g4 + 1) * 4, :, tok].rearrange("c p n -> p c n"), ("gd_ZR", t)), z[:])
        for c in range(8):
            ps = pb[n % 4]
            n += 1
            for kc in range(8):
                P.mm(ps[:], W[:, kc, 3072 + c * 128:3072 + (c + 1) * 128], hb[:, kc, :], start=(kc == 0), stop=(kc == 7))
            P.act(gg[:, c, :], ps[:], AF.Silu)
        P.dma("pool", View(GG.t[:, :, tok].rearrange("c p n -> p c n"), ("gd_GG", t)), gg[:])
        for s in range(4):
            ps = pb[4 + s % 2]
            for kc in range(8):
                P.mm(ps[:, 0:32], hb[:, kc, s * 128:(s + 1) * 128], W[:, kc, 4096:4128], start=(kc == 0), stop=(kc == 7))
            P.tt("dve", gt[:, s, :], ps[:, 0:32], dtb[:], ALU.add)
        P.act(ge[:], gt[:], AF.Exp)
        P.act(ge[:], ge[:], AF.Ln, bias=1.0)
        P.act(gt[:], gt[:], AF.Sigmoid)
        for d in range(2):
            for s in range(4):
                P.tt("dve", gt[:, s, d * 16:d * 16 + 8], ge[:, s, d * 16:d * 16 + 8], nea[:, d * 16:d * 16 + 8], ALU.mult)
        P.dma("sp", View(GT.t[tok, :].rearrange("(s p) f -> p s f", p=128), ("gd_GT", t)), gt[:])
    P.pop()
    P.push()
    pb = [P.ps("g1b_ps%d" % j) for j in range(6)]
    pbt = [P.ps("g1b_pt%d" % j, [128, 1024], BF16) for j in range(2)]
    R = rope_setup(P, C, IN, pb[4:6])
    cw = P.sb("g1b_cw", [128, 24 * 3], F32)
    P.dma("sp", cw[:], IN("gd_conv", [128, 72])[:, :])
    zr = [P.sb("g1b_z%d" % j, [128, 2048], F32) for j in range(2)]
    yy = [P.sb("g1b_y%d" % j, [128, 2048], F32) for j in range(2)]
    sq = P.sb("g1b_sq", [128, 512], F32)
    rn = P.sb("g1b_rn", [128, 512], F32)
    yb = [P.sb("g1b_yb%d" % j, [128, 2048], BF16) for j in range(2)]
    tm = [P.sb("g1b_tm%d" % j, [128, 16, 128], BF16) for j in range(2)]
    epsq = P.sb("g1b_epsq", [128, 1], F32)
    epsk = P.sb("g1b_epsk", [128, 1], F32)
    P.memset("dve", epsq[:], 1e-6 * 128.0)
    P.memset("dve", epsk[:], 1e-6)
    n = 0
    for c in range(24):
        for (t0, ns, is_lat) in SEQS:
            j = n % 2
            n += 1
            z = zr[j]
            y = yy[j]
            P.dma("sp" if n % 2 else "pool", z[:, 0:ns], View(ZR.t[c, :, t0:t0 + ns], ("gd_ZR", None)))
            P.ts("dve", y[:, 0:ns], z[:, 0:ns], cw[:, c * 3 + 1:c * 3 + 2], None, op0=ALU.mult)
            P.stt("pool", y[:, 1:ns], z[:, 0:ns - 1], cw[:, c * 3:c * 3 + 1], y[:, 1:ns], ALU.mult, ALU.add)
            P.stt("dve", y[:, 0:ns - 1], z[:, 1:ns], cw[:, c * 3 + 2:c * 3 + 3], y[:, 0:ns - 1], ALU.mult, ALU.add)
            P.act(y[:, 0:ns], y[:, 0:ns], AF.Silu)
            ybj = yb[j]
            if c < 16:
                for s0 in range(0, ns, 512):
                    w = min(512, ns - s0)
                    sl = slice(s0, s0 + w)
                    P.act(sq[:, 0:w], y[:, sl], AF.Square)
                    ps = pb[n % 2]
                    P.mm(ps[:, 0:w], C.ones[:], sq[:, 0:w], start=True, stop=True)
                    if c < 8:
                        P.act(rn[:, 0:w], ps[:, 0:w], AF.Sqrt, bias=epsq[:], scale=128.0)
                    else:
                        P.act(rn[:, 0:w], ps[:, 0:w], AF.Sqrt, bias=epsk[:], scale=1.0)
                    P.recip(rn[:, 0:w], rn[:, 0:w])
                    if is_lat:
                        P.tt("dve", y[:, sl], y[:, sl], rn[:, 0:w], ALU.mult)
                        pr = pb[2 + (s0 // 512) % 2]
                        P.mm(pr[:, 0:w], R.rotT[:], y[:, sl], start=True, stop=True)
                        P.tt("pool", sq[:, 0:w], y[:, sl], R.cos[:, sl], ALU.mult)
                        P.tt("dve", rn[:, 0:w], pr[:, 0:w], R.sin[:, sl], ALU.mult)
                        P.tt("pool", ybj[:, sl], sq[:, 0:w], rn[:, 0:w], ALU.add)
                    else:
                        P.tt("dve", ybj[:, sl], y[:, sl], rn[:, 0:w], ALU.mult)
                P.dma("sp", View(QK.t[c, :, t0:t0 + ns], ("gd_QK", None)), ybj[:, 0:ns])
            else:
                P.copy("pool", ybj[:, 0:ns], y[:, 0:ns])
            if c >= 8:
                tmj = tm[j]
                for s in range(ns // 128):
                    pt = pbt[s % 2]
                    P.transpose(pt[:, 0:128], ybj[:, s * 128:(s + 1) * 128], C.identb[:])
                    P.copy("act" if s % 2 else "dve", tmj[:, s, :], pt[:, 0:128])
                dst = KT if c < 16 else VT
                hh = (c - 8) % 8
                P.dma("pool", View(dst.t[t0:t0 + ns, hh * 128:(hh + 1) * 128].rearrange("(s p) f -> p s f", p=128), (dst.name, None)),
                      tmj[:, 0:ns // 128, :])
    P.pop()
    P.push()
    pb = [P.ps("g2_ps%d" % j) for j in range(8)]
    qkb = [P.sb("g2_qk%d" % j, [128, 16, TT], BF16) for j in range(2)]
    ktb = [P.sb("g2_kt%d" % j, [64, 8, 1024], BF16) for j in range(2)]
    vtb = [P.sb("g2_vt%d" % j, [64, 8, 1024], BF16) for j in range(2)]
    gtb = [P.sb("g2_gt%d" % j, [64, 8, 32], F32) for j in range(2)]
    S = [P.sb("g2_S%d" % j, [128, GD_H, 128], F32) for j in range(4)]
    Sb = [P.sb("g2_Sb%d" % j, [128, GD_H, 128], BF16) for j in range(4)]
    NB = 2
    eg = [P.sb("g2_eg%d" % j, [64, 32], F32) for j in range(NB)]
    ege = [P.sb("g2_ege%d" % j, [128, 8], F32) for j in range(NB)]
    ob = [P.sb("g2_ob%d" % j, [64, 1024], F32) for j in range(2)]

    def tset(j):
        T = Ctx()
        f = lambda nm, shp=(64, 64), dt=F32: P.sb("g2_%s%d" % (nm, j), list(shp), dt)
        T.ula, T.sla, T.gi, T.gj, T.A = f("ula"), f("sla"), f("gi"), f("gj"), f("A")
        T.x = [f("x%d" % k) for k in range(1, 6)]
        T.y = [f("y%d" % k) for k in range(0, 5)]
        T.p = f("p")
        T.ttb = f("ttb", dt=BF16)
        T.ptb = f("ptb", dt=BF16)
        T.bv = f("bv", (64, 128), BF16)
        T.bk = f("bk", (64, 128), BF16)
        T.kd = f("kd", (64, 128), BF16)
        T.u0 = f("u0", (64, 128))
        T.wx = f("wx", (128, 64), BF16)
        T.dl = f("dl", (64, 128), BF16)
        T.o1 = f("o1", (64, 128))
        return T

    TS = [tset(j) for j in range(2)]
    chains = [(b, d) for b in range(2) for d in range(2)]
    plans = {bd: chunk_plan(*bd) for bd in chains}
    for ci in range(4):
        P.memset("pool", S[ci][:], 0.0)
        P.memset("pool", Sb[ci][:], 0.0)
    nblk = 0
    n = 0
    nch = 0
    for step in range(5):
        for ci, (b, d) in enumerate(chains):
            t0, clist = plans[(b, d)][step]
            ntok = 64 * len(clist)
            nc_ = len(clist)
            j = nblk % 2
            nblk += 1
            P.dma("sp", qkb[j][:, :, 0:ntok], View(QK.t[:, :, t0:t0 + ntok].rearrange("c p n -> p c n"), ("gd_QK", None)))
            P.dma("pool", ktb[j][:, 0:nc_, :], View(KT.t[t0:t0 + ntok, :].rearrange("(c p) f -> p c f", p=64), ("gd_KT", None)))
            P.dma("sp", vtb[j][:, 0:nc_, :], View(VT.t[t0:t0 + ntok, :].rearrange("(c p) f -> p c f", p=64), ("gd_VT", None)))
            P.dma("pool", gtb[j][:, 0:nc_, :], View(GT.t[t0:t0 + ntok, :].rearrange("(c p) f -> p c f", p=64), ("gd_GT", None)))
            for c in clist:
                cs = slice(c * 64, (c + 1) * 64)
                la = gtb[j][:, c, d * 16:d * 16 + 8]
                be = gtb[j][:, c, d * 16 + 8:d * 16 + 16]
                m = nch % NB
                nch += 1
                pg = pb[3 + 4 * (nch % 2)]
                P.mm(pg[0:64, 448:456], cmat(C, "U", d), la, start=True, stop=True)
                P.mm(pg[0:64, 456:464], cmat(C, "ST", d), la, start=True, stop=True)
                P.mm(pg[:, 464:472], C.ones64[:], la, start=True, stop=True)
                P.act(eg[m][:, 0:16], pg[0:64, 448:464], AF.Exp)
                P.act(ege[m][:], pg[:, 464:472], AF.Exp)
                P.tt("dve", eg[m][:, 16:24], eg[m][:, 0:8], be, ALU.mult)
                P.ts("dve", eg[m][:, 24:32], be, -1.0, None, op0=ALU.mult)
                obuf = ob[nch % 2]
                for h in range(GD_H):
                    T = TS[n % 2]
                    b0, b1, b2, b3 = [pb[4 * (n % 2) + q] for q in range(4)]
                    n += 1
                    lah = gtb[j][:, c, d * 16 + h:d * 16 + h + 1]
                    kT = qkb[j][:, 8 + h, cs]
                    qT = qkb[j][:, h, cs]
                    P.ts("dve", T.ula[:], cmat(C, "U", d), lah, None, op0=ALU.mult)
                    P.ts("pool", T.sla[:], cmat(C, "ST", d), lah, None, op0=ALU.mult)
                    P.mm(b0[0:64, 0:64], T.ula[:], cmat(C, "ST", d), start=True, stop=True)
                    P.mm(b0[0:64, 64:128], T.sla[:], cmat(C, "U", d), start=True, stop=True)
                    P.mm(b0[0:64, 128:192], kT, kT, start=True, stop=True)
                    P.mm(b0[0:64, 192:256], kT, qT, start=True, stop=True)
                    P.act(T.gi[:], b0[0:64, 0:64], AF.Exp)
                    P.act(T.gj[:], b0[0:64, 64:128], AF.Exp)
                    P.tt("pool", T.gi[:], T.gi[:], cmat(C, "ST", d), ALU.mult)
                    P.tt("pool", T.gj[:], T.gj[:], cmat(C, "U", d), ALU.mult)
                    P.stt("dve", T.A[:], b0[0:64, 128:192], eg[m][:, 24 + h:25 + h], T.gi[:], ALU.mult, ALU.mult)
                    P.tt("dve", T.ptb[:], b0[0:64, 192:256], T.gj[:], ALU.mult)
                    P.transpose(b0[0:64, 256:320], T.A[:], C.ident[0:64, 0:64])
                    P.copy("act", T.y[0][:], b0[0:64, 256:320])
                    Tm = inv_unit_lower(P, C, T.A, T, b1, b2, b3)
                    P.copy("act", T.ttb[:], Tm[:])
                    P.ts("pool", T.bv[:], vtb[j][:, c, h * 128:(h + 1) * 128], gtb[j][:, c, d * 16 + 8 + h:d * 16 + 9 + h], None, op0=ALU.mult)
                    P.ts("pool", T.bk[:], ktb[j][:, c, h * 128:(h + 1) * 128], eg[m][:, 16 + h:17 + h], None, op0=ALU.mult)
                    P.ts("pool", T.kd[:], ktb[j][:, c, h * 128:(h + 1) * 128], eg[m][:, 8 + h:9 + h], None, op0=ALU.mult)
                    P.mm(b0[0:64, 320:448], T.ttb[:], T.bv[:], start=True, stop=True)
                    P.copy("dve", T.u0[:], b0[0:64, 320:448])
                    P.mm(b1[:, 128:192], T.bk[:], T.ttb[:], start=True, stop=True)
                    P.act(T.wx[:], b1[:, 128:192], AF.Identity, scale=-1.0)
                    P.mm(b2[0:64, 128:256], T.wx[:], Sb[ci][:, h, :], start=True, stop=True)
                    P.tt("dve", T.dl[:], b2[0:64, 128:256], T.u0[:], ALU.add)
                    P.mm(b3[0:64, 128:256], qT, Sb[ci][:, h, :], start=True, stop=True)
                    P.act(T.o1[:], b3[0:64, 128:256], AF.Identity, scale=eg[m][:, h:h + 1])
                    P.mm(b1[0:64, 256:384], T.ptb[:], T.dl[:], start=True, stop=True)
                    P.tt("dve", obuf[:, h * 128:(h + 1) * 128], T.o1[:], b1[0:64, 256:384], ALU.add)
                    P.mm(b2[:, 256:384], T.kd[:], T.dl[:], start=True, stop=True)
                    P.stt("dve", S[ci][:, h, :], S[ci][:, h, :], ege[m][:, h:h + 1], b2[:, 256:384], ALU.mult, ALU.add)
                    P.copy("act", Sb[ci][:, h, :], S[ci][:, h, :])
                tk0 = t0 + c * 64
                P.dma("sp" if nch % 2 else "pool", View(OD[d].t[tk0:tk0 + 64, :], ("gd_OD%d" % d, None)), obuf[:])
    P.pop()
    P.push()
    pb = [P.ps("g3_ps%d" % j) for j in range(8)]
    L = Ctx()
    L.mean = P.sb("g3_mean", [128, TT], F32)
    L.rstd = P.sb("g3_rstd", [128, TT], F32)
    L.sq = P.sb("g3_sq", [128, 8, TT], F32)
    Wo = P.sb("g3_Wo", [128, 8, 1024], BF16)
    stg = [P.sb("g3_stg%d" % j, [128, 8, 512], F32) for j in range(2)]
    load_w_bf16(P, Wo, IN("gdn_w_out", [D, D]).t, 1024, stg)
    ng = P.sb("g3_ng", [128, 1], F32)
    P.dma("sp", ng[:], IN("gd_normg", [128, 1])[:, :])
    xt = P.sb("g3_x", [128, 8, TT], F32)
    yT = P.sb("g3_yT", [128, 8, TT], BF16)
    ggt = P.sb("g3_gg", [128, 8, TT], BF16)
    oa = [P.sb("g3_oa%d" % j, [128, 1024], F32) for j in range(2)]
    obb = [P.sb("g3_ob%d" % j, [128, 1024], F32) for j in range(2)]
    st = [P.sb("g3_st%d" % j, [128, 8], F32) for j in range(2)]
    n = 0
    for t in tiles:
        tok = slice(t * TT, (t + 1) * TT)
        P.dma("sp", xt[:], View(C.XS.t[:, :, tok].rearrange("c p n -> p c n"), ("XS", t)))
        P.dma("pool", ggt[:], View(GG.t[:, :, tok].rearrange("c p n -> p c n"), ("gd_GG", None)))
        for s in range(4):
            a = oa[s % 2]
            bb = obb[s % 2]
            sv = st[s % 2]
            r0 = t * TT + s * 128
            P.dma("sp", a[:], View(OD[0].t[r0:r0 + 128, :], ("gd_OD0", None)))
            P.dma("pool", bb[:], View(OD[1].t[r0:r0 + 128, :], ("gd_OD1", None)))
            P.tt("pool", a[:], a[:], bb[:], ALU.add)
            for h in range(GD_H):
                hs = slice(h * 128, (h + 1) * 128)
                P.act(bb[:, hs], a[:, hs], AF.Square, accum_out=sv[:, h:h + 1])
            P.ts("dve", sv[:], sv[:], 1.0 / 128, 1e-6, op0=ALU.mult, op1=ALU.add)
            P.act(sv[:], sv[:], AF.Sqrt)
            P.recip(sv[:], sv[:])
            for h in range(GD_H):
                hs = slice(h * 128, (h + 1) * 128)
                P.ts("dve" if h % 2 else "pool", a[:, hs], a[:, hs], sv[:, h:h + 1], None, op0=ALU.mult)
            for c in range(8):
                pt = pb[2 + n % 6]
                n += 1
                P.transpose(pt[:, 0:128], a[:, c * 128:(c + 1) * 128], C.ident[:])
                P.stt("dve", yT[:, c, s * 128:(s + 1) * 128], pt[:, 0:128], ng[:, 0:1], ggt[:, c, s * 128:(s + 1) * 128], ALU.mult, ALU.mult)
        out_proj_ln(P, C, L, i, t, xt, yT, Wo, pb)
    P.pop()


RW_H = 16
RW_DEBUG = 0
RW_CUT = 0
HT = 128


def dma_heads_out(P, X, u0, n, src):
    for half in range(2):
        dv = X.t.rearrange("(c two) p n -> two c p n", two=2)[half][:, :, u0:u0 + n].rearrange("c p n -> p c n")
        sv = View(src.ap[half * 64:(half + 1) * 64], src.key)
        P.dma("sp" if half else "pool", View(dv, (X.name, None)), sv)


def phase_rwkv(P, C, IN, i, tiles):
    RF = P.dram("rw_RF", [16, 64, NT], F32)
    KK = P.dram("rw_KK", [16, 64, NT], F32)
    KD = [P.dram("rw_KD%d" % d, [16, 64, NT], F32) for d in range(2)]
    AT = [P.dram("rw_AT%d" % d, [16, 64, NT], F32) for d in range(2)]
    LW = [P.dram("rw_LW%d" % d, [NT, 1024], F32) for d in range(2)]
    VT = P.dram("rw_VT", [NT, 1024], BF16)
    GG = P.dram("rw_GG", [8, 128, NT], BF16)
    BN = P.dram("rw_BN", [8, 128, NT], F32)
    OD = [P.dram("rw_OD%d" % d, [NT, 1024], F32) for d in range(2)]
    P.push()
    pb = [P.ps("r1_ps%d" % j) for j in range(8)]
    Wr = [P.sb("r1_W%d" % j, [128, 8, 1024], BF16) for j in range(3)]
    g1w = P.sb("r1_g1w", [128, 8, 128], BF16)
    a1w = [P.sb("r1_a1w%d" % d, [128, 8, 64], BF16) for d in range(2)]
    w1w = [P.sb("r1_w1w%d" % d, [128, 8, 64], BF16) for d in range(2)]
    g2w = P.sb("r1_g2w", [128, 1024], BF16)
    a2w = [P.sb("r1_a2w%d" % d, [64, 1024], BF16) for d in range(2)]
    w2w = [P.sb("r1_w2w%d" % d, [64, 1024], BF16) for d in range(2)]
    P.push()
    stg = [P.sb("r1_stg%d" % j, [128, 8, 512], F32) for j in range(2)]
    for j in range(3):
        load_w_bf16(P, Wr[j], IN("rwkv_w_rkv", [3, D, D]).t[j], 1024, stg)
    load_w_bf16(P, g1w, IN("rwkv_g1", [D, 128]).t, 128, stg)
    for d in range(2):
        load_w_bf16(P, a1w[d], IN("rwkv_a1", [2, D, 64]).t[d], 64, stg)
        load_w_bf16(P, w1w[d], IN("rwkv_w1", [2, D, 64]).t[d], 64, stg)
    sflat = stg[0].t[:].rearrange("p a b -> p (a b)")
    skey = stg[0][:].key
    P.dma("sp", View(sflat[:, 0:1024], skey), IN("rwkv_g2", [128, D])[:, :])
    P.copy("dve", g2w[:], View(sflat[:, 0:1024], skey))
    for d in range(2):
        P.dma("sp", View(sflat[0:64, 0:1024], skey), IN("rwkv_a2", [2, 64, D])[d])
        P.copy("dve", a2w[d][:], View(sflat[0:64, 0:1024], skey))
        P.dma("sp", View(sflat[0:64, 0:1024], skey), IN("rwkv_w2", [2, 64, D])[d])
        P.copy("dve", w2w[d][:], View(sflat[0:64, 0:1024], skey))
    P.pop()
    w0r = P.sb("r1_w0", [128, 2, 1024], F32)
    P.dma("sp", w0r[:], IN("rw_w0", [128, 2, 1024])[:, :, :])
    cst = P.sb("r1_cst", [128, 13 * 8], F32)
    P.dma("sp", cst[:], IN("rw_cst", [128, 104])[:, :])
    P.ts("dve", cst[:, 64:72], cst[:, 56:64], -1.0, 1.0, op0=ALU.mult, op1=ALU.add)
    bones = P.sb("r1_bones", [128, 128], F32)
    P.dma("sp", bones[:], IN("rw_bones", [128, 128])[:, :])
    eps6 = P.sb("r1_eps6", [128, 1], F32)
    P.memset("dve", eps6[:], 1e-6)
    mhalf = P.sb("r1_mhalf", [128, 1], F32)
    P.memset("dve", mhalf[:], -0.5)
    HL = 64
    HW = HT + 2 * HL
    hx = P.sb("r1_hx", [128, 8, HW], F32)
    dx = P.sb("r1_dx", [128, 8, HT], F32)
    xm = [P.sb("r1_xm%d" % j, [128, 8, HT], BF16) for j in range(2)]
    kf = P.sb("r1_kf", [128, 8, HT], F32)
    vf = P.sb("r1_vf", [128, 8, HT], F32)
    rr = P.sb("r1_rr", [128, 8, HT], F32)
    kkf = P.sb("r1_kkf", [128, 8, HT], F32)
    of = [P.sb("r1_of%d" % j, [128, 8, HT], F32) for j in range(2)]
    ogb = P.sb("r1_ogb", [128, 8, HT], BF16)
    vtt = P.sb("r1_vtt", [128, HT // 128, 1024], BF16)
    lwt = [P.sb("r1_lwt%d" % j, [128, max(HT // 128, 1), 1024], F32) for j in range(2)]
    tmp = [P.sb("r1_tmp%d" % j, [128, 512], F32) for j in range(4)]
    tl = [P.sb("r1_tl%d" % j, [128, HT], BF16) for j in range(2)]
    n = 0
    nm = 0

    def mix(j):
        nonlocal nm
        x = xm[nm % 2]
        nm += 1
        for kc in range(8):
            P.stt("dve", x[:, kc, :], dx[:, kc, :], cst[:, j * 8 + kc:j * 8 + kc + 1], hx[:, kc, HL:HL + HT], ALU.mult, ALU.add)
        return x

    def proj_fm(x, Wt, oc, ncols=128, krows=128, kchunks=8):
        nonlocal n
        ps = pb[n % 6]
        n += 1
        for kc in range(kchunks):
            P.mm(ps[0:ncols, 0:HT], Wt[:, kc, oc * 128:oc * 128 + ncols], x[:, kc, :], start=(kc == 0), stop=(kc == kchunks - 1))
        return ps

    units = []
    for t in tiles:
        units += [(t, hf) for hf in range(TT // HT)]
    for (t, hf) in units:
        seg = seg_of_tile(t)
        u0 = t * TT + hf * HT
        sq0, sqn = [(a, b) for (a, b, _) in SEQS if a <= u0 < a + b][0]
        has_l, has_r = u0 > sq0, u0 + HT < sq0 + sqn
        P.dma("sp", hx[:, :, HL:HL + HT], View(C.XS.t[:, :, u0:u0 + HT].rearrange("c p n -> p c n"), ("XS", t)))
        if has_l:
            P.dma("pool", hx[:, :, 0:HL], View(C.XS.t[:, :, u0 - HL:u0].rearrange("c p n -> p c n"), ("XS", (u0 - 1) // TT)))
        if has_r:
            P.dma("pool", hx[:, :, HL + HT:HW], View(C.XS.t[:, :, u0 + HT:u0 + HT + HL].rearrange("c p n -> p c n"), ("XS", (u0 + HT) // TT)))
        for kc in range(8):
            c_sc = modcol(i, 1, kc, seg)
            c_sh = modcol(i, 0, kc, seg)
            P.ts("dve" if kc % 2 else "pool", hx[:, kc, HL - 1:HL + HT + 1], hx[:, kc, HL - 1:HL + HT + 1], C.mod[:, c_sc:c_sc + 1], C.mod[:, c_sh:c_sh + 1], op0=ALU.mult, op1=ALU.add)
        if not has_l:
            P.memset("pool", hx[:, :, HL - 1:HL], 0.0)
        if not has_r:
            P.memset("pool", hx[:, :, HL + HT:HL + HT + 1], 0.0)
        for kc in range(8):
            P.tt("pool", dx[:, kc, :], hx[:, kc, HL - 1:HL - 1 + HT], hx[:, kc, HL + 1:HL + 1 + HT], ALU.add)
            P.stt("dve", dx[:, kc, :], dx[:, kc, :], 0.5, hx[:, kc, HL:HL + HT], ALU.mult, ALU.subtract)
        x = mix(0)
        o = of[0]
        for c in range(8):
            ps = proj_fm(x, Wr[0], c)
            P.copy("act", o[:, c, :], ps[:, 0:HT])
            P.ts("pool", rr[:, c, :], o[:, c, :], cst[:, 72 + c:73 + c], None, op0=ALU.mult)
        dma_heads_out(P, RF, u0, HT, o[:])
        x = mix(2)
        o = of[1]
        for c in range(8):
            ps = proj_fm(x, Wr[1], c)
            P.copy("act", kf[:, c, :], ps[:, 0:HT])
            t1 = tmp[c % 2]
            P.ts("pool", t1[:, 0:HT], kf[:, c, :], cst[:, 48 + c:49 + c], None, op0=ALU.mult)
            t2 = tmp[2 + c % 2]
            P.act(t2[:, 0:HT], t1[:, 0:HT], AF.Square)
            pq = pb[6 + c % 2]
            P.mm(pq[:, 0:HT], bones[:], t2[:, 0:HT], start=True, stop=True)
            P.act(t2[:, 0:HT], pq[:, 0:HT], AF.Sqrt, bias=eps6[:])
            P.recip(t2[:, 0:HT], t2[:, 0:HT])
            P.tt("pool", kkf[:, c, :], t1[:, 0:HT], t2[:, 0:HT], ALU.mult)
        dma_heads_out(P, KK, u0, HT, kkf[:])
        x = mix(3)
        for c in range(8):
            ps = proj_fm(x, Wr[2], c)
            P.copy("act", vf[:, c, :], ps[:, 0:HT])
        for s in range(HT // 128):
            for blk in range(2):
                ps = pb[n % 6]
                n += 1
                for kc in range(8):
                    P.mm(ps[:], x[:, kc, s * 128:(s + 1) * 128], Wr[2][:, kc, blk * 512:(blk + 1) * 512], start=(kc == 0), stop=(kc == 7))
                P.copy("act" if blk else "dve", vtt[:, s, blk * 512:(blk + 1) * 512], ps[:])
        P.dma("sp", View(VT.t[u0:u0 + HT, :].rearrange("(s p) f -> p s f", p=128), ("rw_VT", None)), vtt[:])
        x = mix(5)
        ps = proj_fm(x, g1w, 0)
        P.act(tl[0][:], ps[:, 0:HT], AF.Sigmoid)
        for c in range(8):
            ps = pb[n % 6]
            n += 1
            P.mm(ps[:, 0:HT], g2w[:, c * 128:(c + 1) * 128], tl[0][:], start=True, stop=True)
            P.copy("act", ogb[:, c, :], ps[:, 0:HT])
        P.dma("pool", View(GG.t[:, :, u0:u0 + HT].rearrange("c p n -> p c n"), ("rw_GG", None)), ogb[:])
        xa = mix(4)
        kdo = [of[0], of[1]]
        for d in range(2):
            ps = proj_fm(xa, a1w[d], 0, ncols=64)
            P.copy("act", tl[d][0:64, :], ps[0:64, 0:HT])
        ato = lwt
        atv = [View(lwt[d].t[:].rearrange("p a b -> p (a b)")[:, 0:8 * HT].rearrange("p (c n) -> p c n", c=8), lwt[d][:].key) for d in range(2)]
        for c in range(8):
            pbn = pb[6 + c % 2]
            for d in range(2):
                ps = pb[n % 6]
                n += 1
                P.mm(ps[:, 0:HT], a2w[d][:, c * 128:(c + 1) * 128], tl[d][0:64, :], start=True, stop=True)
                af = tmp[d]
                P.act(af[:, 0:HT], ps[:, 0:HT], AF.Sigmoid, bias=cst[:, 80 + d * 8 + c:81 + d * 8 + c])
                P.tt("pool", View(atv[d].ap[:, c, :], atv[d].key), af[:, 0:HT], kkf[:, c, :], ALU.mult)
                P.ts("dve", af[:, 0:HT], af[:, 0:HT], cst[:, 56 + c:57 + c], cst[:, 64 + c:65 + c], op0=ALU.mult, op1=ALU.add)
                P.tt("dve", kdo[d][:, c, :], kf[:, c, :], af[:, 0:HT], ALU.mult)
                t2 = tmp[2 + d]
                P.tt("pool", t2[:, 0:HT], rr[:, c, :], kdo[d][:, c, :], ALU.mult)
                P.mm(pbn[:, 0:HT], bones[:], t2[:, 0:HT], start=(d == 0), stop=(d == 1))
            P.tt("dve", vf[:, c, :], vf[:, c, :], pbn[:, 0:HT], ALU.mult)
        for d in range(2):
            dma_heads_out(P, KD[d], u0, HT, kdo[d][:])
            dma_heads_out(P, AT[d], u0, HT, atv[d])
        P.dma("sp", View(BN.t[:, :, u0:u0 + HT].rearrange("c p n -> p c n"), ("rw_BN", None)), vf[:])
        xw = mix(1)
        for d in range(2):
            ps = proj_fm(xw, w1w[d], 0, ncols=64)
            P.act(tl[d][0:64, :], ps[0:64, 0:HT], AF.Tanh)
        for d in range(2):
            lo = lwt[d]
            for s in range(HT // 128):
                for blk in range(2):
                    ps = pb[n % 6]
                    n += 1
                    P.mm(ps[:], tl[d][0:64, s * 128:(s + 1) * 128], w2w[d][:, blk * 512:(blk + 1) * 512], start=True, stop=True)
                    sl = slice(blk * 512, (blk + 1) * 512)
                    tq = tmp[(s * 2 + blk) % 4]
                    P.tt("dve", tq[:], ps[:], w0r[:, d, sl], ALU.add)
                    P.act(tq[:], tq[:], AF.Exp, scale=-1.0)
                    P.act(tq[:], tq[:], AF.Ln, bias=1.0)
                    P.act(tq[:], tq[:], AF.Exp, scale=-1.0, bias=mhalf[:])
                    P.ts("pool", lo[:, s, sl], tq[:], -1.0, None, op0=ALU.mult)
            P.dma("sp" if d else "pool", View(LW[d].t[u0:u0 + HT, :].rearrange("(s p) f -> p s f", p=128), ("rw_LW%d" % d, None)), lo[:])
    P.pop()
    P.push()
    B = [P.ps("r2_ps%d" % j) for j in range(8)]
    HG = 4
    HW4 = HG * 64
    cm4 = P.sb("r2_cm4", [64, 2, 1536], F32)
    P.dma("sp", cm4[:], IN("rw_cm4", [64, 2, 1536])[:, :, :])
    fmb = [[P.sb("r2_fm%d_%d" % (k, j), [64, HG, TT], F32) for k in range(4)] for j in range(2)]
    lwb = [P.sb("r2_lw%d" % j, [64, 8, HW4], F32) for j in range(2)]
    vtb = [P.sb("r2_vt%d" % j, [64, 8, HW4], BF16) for j in range(2)]
    S = P.sb("r2_S", [64, 64, 64], F32)
    Sb = P.sb("r2_Sb", [64, 64, 64], BF16)
    for q in range(8):
        P.memset("pool", S[:, q * 8:(q + 1) * 8, :], 0.0)
        P.memset("dve", Sb[:, q * 8:(q + 1) * 8, :], 0.0)
    ob = [P.sb("r2_ob%d" % j, [64, 8, HW4], F32) for j in range(2)]

    shared = {}

    def tset(j):
        T = Ctx()
        f = lambda nm, shp=(64, HG, 64), dt=F32: P.sb("r2_%s%d" % (nm, j), list(shp), dt)
        T.cls, T.ecl, T.ece, T.encl, T.dec, T.A = f("cls"), f("ecl"), f("ece"), f("encl"), f("dec"), f("A")
        T.br = f("br", (64, HG, 2, 64), BF16)
        T.sk = f("sk", dt=BF16)
        T.sa = f("sa", dt=BF16)
        T.kdec = f("kdec")
        T.adec = f("adec")
        T.kdT = f("kdT", dt=BF16)
        T.nadT = f("nadT", dt=BF16)
        T.g23 = f("g23", (64, HG, 2, 64), BF16)
        T.ng4t = f("ng4t", dt=BF16)
        if j == 0:
            T.x = [f("x%d" % k) for k in range(1, 6)]
            T.y = [f("y0")] + [f("y%d" % k) for k in range(1, 5)]
            shared["x"], shared["y"] = T.x, T.y
        else:
            T.x = shared["x"]
            T.y = [f("y0")] + shared["y"][1:]
        T.p = f("p")
        T.ttb = f("ttb", dt=BF16)
        T.g2v = f("g2v")
        T.zz = f("zz", dt=BF16)
        T.ub = f("ub", dt=BF16)
        T.stmp = f("stmp")
        return T

    TS = [tset(j) for j in range(2)]
    chains = [(b, d) for b in range(2) for d in range(2)]
    plans = {bd: chunk_plan(*bd) for bd in chains}
    nblk = 0
    n = 0

    def f2(v3):
        return View(v3.ap.rearrange("p a b -> p (a b)"), v3.key)

    for step in range(5):
        for ci, (b, d) in enumerate(chains):
            t0, clist = plans[(b, d)][step]
            ntok = 64 * len(clist)
            nc_ = len(clist)
            last = 63 if d == 0 else 0
            U4 = cm4[:, d, 0:256]
            S4 = cm4[:, d, 256:512]
            ST4 = cm4[:, d, 512:768]
            SU4 = cm4[:, d, 768:1280]
            I4 = cm4[:, d, 1280:1536]
            Ud, Sd = cmat(C, "U", d), cmat(C, "S", d)
            for hg in range(RW_H // HG):
                j = nblk % 2
                nblk += 1
                hs = slice(hg * HG, (hg + 1) * HG)
                for k, src in enumerate((RF, KK, KD[d], AT[d])):
                    P.dma("sp" if k % 2 else "pool", fmb[j][k][:, :, 0:ntok], View(src.t[hs, :, t0:t0 + ntok].rearrange("h p n -> p h n"), (src.name, None)))
                P.dma("sp", lwb[j][:, 0:nc_, :], View(LW[d].t[t0:t0 + ntok, hg * HW4:(hg + 1) * HW4].rearrange("(c p) f -> p c f", p=64), ("rw_LW%d" % d, None)))
                P.dma("pool", vtb[j][:, 0:nc_, :], View(VT.t[t0:t0 + ntok, hg * HW4:(hg + 1) * HW4].rearrange("(c p) f -> p c f", p=64), ("rw_VT", None)))
                obuf = ob[j]
                si0 = ci * 16 + hg * HG
                for c in clist:
                    cs = slice(c * 64, (c + 1) * 64)
                    T = TS[n % 2]
                    n += 1
                    H = range(HG)
                    c64 = lambda hh: slice(hh * 64, (hh + 1) * 64)
                    rF, kkF, kdF, atF = [fmb[j][k][:, :, cs] for k in range(4)]
                    for hh in H:
                        lw = lwb[j][:, c, c64(hh)]
                        P.mm(B[0][0:64, hh * 64:(hh + 1) * 64], lw, Ud, start=True, stop=True)
                        P.mm(B[0][0:64, 256 + hh * 64:256 + (hh + 1) * 64], lw, Sd, start=True, stop=True)
                    P.copy("dve", f2(T.cls[:]), B[0][0:64, 0:256])
                    P.act(f2(T.ecl[:]), B[0][0:64, 0:256], AF.Exp)
                    P.act(f2(T.ece[:]), B[0][0:64, 256:512], AF.Exp)
                    P.act(f2(T.encl[:]), B[0][0:64, 0:256], AF.Exp, scale=-1.0)
                    lam = View(T.ecl.t[:, :, last:last + 1].to_broadcast([64, HG, 64]), T.ecl[:].key)
                    P.tt("dve", T.dec[:], T.encl[:], lam, ALU.mult)
                    P.tt("pool", T.br[:, :, 0, :], kkF, T.ece[:], ALU.mult)
                    P.tt("dve", T.br[:, :, 1, :], rF, T.ecl[:], ALU.mult)
                    P.tt("pool", T.sk[:], kdF, T.encl[:], ALU.mult)
                    P.tt("dve", T.sa[:], atF, T.encl[:], ALU.mult)
                    P.tt("pool", T.kdec[:], kdF, T.dec[:], ALU.mult)
                    P.tt("pool", T.adec[:], atF, T.dec[:], ALU.mult)
                    for hh in H:
                        P.transpose(B[7][0:64, hh * 64:(hh + 1) * 64], T.kdec[:, hh, :], C.ident[0:64, 0:64])
                        P.transpose(B[7][0:64, 256 + hh * 64:256 + (hh + 1) * 64], T.adec[:, hh, :], C.ident[0:64, 0:64])
                    P.copy("act", f2(T.kdT[:]), B[7][0:64, 0:256])
                    P.act(f2(T.nadT[:]), B[7][0:64, 256:512], AF.Identity, scale=-1.0)
                    for hh in H:
                        bt = T.br[:, hh, 0, :]
                        brh = View(T.br.t[:, hh].rearrange("p a b -> p (a b)"), T.br[:].key)
                        P.mm(B[1][0:64, hh * 64:(hh + 1) * 64], bt, T.sa[:, hh, :], start=True, stop=True)
                        P.mm(B[2][0:64, hh * 128:(hh + 1) * 128], T.sk[:, hh, :], brh, start=True, stop=True)
                        P.mm(B[3][0:64, hh * 128:(hh + 1) * 128], T.sa[:, hh, :], brh, start=True, stop=True)
                    b3v = B[3].t[0:64, :].rearrange("p (h t k) -> p h t k", h=HG, t=2)
                    P.stt("dve", f2(T.A[:]), B[1][0:64, 0:256], -1.0, ST4, ALU.mult, ALU.mult)
                    P.stt("dve", T.y[0][:], View(b3v[:, :, 0, :], B[3][:].key), -1.0, View(S4.ap.rearrange("p (h k) -> p h k", h=HG), S4.key), ALU.mult, ALU.mult)
                    P.tt("dve", View(T.g23.t[:].rearrange("p a b c -> p (a b c)"), T.g23[:].key), B[2][0:64, :], SU4, ALU.mult)
                    P.stt("dve", T.ng4t[:], View(b3v[:, :, 1, :], B[3][:].key), -1.0, View(U4.ap.rearrange("p (h k) -> p h k", h=HG), U4.key), ALU.mult, ALU.mult)
                    X = [T.A] + T.x
                    Y = T.y
                    Pm = T.p
                    P.tt("pool", f2(Pm[:]), f2(Y[0][:]), I4, ALU.add)
                    for k in range(1, 6):
                        cx = ((k - 1) % 2) * 256
                        for hh in H:
                            P.mm(B[4][0:64, cx + hh * 64:cx + (hh + 1) * 64], Y[k - 1][:, hh, :], X[k - 1][:, hh, :], start=True, stop=True)
                        if k < 5:
                            for hh in H:
                                P.mm(B[5][0:64, cx + hh * 64:cx + (hh + 1) * 64], X[k - 1][:, hh, :], Y[k - 1][:, hh, :], start=True, stop=True)
                        P.copy("act", f2(X[k][:]), B[4][0:64, cx:cx + 256])
                        if k < 5:
                            P.copy("dve", f2(Y[k][:]), B[5][0:64, cx:cx + 256])
                        for hh in H:
                            P.mm(B[6][0:64, cx + hh * 64:cx + (hh + 1) * 64], X[k][:, hh, :], Pm[:, hh, :], start=True, stop=True)
                        P.tt("dve", f2(Pm[:]), f2(Pm[:]), B[6][0:64, cx:cx + 256], ALU.add)
                    P.copy("act", T.ttb[:], Pm[:])
                    for hh in H:
                        P.mm(B[1][0:64, 256 + hh * 64:256 + (hh + 1) * 64], T.g23[:, hh, 0, :], vtb[j][:, c, c64(hh)], start=True, stop=True)
                    P.copy("act", f2(T.g2v[:]), B[1][0:64, 256:512])
                    for hh in H:
                        P.mm(B[0][0:64, hh * 64:(hh + 1) * 64], T.br[:, hh, 0, :], Sb[:, si0 + hh, :], start=True, stop=True)
                    P.tt("dve", f2(T.zz[:]), B[0][0:64, 0:256], f2(T.g2v[:]), ALU.add)
                    for hh in H:
                        P.mm(B[0][0:64, 256 + hh * 64:256 + (hh + 1) * 64], T.ttb[:, hh, :], T.zz[:, hh, :], start=True, stop=True)
                    P.copy("act", f2(T.ub[:]), B[0][0:64, 256:512])
                    for hh in H:
                        o_ = B[2][0:64, hh * 64:(hh + 1) * 64]
                        vh = vtb[j][:, c, c64(hh)]
                        P.mm(o_, T.br[:, hh, 1, :], Sb[:, si0 + hh, :], start=True, stop=False)
                        P.mm(o_, T.g23[:, hh, 1, :], vh, start=False, stop=False)
                        P.mm(o_, T.ng4t[:, hh, :], T.ub[:, hh, :], start=False, stop=True)
                    P.copy("act", obuf[:, c, :], B[2][0:64, 0:256])
                    for hh in H:
                        s_ = B[3][0:64, hh * 64:(hh + 1) * 64]
                        vh = vtb[j][:, c, c64(hh)]
                        P.mm(s_, T.kdT[:, hh, :], vh, start=True, stop=False)
                        P.mm(s_, T.nadT[:, hh, :], T.ub[:, hh, :], start=False, stop=True)
                    Sg = S[:, si0:si0 + HG, :]
                    P.tt("pool", T.stmp[:], Sg, lam, ALU.mult)
                    P.tt("dve", f2(Sg), f2(T.stmp[:]), B[3][0:64, 0:256], ALU.add)
                    P.copy("act", Sb[:, si0:si0 + HG, :], Sg)
                P.dma("sp", View(OD[d].t[t0:t0 + ntok, hg * HW4:(hg + 1) * HW4].rearrange("(c p) f -> p c f", p=64), ("rw_OD%d" % d, None)),
                      obuf[:, 0:nc_, :])
    P.pop()
    if RW_DEBUG in (2, 4):
        return
    P.push()
    pb = [P.ps("r3_ps%d" % j) for j in range(8)]
    L = Ctx()
    L.mean = P.sb("r3_mean", [128, TT], F32)
    L.rstd = P.sb("r3_rstd", [128, TT], F32)
    L.sq = P.sb("r3_sq", [128, 8, TT], F32)
    Wo = P.sb("r3_Wo", [128, 8, 1024], BF16)
    stg = [P.sb("r3_stg%d" % j, [128, 8, 512], F32) for j in range(2)]
    load_w_bf16(P, Wo, IN("rwkv_w_out", [D, D]).t, 1024, stg)
    lx = P.sb("r3_lx", [128, 16], F32)
    P.dma("sp", lx[:], IN("rw_lnx", [128, 16])[:, :])
    xt = P.sb("r3_x", [128, 8, TT], F32)
    yT = P.sb("r3_yT", [128, 8, TT], BF16)
    ggt = P.sb("r3_gg", [128, 8, TT], BF16)
    bnt = P.sb("r3_bn", [128, 8, TT], F32)
    oa = [P.sb("r3_oa%d" % j, [128, 1024], F32) for j in range(2)]
    obb = [P.sb("r3_ob%d" % j, [128, 1024], F32) for j in range(2)]
    st = [P.sb("r3_st%d" % j, [128, 32], F32) for j in range(2)]
    yf = [P.sb("r3_yf%d" % j, [128, 128], F32) for j in range(2)]
    n = 0
    for t in tiles:
        tok = slice(t * TT, (t + 1) * TT)
        P.dma("sp", xt[:], View(C.XS.t[:, :, tok].rearrange("c p n -> p c n"), ("XS", t)))
        P.dma("pool", ggt[:], View(GG.t[:, :, tok].rearrange("c p n -> p c n"), ("rw_GG", None)))
        P.dma("sp", bnt[:], View(BN.t[:, :, tok].rearrange("c p n -> p c n"), ("rw_BN", None)))
        for s in range(4):
            a = oa[s % 2]
            bb = obb[s % 2]
            sv = st[s % 2]
            r0 = t * TT + s * 128
            P.dma("sp", a[:], View(OD[0].t[r0:r0 + 128, :], ("rw_OD0", None)))
            P.dma("pool", bb[:], View(OD[1].t[r0:r0 + 128, :], ("rw_OD1", None)))
            P.tt("pool", a[:], a[:], bb[:], ALU.add)
            P.reduce("dve", sv[:, 0:16], View(a.t[:].rearrange("p (h k) -> p h k", k=64), a[:].key), ALU.add)
            P.ts("dve", sv[:, 0:16], sv[:, 0:16], -1.0 / 64, None, op0=ALU.mult)
            for h in range(RW_H):
                hs = slice(h * 64, (h + 1) * 64)
                P.ts("dve" if h % 2 else "pool", a[:, hs], a[:, hs], sv[:, h:h + 1], None, op0=ALU.add)
                P.act(bb[:, hs], a[:, hs], AF.Square, accum_out=sv[:, 16 + h:17 + h])
            P.ts("dve", sv[:, 16:32], sv[:, 16:32], 1.0 / 64, 64e-5, op0=ALU.mult, op1=ALU.add)
            P.act(sv[:, 16:32], sv[:, 16:32], AF.Sqrt)
            P.recip(sv[:, 16:32], sv[:, 16:32])
            for h in range(RW_H):
                hs = slice(h * 64, (h + 1) * 64)
                P.ts("dve" if h % 2 else "pool", a[:, hs], a[:, hs], sv[:, 16 + h:17 + h], None, op0=ALU.mult)
            for c in range(8):
                pt = pb[2 + n % 6]
                y = yf[n % 2]
                n += 1
                sl = slice(s * 128, (s + 1) * 128)
                P.transpose(pt[:, 0:128], a[:, c * 128:(c + 1) * 128], C.ident[:])
                P.act(y[:], pt[:, 0:128], AF.Identity, scale=lx[:, c:c + 1], bias=lx[:, 8 + c:9 + c])
                P.tt("pool", y[:], y[:], bnt[:, c, sl], ALU.add)
                P.tt("dve", yT[:, c, sl], y[:], ggt[:, c, sl], ALU.mult)
        out_proj_ln(P, C, L, i, t, xt, yT, Wo, pb)
    P.pop()


NA_H = 16


def na_geom(r):
    R0 = min(max(r - 4, 0), 23)
    ty = 0 if r == 0 else 1 if r == 2 else 3 if r == 28 else 4 if r == 30 else 2
    return ty, R0


def dma_heads_out_b(P, X, u0, n, src):
    for half in range(2):
        dv = X.t.rearrange("(c two) p n -> two c p n", two=2)[half][:, :, u0:u0 + n].rearrange("c p n -> p c n")
        sv = View(src.ap[half * 64:(half + 1) * 64], src.key)
        P.dma("sp" if half else "pool", View(dv, (X.name, None)), sv)


def phase_na(P, C, IN, i, tiles):
    QF = P.dram("na_QF", [16, 64, NT], BF16)
    KF = P.dram("na_KF", [16, 64, NT], BF16)
    VT = P.dram("na_VT", [NT, 1024], BF16)
    OT = P.dram("na_OT", [NT, 1024], F32)
    P.push()
    pb = [P.ps("n1_ps%d" % j) for j in range(6)]
    W = P.sb("n1_W", [128, 8, 3072], BF16)
    P.push()
    stg = [P.sb("n1_stg%d" % j, [128, 8, 512], F32) for j in range(2)]
    load_w_bf16(P, W, IN("na_w_in", [D, 3072]).t, 3072, stg)
    P.pop()
    xt = P.sb("n1_x", [128, 8, TT], F32)
    hb = P.sb("n1_hb", [128, 8, TT], BF16)
    qf = P.sb("n1_qf", [128, 8, TT], BF16)
    kfb = P.sb("n1_kf", [128, 8, TT], BF16)
    vt = P.sb("n1_vt", [128, 4, 1024], BF16)
    n = 0
    for t in tiles:
        tok = slice(t * TT, (t + 1) * TT)
        load_xh(P, C, i, t, xt, hb)
        for c in range(16):
            if c < 8 and t == 0:
                continue
            ps = pb[n % 6]
            n += 1
            for kc in range(8):
                P.mm(ps[:], W[:, kc, c * 128:(c + 1) * 128], hb[:, kc, :], start=(kc == 0), stop=(kc == 7))
            if c < 8:
                P.act(qf[:, c, :], ps[:], AF.Identity, scale=0.125)
            else:
                P.copy("dve", kfb[:, c - 8, :], ps[:])
        if t > 0:
            dma_heads_out_b(P, QF, t * TT, TT, qf[:])
        dma_heads_out_b(P, KF, t * TT, TT, kfb[:])
        for s in range(4):
            for blk in range(2):
                ps = pb[n % 6]
                n += 1
                for kc in range(8):
                    P.mm(ps[:], hb[:, kc, s * 128:(s + 1) * 128], W[:, kc, 2048 + blk * 512:2048 + (blk + 1) * 512], start=(kc == 0), stop=(kc == 7))
                P.copy("act" if blk else "dve", vt[:, s, blk * 512:(blk + 1) * 512], ps[:])
        P.dma("sp", View(VT.t[tok, :].rearrange("(s p) f -> p s f", p=128), ("na_VT", None)), vt[:])
    P.pop()
    P.push()
    ps1 = [P.ps("n2_s1%d" % j) for j in range(2)]
    ps2 = [P.ps("n2_s2%d" % j) for j in range(2)]
    pst = [P.ps("n2_pt%d" % j, [128, 1024], BF16) for j in range(2)]
    pso = [P.ps("n2_po%d" % j) for j in range(2)]
    bias = [P.sb("n2_bias%d" % j, [128, 5, 576], F32) for j in range(2)]
    qT = [P.sb("n2_q%d" % j, [64, 2048], BF16) for j in range(2)]
    kT = [P.sb("n2_k%d" % j, [64, 2304], BF16) for j in range(2)]
    va = [P.sb("n2_va%d" % j, [128, 16, 64], BF16) for j in range(2)]
    vb = [P.sb("n2_vb%d" % j, [128, 16, 64], BF16) for j in range(2)]
    vc = [P.sb("n2_vc%d" % j, [128, 2, 64], BF16) for j in range(2)]
    Sm = [P.sb("n2_S%d" % j, [128, 832], F32) for j in range(2)]
    Pm = [P.sb("n2_P%d" % j, [128, 896], BF16) for j in range(2)]
    PT = [P.sb("n2_PT%d" % j, [128, 7, 128], BF16) for j in range(2)]
    sm = [P.sb("n2_sm%d" % j, [128, 4], F32) for j in range(2)]
    oo = [P.sb("n2_o%d" % j, [128, 64], F32) for j in range(2)]
    for j in range(2):
        P.memset("pool", vb[j][:], 0.0)
    bsrc = IN("na_bias", [NA_H, 128, 5, 576])
    n = 0
    nhb = 0
    for h in range(NA_H):
        bj = bias[h % 2]
        P.dma("sp", bj[:], bsrc[h])
        for b in range(2):
            j = nhb % 2
            nhb += 1
            lat0 = 512 + 2048 * b
            ctx0 = 256 * b
            P.dma("sp", qT[j][:], View(QF.t[h, :, lat0:lat0 + 2048], ("na_QF", None)))
            P.dma("pool", kT[j][:, 0:2048], View(KF.t[h, :, lat0:lat0 + 2048], ("na_KF", None)))
            P.dma("pool", kT[j][:, 2048:2304], View(KF.t[h, :, ctx0:ctx0 + 256], ("na_KF", None)))
            P.dma("sp", va[j][:], View(VT.t[lat0:lat0 + 2048, h * 64:(h + 1) * 64].rearrange("(t p) f -> p t f", p=128), ("na_VT", None)))
            P.dma("pool", vb[j][:, 0:15, :], View(VT.t[lat0 + 64:lat0 + 64 + 1920, h * 64:(h + 1) * 64].rearrange("(t p) f -> p t f", p=128), ("na_VT", None)))
            P.dma("sp", vb[j][0:64, 15, :], View(VT.t[lat0 + 1984:lat0 + 2048, h * 64:(h + 1) * 64], ("na_VT", None)))
            P.dma("sp", vc[j][:], View(VT.t[ctx0:ctx0 + 256, h * 64:(h + 1) * 64].rearrange("(t p) f -> p t f", p=128), ("na_VT", None)))
            for r in range(0, 32, 2):
                ty, R0 = na_geom(r)
                u = n % 2
                n += 1
                q = qT[j][:, r * 64:r * 64 + 128]
                k0 = R0 * 64
                p1, p2 = ps1[u], ps2[u]
                P.mm(p1[:], q, kT[j][:, k0:k0 + 512], start=True, stop=True)
                P.mm(p2[:, 0:64], q, kT[j][:, k0 + 512:k0 + 576], start=True, stop=True)
                P.mm(p2[:, 64:320], q, kT[j][:, 2048:2304], start=True, stop=True)
                S = Sm[u]
                P.copy("act", S[:, 0:256], p2[:, 64:320])
                P.tt("dve", S[:, 256:768], p1[:], bj[:, ty, 0:512], ALU.add)
                P.tt("dve", S[:, 768:832], p2[:, 0:64], bj[:, ty, 512:576], ALU.add)
                st = sm[u]
                P.reduce("dve", st[:, 0:1], S[:], ALU.max)
                P.ts("dve", st[:, 0:1], st[:, 0:1], -1.0, None, op0=ALU.mult)
                Pb = Pm[u]
                P.act(Pb[:, 0:832], S[:], AF.Exp, bias=st[:, 0:1], accum_out=st[:, 1:2])
                P.recip(st[:, 2:3], st[:, 1:2])
                pt = pst[u]
                ptr = PT[u]
                for kt in range(7):
                    w = 128 if kt < 6 else 64
                    P.transpose(pt[0:w, kt * 128:(kt + 1) * 128], Pb[:, kt * 128:kt * 128 + w], C.identb[:])
                P.copy("act", ptr[:, 0:3, :], View(pt.t[:, 0:384].rearrange("p (a b) -> p a b", b=128), pt[:].key))
                P.copy("dve", ptr[:, 3:6, :], View(pt.t[:, 384:768].rearrange("p (a b) -> p a b", b=128), pt[:].key))
                P.copy("act", ptr[0:64, 6, :], pt[0:64, 768:896])
                po = pso[u]
                vx = va[j] if R0 % 2 == 0 else vb[j]
                t0 = R0 // 2
                P.mm(po[:, 0:64], ptr[:, 0, :], vc[j][:, 0, :], start=True, stop=False)
                P.mm(po[:, 0:64], ptr[:, 1, :], vc[j][:, 1, :], start=False, stop=False)
                for kt in range(4):
                    P.mm(po[:, 0:64], ptr[:, 2 + kt, :], vx[:, t0 + kt, :], start=False, stop=False)
                P.mm(po[:, 0:64], ptr[0:64, 6, :], vx[0:64, t0 + 4, :], start=False, stop=True)
                o = oo[u]
                P.act(o[:], po[:, 0:64], AF.Identity, scale=st[:, 2:3])
                q0 = lat0 + r * 64
                P.dma("sp" if n % 2 else "pool", View(OT.t[q0:q0 + 128, h * 64:(h + 1) * 64], ("na_OT", None)), o[:])
    P.pop()
    P.push()
    pb = [P.ps("n3_ps%d" % j) for j in range(8)]
    L = Ctx()
    L.mean = P.sb("n3_mean", [128, TT], F32)
    L.rstd = P.sb("n3_rstd", [128, TT], F32)
    L.sq = P.sb("n3_sq", [128, 8, TT], F32)
    Wo = P.sb("n3_Wo", [128, 8, 1024], BF16)
    stg = [P.sb("n3_stg%d" % j, [128, 8, 512], F32) for j in range(2)]
    load_w_bf16(P, Wo, IN("na_w_out", [D, D]).t, 1024, stg)
    xt = P.sb("n3_x", [128, 8, TT], F32)
    yT = P.sb("n3_yT", [128, 8, TT], BF16)
    oa = [P.sb("n3_oa%d" % j, [128, 1024], F32) for j in range(2)]
    n = 0
    for t in tiles:
        if t == 0:
            continue
        tok = slice(t * TT, (t + 1) * TT)
        P.dma("sp", xt[:], View(C.XS.t[:, :, tok].rearrange("c p n -> p c n"), ("XS", t)))
        for s in range(4):
            a = oa[s % 2]
            r0 = t * TT + s * 128
            P.dma("sp" if s % 2 else "pool", a[:], View(OT.t[r0:r0 + 128, :], ("na_OT", None)))
            for c in range(8):
                pt = pb[2 + n % 6]
                n += 1
                P.transpose(pt[:, 0:128], a[:, c * 128:(c + 1) * 128], C.ident[:])
                P.copy("act" if c % 2 else "dve", yT[:, c, s * 128:(s + 1) * 128], pt[:, 0:128])
        out_proj_ln(P, C, L, i, t, xt, yT, Wo, pb)
    P.pop()


class Inputs:
    def __init__(self, P):
        self.P = P
        self.d = {}

    def __call__(self, name, shape=None, dt=F32):
        if name not in self.d:
            self.d[name] = self.P.dram(name, SHAPES[name] if shape is None else shape, dt, kind="ExternalInput")
        return self.d[name]


SHAPES = {
    "xT": [8, 128, NT], "cT": [128, 8, 3], "adab": [128, DEPTH * 48 * 3], "lng": [128, DEPTH * 2 * 8], "lnb": [128, DEPTH * 2 * 8],
    "ident": [128, 128], "selE": [NE, NE * 128], "rb": [128, NE], "router_w": [D, NE],
}


def build(stages, tiles=None):
    nc = bass.Bass("TRN2", target_bir_lowering=False)
    P = Prog(nc)
    C = Ctx()
    IN = Inputs(P)
    OUT = P.dram("out", [8, 128, NT], F32, kind="ExternalOutput")
    C.XS = P.dram("XS", [8, 128, NT], F32)
    C.W1B = P.dram("W1B", [NE, 128, 8 * DE], BF16)
    C.W3B = P.dram("W3B", [NE, 128, 8 * DE], BF16)
    C.W2B = P.dram("W2B", [NE, 2, 128, 4 * 512], BF16)
    tiles = list(range(NTILE)) if tiles is None else tiles
    for c in range(8):
        P.dma("sp" if c % 2 else "pool", C.XS[c], IN("xT")[c])
    phase_consts(P, C, IN)
    mixer_consts(P, C, IN)
    layers = sorted(set(i for (_, i) in stages))
    phase_mod(P, C, IN, layers)
    for (kind, i) in stages:
        if kind == "moe":
            phase_wprep(P, C, IN, i)
            phase_moe(P, C, IN, i, tiles if i < DEPTH - 1 else [t for t in tiles if t > 0])
        elif kind == "mixer":
            if i % 4 == 0:
                phase_gdn(P, C, IN, i, tiles)
            elif i % 4 == 1:
                phase_mlstm(P, C, IN, i, tiles)
            elif i % 4 == 2:
                phase_rwkv(P, C, IN, i, tiles)
            else:
                phase_na(P, C, IN, i, tiles)
    P.pop()
    P.push()
    for c in range(8):
        P.dma("sp" if c % 2 else "pool", OUT[c], C.XS[c])
    P.emit()
    return nc, P, IN


def host_inputs(inputs, core):
    f = np.float32
    b0, b1 = 2 * core, 2 * core + 1
    x, ctx = inputs["x"], inputs["ctx"]
    tok = np.concatenate([ctx[b0], ctx[b1], x[b0], x[b1]], axis=0)
    m = {}
    m["xT"] = np.ascontiguousarray(tok.T.reshape(8, 128, NT)).astype(f)
    c3 = np.stack([inputs["c"][b0], inputs["c"][b1], inputs["c_ctx"]], axis=0)
    m["cT"] = np.ascontiguousarray(c3.reshape(3, 8, 128).transpose(2, 1, 0)).astype(f)
    return m


def rowrep(v, n=128):
    v = np.asarray(v, np.float32).reshape(1, -1)
    return np.ascontiguousarray(np.broadcast_to(v, (n, v.shape[1])))


def fm(v):
    v = np.asarray(v, np.float32)
    return np.ascontiguousarray(v.reshape(-1, 128).T)


def host_shared(inputs, names):
    f = np.float32
    m = {}
    ab = inputs["ada_b"].reshape(DEPTH, 48, 128).transpose(2, 0, 1)
    m["adab"] = np.ascontiguousarray(np.repeat(ab[..., None], 3, axis=-1).reshape(128, -1)).astype(f)
    m["lng"] = np.ascontiguousarray(inputs["ln_g"].reshape(DEPTH, 2, 8, 128).transpose(3, 0, 1, 2).reshape(128, -1)).astype(f)
    m["lnb"] = np.ascontiguousarray(inputs["ln_b"].reshape(DEPTH, 2, 8, 128).transpose(3, 0, 1, 2).reshape(128, -1)).astype(f)
    m["ident"] = np.eye(128, dtype=f)
    sel = np.zeros((NE, NE, 128), f)
    for e in range(NE):
        sel[e, e, :] = 1.0
    m["selE"] = sel.reshape(NE, NE * 128)
    m["rb"] = rowrep(inputs["router_b"])
    m["router_w"] = inputs["router_w"]
    for i in range(DEPTH):
        m["ada_w_%d" % i] = inputs["ada_w"][i]
        m["moe_w1_%d" % i] = inputs["moe_w1"][i]
        m["moe_w3_%d" % i] = inputs["moe_w3"][i]
        m["moe_w2_%d" % i] = inputs["moe_w2"][i]
    a = np.arange(64)
    Uf = (a[:, None] <= a[None, :]).astype(f)
    Sf = (a[:, None] < a[None, :]).astype(f)
    m["cmats"] = np.concatenate([Uf, Uf.T, Sf, Sf.T], axis=1)
    p = np.arange(128)
    inv = (10000.0 ** (-(p % 32).astype(np.float64) / 32.0))
    t = np.arange(2048)
    posv = np.where((p // 64)[:, None] == 0, (t // 64)[None, :], (t % 64)[None, :]).astype(np.float64)
    ang = posv * inv[:, None]
    m["ropecos"] = np.cos(ang).astype(f)
    m["ropesin"] = np.sin(ang).astype(f)
    R = np.zeros((128, 128), f)
    for mm_ in range(128):
        if (mm_ % 64) < 32:
            R[mm_, mm_ + 32] = -1.0
        else:
            R[mm_, mm_ - 32] = 1.0
    m["rotT"] = np.ascontiguousarray(R.T)
    m["mlstm_w_in"] = inputs["mlstm_w_in"]
    m["mlstm_w_out"] = inputs["mlstm_w_out"]
    m["ml_gateb"] = rowrep(inputs["mlstm_gate_b"].reshape(-1))
    m["ml_normg"] = fm(inputs["mlstm_norm_g"])
    m["gdn_w_in"] = inputs["gdn_w_in"]
    m["gdn_w_out"] = inputs["gdn_w_out"]
    dtb = np.zeros((2, 16), f)
    dtb[:, 0:8] = inputs["gdn_dt_bias"]
    m["gd_dtb"] = rowrep(dtb.reshape(-1))
    al = np.zeros((2, 16), f)
    al[:, 0:8] = inputs["gdn_a_log"]
    m["gd_alog"] = rowrep(al.reshape(-1))
    m["gd_conv"] = np.ascontiguousarray(inputs["gdn_conv"].T.reshape(24, 128, 3).transpose(1, 0, 2).reshape(128, 72))
    m["gd_normg"] = np.asarray(inputs["gdn_norm_g"], f).reshape(128, 1)
    for k in ("rwkv_w_rkv", "rwkv_g1", "rwkv_a1", "rwkv_w1", "rwkv_g2", "rwkv_a2", "rwkv_w2", "rwkv_w_out"):
        m[k] = inputs[k]
    m["rw_w0"] = np.ascontiguousarray(np.broadcast_to(np.asarray(inputs["rwkv_w0"], f)[None], (128, 2, 1024)))
    cst = [fm(inputs["rwkv_mu"][j]) for j in range(6)]
    cst += [fm(inputs["rwkv_k_k"]), fm(inputs["rwkv_k_a"]), np.zeros((128, 8), f), fm(inputs["rwkv_r_k"].reshape(-1))]
    cst += [fm(inputs["rwkv_a0"][0]), fm(inputs["rwkv_a0"][1]), np.zeros((128, 8), f)]
    m["rw_cst"] = np.concatenate(cst, axis=1)
    bo = np.zeros((128, 128), f)
    bo[0:64, 0:64] = 1.0
    bo[64:128, 64:128] = 1.0
    m["rw_bones"] = bo
    m["rw_lnx"] = np.concatenate([fm(inputs["rwkv_lnx_g"]), fm(inputs["rwkv_lnx_b"])], axis=1)
    cm4 = np.zeros((64, 2, 1536), f)
    for d_ in range(2):
        U_ = Uf if d_ == 0 else Uf.T
        S_ = Sf if d_ == 0 else Sf.T
        cm4[:, d_, 0:256] = np.tile(U_, (1, 4))
        cm4[:, d_, 256:512] = np.tile(S_, (1, 4))
        cm4[:, d_, 512:768] = np.tile(S_.T, (1, 4))
        cm4[:, d_, 768:1280] = np.tile(np.concatenate([S_, U_], axis=1), (1, 4))
        cm4[:, d_, 1280:1536] = np.tile(np.eye(64, dtype=f), (1, 4))
    m["rw_cm4"] = cm4
    m["na_w_in"] = inputs["na_w_in"]
    m["na_w_out"] = inputs["na_w_out"]
    if "na_bias" in names:
        rpb = np.asarray(inputs["na_rpb"], f)
        bt = np.full((NA_H, 128, 5, 576), -30000.0, f)
        w = np.arange(64)
        c0 = np.clip(w - 8, 0, 48)
        for ty, (r, R0, r00, r01) in enumerate([(0, 0, 0, 0), (2, 0, 0, 0), (6, 2, 2, 3), (28, 23, 24, 24), (30, 23, 24, 24)]):
            for dq, r0q in ((0, r00), (1, r01)):
                qrow = r + dq
                for kr in range(9):
                    krow = R0 + kr
                    if not (r0q <= krow < r0q + 8):
                        continue
                    dr = krow - qrow + 7
                    wp = np.arange(64)
                    valid = (wp[None, :] >= c0[:, None]) & (wp[None, :] < c0[:, None] + 16)
                    dc = np.clip(wp[None, :] - w[:, None] + 15, 0, 30)
                    vals = rpb[:, dr, :][:, dc]
                    blk = np.where(valid[None], vals, f(-30000.0))
                    bt[:, dq * 64:(dq + 1) * 64, ty, kr * 64:(kr + 1) * 64] = blk
        m["na_bias"] = bt
    return {k: np.ascontiguousarray(np.asarray(v, f)) for k, v in m.items() if k in names}


def unpack_out(res_core):
    o = res_core["out"].reshape(D, NT).T
    return o[512:512 + 2048], o[512 + 2048:], o[0:256], o[256:512]


ALL_STAGES = [(k, i) for i in range(DEPTH) for k in ("mixer", "moe")]


def kernel(**inputs):
    inputs = {k: np.asarray(v) for k, v in inputs.items()}
    nc, _, IN = build(ALL_STAGES)
    shared = host_shared(inputs, set(IN.d.keys()))
    in_maps = []
    for core in range(NCORES):
        m = dict(shared)
        m.update(host_inputs(inputs, core))
        in_maps.append(m)
    res = run_bass_kernel_spmd(nc, in_maps, core_ids=list(range(NCORES)))
    out = np.zeros((16, 2048, D), np.float32)
    for core in range(NCORES):
        l0, l1, _, _ = unpack_out(res.results[core])
        out[2 * core] = l0
        out[2 * core + 1] = l1
    return out
```

```python
import numpy as np
import concourse.bass as bass
import concourse.mybir as mybir
from concourse.bass_utils import run_bass_kernel_spmd

F32 = mybir.dt.float32
BF16 = mybir.dt.bfloat16
AF = mybir.ActivationFunctionType
ALU = mybir.AluOpType
AX = mybir.AxisListType

D = 1024
DEPTH = 4
NT = 4608
TT = 512
NTILE = NT // TT
NE = 16
DE = 512
ALPHA = float((2 * DEPTH) ** 0.25)
LN_EPS = 1e-5
NCORES = 8


class View:
    __slots__ = ("ap", "key")

    def __init__(self, ap, key):
        self.ap = ap
        self.key = key

    def bc(self, shape):
        return View(self.ap.to_broadcast(list(shape)), self.key)


class Buf:
    def __init__(self, name, t):
        self.name = name
        self.t = t

    def __getitem__(self, idx):
        return View(self.t[idx], (self.name, None))

    def k(self, sub):
        return _SubBuf(self, sub)


class _SubBuf:
    def __init__(self, buf, sub):
        self.buf = buf
        self.sub = sub

    def __getitem__(self, idx):
        return View(self.buf.t[idx], (self.buf.name, self.sub))


class Op:
    __slots__ = ("eng", "fn", "deps", "is_dma", "id", "inc", "ticket", "dsem", "dval", "is_mm")


ENGS = ("pe", "act", "dve", "pool", "sp")
NDSEM = 14


class Prog:
    def __init__(self, nc):
        self.nc = nc
        self.ops = []
        self.state = {}
        self.scopes = [[]]
        self.pending = {}
        self.last = {}
        self.open_dmas = set()

    def _enter(self, cm):
        t = cm.__enter__()
        self.scopes[-1].append(cm)
        return t

    def sb(self, name, shape, dt):
        self.uid = getattr(self, "uid", 0) + 1
        nm = "S%d_%s" % (self.uid, name)
        return Buf(nm, self._enter(self.nc.sbuf_tensor(nm, list(shape), dt)))

    def ps(self, name, shape=(128, 512), dt=F32):
        self.uid = getattr(self, "uid", 0) + 1
        nm = "P_%d_%s" % (self.uid, name)
        return Buf(nm, self._enter(self.nc.psum_tensor(nm, list(shape), dt)))

    def dram(self, name, shape, dt, kind="Internal"):
        return Buf(name, self.nc.dram_tensor(name, list(shape), dt, kind=kind).ap())

    def push(self):
        self.scopes.append([])

    def pop(self):
        for cm in reversed(self.scopes.pop()):
            cm.__exit__(None, None, None)
        bar = set(self.last.values()) | set(self.open_dmas)
        self.open_dmas = set()
        self.pending = {e: set(bar) | self.pending.get(e, set()) for e in ENGS}

    def _deps(self, key, is_write):
        name, sub = key
        st = self.state.get(name)
        if not st:
            return set()
        subs = list(st.keys()) if sub is None else [s for s in (sub, None) if s in st]
        deps = set()
        for s in subs:
            w, rs = st[s]
            if w is not None:
                deps.add(w)
            if is_write:
                deps.update(rs)
        return deps

    def _record(self, key, is_write, opid):
        name, sub = key
        st = self.state.setdefault(name, {})
        if is_write:
            if sub is None:
                st.clear()
            st[sub] = [opid, []]
        else:
            if sub not in st:
                st[sub] = [None, []]
            st[sub][1].append(opid)

    def add(self, eng, fn, writes, reads, is_dma=False, is_mm=False):
        op = Op()
        op.eng, op.fn, op.is_dma, op.is_mm = eng, fn, is_dma, is_mm
        op.inc, op.ticket, op.dsem, op.dval = False, 0, None, 0
        op.deps = self.pending.pop(eng, set())
        op.id = len(self.ops)
        reads = self._vs(*reads)
        writes = self._vs(*writes)
        for v in reads:
            dr = self._deps(v.key, False)
            op.deps |= dr
            if v.key[0].startswith("P_"):
                for x in self._deps(v.key, True) - dr:
                    if self.ops[x].eng != eng:
                        op.deps.add(x)
        for v in writes:
            op.deps |= self._deps(v.key, True)
        for v in reads:
            self._record(v.key, False, op.id)
        for v in writes:
            self._record(v.key, True, op.id)
        self.ops.append(op)
        if is_dma:
            self.open_dmas.add(op.id)
        else:
            self.last[eng] = op.id
        return op

    @staticmethod
    def _a(x):
        return x.ap if isinstance(x, View) else x

    @staticmethod
    def _vs(*xs):
        return [x for x in xs if isinstance(x, View)]

    def mm(self, out, lhsT, rhs, start=True, stop=True, **kw):
        a = self._a
        return self.add("pe", lambda e: e.matmul(a(out), a(lhsT), a(rhs), start=start, stop=stop, **kw),
                        [out], [lhsT, rhs], is_mm=True)

    def transpose(self, out, in_, ident):
        a = self._a
        return self.add("pe", lambda e: e.transpose(a(out), a(in_), a(ident)), [out], [in_, ident], is_mm=True)

    def act(self, out, in_, func, bias=0.0, scale=1.0, accum_out=None):
        a = self._a
        kw = {}
        if accum_out is not None:
            kw["accum_out"] = a(accum_out)
        return self.add("act", lambda e: e.activation(out=a(out), in_=a(in_), func=func, bias=a(bias), scale=a(scale), **kw),
                        self._vs(out, accum_out), self._vs(in_, bias, scale))

    def copy(self, eng, out, in_):
        a = self._a
        if eng == "act":
            return self.add(eng, lambda e: e.copy(out=a(out), in_=a(in_)), [out], [in_])
        return self.add(eng, lambda e: e.tensor_copy(out=a(out), in_=a(in_)), [out], [in_])

    def tt(self, eng, out, in0, in1, op):
        a = self._a
        return self.add(eng, lambda e: e.tensor_tensor(out=a(out), in0=a(in0), in1=a(in1), op=op), [out], [in0, in1])

    def ts(self, eng, out, in0, s1, s2=None, op0=ALU.mult, op1=None, accum_out=None):
        a = self._a
        kw = {}
        if op1 is not None:
            kw["op1"] = op1
        if accum_out is not None:
            kw["accum_out"] = a(accum_out)
        return self.add(eng, lambda e: e.tensor_scalar(out=a(out), in0=a(in0), scalar1=a(s1), scalar2=a(s2), op0=op0, **kw),
                        self._vs(out, accum_out), self._vs(in0, s1, s2))

    def stt(self, eng, out, in0, scalar, in1, op0, op1):
        a = self._a
        eng = "dve"
        return self.add(eng, lambda e: e.scalar_tensor_tensor(out=a(out), in0=a(in0), scalar=a(scalar), in1=a(in1), op0=op0, op1=op1),
                        [out], self._vs(in0, scalar, in1))

    def memset(self, eng, out, val):
        a = self._a
        return self.add(eng, lambda e: e.memset(a(out), val), [out], [])

    def reduce(self, eng, out, in_, op, axis=AX.X):
        a = self._a
        return self.add(eng, lambda e: e.tensor_reduce(out=a(out), in_=a(in_), axis=axis, op=op), [out], [in_])

    def recip(self, out, in_):
        a = self._a
        return self.add("dve", lambda e: e.reciprocal(out=a(out), in_=a(in_)), [out], [in_])

    def dma(self, q, out, in_, **kw):
        a = self._a
        return self.add(q, lambda e: e.dma_start(out=a(out), in_=a(in_), **kw), [out], [in_], is_dma=True)

    def emit(self):
        nc = self.nc
        ops = self.ops

        def pe_chain(op, dop):
            return dop.eng == "pe" and op.eng == "pe" and op.is_mm and dop.is_mm

        for op in ops:
            for d in op.deps:
                dop = ops[d]
                if dop.is_dma or pe_chain(op, dop):
                    continue
                dop.inc = True
        semctx = []

        def newsem(name):
            cm = nc.semaphore(name)
            s = cm.__enter__()
            semctx.append(cm)
            return s

        esem = {e: newsem("s_" + e) for e in ENGS}
        dq = ("sp", "pool", "act")
        dsems = {q: [newsem("d_%s_%d" % (q, i)) for i in range(NDSEM)] for q in dq}
        dcount = {q: [0] * NDSEM for q in dq}
        drr = {q: 0 for q in dq}
        tick = {e: 0 for e in ENGS}
        per_eng = {e: [] for e in ENGS}
        waits = {}
        seen = {e: {} for e in ENGS}
        for op in ops:
            w = []
            sn = seen[op.eng]

            def want(sem, val, key):
                if sn.get(key, 0) >= val:
                    return
                sn[key] = val
                w.append((sem, val))

            if op.is_dma:
                q = op.eng
                i = drr[q]
                drr[q] = (i + 1) % NDSEM
                if dcount[q][i] > 0:
                    want(dsems[q][i], dcount[q][i] * 16, ("d", q, i))
                dcount[q][i] += 1
                op.dsem = (q, i)
                op.dval = dcount[q][i] * 16
            for d in sorted(op.deps):
                dop = ops[d]
                if dop.is_dma:
                    q, i = dop.dsem
                    want(dsems[q][i], dop.dval, ("d", q, i))
                elif not pe_chain(op, dop):
                    want(esem[dop.eng], dop.ticket, ("e", dop.eng))
            if (not op.is_dma) and op.inc:
                tick[op.eng] += 1
                op.ticket = tick[op.eng]
            waits[op.id] = w
            per_eng[op.eng].append(op)
        self.n_waits = sum(len(w) for w in waits.values())
        final = []
        for q in dq:
            for i in range(NDSEM):
                if dcount[q][i] > 0:
                    final.append((dsems[q][i], dcount[q][i] * 16))

        def run_engine(e, name):
            for op in per_eng[name]:
                for (sem, val) in waits[op.id]:
                    e.wait_ge(sem, val)
                ins = op.fn(e)
                if op.is_dma:
                    q, i = op.dsem
                    ins.then_inc(dsems[q][i], 16)
                elif op.inc:
                    ins.then_inc(esem[name], 1)
            if name == "sp":
                for (sem, val) in final:
                    e.wait_ge(sem, val)

        with nc.Block() as block:
            @block.sync
            def _(e):
                run_engine(e, "sp")

            @block.tensor
            def _(e):
                run_engine(e, "pe")

            @block.scalar
            def _(e):
                run_engine(e, "act")

            @block.vector
            def _(e):
                run_engine(e, "dve")

            @block.gpsimd
            def _(e):
                run_engine(e, "pool")
        for cm in reversed(semctx):
            cm.__exit__(None, None, None)
        while self.scopes:
            for cm in reversed(self.scopes.pop()):
                cm.__exit__(None, None, None)


def seg_of_tile(t):
    return 2 if t == 0 else (0 if t <= 4 else 1)


def modcol(i, grp, kc, seg):
    return (i * 48 + grp * 8 + kc) * 3 + seg


class Ctx:
    pass


def phase_consts(P, C, IN):
    C.ones = P.sb("ones", [128, 128], F32)
    C.ident = P.sb("ident", [128, 128], F32)
    C.identb = P.sb("identb", [128, 128], BF16)
    C.mod = P.sb("mod", [128, DEPTH * 48 * 3], F32)
    C.lng = P.sb("lng", [128, DEPTH * 2 * 8], F32)
    C.lnb = P.sb("lnb", [128, DEPTH * 2 * 8], F32)
    C.rw = P.sb("rw", [128, 8, NE], F32)
    C.rb = P.sb("rb", [128, NE], F32)
    C.selE = P.sb("selE", [NE, NE * 128], F32)
    C.eps = P.sb("epsc", [128, 1], F32)
    P.memset("dve", C.ones[:], 1.0)
    P.memset("dve", C.eps[:], LN_EPS)
    P.dma("sp", C.ident[:], IN("ident")[:, :])
    P.copy("dve", C.identb[:], C.ident[:])
    P.dma("sp", C.lng[:], IN("lng")[:, :])
    P.dma("sp", C.lnb[:], IN("lnb")[:, :])
    P.dma("sp", C.rw[:], IN("router_w").t.rearrange("(kc p) e -> p kc e", p=128))
    P.dma("sp", C.rb[:], IN("rb")[:, :])
    P.dma("sp", C.selE[:], IN("selE")[:, :])


def phase_mod(P, C, IN, layers):
    P.push()
    cT = P.sb("cT", [128, 8, 3], F32)
    cv = P.sb("cv", [128, 8, 3], F32)
    adab = P.sb("adab", [128, DEPTH * 48 * 3], F32)
    aw = [P.sb("aw%d" % j, [128, 8, 512], F32) for j in range(2)]
    pm = [P.ps("pm%d" % j) for j in range(2)]
    P.dma("sp", cT[:], IN("cT")[:, :, :])
    P.dma("sp", adab[:], IN("adab")[:, :])
    P.act(cv[:], cT[:], AF.Silu)
    n = 0
    for i in layers:
        awv = IN("ada_w_%d" % i, [D, 6 * D]).t.rearrange("(kc p) n -> p kc n", p=128)
        for p in range(12):
            a = aw[n % 2]
            ps = pm[n % 2]
            n += 1
            P.dma("sp" if n % 2 else "pool", a[:], awv[:, :, p * 512:(p + 1) * 512])
            for f in range(4):
                for kc in range(8):
                    P.mm(ps[:, f * 3:(f + 1) * 3], a[:, kc, f * 128:(f + 1) * 128], cv[:, kc, :], start=(kc == 0), stop=(kc == 7))
            c0 = (i * 48 + p * 4) * 3
            P.tt("dve", C.mod[:, c0:c0 + 12], ps[:, 0:12], adab[:, c0:c0 + 12], ALU.add)
        for grp in (1, 4):
            c0 = (i * 48 + grp * 8) * 3
            P.ts("dve", C.mod[:, c0:c0 + 24], C.mod[:, c0:c0 + 24], 1.0, None, op0=ALU.add)
    P.pop()


def ln_tile(P, C, L, y, out, i, which, ps_a, ps_b):
    mean, rstd, sq = L.mean, L.rstd, L.sq
    for kc in range(8):
        P.mm(ps_a[:], C.ones[:], y[:, kc, :], start=(kc == 0), stop=(kc == 7))
    P.act(mean[:], ps_a[:], AF.Identity, scale=1.0 / D)
    for kc in range(8):
        P.tt("pool" if kc % 2 else "dve", y[:, kc, :], y[:, kc, :], mean[:], ALU.subtract)
        P.act(sq[:, kc, :], y[:, kc, :], AF.Square)
    for kc in range(8):
        P.mm(ps_b[:], C.ones[:], sq[:, kc, :], start=(kc == 0), stop=(kc == 7))
    P.act(rstd[:], ps_b[:], AF.Sqrt, bias=C.eps[:], scale=1.0 / D)
    P.recip(rstd[:], rstd[:])
    for kc in range(8):
        col = (i * 2 + which) * 8 + kc
        P.stt("dve", sq[:, kc, :], y[:, kc, :], C.lng[:, col:col + 1], rstd[:], ALU.mult, ALU.mult)
        P.act(out[:, kc, :], sq[:, kc, :], AF.Identity, bias=C.lnb[:, col:col + 1])


def phase_wprep(P, C, IN, i):
    P.push()
    st = [P.sb("wp_f%d" % j, [128, 8, 512], F32) for j in range(3)]
    sb = [P.sb("wp_b%d" % j, [128, 8, 512], BF16) for j in range(3)]
    n = 0
    engs = ("pool", "act", "dve")
    for e in range(NE):
        for (src, dst, pat, isw2) in ((IN("moe_w1_%d" % i, [NE, D, DE]), C.W1B, "(kc p) n -> p kc n", False),
                                      (IN("moe_w3_%d" % i, [NE, D, DE]), C.W3B, "(kc p) n -> p kc n", False),
                                      (IN("moe_w2_%d" % i, [NE, DE, D]), C.W2B, "", True)):
            j = n % 3
            n += 1
            if isw2:
                sv = src.t[e].rearrange("(kc p) (h n) -> p kc h n", p=128, h=2)
                stv = View(st[j].t[:].rearrange("p (kc h) n -> p kc h n", h=2), st[j][:].key)
                P.dma("sp", stv, sv)
                P.copy(engs[j], sb[j][:], st[j][:])
                for half in range(2):
                    dv = dst.t[e, half].rearrange("p (kc n) -> p kc n", kc=4)
                    sbv = View(sb[j].t[:].rearrange("p (kc h) n -> p kc h n", h=2)[:, :, half, :], sb[j][:].key)
                    P.dma("pool", View(dv, (dst.name, e)), sbv)
            else:
                sv = src.t[e].rearrange(pat, p=128)
                dv = dst.t[e].rearrange("p (kc n) -> p kc n", kc=8)
                P.dma("sp", st[j][:], sv)
                P.copy(engs[j], sb[j][:], st[j][:])
                P.dma("pool", View(dv, (dst.name, e)), sb[j][:])
    P.pop()


def phase_moe(P, C, IN, i, tiles):
    P.push()
    xts = [P.sb("m_x%d" % j, [128, 8, TT], F32) for j in range(2)]
    h2fs = [P.sb("m_h2f%d" % j, [128, 8, TT], F32) for j in range(2)]
    h2bs = [P.sb("m_h2b%d" % j, [128, 8, TT], BF16) for j in range(2)]
    combTs = [P.sb("m_combT%d" % j, [NE, TT], F32) for j in range(2)]
    hid = P.sb("m_hid", [128, NE * 4, TT], BF16)
    wsl = [P.sb("m_w%d" % j, [128, 8, 512], BF16) for j in range(3)]
    w2st = [P.sb("m_w2%d" % j, [128, 4, 512], BF16) for j in range(2)]
    Ls = []
    mean_ = P.sb("m_mean", [128, TT], F32)
    rstd_ = P.sb("m_rstd", [128, TT], F32)
    for j in range(2):
        L = Ctx()
        L.mean = mean_
        L.rstd = rstd_
        L.sq = h2fs[j]
        Ls.append(L)
    cbs = [P.sb("m_cb%d" % j, [128, TT], F32) for j in range(1)]
    gS = [P.sb("m_g%d" % j, [128, TT], F32) for j in range(2)]
    nws = [0]
    R = [dict((nm, P.sb("r_%s%d" % (nm, j), [128, w], F32)) for nm, w in
              (("lg", 16), ("e", 16), ("pr", 16), ("sel", 16), ("eq", 16), ("s2", 16), ("msk", 16), ("pw", 16), ("cmb", 16),
               ("mx", 1), ("se", 1), ("m1", 4), ("m2", 4), ("gs", 4), ("gm", 1), ("ing", 4), ("sw", 1))) for j in range(2)]
    pb = [P.ps("m_ps%d" % j) for j in range(8)]
    rr = [0]

    def prologue(t, pj):
        ops = []
        xt, h2f, h2b, combT = xts[pj], h2fs[pj], h2bs[pj], combTs[pj]
        seg = seg_of_tile(t)
        tok = slice(t * TT, (t + 1) * TT)
        ops.append(lambda: P.dma("sp", xt[:], View(C.XS.t[:, :, tok].rearrange("c p n -> p c n"), ("XS", t))))
        for kc in range(8):
            def f(kc=kc):
                c_sc = modcol(i, 4, kc, seg)
                c_sh = modcol(i, 3, kc, seg)
                P.ts("dve" if kc % 2 else "pool", h2f[:, kc, :], xt[:, kc, :], C.mod[:, c_sc:c_sc + 1], C.mod[:, c_sh:c_sh + 1], op0=ALU.mult, op1=ALU.add)
                P.copy("act", h2b[:, kc, :], h2f[:, kc, :])
            ops.append(f)
        for s in range(4):
            r = R[0]
            rr[0] += 1
            pl = pb[6 + (s % 2)]

            def f1(s=s, r=r, pl=pl):
                for kc in range(8):
                    P.mm(pl[:, 0:16], h2f[:, kc, s * 128:(s + 1) * 128], C.rw[:, kc, :], start=(kc == 0), stop=(kc == 7))
                P.copy("act", r["lg"][:], pl[:, 0:16])
            ops.append(f1)
            seq = [
                lambda r=r: P.reduce("dve", r["mx"][:], r["lg"][:], ALU.max),
                lambda r=r: P.ts("dve", r["mx"][:], r["mx"][:], -1.0, None, op0=ALU.mult),
                lambda r=r: P.act(r["e"][:], r["lg"][:], AF.Exp, bias=r["mx"][:], accum_out=r["se"][:]),
                lambda r=r: P.recip(r["se"][:], r["se"][:]),
                lambda r=r: P.ts("dve", r["pr"][:], r["e"][:], r["se"][:], None, op0=ALU.mult),
                lambda r=r: P.tt("dve", r["sel"][:], r["pr"][:], C.rb[:], ALU.add),
                lambda r=r: P.reduce("dve", r["m1"][:], View(r["sel"].t[:].rearrange("p (g k) -> p g k", k=4), r["sel"][:].key), ALU.max),
            ]
            for g in range(4):
                seq.append(lambda r=r, g=g: P.ts("dve", r["eq"][:, g * 4:(g + 1) * 4], r["sel"][:, g * 4:(g + 1) * 4], r["m1"][:, g:g + 1], None, op0=ALU.is_equal))
            seq += [
                lambda r=r: P.stt("dve", r["s2"][:], r["eq"][:], -1e9, r["sel"][:], ALU.mult, ALU.add),
                lambda r=r: P.reduce("dve", r["m2"][:], View(r["s2"].t[:].rearrange("p (g k) -> p g k", k=4), r["s2"][:].key), ALU.max),
                lambda r=r: P.tt("dve", r["gs"][:], r["m1"][:], r["m2"][:], ALU.add),
                lambda r=r: P.reduce("dve", r["gm"][:], r["gs"][:], ALU.max),
                lambda r=r: P.ts("dve", r["ing"][:], r["gs"][:], r["gm"][:], None, op0=ALU.is_equal),
            ]
            for g in range(4):
                seq.append(lambda r=r, g=g: P.ts("dve", r["msk"][:, g * 4:(g + 1) * 4], r["sel"][:, g * 4:(g + 1) * 4], r["m2"][:, g:g + 1], r["ing"][:, g:g + 1],
                                                 op0=ALU.is_ge, op1=ALU.mult))
            seq += [
                lambda r=r: P.tt("dve", r["pw"][:], r["pr"][:], r["msk"][:], ALU.mult),
                lambda r=r: P.reduce("dve", r["sw"][:], r["pw"][:], ALU.add),
                lambda r=r: P.recip(r["sw"][:], r["sw"][:]),
                lambda r=r: P.ts("dve", r["cmb"][:], r["pw"][:], r["sw"][:], None, op0=ALU.mult),
            ]
            ops += seq

            def f2(s=s, r=r, pl=pl):
                P.transpose(pl[0:16, 128:256], r["cmb"][:], C.ident[:])
                P.copy("act", combT[:, s * 128:(s + 1) * 128], pl[0:16, 128:256])
            ops.append(f2)
        return ops

    def epilogue(t, pj):
        ops = []
        xt, L = xts[pj], Ls[pj]
        tok = slice(t * TT, (t + 1) * TT)
        y = xt
        mean, rstd, sq = L.mean, L.rstd, L.sq
        ps_a, ps_b = pb[6], pb[7]

        def s1():
            for kc in range(8):
                P.mm(ps_a[:], C.ones[:], y[:, kc, :], start=(kc == 0), stop=(kc == 7))
            P.act(mean[:], ps_a[:], AF.Identity, scale=1.0 / D)
        ops.append(s1)
        for kc in range(8):
            def f(kc=kc):
                P.tt("pool" if kc % 2 else "dve", y[:, kc, :], y[:, kc, :], mean[:], ALU.subtract)
                P.act(sq[:, kc, :], y[:, kc, :], AF.Square)
            ops.append(f)

        def s2():
            for kc in range(8):
                P.mm(ps_b[:], C.ones[:], sq[:, kc, :], start=(kc == 0), stop=(kc == 7))
            P.act(rstd[:], ps_b[:], AF.Sqrt, bias=C.eps[:], scale=1.0 / D)
            P.recip(rstd[:], rstd[:])
        ops.append(s2)
        for kc in range(8):
            def f(kc=kc):
                col = (i * 2 + 1) * 8 + kc
                P.stt("dve", sq[:, kc, :], y[:, kc, :], C.lng[:, col:col + 1], rstd[:], ALU.mult, ALU.mult)
                P.act(xt[:, kc, :], sq[:, kc, :], AF.Identity, bias=C.lnb[:, col:col + 1])
            ops.append(f)
        ops.append(lambda: P.dma("sp", View(C.XS.t[:, :, tok].rearrange("c p n -> p c n"), ("XS", t)), xt[:]))
        return ops

    def drain(q, k):
        for _ in range(k):
            if q:
                q.pop(0)()

    for f in prologue(tiles[0], 0):
        f()
    pend_epi = []
    for ti, t in enumerate(tiles):
        pj = ti % 2
        xt, h2f, h2b, combT = xts[pj], h2fs[pj], h2bs[pj], combTs[pj]
        seg = seg_of_tile(t)
        pend_pro = prologue(tiles[ti + 1], 1 - pj) if ti + 1 < len(tiles) else []
        k_epi = (len(pend_epi) + 5) // 6
        k_pro = (len(pend_pro) + 13) // 14
        n = 0
        for e in range(NE):
            wa = wsl[nws[0] % 3]
            wb_ = wsl[(nws[0] + 1) % 3]
            nws[0] += 2
            P.dma("sp", wa[:], View(C.W1B.t[e].rearrange("p (kc n) -> p kc n", kc=8), ("W1B", e)))
            P.dma("pool", wb_[:], View(C.W3B.t[e].rearrange("p (kc n) -> p kc n", kc=8), ("W3B", e)))
            cb = cbs[0]
            pc = pb[6 + (e % 2)]
            P.mm(pc[:], C.selE[:, e * 128:(e + 1) * 128], combT[:], start=True, stop=True)
            P.copy("act", cb[:], pc[:])
            for fc in range(4):
                p1 = pb[(n % 3) * 2]
                p3 = pb[(n % 3) * 2 + 1]
                g = gS[n % 2]
                u = g
                n += 1
                for kc in range(8):
                    P.mm(p1[:], wa[:, kc, fc * 128:(fc + 1) * 128], h2b[:, kc, :], start=(kc == 0), stop=(kc == 7))
                for kc in range(8):
                    P.mm(p3[:], wb_[:, kc, fc * 128:(fc + 1) * 128], h2b[:, kc, :], start=(kc == 0), stop=(kc == 7))
                P.act(g[:], p1[:], AF.Silu)
                P.tt("dve", u[:], g[:], p3[:], ALU.mult)
                P.tt("pool", hid[:, e * 4 + fc, :], u[:], cb[:], ALU.mult)
            if pend_epi:
                drain(pend_epi, k_epi)
            else:
                drain(pend_pro, k_pro)
        drain(pend_epi, len(pend_epi))
        n = 0
        for half in range(2):
            for e in range(NE):
                w2 = w2st[n % 2]
                n += 1
                P.dma("sp" if n % 2 else "pool", w2[:], View(C.W2B.t[e, half].rearrange("p (fc n) -> p fc n", fc=4), ("W2B", e)))
                for fc in range(4):
                    for o in range(4):
                        P.mm(pb[half * 4 + o][:], w2[:, fc, o * 128:(o + 1) * 128], hid[:, e * 4 + fc, :],
                             start=(e == 0 and fc == 0), stop=(e == NE - 1 and fc == 3))
                if half == 0:
                    drain(pend_pro, k_pro)
            for o in range(4):
                oc = half * 4 + o
                cg = modcol(i, 5, oc, seg)
                P.act(h2f[:, oc, :], pb[half * 4 + o][:], AF.Identity, scale=C.mod[:, cg:cg + 1])
                P.stt("dve", xt[:, oc, :], xt[:, oc, :], ALPHA, h2f[:, oc, :], ALU.mult, ALU.add)
        drain(pend_pro, len(pend_pro))
        pend_epi = epilogue(t, pj)
    drain(pend_epi, len(pend_epi))
    P.pop()


CH = 64


def chunk_plan(b, d):
    ctx0 = 256 * b
    lat0 = 512 + 2048 * b
    blocks = [(ctx0, 4)] + [(lat0 + 512 * k, 8) for k in range(4)]
    if d == 0:
        return [(t0, list(range(n))) for (t0, n) in blocks]
    return [(blocks[0][0], [3, 2, 1, 0])] + [(t0, list(range(n - 1, -1, -1))) for (t0, n) in reversed(blocks[1:])]


def load_w_bf16(P, dst, src_ap, ncols, stg, engs=("pool", "act", "dve"), ctr=[0]):
    sv = src_ap.rearrange("(kc p) n -> p kc n", p=128)
    c0 = 0
    while c0 < ncols:
        w = min(512, ncols - c0)
        j = ctr[0] % len(stg)
        ctr[0] += 1
        P.dma("sp" if j % 2 else "pool", stg[j][:, :, 0:w], sv[:, :, c0:c0 + w])
        P.copy(engs[j % 3], dst[:, :, c0:c0 + w], stg[j][:, :, 0:w])
        c0 += w


def mixer_consts(P, C, IN):
    C.cm = P.sb("cm", [64, 4 * 64], F32)
    P.dma("sp", C.cm[:], IN("cmats", [64, 256])[:, :])
    C.ones64 = P.sb("ones64", [64, 128], F32)
    P.memset("dve", C.ones64[:], 1.0)


def cmat(C, name, d):
    idx = {"U": 0, "UT": 1, "S": 2, "ST": 3}[name]
    if d == 1:
        idx = idx ^ 1
    return C.cm[:, idx * 64:(idx + 1) * 64]


def load_xh(P, C, i, t, xt, hb, halo=False):
    seg = seg_of_tile(t)
    tok = slice(t * TT, (t + 1) * TT)
    P.dma("sp", xt[:], View(C.XS.t[:, :, tok].rearrange("c p n -> p c n"), ("XS", t)))
    for kc in range(8):
        c_sc = modcol(i, 1, kc, seg)
        c_sh = modcol(i, 0, kc, seg)
        P.ts("dve" if kc % 2 else "pool", hb[:, kc, :], xt[:, kc, :], C.mod[:, c_sc:c_sc + 1], C.mod[:, c_sh:c_sh + 1], op0=ALU.mult, op1=ALU.add)


def out_proj_ln(P, C, L, i, t, xt, yT, Wo, pb):
    seg = seg_of_tile(t)
    tok = slice(t * TT, (t + 1) * TT)
    for oc in range(8):
        ps = pb[2 + oc % 4]
        for kc in range(8):
            P.mm(ps[:], Wo[:, kc, oc * 128:(oc + 1) * 128], yT[:, kc, :], start=(kc == 0), stop=(kc == 7))
        cg = modcol(i, 2, oc, seg)
        P.act(L.sq[:, oc, :], ps[:], AF.Identity, scale=C.mod[:, cg:cg + 1])
        P.stt("dve", xt[:, oc, :], xt[:, oc, :], ALPHA, L.sq[:, oc, :], ALU.mult, ALU.add)
    ln_tile(P, C, L, xt, xt, i, 0, pb[0], pb[1])
    P.dma("sp", View(C.XS.t[:, :, tok].rearrange("c p n -> p c n"), ("XS", t)), xt[:])


def rope_evac(P, C, R, ps, dst, t, scale, n):
    y = R.y[n % 2]
    if t == 0:
        P.act(dst, ps[:], AF.Identity, scale=scale)
        return
    pos = ((t - 1) % 4) * TT
    P.act(y[:], ps[:], AF.Identity, scale=scale)
    pr = R.pr[n % 2]
    P.mm(pr[:], R.rotT[:], y[:], start=True, stop=True)
    y1 = R.y1[n % 2]
    P.tt("pool", y1[:], y[:], R.cos[:, pos:pos + TT], ALU.mult)
    y2 = R.y2[n % 2]
    P.tt("dve", y2[:], pr[:], R.sin[:, pos:pos + TT], ALU.mult)
    P.tt("pool", dst, y1[:], y2[:], ALU.add)


def rope_setup(P, C, IN, pbanks):
    R = Ctx()
    R.cos = P.sb("ropecos", [128, 2048], F32)
    R.sin = P.sb("ropesin", [128, 2048], F32)
    R.rotT = P.sb("rotT", [128, 128], F32)
    P.dma("sp", R.cos[:], IN("ropecos", [128, 2048])[:, :])
    P.dma("pool", R.sin[:], IN("ropesin", [128, 2048])[:, :])
    P.dma("sp", R.rotT[:], IN("rotT", [128, 128])[:, :])
    R.y = [P.sb("rp_y%d" % j, [128, TT], F32) for j in range(2)]
    R.y1 = [P.sb("rp_y1%d" % j, [128, TT], F32) for j in range(2)]
    R.y2 = [P.sb("rp_y2%d" % j, [128, TT], F32) for j in range(2)]
    R.pr = pbanks
    return R


ML_H = 4
ML_DV = 256
ML_VW = ML_DV + 1


def phase_mlstm(P, C, IN, i, tiles):
    QK = P.dram("ml_QK", [8, 128, NT], BF16)
    OG = P.dram("ml_OG", [8, 128, NT], BF16)
    KT = P.dram("ml_KT", [NT, 512], BF16)
    VT = P.dram("ml_VT", [NT, ML_H * ML_VW], BF16)
    GT = P.dram("ml_GT", [NT, 16], F32)
    OD = [P.dram("ml_OD%d" % d, [NT, 1024], F32) for d in range(2)]
    w_in = IN("mlstm_w_in", [D, 3088])
    P.push()
    pb = [P.ps("ml_ps%d" % j) for j in range(6)]
    pbt = [P.ps("ml_pt%d" % j, [128, 1024], BF16) for j in range(2)]
    W = P.sb("ml_W", [128, 8, 3088], BF16)
    stg = [P.sb("ml_stg%d" % j, [128, 8, 512], F32) for j in range(2)]
    load_w_bf16(P, W, w_in.t, 3088, stg)
    R = rope_setup(P, C, IN, pb[4:6])
    gb = P.sb("ml_gb", [128, 16], F32)
    P.dma("sp", gb[:], IN("ml_gateb", [128, 16])[:, :])
    xt = P.sb("ml_x", [128, 8, TT], F32)
    hb = P.sb("ml_hb", [128, 8, TT], BF16)
    qk = P.sb("ml_qk", [128, 8, TT], BF16)
    og = P.sb("ml_og", [128, 8, TT], BF16)
    kt = P.sb("ml_kt", [128, 4, 512], BF16)
    vt = P.sb("ml_vt", [128, 4, ML_H * ML_VW], BF16)
    gt = P.sb("ml_gt", [128, 4, 16], F32)
    ge = P.sb("ml_ge", [128, 4, 16], F32)
    P.memset("dve", vt[:], 1.0)
    n = 0
    for t in tiles:
        tok = slice(t * TT, (t + 1) * TT)
        load_xh(P, C, i, t, xt, hb)
        for oc in range(8):
            ps = pb[n % 4]
            for kc in range(8):
                P.mm(ps[:], W[:, kc, oc * 128:(oc + 1) * 128], hb[:, kc, :], start=(kc == 0), stop=(kc == 7))
            rope_evac(P, C, R, ps, qk[:, oc, :], t, (128.0 ** -0.5) if oc < 4 else 1.0, n)
            n += 1
            if oc >= 4:
                for s in range(4):
                    pt = pbt[s % 2]
                    P.transpose(pt[:, 0:128], qk[:, oc, s * 128:(s + 1) * 128], C.identb[:])
                    P.copy("act" if s % 2 else "dve", kt[:, s, (oc - 4) * 128:(oc - 3) * 128], pt[:, 0:128])
        for c in range(8):
            ps = pb[n % 4]
            n += 1
            for kc in range(8):
                P.mm(ps[:], W[:, kc, 2048 + c * 128:2048 + (c + 1) * 128], hb[:, kc, :], start=(kc == 0), stop=(kc == 7))
            P.act(og[:, c, :], ps[:], AF.Sigmoid)
        for s in range(4):
            for blk in range(2):
                ps = pb[n % 4]
                n += 1
                for kc in range(8):
                    P.mm(ps[:], hb[:, kc, s * 128:(s + 1) * 128], W[:, kc, 1024 + blk * 512:1024 + (blk + 1) * 512], start=(kc == 0), stop=(kc == 7))
                for hh in range(2):
                    h = blk * 2 + hh
                    P.copy("act" if hh else "dve", vt[:, s, h * ML_VW:h * ML_VW + ML_DV], ps[:, hh * 256:(hh + 1) * 256])
            ps = pb[n % 4]
            n += 1
            for kc in range(8):
                P.mm(ps[:, 0:16], hb[:, kc, s * 128:(s + 1) * 128], W[:, kc, 3072:3088], start=(kc == 0), stop=(kc == 7))
            P.tt("dve", gt[:, s, :], ps[:, 0:16], gb[:], ALU.add)
        P.act(ge[:], gt[:], AF.Exp, scale=-1.0)
        P.act(ge[:], ge[:], AF.Ln, bias=1.0)
        for d in range(2):
            P.ts("dve", gt[:, :, d * 8 + 4:d * 8 + 8], ge[:, :, d * 8 + 4:d * 8 + 8], -1.0, None, op0=ALU.mult)
        P.dma("sp", View(QK.t[:, :, tok].rearrange("c p n -> p c n"), ("ml_QK", t)), qk[:])
        P.dma("pool", View(OG.t[:, :, tok].rearrange("c p n -> p c n"), ("ml_OG", t)), og[:])
        P.dma("sp", View(KT.t[tok, :].rearrange("(s p) f -> p s f", p=128), ("ml_KT", t)), kt[:])
        P.dma("pool", View(VT.t[tok, :].rearrange("(s p) f -> p s f", p=128), ("ml_VT", t)), vt[:])
        P.dma("sp", View(GT.t[tok, :].rearrange("(s p) f -> p s f", p=128), ("ml_GT", t)), gt[:])
    P.pop()
    P.push()
    pb = [P.ps("m2_ps%d" % j) for j in range(8)]
    qkb = [P.sb("m2_qk%d" % j, [128, 8, TT], BF16) for j in range(2)]
    ktb = [P.sb("m2_kt%d" % j, [64, 8, 512], BF16) for j in range(2)]
    vtb = [P.sb("m2_vt%d" % j, [64, 8, ML_H * ML_VW], BF16) for j in range(2)]
    gtb = [P.sb("m2_gt%d" % j, [64, 8, 16], F32) for j in range(2)]
    S = [P.sb("m2_S%d" % j, [128, ML_H, ML_VW], F32) for j in range(4)]
    Sb = [P.sb("m2_Sb%d" % j, [128, ML_H, ML_VW], BF16) for j in range(4)]
    NB = 3
    eg = [P.sb("m2_eg%d" % j, [64, 8], F32) for j in range(NB)]
    ege = [P.sb("m2_ege%d" % j, [128, 4], F32) for j in range(NB)]
    e1la = [P.sb("m2_e1la%d" % j, [64, 64], F32) for j in range(NB)]
    gm = [P.sb("m2_gm%d" % j, [64, 64], F32) for j in range(NB)]
    ptb = [P.sb("m2_pt%d" % j, [64, 64], BF16) for j in range(NB)]
    kd = [P.sb("m2_kd%d" % j, [64, 128], BF16) for j in range(NB)]
    o1 = [P.sb("m2_o1%d" % j, [64, ML_VW], F32) for j in range(NB)]
    den = [P.sb("m2_den%d" % j, [64, 1], F32) for j in range(NB)]
    ob = [P.sb("m2_ob%d" % j, [64, 1024], F32) for j in range(2)]
    nblk = 0
    n = 0
    nch = 0
    chains = [(b, d) for b in range(2) for d in range(2)]
    plans = {bd: chunk_plan(*bd) for bd in chains}
    for ci, bd in enumerate(chains):
        P.memset("pool", S[ci][:], 0.0)
        P.memset("pool", Sb[ci][:], 0.0)
    for step in range(5):
        for ci, (b, d) in enumerate(chains):
            t0, clist = plans[(b, d)][step]
            ntok = 64 * len(clist)
            j = nblk % 2
            nblk += 1
            tk = ("blk", t0)
            P.dma("sp", qkb[j][:, :, 0:ntok], View(QK.t[:, :, t0:t0 + ntok].rearrange("c p n -> p c n"), ("ml_QK", None)))
            P.dma("pool", ktb[j][:, 0:len(clist), :], View(KT.t[t0:t0 + ntok, :].rearrange("(c p) f -> p c f", p=64), ("ml_KT", None)))
            P.dma("sp", vtb[j][:, 0:len(clist), :], View(VT.t[t0:t0 + ntok, :].rearrange("(c p) f -> p c f", p=64), ("ml_VT", None)))
            P.dma("pool", gtb[j][:, 0:len(clist), :], View(GT.t[t0:t0 + ntok, :].rearrange("(c p) f -> p c f", p=64), ("ml_GT", None)))
            for c in clist:
                cs = slice(c * 64, (c + 1) * 64)
                la = gtb[j][:, c, d * 8 + 4:d * 8 + 8]
                ip = gtb[j][:, c, d * 8:d * 8 + 4]
                m = nch % NB
                nch += 1
                pg = pb[6 + nch % 2]
                P.mm(pg[0:64, 0:4], cmat(C, "U", d), la, start=True, stop=True)
                P.mm(pg[0:64, 4:8], cmat(C, "ST", d), la, start=True, stop=True)
                P.mm(pg[:, 8:12], C.ones64[:], la, start=True, stop=True)
                P.tt("dve", eg[m][:, 4:8], pg[0:64, 4:8], ip, ALU.add)
                P.act(eg[m][:, 4:8], eg[m][:, 4:8], AF.Exp)
                P.act(eg[m][:, 0:4], pg[0:64, 0:4], AF.Exp)
                P.act(ege[m][:], pg[:, 8:12], AF.Exp)
                obuf = ob[nch % 2]
                for h in range(ML_H):
                    u = n % NB
                    n += 1
                    P.ts("dve", e1la[u][:], cmat(C, "ST", d), la[:, h:h + 1] if False else gtb[j][:, c, d * 8 + 4 + h:d * 8 + 5 + h], None, op0=ALU.mult)
                    pl = pb[(n % 3) * 2]
                    P.mm(pl[0:64, 0:64], e1la[u][:], cmat(C, "U", d), start=True, stop=True)
                    P.act(gm[u][:], pl[0:64, 0:64], AF.Exp, bias=gtb[j][:, c, d * 8 + h:d * 8 + h + 1])
                    P.tt("pool", gm[u][:], gm[u][:], cmat(C, "U", d), ALU.mult)
                    P.mm(pl[0:64, 64:128], qkb[j][:, 4 + h, cs], qkb[j][:, h, cs], start=True, stop=True)
                    P.tt("dve", ptb[u][:], pl[0:64, 64:128], gm[u][:], ALU.mult)
                    P.ts("pool", kd[u][:], ktb[j][:, c, h * 128:(h + 1) * 128], eg[m][:, 4 + h:5 + h], None, op0=ALU.mult)
                    vh = vtb[j][:, c, h * ML_VW:(h + 1) * ML_VW]
                    po = pb[(n % 3) * 2 + 1]
                    P.mm(po[0:64, 0:ML_VW], qkb[j][:, h, cs], Sb[ci][:, h, :], start=True, stop=True)
                    P.act(o1[u][:], po[0:64, 0:ML_VW], AF.Identity, scale=eg[m][:, h:h + 1])
                    P.mm(pl[0:64, 128:128 + ML_VW], ptb[u][:], vh, start=True, stop=True)
                    P.tt("dve", o1[u][:], o1[u][:], pl[0:64, 128:128 + ML_VW], ALU.add)
                    P.act(den[u][:], o1[u][:, ML_DV:ML_VW], AF.Abs)
                    P.ts("dve", den[u][:], den[u][:], 1.0, None, op0=ALU.max)
                    P.recip(den[u][:], den[u][:])
                    P.ts("pool", obuf[:, h * ML_DV:(h + 1) * ML_DV], o1[u][:, 0:ML_DV], den[u][:], None, op0=ALU.mult)
                    P.mm(po[:, 0:ML_VW], kd[u][:], vh, start=True, stop=True)
                    P.stt("dve", S[ci][:, h, :], S[ci][:, h, :], ege[m][:, h:h + 1], po[:, 0:ML_VW], ALU.mult, ALU.add)
                    P.copy("act", Sb[ci][:, h, :], S[ci][:, h, :])
                tk0 = t0 + c * 64
                P.dma("sp" if nch % 2 else "pool", View(OD[d].t[tk0:tk0 + 64, :], ("ml_OD%d" % d, None)), obuf[:])
    P.pop()
    P.push()
    pb = [P.ps("m3_ps%d" % j) for j in range(8)]
    L = Ctx()
    L.mean = P.sb("m3_mean", [128, TT], F32)
    L.rstd = P.sb("m3_rstd", [128, TT], F32)
    L.sq = P.sb("m3_sq", [128, 8, TT], F32)
    Wo = P.sb("m3_Wo", [128, 8, 1024], BF16)
    stg = [P.sb("m3_stg%d" % j, [128, 8, 512], F32) for j in range(2)]
    load_w_bf16(P, Wo, IN("mlstm_w_out", [D, D]).t, 1024, stg)
    ng = P.sb("m3_ng", [128, 8], F32)
    P.dma("sp", ng[:], IN("ml_normg", [128, 8])[:, :])
    xt = P.sb("m3_x", [128, 8, TT], F32)
    yT = P.sb("m3_yT", [128, 8, TT], BF16)
    ogt = P.sb("m3_og", [128, 8, TT], BF16)
    oa = [P.sb("m3_oa%d" % j, [128, 1024], F32) for j in range(2)]
    obb = [P.sb("m3_ob%d" % j, [128, 1024], F32) for j in range(2)]
    st = [P.sb("m3_st%d" % j, [128, 8], F32) for j in range(2)]
    n = 0
    for t in tiles:
        seg = seg_of_tile(t)
        tok = slice(t * TT, (t + 1) * TT)
        P.dma("sp", xt[:], View(C.XS.t[:, :, tok].rearrange("c p n -> p c n"), ("XS", t)))
        P.dma("pool", ogt[:], View(OG.t[:, :, tok].rearrange("c p n -> p c n"), ("ml_OG", None)))
        for s in range(4):
            a = oa[s % 2]
            bb = obb[s % 2]
            sv = st[s % 2]
            r0 = t * TT + s * 128
            P.dma("sp", a[:], View(OD[0].t[r0:r0 + 128, :], ("ml_OD0", None)))
            P.dma("pool", bb[:], View(OD[1].t[r0:r0 + 128, :], ("ml_OD1", None)))
            P.tt("pool", a[:], a[:], bb[:], ALU.add)
            P.reduce("dve", sv[:, 0:4], View(a.t[:].rearrange("p (h k) -> p h k", k=ML_DV), a[:].key), ALU.add)
            P.ts("dve", sv[:, 0:4], sv[:, 0:4], -1.0 / ML_DV, None, op0=ALU.mult)
            for h in range(ML_H):
                hs = slice(h * ML_DV, (h + 1) * ML_DV)
                P.ts("dve" if h % 2 else "pool", a[:, hs], a[:, hs], sv[:, h:h + 1], None, op0=ALU.add)
                P.act(bb[:, hs], a[:, hs], AF.Square, accum_out=sv[:, 4 + h:5 + h])
            P.ts("dve", sv[:, 4:8], sv[:, 4:8], 1.0 / ML_DV, 1e-6, op0=ALU.mult, op1=ALU.add)
            P.act(sv[:, 4:8], sv[:, 4:8], AF.Sqrt)
            P.recip(sv[:, 4:8], sv[:, 4:8])
            for h in range(ML_H):
                hs = slice(h * ML_DV, (h + 1) * ML_DV)
                P.ts("dve" if h % 2 else "pool", a[:, hs], a[:, hs], sv[:, 4 + h:5 + h], None, op0=ALU.mult)
            for c in range(8):
                pt = pb[2 + n % 6]
                n += 1
                P.transpose(pt[:, 0:128], a[:, c * 128:(c + 1) * 128], C.ident[:])
                P.stt("dve", yT[:, c, s * 128:(s + 1) * 128], pt[:, 0:128], ng[:, c:c + 1], ogt[:, c, s * 128:(s + 1) * 128], ALU.mult, ALU.mult)
        out_proj_ln(P, C, L, i, t, xt, yT, Wo, pb)
    P.pop()


GD_H = 8
SEQS = [(0, 256, False), (256, 256, False), (512, 2048, True), (2560, 2048, True)]


def inv_unit_lower(P, C, Bm, W, pbX, pbY, pbP):
    X = [Bm] + W.x
    Y = W.y
    Pm = W.p
    P.tt("pool", Pm[:], Y[0][:], C.ident[0:64, 0:64], ALU.add)
    for k in range(1, 6):
        cx = ((k - 1) % 2) * 64
        P.mm(pbX[0:64, cx:cx + 64], Y[k - 1][:], X[k - 1][:], start=True, stop=True)
        if k < 5:
            P.mm(pbY[0:64, cx:cx + 64], X[k - 1][:], Y[k - 1][:], start=True, stop=True)
        P.copy("act", X[k][:], pbX[0:64, cx:cx + 64])
        if k < 5:
            P.copy("dve", Y[k][:], pbY[0:64, cx:cx + 64])
        P.mm(pbP[0:64, cx:cx + 64], X[k][:], Pm[:], start=True, stop=True)
        P.tt("dve", Pm[:], Pm[:], pbP[0:64, cx:cx + 64], ALU.add)
    return Pm


def phase_gdn(P, C, IN, i, tiles):
    ZR = P.dram("gd_ZR", [24, 128, NT], F32)
    GG = P.dram("gd_GG", [8, 128, NT], BF16)
    QK = P.dram("gd_QK", [16, 128, NT], BF16)
    KT = P.dram("gd_KT", [NT, 1024], BF16)
    VT = P.dram("gd_VT", [NT, 1024], BF16)
    GT = P.dram("gd_GT", [NT, 32], F32)
    OD = [P.dram("gd_OD%d" % d, [NT, 1024], F32) for d in range(2)]
    P.push()
    pb = [P.ps("g1_ps%d" % j) for j in range(6)]
    W = P.sb("g1_W", [128, 8, 4128], BF16)
    stg = [P.sb("g1_stg%d" % j, [128, 8, 512], F32) for j in range(2)]
    load_w_bf16(P, W, IN("gdn_w_in", [D, 4128]).t, 4128, stg)
    dtb = P.sb("g1_dtb", [128, 32], F32)
    nea = P.sb("g1_nea", [128, 32], F32)
    P.dma("sp", dtb[:], IN("gd_dtb", [128, 32])[:, :])
    P.dma("sp", nea[:], IN("gd_alog", [128, 32])[:, :])
    P.act(nea[:], nea[:], AF.Exp)
    P.ts("dve", nea[:], nea[:], -1.0, None, op0=ALU.mult)
    xt = P.sb("g1_x", [128, 8, TT], F32)
    hb = P.sb("g1_hb", [128, 8, TT], BF16)
    zt = [P.sb("g1_z%d" % j, [128, 4, TT], F32) for j in range(2)]
    gg = P.sb("g1_gg", [128, 8, TT], BF16)
    gt = P.sb("g1_gt", [128, 4, 32], F32)
    ge = P.sb("g1_ge", [128, 4, 32], F32)
    n = 0
    for t in tiles:
        tok = slice(t * TT, (t + 1) * TT)
        load_xh(P, C, i, t, xt, hb)
        for g4 in range(6):
            z = zt[g4 % 2]
            for cc in range(4):
                oc = g4 * 4 + cc
                ps = pb[n % 4]
                n += 1
                for kc in range(8):
                    P.mm(ps[:], W[:, kc, oc * 128:(oc + 1) * 128], hb[:, kc, :], start=(kc == 0), stop=(kc == 7))
                P.copy("act" if cc % 2 else "dve", z[:, cc, :], ps[:])
            P.dma("sp" if g4 % 2 else "pool", View(ZR.t[g4 * 4:(g4 + 1) * 4, :, tok].rearrange("c p n -> p c n"), ("gd_ZR", t)), z[:])
        for c in range(8):
            ps = pb[n % 4]
            n += 1
            for kc in range(8):
                P.mm(ps[:], W[:, kc, 3072 + c * 128:3072 + (c + 1) * 128], hb[:, kc, :], start=(kc == 0), stop=(kc == 7))
            P.act(gg[:, c, :], ps[:], AF.Silu)
        P.dma("pool", View(GG.t[:, :, tok].rearrange("c p n -> p c n"), ("gd_GG", t)), gg[:])
        for s in range(4):
            ps = pb[4 + s % 2]
            for kc in range(8):
                P.mm(ps[:, 0:32], hb[:, kc, s * 128:(s + 1) * 128], W[:, kc, 4096:4128], start=(kc == 0), stop=(kc == 7))
            P.tt("dve", gt[:, s, :], ps[:, 0:32], dtb[:], ALU.add)
        P.act(ge[:], gt[:], AF.Exp)
        P.act(ge[:], ge[:], AF.Ln, bias=1.0)
        P.act(gt[:], gt[:], AF.Sigmoid)
        for d in range(2):
            for s in range(4):
                P.tt("dve", gt[:, s, d * 16:d * 16 + 8], ge[:, s, d * 16:d * 16 + 8], nea[:, d * 16:d * 16 + 8], ALU.mult)
        P.dma("sp", View(GT.t[tok, :].rearrange("(s p) f -> p s f", p=128), ("gd_GT", t)), gt[:])
    P.pop()
    P.push()
    pb = [P.ps("g1b_ps%d" % j) for j in range(6)]
    pbt = [P.ps("g1b_pt%d" % j, [128, 1024], BF16) for j in range(2)]
    R = rope_setup(P, C, IN, pb[4:6])
    cw = P.sb("g1b_cw", [128, 24 * 3], F32)
    P.dma("sp", cw[:], IN("gd_conv", [128, 72])[:, :])
    zr = [P.sb("g1b_z%d" % j, [128, 2048], F32) for j in range(2)]
    yy = [P.sb("g1b_y%d" % j, [128, 2048], F32) for j in range(2)]
    sq = P.sb("g1b_sq", [128, 512], F32)
    rn = P.sb("g1b_rn", [128, 512], F32)
    yb = [P.sb("g1b_yb%d" % j, [128, 2048], BF16) for j in range(2)]
    tm = [P.sb("g1b_tm%d" % j, [128, 16, 128], BF16) for j in range(2)]
    epsq = P.sb("g1b_epsq", [128, 1], F32)
    epsk = P.sb("g1b_epsk", [128, 1], F32)
    P.memset("dve", epsq[:], 1e-6 * 128.0)
    P.memset("dve", epsk[:], 1e-6)
    n = 0
    for c in range(24):
        for (t0, ns, is_lat) in SEQS:
            j = n % 2
            n += 1
            z = zr[j]
            y = yy[j]
            P.dma("sp" if n % 2 else "pool", z[:, 0:ns], View(ZR.t[c, :, t0:t0 + ns], ("gd_ZR", None)))
            P.ts("dve", y[:, 0:ns], z[:, 0:ns], cw[:, c * 3 + 1:c * 3 + 2], None, op0=ALU.mult)
            P.stt("pool", y[:, 1:ns], z[:, 0:ns - 1], cw[:, c * 3:c * 3 + 1], y[:, 1:ns], ALU.mult, ALU.add)
            P.stt("dve", y[:, 0:ns - 1], z[:, 1:ns], cw[:, c * 3 + 2:c * 3 + 3], y[:, 0:ns - 1], ALU.mult, ALU.add)
            P.act(y[:, 0:ns], y[:, 0:ns], AF.Silu)
            ybj = yb[j]
            if c < 16:
                for s0 in range(0, ns, 512):
                    w = min(512, ns - s0)
                    sl = slice(s0, s0 + w)
                    P.act(sq[:, 0:w], y[:, sl], AF.Square)
                    ps = pb[n % 2]
                    P.mm(ps[:, 0:w], C.ones[:], sq[:, 0:w], start=True, stop=True)
                    if c < 8:
                        P.act(rn[:, 0:w], ps[:, 0:w], AF.Sqrt, bias=epsq[:], scale=128.0)
                    else:
                        P.act(rn[:, 0:w], ps[:, 0:w], AF.Sqrt, bias=epsk[:], scale=1.0)
                    P.recip(rn[:, 0:w], rn[:, 0:w])
                    if is_lat:
                        P.tt("dve", y[:, sl], y[:, sl], rn[:, 0:w], ALU.mult)
                        pr = pb[2 + (s0 // 512) % 2]
                        P.mm(pr[:, 0:w], R.rotT[:], y[:, sl], start=True, stop=True)
                        P.tt("pool", sq[:, 0:w], y[:, sl], R.cos[:, sl], ALU.mult)
                        P.tt("dve", rn[:, 0:w], pr[:, 0:w], R.sin[:, sl], ALU.mult)
                        P.tt("pool", ybj[:, sl], sq[:, 0:w], rn[:, 0:w], ALU.add)
                    else:
                        P.tt("dve", ybj[:, sl], y[:, sl], rn[:, 0:w], ALU.mult)
                P.dma("sp", View(QK.t[c, :, t0:t0 + ns], ("gd_QK", None)), ybj[:, 0:ns])
            else:
                P.copy("pool", ybj[:, 0:ns], y[:, 0:ns])
            if c >= 8:
                tmj = tm[j]
                for s in range(ns // 128):
                    pt = pbt[s % 2]
                    P.transpose(pt[:, 0:128], ybj[:, s * 128:(s + 1) * 128], C.identb[:])
                    P.copy("act" if s % 2 else "dve", tmj[:, s, :], pt[:, 0:128])
                dst = KT if c < 16 else VT
                hh = (c - 8) % 8
                P.dma("pool", View(dst.t[t0:t0 + ns, hh * 128:(hh + 1) * 128].rearrange("(s p) f -> p s f", p=128), (dst.name, None)),
                      tmj[:, 0:ns // 128, :])
    P.pop()
    P.push()
    B = [P.ps("g2_ps%d" % j) for j in range(8)]
    HG = 4
    cm4 = P.sb("g2_cm4", [64, 2, 1536], F32)
    P.dma("sp", cm4[:], IN("rw_cm4", [64, 2, 1536])[:, :, :])
    qkb = [P.sb("g2_qk%d" % j, [128, 16, TT], BF16) for j in range(2)]
    ktb = [P.sb("g2_kt%d" % j, [64, 8, 1024], BF16) for j in range(2)]
    vtb = [P.sb("g2_vt%d" % j, [64, 8, 1024], BF16) for j in range(2)]
    gtb = [P.sb("g2_gt%d" % j, [64, 8, 32], F32) for j in range(2)]
    S = [P.sb("g2_S%d" % j, [128, GD_H, 128], F32) for j in range(4)]
    Sb = [P.sb("g2_Sb%d" % j, [128, GD_H, 128], BF16) for j in range(4)]
    NB = 2
    eg = [P.sb("g2_eg%d" % j, [64, 32], F32) for j in range(NB)]
    ege = [P.sb("g2_ege%d" % j, [128, 8], F32) for j in range(NB)]
    ob = [P.sb("g2_ob%d" % j, [64, 1024], F32) for j in range(2)]
    shared = {}

    def tset(j):
        T = Ctx()
        f = lambda nm, shp=(64, HG, 64), dt=F32: P.sb("g2_%s%d" % (nm, j), list(shp), dt)
        T.ula, T.sla, T.gi, T.gj, T.A = f("ula"), f("sla"), f("gi"), f("gj"), f("A")
        if j == 0:
            T.x = [f("x%d" % k) for k in range(1, 6)]
            T.y = [f("y0")] + [f("y%d" % k) for k in range(1, 5)]
            shared["x"], shared["y"] = T.x, T.y
        else:
            T.x = shared["x"]
            T.y = [f("y0")] + shared["y"][1:]
        T.p = f("p")
        T.ttb = f("ttb", dt=BF16)
        T.ptb = f("ptb", dt=BF16)
        T.bv = f("bv", (64, HG, 128), BF16)
        T.bk = f("bk", (64, HG, 128), BF16)
        T.kd = f("kd", (64, HG, 128), BF16)
        T.u0 = f("u0", (64, HG, 128))
        T.wx = f("wx", (128, HG, 64), BF16)
        T.dl = f("dl", (64, HG, 128), BF16)
        T.o1 = f("o1", (64, HG, 128))
        T.stmp = f("stmp", (128, HG, 128))
        return T

    TS = [tset(j) for j in range(2)]
    chains = [(b, d) for b in range(2) for d in range(2)]
    plans = {bd: chunk_plan(*bd) for bd in chains}
    for ci in range(4):
        P.memset("pool", S[ci][:], 0.0)
        P.memset("pool", Sb[ci][:], 0.0)
    nblk = 0
    n = 0
    nch = 0

    def f2(v3):
        return View(v3.ap.rearrange("p a b -> p (a b)"), v3.key)

    def bc(v2, w):
        return View(v2.ap.rearrange("p (h o) -> p h o", o=1).to_broadcast([v2.ap.shape[0], HG, w]), v2.key)

    for step in range(5):
        for ci, (b, d) in enumerate(chains):
            t0, clist = plans[(b, d)][step]
            ntok = 64 * len(clist)
            nc_ = len(clist)
            j = nblk % 2
            nblk += 1
            U4 = cm4[:, d, 0:256]
            ST4 = cm4[:, d, 512:768]
            I4 = cm4[:, d, 1280:1536]
            Ud, STd = cmat(C, "U", d), cmat(C, "ST", d)
            P.dma("sp", qkb[j][:, :, 0:ntok], View(QK.t[:, :, t0:t0 + ntok].rearrange("c p n -> p c n"), ("gd_QK", None)))
            P.dma("pool", ktb[j][:, 0:nc_, :], View(KT.t[t0:t0 + ntok, :].rearrange("(c p) f -> p c f", p=64), ("gd_KT", None)))
            P.dma("sp", vtb[j][:, 0:nc_, :], View(VT.t[t0:t0 + ntok, :].rearrange("(c p) f -> p c f", p=64), ("gd_VT", None)))
            P.dma("pool", gtb[j][:, 0:nc_, :], View(GT.t[t0:t0 + ntok, :].rearrange("(c p) f -> p c f", p=64), ("gd_GT", None)))
            for c in clist:
                cs = slice(c * 64, (c + 1) * 64)
                la = gtb[j][:, c, d * 16:d * 16 + 8]
                be = gtb[j][:, c, d * 16 + 8:d * 16 + 16]
                m_ = nch % NB
                nch += 1
                pg = B[7]
                P.mm(pg[0:64, 448:456], Ud, la, start=True, stop=True)
                P.mm(pg[0:64, 456:464], STd, la, start=True, stop=True)
                P.mm(pg[:, 464:472], C.ones64[:], la, start=True, stop=True)
                P.act(eg[m_][:, 0:16], pg[0:64, 448:464], AF.Exp)
                P.act(ege[m_][:], pg[:, 464:472], AF.Exp)
                P.tt("dve", eg[m_][:, 16:24], eg[m_][:, 0:8], be, ALU.mult)
                P.ts("dve", eg[m_][:, 24:32], be, -1.0, None, op0=ALU.mult)
                obuf = ob[nch % 2]
                for g in range(GD_H // HG):
                    T = TS[n % 2]
                    n += 1
                    H = range(HG)
                    h0 = g * HG
                    lag = gtb[j][:, c, d * 16 + h0:d * 16 + h0 + HG]
                    P.tt("dve", T.ula[:], View(U4.ap.rearrange("p (h k) -> p h k", h=HG), U4.key), bc(lag, 64), ALU.mult)
                    P.tt("pool", T.sla[:], View(ST4.ap.rearrange("p (h k) -> p h k", h=HG), ST4.key), bc(lag, 64), ALU.mult)
                    for hh in H:
                        h = h0 + hh
                        kT = qkb[j][:, 8 + h, cs]
                        qT = qkb[j][:, h, cs]
                        P.mm(B[0][0:64, hh * 64:(hh + 1) * 64], T.ula[:, hh, :], STd, start=True, stop=True)
                        P.mm(B[0][0:64, 256 + hh * 64:256 + (hh + 1) * 64], T.sla[:, hh, :], Ud, start=True, stop=True)
                        P.mm(B[1][0:64, hh * 64:(hh + 1) * 64], kT, kT, start=True, stop=True)
                        P.mm(B[1][0:64, 256 + hh * 64:256 + (hh + 1) * 64], kT, qT, start=True, stop=True)
                    P.act(f2(T.gi[:]), B[0][0:64, 0:256], AF.Exp)
                    P.act(f2(T.gj[:]), B[0][0:64, 256:512], AF.Exp)
                    P.tt("pool", f2(T.gi[:]), f2(T.gi[:]), ST4, ALU.mult)
                    P.tt("pool", f2(T.gj[:]), f2(T.gj[:]), U4, ALU.mult)
                    P.tt("dve", f2(T.gi[:]), B[1][0:64, 0:256], f2(T.gi[:]), ALU.mult)
                    P.tt("dve", T.A[:], T.gi[:], bc(eg[m_][:, 24 + h0:24 + h0 + HG], 64), ALU.mult)
                    P.tt("dve", f2(T.ptb[:]), B[1][0:64, 256:512], f2(T.gj[:]), ALU.mult)
                    for hh in H:
                        P.transpose(B[2][0:64, hh * 64:(hh + 1) * 64], T.A[:, hh, :], C.ident[0:64, 0:64])
                    P.copy("act", f2(T.y[0][:]), B[2][0:64, 0:256])
                    X = [T.A] + T.x
                    Y = T.y
                    Pm = T.p
                    P.tt("pool", f2(Pm[:]), f2(Y[0][:]), I4, ALU.add)
                    for k in range(1, 6):
                        cx = ((k - 1) % 2) * 256
                        for hh in H:
                            P.mm(B[4][0:64, cx + hh * 64:cx + (hh + 1) * 64], Y[k - 1][:, hh, :], X[k - 1][:, hh, :], start=True, stop=True)
                        if k < 5:
                            for hh in H:
                                P.mm(B[5][0:64, cx + hh * 64:cx + (hh + 1) * 64], X[k - 1][:, hh, :], Y[k - 1][:, hh, :], start=True, stop=True)
                        P.copy("act", f2(X[k][:]), B[4][0:64, cx:cx + 256])
                        if k < 5:
                            P.copy("dve", f2(Y[k][:]), B[5][0:64, cx:cx + 256])
                        for hh in H:
                            P.mm(B[6][0:64, cx + hh * 64:cx + (hh + 1) * 64], X[k][:, hh, :], Pm[:, hh, :], start=True, stop=True)
                        P.tt("dve", f2(Pm[:]), f2(Pm[:]), B[6][0:64, cx:cx + 256], ALU.add)
                    P.copy("act", T.ttb[:], Pm[:])
                    k3 = View(ktb[j].t[:, c, h0 * 128:(h0 + HG) * 128].rearrange("p (h k) -> p h k", h=HG), ktb[j][:].key)
                    v3 = View(vtb[j].t[:, c, h0 * 128:(h0 + HG) * 128].rearrange("p (h k) -> p h k", h=HG), vtb[j][:].key)
                    P.tt("pool", T.bv[:], v3, bc(gtb[j][:, c, d * 16 + 8 + h0:d * 16 + 8 + h0 + HG], 128), ALU.mult)
                    P.tt("pool", T.bk[:], k3, bc(eg[m_][:, 16 + h0:16 + h0 + HG], 128), ALU.mult)
                    P.tt("pool", T.kd[:], k3, bc(eg[m_][:, 8 + h0:8 + h0 + HG], 128), ALU.mult)
                    for hh in H:
                        P.mm(B[3][0:64, hh * 128:(hh + 1) * 128], T.ttb[:, hh, :], T.bv[:, hh, :], start=True, stop=True)
                        P.mm(B[7][:, hh * 64:(hh + 1) * 64], T.bk[:, hh, :], T.ttb[:, hh, :], start=True, stop=True)
                    P.copy("dve", f2(T.u0[:]), B[3][0:64, :])
                    P.act(f2(T.wx[:]), B[7][:, 0:256], AF.Identity, scale=-1.0)
                    for hh in H:
                        P.mm(B[0][0:64, hh * 128:(hh + 1) * 128], T.wx[:, hh, :], Sb[ci][:, h0 + hh, :], start=True, stop=True)
                        P.mm(B[1][0:64, hh * 128:(hh + 1) * 128], qkb[j][:, h0 + hh, cs], Sb[ci][:, h0 + hh, :], start=True, stop=True)
                    P.tt("dve", f2(T.dl[:]), B[0][0:64, :], f2(T.u0[:]), ALU.add)
                    o13 = View(B[1].t[0:64, :].rearrange("p (h k) -> p h k", h=HG), B[1][:].key)
                    P.tt("dve", T.o1[:], o13, bc(eg[m_][:, h0:h0 + HG], 128), ALU.mult)
                    for hh in H:
                        P.mm(B[2][0:64, hh * 128:(hh + 1) * 128], T.ptb[:, hh, :], T.dl[:, hh, :], start=True, stop=True)
                        P.mm(B[3][:, hh * 128:(hh + 1) * 128], T.kd[:, hh, :], T.dl[:, hh, :], start=True, stop=True)
                    P.tt("dve", obuf[:, h0 * 128:(h0 + HG) * 128], f2(T.o1[:]), B[2][0:64, :], ALU.add)
                    Sg = S[ci][:, h0:h0 + HG, :]
                    P.tt("pool", T.stmp[:], Sg, bc(ege[m_][:, h0:h0 + HG], 128), ALU.mult)
                    P.tt("dve", f2(Sg), f2(T.stmp[:]), B[3][:, :], ALU.add)
                    P.copy("act", Sb[ci][:, h0:h0 + HG, :], Sg)
                tk0 = t0 + c * 64
                P.dma("sp" if nch % 2 else "pool", View(OD[d].t[tk0:tk0 + 64, :], ("gd_OD%d" % d, None)), obuf[:])
    P.pop()
    P.push()
    pb = [P.ps("g3_ps%d" % j) for j in range(8)]
    L = Ctx()
    L.mean = P.sb("g3_mean", [128, TT], F32)
    L.rstd = P.sb("g3_rstd", [128, TT], F32)
    L.sq = P.sb("g3_sq", [128, 8, TT], F32)
    Wo = P.sb("g3_Wo", [128, 8, 1024], BF16)
    stg = [P.sb("g3_stg%d" % j, [128, 8, 512], F32) for j in range(2)]
    load_w_bf16(P, Wo, IN("gdn_w_out", [D, D]).t, 1024, stg)
    ng = P.sb("g3_ng", [128, 1], F32)
    P.dma("sp", ng[:], IN("gd_normg", [128, 1])[:, :])
    xt = P.sb("g3_x", [128, 8, TT], F32)
    yT = P.sb("g3_yT", [128, 8, TT], BF16)
    ggt = P.sb("g3_gg", [128, 8, TT], BF16)
    oa = [P.sb("g3_oa%d" % j, [128, 1024], F32) for j in range(2)]
    obb = [P.sb("g3_ob%d" % j, [128, 1024], F32) for j in range(2)]
    st = [P.sb("g3_st%d" % j, [128, 8], F32) for j in range(2)]
    n = 0
    for t in tiles:
        tok = slice(t * TT, (t + 1) * TT)
        P.dma("sp", xt[:], View(C.XS.t[:, :, tok].rearrange("c p n -> p c n"), ("XS", t)))
        P.dma("pool", ggt[:], View(GG.t[:, :, tok].rearrange("c p n -> p c n"), ("gd_GG", None)))
        for s in range(4):
            a = oa[s % 2]
            bb = obb[s % 2]
            sv = st[s % 2]
            r0 = t * TT + s * 128
            P.dma("sp", a[:], View(OD[0].t[r0:r0 + 128, :], ("gd_OD0", None)))
            P.dma("pool", bb[:], View(OD[1].t[r0:r0 + 128, :], ("gd_OD1", None)))
            P.tt("pool", a[:], a[:], bb[:], ALU.add)
            for h in range(GD_H):
                hs = slice(h * 128, (h + 1) * 128)
                P.act(bb[:, hs], a[:, hs], AF.Square, accum_out=sv[:, h:h + 1])
            P.ts("dve", sv[:], sv[:], 1.0 / 128, 1e-6, op0=ALU.mult, op1=ALU.add)
            P.act(sv[:], sv[:], AF.Sqrt)
            P.recip(sv[:], sv[:])
            for h in range(GD_H):
                hs = slice(h * 128, (h + 1) * 128)
                P.ts("dve" if h % 2 else "pool", a[:, hs], a[:, hs], sv[:, h:h + 1], None, op0=ALU.mult)
            for c in range(8):
                pt = pb[2 + n % 6]
                n += 1
                P.transpose(pt[:, 0:128], a[:, c * 128:(c + 1) * 128], C.ident[:])
                P.stt("dve", yT[:, c, s * 128:(s + 1) * 128], pt[:, 0:128], ng[:, 0:1], ggt[:, c, s * 128:(s + 1) * 128], ALU.mult, ALU.mult)
        out_proj_ln(P, C, L, i, t, xt, yT, Wo, pb)
    P.pop()


RW_H = 16
RW_DEBUG = 0
RW_CUT = 0
HT = 128


def dma_heads_out(P, X, u0, n, src):
    for half in range(2):
        dv = X.t.rearrange("(c two) p n -> two c p n", two=2)[half][:, :, u0:u0 + n].rearrange("c p n -> p c n")
        sv = View(src.ap[half * 64:(half + 1) * 64], src.key)
        P.dma("sp" if half else "pool", View(dv, (X.name, None)), sv)


def phase_rwkv(P, C, IN, i, tiles):
    RF = P.dram("rw_RF", [16, 64, NT], F32)
    KK = P.dram("rw_KK", [16, 64, NT], F32)
    KD = [P.dram("rw_KD%d" % d, [16, 64, NT], F32) for d in range(2)]
    AT = [P.dram("rw_AT%d" % d, [16, 64, NT], F32) for d in range(2)]
    LW = [P.dram("rw_LW%d" % d, [NT, 1024], F32) for d in range(2)]
    VT = P.dram("rw_VT", [NT, 1024], BF16)
    GG = P.dram("rw_GG", [8, 128, NT], BF16)
    BN = P.dram("rw_BN", [8, 128, NT], F32)
    OD = [P.dram("rw_OD%d" % d, [NT, 1024], F32) for d in range(2)]
    P.push()
    pb = [P.ps("r1_ps%d" % j) for j in range(8)]
    Wr = [P.sb("r1_W%d" % j, [128, 8, 1024], BF16) for j in range(3)]
    g1w = P.sb("r1_g1w", [128, 8, 128], BF16)
    a1w = [P.sb("r1_a1w%d" % d, [128, 8, 64], BF16) for d in range(2)]
    w1w = [P.sb("r1_w1w%d" % d, [128, 8, 64], BF16) for d in range(2)]
    g2w = P.sb("r1_g2w", [128, 1024], BF16)
    a2w = [P.sb("r1_a2w%d" % d, [64, 1024], BF16) for d in range(2)]
    w2w = [P.sb("r1_w2w%d" % d, [64, 1024], BF16) for d in range(2)]
    P.push()
    stg = [P.sb("r1_stg%d" % j, [128, 8, 512], F32) for j in range(2)]
    for j in range(3):
        load_w_bf16(P, Wr[j], IN("rwkv_w_rkv", [3, D, D]).t[j], 1024, stg)
    load_w_bf16(P, g1w, IN("rwkv_g1", [D, 128]).t, 128, stg)
    for d in range(2):
        load_w_bf16(P, a1w[d], IN("rwkv_a1", [2, D, 64]).t[d], 64, stg)
        load_w_bf16(P, w1w[d], IN("rwkv_w1", [2, D, 64]).t[d], 64, stg)
    sflat = stg[0].t[:].rearrange("p a b -> p (a b)")
    skey = stg[0][:].key
    P.dma("sp", View(sflat[:, 0:1024], skey), IN("rwkv_g2", [128, D])[:, :])
    P.copy("dve", g2w[:], View(sflat[:, 0:1024], skey))
    for d in range(2):
        P.dma("sp", View(sflat[0:64, 0:1024], skey), IN("rwkv_a2", [2, 64, D])[d])
        P.copy("dve", a2w[d][:], View(sflat[0:64, 0:1024], skey))
        P.dma("sp", View(sflat[0:64, 0:1024], skey), IN("rwkv_w2", [2, 64, D])[d])
        P.copy("dve", w2w[d][:], View(sflat[0:64, 0:1024], skey))
    P.pop()
    w0r = P.sb("r1_w0", [128, 2, 1024], F32)
    P.dma("sp", w0r[:], IN("rw_w0", [128, 2, 1024])[:, :, :])
    cst = P.sb("r1_cst", [128, 13 * 8], F32)
    P.dma("sp", cst[:], IN("rw_cst", [128, 104])[:, :])
    P.ts("dve", cst[:, 64:72], cst[:, 56:64], -1.0, 1.0, op0=ALU.mult, op1=ALU.add)
    bones = P.sb("r1_bones", [128, 128], F32)
    P.dma("sp", bones[:], IN("rw_bones", [128, 128])[:, :])
    eps6 = P.sb("r1_eps6", [128, 1], F32)
    P.memset("dve", eps6[:], 1e-6)
    mhalf = P.sb("r1_mhalf", [128, 1], F32)
    P.memset("dve", mhalf[:], -0.5)
    HL = 64
    HW = HT + 2 * HL
    hx = P.sb("r1_hx", [128, 8, HW], F32)
    dx = P.sb("r1_dx", [128, 8, HT], F32)
    xm = [P.sb("r1_xm%d" % j, [128, 8, HT], BF16) for j in range(2)]
    kf = P.sb("r1_kf", [128, 8, HT], F32)
    vf = P.sb("r1_vf", [128, 8, HT], F32)
    rr = P.sb("r1_rr", [128, 8, HT], F32)
    kkf = P.sb("r1_kkf", [128, 8, HT], F32)
    of = [P.sb("r1_of%d" % j, [128, 8, HT], F32) for j in range(2)]
    ogb = P.sb("r1_ogb", [128, 8, HT], BF16)
    vtt = P.sb("r1_vtt", [128, HT // 128, 1024], BF16)
    lwt = [P.sb("r1_lwt%d" % j, [128, max(HT // 128, 1), 1024], F32) for j in range(2)]
    tmp = [P.sb("r1_tmp%d" % j, [128, 512], F32) for j in range(4)]
    tl = [P.sb("r1_tl%d" % j, [128, HT], BF16) for j in range(2)]
    n = 0
    nm = 0

    def mix(j):
        nonlocal nm
        x = xm[nm % 2]
        nm += 1
        for kc in range(8):
            P.stt("dve", x[:, kc, :], dx[:, kc, :], cst[:, j * 8 + kc:j * 8 + kc + 1], hx[:, kc, HL:HL + HT], ALU.mult, ALU.add)
        return x

    def proj_fm(x, Wt, oc, ncols=128, krows=128, kchunks=8):
        nonlocal n
        ps = pb[n % 6]
        n += 1
        for kc in range(kchunks):
            P.mm(ps[0:ncols, 0:HT], Wt[:, kc, oc * 128:oc * 128 + ncols], x[:, kc, :], start=(kc == 0), stop=(kc == kchunks - 1))
        return ps

    units = []
    for t in tiles:
        units += [(t, hf) for hf in range(TT // HT)]
    for (t, hf) in units:
        seg = seg_of_tile(t)
        u0 = t * TT + hf * HT
        sq0, sqn = [(a, b) for (a, b, _) in SEQS if a <= u0 < a + b][0]
        has_l, has_r = u0 > sq0, u0 + HT < sq0 + sqn
        P.dma("sp", hx[:, :, HL:HL + HT], View(C.XS.t[:, :, u0:u0 + HT].rearrange("c p n -> p c n"), ("XS", t)))
        if has_l:
            P.dma("pool", hx[:, :, 0:HL], View(C.XS.t[:, :, u0 - HL:u0].rearrange("c p n -> p c n"), ("XS", (u0 - 1) // TT)))
        if has_r:
            P.dma("pool", hx[:, :, HL + HT:HW], View(C.XS.t[:, :, u0 + HT:u0 + HT + HL].rearrange("c p n -> p c n"), ("XS", (u0 + HT) // TT)))
        for kc in range(8):
            c_sc = modcol(i, 1, kc, seg)
            c_sh = modcol(i, 0, kc, seg)
            P.ts("dve" if kc % 2 else "pool", hx[:, kc, HL - 1:HL + HT + 1], hx[:, kc, HL - 1:HL + HT + 1], C.mod[:, c_sc:c_sc + 1], C.mod[:, c_sh:c_sh + 1], op0=ALU.mult, op1=ALU.add)
        if not has_l:
            P.memset("pool", hx[:, :, HL - 1:HL], 0.0)
        if not has_r:
            P.memset("pool", hx[:, :, HL + HT:HL + HT + 1], 0.0)
        for kc in range(8):
            P.tt("pool", dx[:, kc, :], hx[:, kc, HL - 1:HL - 1 + HT], hx[:, kc, HL + 1:HL + 1 + HT], ALU.add)
            P.stt("dve", dx[:, kc, :], dx[:, kc, :], 0.5, hx[:, kc, HL:HL + HT], ALU.mult, ALU.subtract)
        x = mix(0)
        o = of[0]
        for c in range(8):
            ps = proj_fm(x, Wr[0], c)
            P.copy("act", o[:, c, :], ps[:, 0:HT])
            P.ts("pool", rr[:, c, :], o[:, c, :], cst[:, 72 + c:73 + c], None, op0=ALU.mult)
        dma_heads_out(P, RF, u0, HT, o[:])
        x = mix(2)
        o = of[1]
        for c in range(8):
            ps = proj_fm(x, Wr[1], c)
            P.copy("act", kf[:, c, :], ps[:, 0:HT])
            t1 = tmp[c % 2]
            P.ts("pool", t1[:, 0:HT], kf[:, c, :], cst[:, 48 + c:49 + c], None, op0=ALU.mult)
            t2 = tmp[2 + c % 2]
            P.act(t2[:, 0:HT], t1[:, 0:HT], AF.Square)
            pq = pb[6 + c % 2]
            P.mm(pq[:, 0:HT], bones[:], t2[:, 0:HT], start=True, stop=True)
            P.act(t2[:, 0:HT], pq[:, 0:HT], AF.Sqrt, bias=eps6[:])
            P.recip(t2[:, 0:HT], t2[:, 0:HT])
            P.tt("pool", kkf[:, c, :], t1[:, 0:HT], t2[:, 0:HT], ALU.mult)
        dma_heads_out(P, KK, u0, HT, kkf[:])
        x = mix(3)
        for c in range(8):
            ps = proj_fm(x, Wr[2], c)
            P.copy("act", vf[:, c, :], ps[:, 0:HT])
        for s in range(HT // 128):
            for blk in range(2):
                ps = pb[n % 6]
                n += 1
                for kc in range(8):
                    P.mm(ps[:], x[:, kc, s * 128:(s + 1) * 128], Wr[2][:, kc, blk * 512:(blk + 1) * 512], start=(kc == 0), stop=(kc == 7))
                P.copy("act" if blk else "dve", vtt[:, s, blk * 512:(blk + 1) * 512], ps[:])
        P.dma("sp", View(VT.t[u0:u0 + HT, :].rearrange("(s p) f -> p s f", p=128), ("rw_VT", None)), vtt[:])
        x = mix(5)
        ps = proj_fm(x, g1w, 0)
        P.act(tl[0][:], ps[:, 0:HT], AF.Sigmoid)
        for c in range(8):
            ps = pb[n % 6]
            n += 1
            P.mm(ps[:, 0:HT], g2w[:, c * 128:(c + 1) * 128], tl[0][:], start=True, stop=True)
            P.copy("act", ogb[:, c, :], ps[:, 0:HT])
        P.dma("pool", View(GG.t[:, :, u0:u0 + HT].rearrange("c p n -> p c n"), ("rw_GG", None)), ogb[:])
        xa = mix(4)
        kdo = [of[0], of[1]]
        for d in range(2):
            ps = proj_fm(xa, a1w[d], 0, ncols=64)
            P.copy("act", tl[d][0:64, :], ps[0:64, 0:HT])
        ato = lwt
        atv = [View(lwt[d].t[:].rearrange("p a b -> p (a b)")[:, 0:8 * HT].rearrange("p (c n) -> p c n", c=8), lwt[d][:].key) for d in range(2)]
        for c in range(8):
            pbn = pb[6 + c % 2]
            for d in range(2):
                ps = pb[n % 6]
                n += 1
                P.mm(ps[:, 0:HT], a2w[d][:, c * 128:(c + 1) * 128], tl[d][0:64, :], start=True, stop=True)
                af = tmp[d]
                P.act(af[:, 0:HT], ps[:, 0:HT], AF.Sigmoid, bias=cst[:, 80 + d * 8 + c:81 + d * 8 + c])
                P.tt("pool", View(atv[d].ap[:, c, :], atv[d].key), af[:, 0:HT], kkf[:, c, :], ALU.mult)
                P.ts("dve", af[:, 0:HT], af[:, 0:HT], cst[:, 56 + c:57 + c], cst[:, 64 + c:65 + c], op0=ALU.mult, op1=ALU.add)
                P.tt("dve", kdo[d][:, c, :], kf[:, c, :], af[:, 0:HT], ALU.mult)
                t2 = tmp[2 + d]
                P.tt("pool", t2[:, 0:HT], rr[:, c, :], kdo[d][:, c, :], ALU.mult)
                P.mm(pbn[:, 0:HT], bones[:], t2[:, 0:HT], start=(d == 0), stop=(d == 1))
            P.tt("dve", vf[:, c, :], vf[:, c, :], pbn[:, 0:HT], ALU.mult)
        for d in range(2):
            dma_heads_out(P, KD[d], u0, HT, kdo[d][:])
            dma_heads_out(P, AT[d], u0, HT, atv[d])
        P.dma("sp", View(BN.t[:, :, u0:u0 + HT].rearrange("c p n -> p c n"), ("rw_BN", None)), vf[:])
        xw = mix(1)
        for d in range(2):
            ps = proj_fm(xw, w1w[d], 0, ncols=64)
            P.act(tl[d][0:64, :], ps[0:64, 0:HT], AF.Tanh)
        for d in range(2):
            lo = lwt[d]
            for s in range(HT // 128):
                for blk in range(2):
                    ps = pb[n % 6]
                    n += 1
                    P.mm(ps[:], tl[d][0:64, s * 128:(s + 1) * 128], w2w[d][:, blk * 512:(blk + 1) * 512], start=True, stop=True)
                    sl = slice(blk * 512, (blk + 1) * 512)
                    tq = tmp[(s * 2 + blk) % 4]
                    P.tt("dve", tq[:], ps[:], w0r[:, d, sl], ALU.add)
                    P.act(tq[:], tq[:], AF.Exp, scale=-1.0)
                    P.act(tq[:], tq[:], AF.Ln, bias=1.0)
                    P.act(tq[:], tq[:], AF.Exp, scale=-1.0, bias=mhalf[:])
                    P.ts("pool", lo[:, s, sl], tq[:], -1.0, None, op0=ALU.mult)
            P.dma("sp" if d else "pool", View(LW[d].t[u0:u0 + HT, :].rearrange("(s p) f -> p s f", p=128), ("rw_LW%d" % d, None)), lo[:])
    P.pop()
    P.push()
    B = [P.ps("r2_ps%d" % j) for j in range(8)]
    HG = 4
    HW4 = HG * 64
    cm4 = P.sb("r2_cm4", [64, 2, 1536], F32)
    P.dma("sp", cm4[:], IN("rw_cm4", [64, 2, 1536])[:, :, :])
    fmb = [[P.sb("r2_fm%d_%d" % (k, j), [64, HG, TT], F32) for k in range(4)] for j in range(2)]
    lwb = [P.sb("r2_lw%d" % j, [64, 8, HW4], F32) for j in range(2)]
    vtb = [P.sb("r2_vt%d" % j, [64, 8, HW4], BF16) for j in range(2)]
    S = P.sb("r2_S", [64, 64, 64], F32)
    Sb = P.sb("r2_Sb", [64, 64, 64], BF16)
    for q in range(8):
        P.memset("pool", S[:, q * 8:(q + 1) * 8, :], 0.0)
        P.memset("dve", Sb[:, q * 8:(q + 1) * 8, :], 0.0)
    ob = [P.sb("r2_ob%d" % j, [64, 8, HW4], F32) for j in range(2)]

    shared = {}

    def tset(j):
        T = Ctx()
        f = lambda nm, shp=(64, HG, 64), dt=F32: P.sb("r2_%s%d" % (nm, j), list(shp), dt)
        T.cls, T.ecl, T.ece, T.encl, T.dec, T.A = f("cls"), f("ecl"), f("ece"), f("encl"), f("dec"), f("A")
        T.br = f("br", (64, HG, 2, 64), BF16)
        T.sk = f("sk", dt=BF16)
        T.sa = f("sa", dt=BF16)
        T.kdec = f("kdec")
        T.adec = f("adec")
        T.kdT = f("kdT", dt=BF16)
        T.nadT = f("nadT", dt=BF16)
        T.g23 = f("g23", (64, HG, 2, 64), BF16)
        T.ng4t = f("ng4t", dt=BF16)
        if j == 0:
            T.x = [f("x%d" % k) for k in range(1, 6)]
            T.y = [f("y0")] + [f("y%d" % k) for k in range(1, 5)]
            shared["x"], shared["y"] = T.x, T.y
        else:
            T.x = shared["x"]
            T.y = [f("y0")] + shared["y"][1:]
        T.p = f("p")
        T.ttb = f("ttb", dt=BF16)
        T.g2v = f("g2v")
        T.zz = f("zz", dt=BF16)
        T.ub = f("ub", dt=BF16)
        T.stmp = f("stmp")
        return T

    TS = [tset(j) for j in range(2)]
    chains = [(b, d) for b in range(2) for d in range(2)]
    plans = {bd: chunk_plan(*bd) for bd in chains}
    nblk = 0
    n = 0

    def f2(v3):
        return View(v3.ap.rearrange("p a b -> p (a b)"), v3.key)

    for step in range(5):
        for ci, (b, d) in enumerate(chains):
            t0, clist = plans[(b, d)][step]
            ntok = 64 * len(clist)
            nc_ = len(clist)
            last = 63 if d == 0 else 0
            U4 = cm4[:, d, 0:256]
            S4 = cm4[:, d, 256:512]
            ST4 = cm4[:, d, 512:768]
            SU4 = cm4[:, d, 768:1280]
            I4 = cm4[:, d, 1280:1536]
            Ud, Sd = cmat(C, "U", d), cmat(C, "S", d)
            for hg in range(RW_H // HG):
                j = nblk % 2
                nblk += 1
                hs = slice(hg * HG, (hg + 1) * HG)
                for k, src in enumerate((RF, KK, KD[d], AT[d])):
                    P.dma("sp" if k % 2 else "pool", fmb[j][k][:, :, 0:ntok], View(src.t[hs, :, t0:t0 + ntok].rearrange("h p n -> p h n"), (src.name, None)))
                P.dma("sp", lwb[j][:, 0:nc_, :], View(LW[d].t[t0:t0 + ntok, hg * HW4:(hg + 1) * HW4].rearrange("(c p) f -> p c f", p=64), ("rw_LW%d" % d, None)))
                P.dma("pool", vtb[j][:, 0:nc_, :], View(VT.t[t0:t0 + ntok, hg * HW4:(hg + 1) * HW4].rearrange("(c p) f -> p c f", p=64), ("rw_VT", None)))
                obuf = ob[j]
                si0 = ci * 16 + hg * HG
                for c in clist:
                    cs = slice(c * 64, (c + 1) * 64)
                    T = TS[n % 2]
                    n += 1
                    H = range(HG)
                    c64 = lambda hh: slice(hh * 64, (hh + 1) * 64)
                    rF, kkF, kdF, atF = [fmb[j][k][:, :, cs] for k in range(4)]
                    for hh in H:
                        lw = lwb[j][:, c, c64(hh)]
                        P.mm(B[0][0:64, hh * 64:(hh + 1) * 64], lw, Ud, start=True, stop=True)
                        P.mm(B[0][0:64, 256 + hh * 64:256 + (hh + 1) * 64], lw, Sd, start=True, stop=True)
                    P.copy("dve", f2(T.cls[:]), B[0][0:64, 0:256])
                    P.act(f2(T.ecl[:]), B[0][0:64, 0:256], AF.Exp)
                    P.act(f2(T.ece[:]), B[0][0:64, 256:512], AF.Exp)
                    P.act(f2(T.encl[:]), B[0][0:64, 0:256], AF.Exp, scale=-1.0)
                    lam = View(T.ecl.t[:, :, last:last + 1].to_broadcast([64, HG, 64]), T.ecl[:].key)
                    P.tt("dve", T.dec[:], T.encl[:], lam, ALU.mult)
                    P.tt("pool", T.br[:, :, 0, :], kkF, T.ece[:], ALU.mult)
                    P.tt("dve", T.br[:, :, 1, :], rF, T.ecl[:], ALU.mult)
                    P.tt("pool", T.sk[:], kdF, T.encl[:], ALU.mult)
                    P.tt("dve", T.sa[:], atF, T.encl[:], ALU.mult)
                    P.tt("pool", T.kdec[:], kdF, T.dec[:], ALU.mult)
                    P.tt("pool", T.adec[:], atF, T.dec[:], ALU.mult)
                    for hh in H:
                        P.transpose(B[7][0:64, hh * 64:(hh + 1) * 64], T.kdec[:, hh, :], C.ident[0:64, 0:64])
                        P.transpose(B[7][0:64, 256 + hh * 64:256 + (hh + 1) * 64], T.adec[:, hh, :], C.ident[0:64, 0:64])
                    P.copy("act", f2(T.kdT[:]), B[7][0:64, 0:256])
                    P.act(f2(T.nadT[:]), B[7][0:64, 256:512], AF.Identity, scale=-1.0)
                    for hh in H:
                        bt = T.br[:, hh, 0, :]
                        brh = View(T.br.t[:, hh].rearrange("p a b -> p (a b)"), T.br[:].key)
                        P.mm(B[1][0:64, hh * 64:(hh + 1) * 64], bt, T.sa[:, hh, :], start=True, stop=True)
                        P.mm(B[2][0:64, hh * 128:(hh + 1) * 128], T.sk[:, hh, :], brh, start=True, stop=True)
                        P.mm(B[3][0:64, hh * 128:(hh + 1) * 128], T.sa[:, hh, :], brh, start=True, stop=True)
                    b3v = B[3].t[0:64, :].rearrange("p (h t k) -> p h t k", h=HG, t=2)
                    P.stt("dve", f2(T.A[:]), B[1][0:64, 0:256], -1.0, ST4, ALU.mult, ALU.mult)
                    P.stt("dve", T.y[0][:], View(b3v[:, :, 0, :], B[3][:].key), -1.0, View(S4.ap.rearrange("p (h k) -> p h k", h=HG), S4.key), ALU.mult, ALU.mult)
                    P.tt("dve", View(T.g23.t[:].rearrange("p a b c -> p (a b c)"), T.g23[:].key), B[2][0:64, :], SU4, ALU.mult)
                    P.stt("dve", T.ng4t[:], View(b3v[:, :, 1, :], B[3][:].key), -1.0, View(U4.ap.rearrange("p (h k) -> p h k", h=HG), U4.key), ALU.mult, ALU.mult)
                    X = [T.A] + T.x
                    Y = T.y
                    Pm = T.p
                    P.tt("pool", f2(Pm[:]), f2(Y[0][:]), I4, ALU.add)
                    for k in range(1, 6):
                        cx = ((k - 1) % 2) * 256
                        for hh in H:
                            P.mm(B[4][0:64, cx + hh * 64:cx + (hh + 1) * 64], Y[k - 1][:, hh, :], X[k - 1][:, hh, :], start=True, stop=True)
                        if k < 5:
                            for hh in H:
                                P.mm(B[5][0:64, cx + hh * 64:cx + (hh + 1) * 64], X[k - 1][:, hh, :], Y[k - 1][:, hh, :], start=True, stop=True)
                        P.copy("act", f2(X[k][:]), B[4][0:64, cx:cx + 256])
                        if k < 5:
                            P.copy("dve", f2(Y[k][:]), B[5][0:64, cx:cx + 256])
                        for hh in H:
                            P.mm(B[6][0:64, cx + hh * 64:cx + (hh + 1) * 64], X[k][:, hh, :], Pm[:, hh, :], start=True, stop=True)
                        P.tt("dve", f2(Pm[:]), f2(Pm[:]), B[6][0:64, cx:cx + 256], ALU.add)
                    P.copy("act", T.ttb[:], Pm[:])
                    for hh in H:
                        P.mm(B[1][0:64, 256 + hh * 64:256 + (hh + 1) * 64], T.g23[:, hh, 0, :], vtb[j][:, c, c64(hh)], start=True, stop=True)
                    P.copy("act", f2(T.g2v[:]), B[1][0:64, 256:512])
                    for hh in H:
                        P.mm(B[0][0:64, hh * 64:(hh + 1) * 64], T.br[:, hh, 0, :], Sb[:, si0 + hh, :], start=True, stop=True)
                    P.tt("dve", f2(T.zz[:]), B[0][0:64, 0:256], f2(T.g2v[:]), ALU.add)
                    for hh in H:
                        P.mm(B[0][0:64, 256 + hh * 64:256 + (hh + 1) * 64], T.ttb[:, hh, :], T.zz[:, hh, :], start=True, stop=True)
                    P.copy("act", f2(T.ub[:]), B[0][0:64, 256:512])
                    for hh in H:
                        o_ = B[2][0:64, hh * 64:(hh + 1) * 64]
                        vh = vtb[j][:, c, c64(hh)]
                        P.mm(o_, T.br[:, hh, 1, :], Sb[:, si0 + hh, :], start=True, stop=False)
                        P.mm(o_, T.g23[:, hh, 1, :], vh, start=False, stop=False)
                        P.mm(o_, T.ng4t[:, hh, :], T.ub[:, hh, :], start=False, stop=True)
                    P.copy("act", obuf[:, c, :], B[2][0:64, 0:256])
                    for hh in H:
                        s_ = B[3][0:64, hh * 64:(hh + 1) * 64]
                        vh = vtb[j][:, c, c64(hh)]
                        P.mm(s_, T.kdT[:, hh, :], vh, start=True, stop=False)
                        P.mm(s_, T.nadT[:, hh, :], T.ub[:, hh, :], start=False, stop=True)
                    Sg = S[:, si0:si0 + HG, :]
                    P.tt("pool", T.stmp[:], Sg, lam, ALU.mult)
                    P.tt("dve", f2(Sg), f2(T.stmp[:]), B[3][0:64, 0:256], ALU.add)
                    P.copy("act", Sb[:, si0:si0 + HG, :], Sg)
                P.dma("sp", View(OD[d].t[t0:t0 + ntok, hg * HW4:(hg + 1) * HW4].rearrange("(c p) f -> p c f", p=64), ("rw_OD%d" % d, None)),
                      obuf[:, 0:nc_, :])
    P.pop()
    if RW_DEBUG in (2, 4):
        return
    P.push()
    pb = [P.ps("r3_ps%d" % j) for j in range(8)]
    L = Ctx()
    L.mean = P.sb("r3_mean", [128, TT], F32)
    L.rstd = P.sb("r3_rstd", [128, TT], F32)
    L.sq = P.sb("r3_sq", [128, 8, TT], F32)
    Wo = P.sb("r3_Wo", [128, 8, 1024], BF16)
    stg = [P.sb("r3_stg%d" % j, [128, 8, 512], F32) for j in range(2)]
    load_w_bf16(P, Wo, IN("rwkv_w_out", [D, D]).t, 1024, stg)
    lx = P.sb("r3_lx", [128, 16], F32)
    P.dma("sp", lx[:], IN("rw_lnx", [128, 16])[:, :])
    xt = P.sb("r3_x", [128, 8, TT], F32)
    yT = P.sb("r3_yT", [128, 8, TT], BF16)
    ggt = P.sb("r3_gg", [128, 8, TT], BF16)
    bnt = P.sb("r3_bn", [128, 8, TT], F32)
    oa = [P.sb("r3_oa%d" % j, [128, 1024], F32) for j in range(2)]
    obb = [P.sb("r3_ob%d" % j, [128, 1024], F32) for j in range(2)]
    st = [P.sb("r3_st%d" % j, [128, 32], F32) for j in range(2)]
    yf = [P.sb("r3_yf%d" % j, [128, 128], F32) for j in range(2)]
    n = 0
    for t in tiles:
        tok = slice(t * TT, (t + 1) * TT)
        P.dma("sp", xt[:], View(C.XS.t[:, :, tok].rearrange("c p n -> p c n"), ("XS", t)))
        P.dma("pool", ggt[:], View(GG.t[:, :, tok].rearrange("c p n -> p c n"), ("rw_GG", None)))
        P.dma("sp", bnt[:], View(BN.t[:, :, tok].rearrange("c p n -> p c n"), ("rw_BN", None)))
        for s in range(4):
            a = oa[s % 2]
            bb = obb[s % 2]
            sv = st[s % 2]
            r0 = t * TT + s * 128
            P.dma("sp", a[:], View(OD[0].t[r0:r0 + 128, :], ("rw_OD0", None)))
            P.dma("pool", bb[:], View(OD[1].t[r0:r0 + 128, :], ("rw_OD1", None)))
            P.tt("pool", a[:], a[:], bb[:], ALU.add)
            P.reduce("dve", sv[:, 0:16], View(a.t[:].rearrange("p (h k) -> p h k", k=64), a[:].key), ALU.add)
            P.ts("dve", sv[:, 0:16], sv[:, 0:16], -1.0 / 64, None, op0=ALU.mult)
            for h in range(RW_H):
                hs = slice(h * 64, (h + 1) * 64)
                P.ts("dve" if h % 2 else "pool", a[:, hs], a[:, hs], sv[:, h:h + 1], None, op0=ALU.add)
                P.act(bb[:, hs], a[:, hs], AF.Square, accum_out=sv[:, 16 + h:17 + h])
            P.ts("dve", sv[:, 16:32], sv[:, 16:32], 1.0 / 64, 64e-5, op0=ALU.mult, op1=ALU.add)
            P.act(sv[:, 16:32], sv[:, 16:32], AF.Sqrt)
            P.recip(sv[:, 16:32], sv[:, 16:32])
            for h in range(RW_H):
                hs = slice(h * 64, (h + 1) * 64)
                P.ts("dve" if h % 2 else "pool", a[:, hs], a[:, hs], sv[:, 16 + h:17 + h], None, op0=ALU.mult)
            for c in range(8):
                pt = pb[2 + n % 6]
                y = yf[n % 2]
                n += 1
                sl = slice(s * 128, (s + 1) * 128)
                P.transpose(pt[:, 0:128], a[:, c * 128:(c + 1) * 128], C.ident[:])
                P.act(y[:], pt[:, 0:128], AF.Identity, scale=lx[:, c:c + 1], bias=lx[:, 8 + c:9 + c])
                P.tt("pool", y[:], y[:], bnt[:, c, sl], ALU.add)
                P.tt("dve", yT[:, c, sl], y[:], ggt[:, c, sl], ALU.mult)
        out_proj_ln(P, C, L, i, t, xt, yT, Wo, pb)
    P.pop()


NA_H = 16


def na_geom(r):
    R0 = min(max(r - 4, 0), 23)
    ty = 0 if r == 0 else 1 if r == 2 else 3 if r == 28 else 4 if r == 30 else 2
    return ty, R0


def dma_heads_out_b(P, X, u0, n, src):
    for half in range(2):
        dv = X.t.rearrange("(c two) p n -> two c p n", two=2)[half][:, :, u0:u0 + n].rearrange("c p n -> p c n")
        sv = View(src.ap[half * 64:(half + 1) * 64], src.key)
        P.dma("sp" if half else "pool", View(dv, (X.name, None)), sv)


def phase_na(P, C, IN, i, tiles):
    QF = P.dram("na_QF", [16, 64, NT], BF16)
    KF = P.dram("na_KF", [16, 64, NT], BF16)
    VT = P.dram("na_VT", [NT, 1024], BF16)
    OT = P.dram("na_OT", [NT, 1024], F32)
    P.push()
    pb = [P.ps("n1_ps%d" % j) for j in range(6)]
    W = P.sb("n1_W", [128, 8, 3072], BF16)
    P.push()
    stg = [P.sb("n1_stg%d" % j, [128, 8, 512], F32) for j in range(2)]
    load_w_bf16(P, W, IN("na_w_in", [D, 3072]).t, 3072, stg)
    P.pop()
    xt = P.sb("n1_x", [128, 8, TT], F32)
    hb = P.sb("n1_hb", [128, 8, TT], BF16)
    qf = P.sb("n1_qf", [128, 8, TT], BF16)
    kfb = P.sb("n1_kf", [128, 8, TT], BF16)
    vt = P.sb("n1_vt", [128, 4, 1024], BF16)
    n = 0
    for t in tiles:
        tok = slice(t * TT, (t + 1) * TT)
        load_xh(P, C, i, t, xt, hb)
        for c in range(16):
            if c < 8 and t == 0:
                continue
            ps = pb[n % 6]
            n += 1
            for kc in range(8):
                P.mm(ps[:], W[:, kc, c * 128:(c + 1) * 128], hb[:, kc, :], start=(kc == 0), stop=(kc == 7))
            if c < 8:
                P.act(qf[:, c, :], ps[:], AF.Identity, scale=0.125)
            else:
                P.copy("dve", kfb[:, c - 8, :], ps[:])
        if t > 0:
            dma_heads_out_b(P, QF, t * TT, TT, qf[:])
        dma_heads_out_b(P, KF, t * TT, TT, kfb[:])
        for s in range(4):
            for blk in range(2):
                ps = pb[n % 6]
                n += 1
                for kc in range(8):
                    P.mm(ps[:], hb[:, kc, s * 128:(s + 1) * 128], W[:, kc, 2048 + blk * 512:2048 + (blk + 1) * 512], start=(kc == 0), stop=(kc == 7))
                P.copy("act" if blk else "dve", vt[:, s, blk * 512:(blk + 1) * 512], ps[:])
        P.dma("sp", View(VT.t[tok, :].rearrange("(s p) f -> p s f", p=128), ("na_VT", None)), vt[:])
    P.pop()
    P.push()
    ps1 = [P.ps("n2_s1%d" % j) for j in range(2)]
    ps2 = [P.ps("n2_s2%d" % j) for j in range(2)]
    pst = [P.ps("n2_pt%d" % j, [128, 1024], BF16) for j in range(2)]
    pso = [P.ps("n2_po%d" % j) for j in range(2)]
    bias = [P.sb("n2_bias%d" % j, [128, 5, 576], F32) for j in range(2)]
    qT = [P.sb("n2_q%d" % j, [64, 2048], BF16) for j in range(2)]
    kT = [P.sb("n2_k%d" % j, [64, 2304], BF16) for j in range(2)]
    va = [P.sb("n2_va%d" % j, [128, 16, 64], BF16) for j in range(2)]
    vb = [P.sb("n2_vb%d" % j, [128, 16, 64], BF16) for j in range(2)]
    vc = [P.sb("n2_vc%d" % j, [128, 2, 64], BF16) for j in range(2)]
    Sm = [P.sb("n2_S%d" % j, [128, 832], F32) for j in range(2)]
    Pm = [P.sb("n2_P%d" % j, [128, 896], BF16) for j in range(2)]
    PT = [P.sb("n2_PT%d" % j, [128, 7, 128], BF16) for j in range(2)]
    sm = [P.sb("n2_sm%d" % j, [128, 4], F32) for j in range(2)]
    oo = [P.sb("n2_o%d" % j, [128, 64], F32) for j in range(2)]
    for j in range(2):
        P.memset("pool", vb[j][:], 0.0)
    bsrc = IN("na_bias", [NA_H, 128, 5, 576])
    n = 0
    nhb = 0
    for h in range(NA_H):
        bj = bias[h % 2]
        P.dma("sp", bj[:], bsrc[h])
        for b in range(2):
            j = nhb % 2
            nhb += 1
            lat0 = 512 + 2048 * b
            ctx0 = 256 * b
            P.dma("sp", qT[j][:], View(QF.t[h, :, lat0:lat0 + 2048], ("na_QF", None)))
            P.dma("pool", kT[j][:, 0:2048], View(KF.t[h, :, lat0:lat0 + 2048], ("na_KF", None)))
            P.dma("pool", kT[j][:, 2048:2304], View(KF.t[h, :, ctx0:ctx0 + 256], ("na_KF", None)))
            P.dma("sp", va[j][:], View(VT.t[lat0:lat0 + 2048, h * 64:(h + 1) * 64].rearrange("(t p) f -> p t f", p=128), ("na_VT", None)))
            P.dma("pool", vb[j][:, 0:15, :], View(VT.t[lat0 + 64:lat0 + 64 + 1920, h * 64:(h + 1) * 64].rearrange("(t p) f -> p t f", p=128), ("na_VT", None)))
            P.dma("sp", vb[j][0:64, 15, :], View(VT.t[lat0 + 1984:lat0 + 2048, h * 64:(h + 1) * 64], ("na_VT", None)))
            P.dma("sp", vc[j][:], View(VT.t[ctx0:ctx0 + 256, h * 64:(h + 1) * 64].rearrange("(t p) f -> p t f", p=128), ("na_VT", None)))
            for r in range(0, 32, 2):
                ty, R0 = na_geom(r)
                u = n % 2
                n += 1
                q = qT[j][:, r * 64:r * 64 + 128]
                k0 = R0 * 64
                p1, p2 = ps1[u], ps2[u]
                P.mm(p1[:], q, kT[j][:, k0:k0 + 512], start=True, stop=True)
                P.mm(p2[:, 0:64], q, kT[j][:, k0 + 512:k0 + 576], start=True, stop=True)
                P.mm(p2[:, 64:320], q, kT[j][:, 2048:2304], start=True, stop=True)
                S = Sm[u]
                P.copy("act", S[:, 0:256], p2[:, 64:320])
                P.tt("dve", S[:, 256:768], p1[:], bj[:, ty, 0:512], ALU.add)
                P.tt("dve", S[:, 768:832], p2[:, 0:64], bj[:, ty, 512:576], ALU.add)
                st = sm[u]
                P.reduce("dve", st[:, 0:1], S[:], ALU.max)
                P.ts("dve", st[:, 0:1], st[:, 0:1], -1.0, None, op0=ALU.mult)
                Pb = Pm[u]
                P.act(Pb[:, 0:832], S[:], AF.Exp, bias=st[:, 0:1], accum_out=st[:, 1:2])
                P.recip(st[:, 2:3], st[:, 1:2])
                pt = pst[u]
                ptr = PT[u]
                for kt in range(7):
                    w = 128 if kt < 6 else 64
                    P.transpose(pt[0:w, kt * 128:(kt + 1) * 128], Pb[:, kt * 128:kt * 128 + w], C.identb[:])
                P.copy("act", ptr[:, 0:3, :], View(pt.t[:, 0:384].rearrange("p (a b) -> p a b", b=128), pt[:].key))
                P.copy("dve", ptr[:, 3:6, :], View(pt.t[:, 384:768].rearrange("p (a b) -> p a b", b=128), pt[:].key))
                P.copy("act", ptr[0:64, 6, :], pt[0:64, 768:896])
                po = pso[u]
                vx = va[j] if R0 % 2 == 0 else vb[j]
                t0 = R0 // 2
                P.mm(po[:, 0:64], ptr[:, 0, :], vc[j][:, 0, :], start=True, stop=False)
                P.mm(po[:, 0:64], ptr[:, 1, :], vc[j][:, 1, :], start=False, stop=False)
                for kt in range(4):
                    P.mm(po[:, 0:64], ptr[:, 2 + kt, :], vx[:, t0 + kt, :], start=False, stop=False)
                P.mm(po[:, 0:64], ptr[0:64, 6, :], vx[0:64, t0 + 4, :], start=False, stop=True)
                o = oo[u]
                P.act(o[:], po[:, 0:64], AF.Identity, scale=st[:, 2:3])
                q0 = lat0 + r * 64
                P.dma("sp" if n % 2 else "pool", View(OT.t[q0:q0 + 128, h * 64:(h + 1) * 64], ("na_OT", None)), o[:])
    P.pop()
    P.push()
    pb = [P.ps("n3_ps%d" % j) for j in range(8)]
    L = Ctx()
    L.mean = P.sb("n3_mean", [128, TT], F32)
    L.rstd = P.sb("n3_rstd", [128, TT], F32)
    L.sq = P.sb("n3_sq", [128, 8, TT], F32)
    Wo = P.sb("n3_Wo", [128, 8, 1024], BF16)
    stg = [P.sb("n3_stg%d" % j, [128, 8, 512], F32) for j in range(2)]
    load_w_bf16(P, Wo, IN("na_w_out", [D, D]).t, 1024, stg)
    xt = P.sb("n3_x", [128, 8, TT], F32)
    yT = P.sb("n3_yT", [128, 8, TT], BF16)
    oa = [P.sb("n3_oa%d" % j, [128, 1024], F32) for j in range(2)]
    n = 0
    for t in tiles:
        if t == 0:
            continue
        tok = slice(t * TT, (t + 1) * TT)
        P.dma("sp", xt[:], View(C.XS.t[:, :, tok].rearrange("c p n -> p c n"), ("XS", t)))
        for s in range(4):
            a = oa[s % 2]
            r0 = t * TT + s * 128
            P.dma("sp" if s % 2 else "pool", a[:], View(OT.t[r0:r0 + 128, :], ("na_OT", None)))
            for c in range(8):
                pt = pb[2 + n % 6]
                n += 1
                P.transpose(pt[:, 0:128], a[:, c * 128:(c + 1) * 128], C.ident[:])
                P.copy("act" if c % 2 else "dve", yT[:, c, s * 128:(s + 1) * 128], pt[:, 0:128])
        out_proj_ln(P, C, L, i, t, xt, yT, Wo, pb)
    P.pop()


class Inputs:
    def __init__(self, P):
        self.P = P
        self.d = {}

    def __call__(self, name, shape=None, dt=F32):
        if name not in self.d:
            self.d[name] = self.P.dram(name, SHAPES[name] if shape is None else shape, dt, kind="ExternalInput")
        return self.d[name]


SHAPES = {
    "xT": [8, 128, NT], "cT": [128, 8, 3], "adab": [128, DEPTH * 48 * 3], "lng": [128, DEPTH * 2 * 8], "lnb": [128, DEPTH * 2 * 8],
    "ident": [128, 128], "selE": [NE, NE * 128], "rb": [128, NE], "router_w": [D, NE],
}


def build(stages, tiles=None):
    nc = bass.Bass("TRN2", target_bir_lowering=False)
    P = Prog(nc)
    C = Ctx()
    IN = Inputs(P)
    OUT = P.dram("out", [8, 128, NT], F32, kind="ExternalOutput")
    C.XS = P.dram("XS", [8, 128, NT], F32)
    C.W1B = P.dram("W1B", [NE, 128, 8 * DE], BF16)
    C.W3B = P.dram("W3B", [NE, 128, 8 * DE], BF16)
    C.W2B = P.dram("W2B", [NE, 2, 128, 4 * 512], BF16)
    tiles = list(range(NTILE)) if tiles is None else tiles
    for c in range(8):
        P.dma("sp" if c % 2 else "pool", C.XS[c], IN("xT")[c])
    phase_consts(P, C, IN)
    mixer_consts(P, C, IN)
    layers = sorted(set(i for (_, i) in stages))
    phase_mod(P, C, IN, layers)
    for (kind, i) in stages:
        if kind == "moe":
            phase_wprep(P, C, IN, i)
            phase_moe(P, C, IN, i, tiles if i < DEPTH - 1 else [t for t in tiles if t > 0])
        elif kind == "mixer":
            if i % 4 == 0:
                phase_gdn(P, C, IN, i, tiles)
            elif i % 4 == 1:
                phase_mlstm(P, C, IN, i, tiles)
            elif i % 4 == 2:
                phase_rwkv(P, C, IN, i, tiles)
            else:
                phase_na(P, C, IN, i, tiles)
    P.pop()
    P.push()
    for c in range(8):
        P.dma("sp" if c % 2 else "pool", OUT[c], C.XS[c])
    P.emit()
    return nc, P, IN


def host_inputs(inputs, core):
    f = np.float32
    b0, b1 = 2 * core, 2 * core + 1
    x, ctx = inputs["x"], inputs["ctx"]
    tok = np.concatenate([ctx[b0], ctx[b1], x[b0], x[b1]], axis=0)
    m = {}
    m["xT"] = np.ascontiguousarray(tok.T.reshape(8, 128, NT)).astype(f)
    c3 = np.stack([inputs["c"][b0], inputs["c"][b1], inputs["c_ctx"]], axis=0)
    m["cT"] = np.ascontiguousarray(c3.reshape(3, 8, 128).transpose(2, 1, 0)).astype(f)
    return m


def rowrep(v, n=128):
    v = np.asarray(v, np.float32).reshape(1, -1)
    return np.ascontiguousarray(np.broadcast_to(v, (n, v.shape[1])))


def fm(v):
    v = np.asarray(v, np.float32)
    return np.ascontiguousarray(v.reshape(-1, 128).T)


def host_shared(inputs, names):
    f = np.float32
    m = {}
    ab = inputs["ada_b"].reshape(DEPTH, 48, 128).transpose(2, 0, 1)
    m["adab"] = np.ascontiguousarray(np.repeat(ab[..., None], 3, axis=-1).reshape(128, -1)).astype(f)
    m["lng"] = np.ascontiguousarray(inputs["ln_g"].reshape(DEPTH, 2, 8, 128).transpose(3, 0, 1, 2).reshape(128, -1)).astype(f)
    m["lnb"] = np.ascontiguousarray(inputs["ln_b"].reshape(DEPTH, 2, 8, 128).transpose(3, 0, 1, 2).reshape(128, -1)).astype(f)
    m["ident"] = np.eye(128, dtype=f)
    sel = np.zeros((NE, NE, 128), f)
    for e in range(NE):
        sel[e, e, :] = 1.0
    m["selE"] = sel.reshape(NE, NE * 128)
    m["rb"] = rowrep(inputs["router_b"])
    m["router_w"] = inputs["router_w"]
    for i in range(DEPTH):
        m["ada_w_%d" % i] = inputs["ada_w"][i]
        m["moe_w1_%d" % i] = inputs["moe_w1"][i]
        m["moe_w3_%d" % i] = inputs["moe_w3"][i]
        m["moe_w2_%d" % i] = inputs["moe_w2"][i]
    a = np.arange(64)
    Uf = (a[:, None] <= a[None, :]).astype(f)
    Sf = (a[:, None] < a[None, :]).astype(f)
    m["cmats"] = np.concatenate([Uf, Uf.T, Sf, Sf.T], axis=1)
    p = np.arange(128)
    inv = (10000.0 ** (-(p % 32).astype(np.float64) / 32.0))
    t = np.arange(2048)
    posv = np.where((p // 64)[:, None] == 0, (t // 64)[None, :], (t % 64)[None, :]).astype(np.float64)
    ang = posv * inv[:, None]
    m["ropecos"] = np.cos(ang).astype(f)
    m["ropesin"] = np.sin(ang).astype(f)
    R = np.zeros((128, 128), f)
    for mm_ in range(128):
        if (mm_ % 64) < 32:
            R[mm_, mm_ + 32] = -1.0
        else:
            R[mm_, mm_ - 32] = 1.0
    m["rotT"] = np.ascontiguousarray(R.T)
    m["mlstm_w_in"] = inputs["mlstm_w_in"]
    m["mlstm_w_out"] = inputs["mlstm_w_out"]
    m["ml_gateb"] = rowrep(inputs["mlstm_gate_b"].reshape(-1))
    m["ml_normg"] = fm(inputs["mlstm_norm_g"])
    m["gdn_w_in"] = inputs["gdn_w_in"]
    m["gdn_w_out"] = inputs["gdn_w_out"]
    dtb = np.zeros((2, 16), f)
    dtb[:, 0:8] = inputs["gdn_dt_bias"]
    m["gd_dtb"] = rowrep(dtb.reshape(-1))
    al = np.zeros((2, 16), f)
    al[:, 0:8] = inputs["gdn_a_log"]
    m["gd_alog"] = rowrep(al.reshape(-1))
    m["gd_conv"] = np.ascontiguousarray(inputs["gdn_conv"].T.reshape(24, 128, 3).transpose(1, 0, 2).reshape(128, 72))
    m["gd_normg"] = np.asarray(inputs["gdn_norm_g"], f).reshape(128, 1)
    for k in ("rwkv_w_rkv", "rwkv_g1", "rwkv_a1", "rwkv_w1", "rwkv_g2", "rwkv_a2", "rwkv_w2", "rwkv_w_out"):
        m[k] = inputs[k]
    m["rw_w0"] = np.ascontiguousarray(np.broadcast_to(np.asarray(inputs["rwkv_w0"], f)[None], (128, 2, 1024)))
    cst = [fm(inputs["rwkv_mu"][j]) for j in range(6)]
    cst += [fm(inputs["rwkv_k_k"]), fm(inputs["rwkv_k_a"]), np.zeros((128, 8), f), fm(inputs["rwkv_r_k"].reshape(-1))]
    cst += [fm(inputs["rwkv_a0"][0]), fm(inputs["rwkv_a0"][1]), np.zeros((128, 8), f)]
    m["rw_cst"] = np.concatenate(cst, axis=1)
    bo = np.zeros((128, 128), f)
    bo[0:64, 0:64] = 1.0
    bo[64:128, 64:128] = 1.0
    m["rw_bones"] = bo
    m["rw_lnx"] = np.concatenate([fm(inputs["rwkv_lnx_g"]), fm(inputs["rwkv_lnx_b"])], axis=1)
    cm4 = np.zeros((64, 2, 1536), f)
    for d_ in range(2):
        U_ = Uf if d_ == 0 else Uf.T
        S_ = Sf if d_ == 0 else Sf.T
        cm4[:, d_, 0:256] = np.tile(U_, (1, 4))
        cm4[:, d_, 256:512] = np.tile(S_, (1, 4))
        cm4[:, d_, 512:768] = np.tile(S_.T, (1, 4))
        cm4[:, d_, 768:1280] = np.tile(np.concatenate([S_, U_], axis=1), (1, 4))
        cm4[:, d_, 1280:1536] = np.tile(np.eye(64, dtype=f), (1, 4))
    m["rw_cm4"] = cm4
    m["na_w_in"] = inputs["na_w_in"]
    m["na_w_out"] = inputs["na_w_out"]
    if "na_bias" in names:
        rpb = np.asarray(inputs["na_rpb"], f)
        bt = np.full((NA_H, 128, 5, 576), -30000.0, f)
        w = np.arange(64)
        c0 = np.clip(w - 8, 0, 48)
        for ty, (r, R0, r00, r01) in enumerate([(0, 0, 0, 0), (2, 0, 0, 0), (6, 2, 2, 3), (28, 23, 24, 24), (30, 23, 24, 24)]):
            for dq, r0q in ((0, r00), (1, r01)):
                qrow = r + dq
                for kr in range(9):
                    krow = R0 + kr
                    if not (r0q <= krow < r0q + 8):
                        continue
                    dr = krow - qrow + 7
                    wp = np.arange(64)
                    valid = (wp[None, :] >= c0[:, None]) & (wp[None, :] < c0[:, None] + 16)
                    dc = np.clip(wp[None, :] - w[:, None] + 15, 0, 30)
                    vals = rpb[:, dr, :][:, dc]
                    blk = np.where(valid[None], vals, f(-30000.0))
                    bt[:, dq * 64:(dq + 1) * 64, ty, kr * 64:(kr + 1) * 64] = blk
        m["na_bias"] = bt
    return {k: np.ascontiguousarray(np.asarray(v, f)) for k, v in m.items() if k in names}


def unpack_out(res_core):
    o = res_core["out"].reshape(D, NT).T
    return o[512:512 + 2048], o[512 + 2048:], o[0:256], o[256:512]


ALL_STAGES = [(k, i) for i in range(DEPTH) for k in ("mixer", "moe")]


def kernel(**inputs):
    inputs = {k: np.asarray(v) for k, v in inputs.items()}
    nc, _, IN = build(ALL_STAGES)
    shared = host_shared(inputs, set(IN.d.keys()))
    in_maps = []
    for core in range(NCORES):
        m = dict(shared)
        m.update(host_inputs(inputs, core))
        in_maps.append(m)
    res = run_bass_kernel_spmd(nc, in_maps, core_ids=list(range(NCORES)))
    out = np.zeros((16, 2048, D), np.float32)
    for core in range(NCORES):
        l0, l1, _, _ = unpack_out(res.results[core])
        out[2 * core] = l0
        out[2 * core + 1] = l1
    return out
```

```python
import numpy as np
import concourse.bass as bass
import concourse.mybir as mybir
from concourse.bass_utils import run_bass_kernel_spmd

F32 = mybir.dt.float32
BF16 = mybir.dt.bfloat16
AF = mybir.ActivationFunctionType
ALU = mybir.AluOpType
AX = mybir.AxisListType

D = 1024
DEPTH = 4
NT = 4608
TT = 512
NTILE = NT // TT
NE = 16
DE = 512
ALPHA = float((2 * DEPTH) ** 0.25)
LN_EPS = 1e-5
NCORES = 8


class View:
    __slots__ = ("ap", "key")

    def __init__(self, ap, key):
        self.ap = ap
        self.key = key

    def bc(self, shape):
        return View(self.ap.to_broadcast(list(shape)), self.key)


class Buf:
    def __init__(self, name, t):
        self.name = name
        self.t = t

    def __getitem__(self, idx):
        return View(self.t[idx], (self.name, None))

    def k(self, sub):
        return _SubBuf(self, sub)


class _SubBuf:
    def __init__(self, buf, sub):
        self.buf = buf
        self.sub = sub

    def __getitem__(self, idx):
        return View(self.buf.t[idx], (self.buf.name, self.sub))


class Op:
    __slots__ = ("eng", "fn", "deps", "is_dma", "id", "inc", "ticket", "dsem", "dval", "is_mm")


ENGS = ("pe", "act", "dve", "pool", "sp")
NDSEM = 14


class Prog:
    def __init__(self, nc):
        self.nc = nc
        self.ops = []
        self.state = {}
        self.scopes = [[]]
        self.pending = {}
        self.last = {}
        self.open_dmas = set()

    def _enter(self, cm):
        t = cm.__enter__()
        self.scopes[-1].append(cm)
        return t

    def sb(self, name, shape, dt):
        self.uid = getattr(self, "uid", 0) + 1
        nm = "S%d_%s" % (self.uid, name)
        return Buf(nm, self._enter(self.nc.sbuf_tensor(nm, list(shape), dt)))

    def ps(self, name, shape=(128, 512), dt=F32):
        self.uid = getattr(self, "uid", 0) + 1
        nm = "P_%d_%s" % (self.uid, name)
        return Buf(nm, self._enter(self.nc.psum_tensor(nm, list(shape), dt)))

    def dram(self, name, shape, dt, kind="Internal"):
        return Buf(name, self.nc.dram_tensor(name, list(shape), dt, kind=kind).ap())

    def push(self):
        self.scopes.append([])

    def pop(self):
        for cm in reversed(self.scopes.pop()):
            cm.__exit__(None, None, None)
        bar = set(self.last.values()) | set(self.open_dmas)
        self.open_dmas = set()
        self.pending = {e: set(bar) | self.pending.get(e, set()) for e in ENGS}

    def _deps(self, key, is_write):
        name, sub = key
        st = self.state.get(name)
        if not st:
            return set()
        subs = list(st.keys()) if sub is None else [s for s in (sub, None) if s in st]
        deps = set()
        for s in subs:
            w, rs = st[s]
            if w is not None:
                deps.add(w)
            if is_write:
                deps.update(rs)
        return deps

    def _record(self, key, is_write, opid):
        name, sub = key
        st = self.state.setdefault(name, {})
        if is_write:
            if sub is None:
                st.clear()
            st[sub] = [opid, []]
        else:
            if sub not in st:
                st[sub] = [None, []]
            st[sub][1].append(opid)

    def add(self, eng, fn, writes, reads, is_dma=False, is_mm=False):
        op = Op()
        op.eng, op.fn, op.is_dma, op.is_mm = eng, fn, is_dma, is_mm
        op.inc, op.ticket, op.dsem, op.dval = False, 0, None, 0
        op.deps = self.pending.pop(eng, set())
        op.id = len(self.ops)
        reads = self._vs(*reads)
        writes = self._vs(*writes)
        for v in reads:
            dr = self._deps(v.key, False)
            op.deps |= dr
            if v.key[0].startswith("P_"):
                for x in self._deps(v.key, True) - dr:
                    if self.ops[x].eng != eng:
                        op.deps.add(x)
        for v in writes:
            op.deps |= self._deps(v.key, True)
        for v in reads:
            self._record(v.key, False, op.id)
        for v in writes:
            self._record(v.key, True, op.id)
        self.ops.append(op)
        if is_dma:
            self.open_dmas.add(op.id)
        else:
            self.last[eng] = op.id
        return op

    @staticmethod
    def _a(x):
        return x.ap if isinstance(x, View) else x

    @staticmethod
    def _vs(*xs):
        return [x for x in xs if isinstance(x, View)]

    def mm(self, out, lhsT, rhs, start=True, stop=True, **kw):
        a = self._a
        return self.add("pe", lambda e: e.matmul(a(out), a(lhsT), a(rhs), start=start, stop=stop, **kw),
                        [out], [lhsT, rhs], is_mm=True)

    def transpose(self, out, in_, ident):
        a = self._a
        return self.add("pe", lambda e: e.transpose(a(out), a(in_), a(ident)), [out], [in_, ident], is_mm=True)

    def act(self, out, in_, func, bias=0.0, scale=1.0, accum_out=None):
        a = self._a
        kw = {}
        if accum_out is not None:
            kw["accum_out"] = a(accum_out)
        return self.add("act", lambda e: e.activation(out=a(out), in_=a(in_), func=func, bias=a(bias), scale=a(scale), **kw),
                        self._vs(out, accum_out), self._vs(in_, bias, scale))

    def copy(self, eng, out, in_):
        a = self._a
        if eng == "act":
            return self.add(eng, lambda e: e.copy(out=a(out), in_=a(in_)), [out], [in_])
        return self.add(eng, lambda e: e.tensor_copy(out=a(out), in_=a(in_)), [out], [in_])

    def tt(self, eng, out, in0, in1, op):
        a = self._a
        return self.add(eng, lambda e: e.tensor_tensor(out=a(out), in0=a(in0), in1=a(in1), op=op), [out], [in0, in1])

    def ts(self, eng, out, in0, s1, s2=None, op0=ALU.mult, op1=None, accum_out=None):
        a = self._a
        kw = {}
        if op1 is not None:
            kw["op1"] = op1
        if accum_out is not None:
            kw["accum_out"] = a(accum_out)
        return self.add(eng, lambda e: e.tensor_scalar(out=a(out), in0=a(in0), scalar1=a(s1), scalar2=a(s2), op0=op0, **kw),
                        self._vs(out, accum_out), self._vs(in0, s1, s2))

    def stt(self, eng, out, in0, scalar, in1, op0, op1):
        a = self._a
        eng = "dve"
        return self.add(eng, lambda e: e.scalar_tensor_tensor(out=a(out), in0=a(in0), scalar=a(scalar), in1=a(in1), op0=op0, op1=op1),
                        [out], self._vs(in0, scalar, in1))

    def memset(self, eng, out, val):
        a = self._a
        return self.add(eng, lambda e: e.memset(a(out), val), [out], [])

    def reduce(self, eng, out, in_, op, axis=AX.X):
        a = self._a
        return self.add(eng, lambda e: e.tensor_reduce(out=a(out), in_=a(in_), axis=axis, op=op), [out], [in_])

    def recip(self, out, in_):
        a = self._a
        return self.add("dve", lambda e: e.reciprocal(out=a(out), in_=a(in_)), [out], [in_])

    def dma(self, q, out, in_, **kw):
        a = self._a
        q = "sp"
        return self.add(q, lambda e: e.dma_start(out=a(out), in_=a(in_), **kw), [out], [in_], is_dma=True)

    def emit(self):
        nc = self.nc
        ops = self.ops

        def pe_chain(op, dop):
            return dop.eng == "pe" and op.eng == "pe" and op.is_mm and dop.is_mm

        for op in ops:
            for d in op.deps:
                dop = ops[d]
                if dop.is_dma or pe_chain(op, dop):
                    continue
                dop.inc = True
        semctx = []

        def newsem(name):
            cm = nc.semaphore(name)
            s = cm.__enter__()
            semctx.append(cm)
            return s

        esem = {e: newsem("s_" + e) for e in ENGS}
        dq = ("sp", "pool", "act")
        dsems = {q: [newsem("d_%s_%d" % (q, i)) for i in range(NDSEM)] for q in dq}
        dcount = {q: [0] * NDSEM for q in dq}
        drr = {q: 0 for q in dq}
        tick = {e: 0 for e in ENGS}
        per_eng = {e: [] for e in ENGS}
        waits = {}
        seen = {e: {} for e in ENGS}
        for op in ops:
            w = []
            sn = seen[op.eng]

            def want(sem, val, key):
                if sn.get(key, 0) >= val:
                    return
                sn[key] = val
                w.append((sem, val))

            if op.is_dma:
                q = op.eng
                i = drr[q]
                drr[q] = (i + 1) % NDSEM
                if dcount[q][i] > 0:
                    want(dsems[q][i], dcount[q][i] * 16, ("d", q, i))
                dcount[q][i] += 1
                op.dsem = (q, i)
                op.dval = dcount[q][i] * 16
            for d in sorted(op.deps):
                dop = ops[d]
                if dop.is_dma:
                    q, i = dop.dsem
                    want(dsems[q][i], dop.dval, ("d", q, i))
                elif not pe_chain(op, dop):
                    want(esem[dop.eng], dop.ticket, ("e", dop.eng))
            if (not op.is_dma) and op.inc:
                tick[op.eng] += 1
                op.ticket = tick[op.eng]
            waits[op.id] = w
            per_eng[op.eng].append(op)
        self.n_waits = sum(len(w) for w in waits.values())
        final = []
        for q in dq:
            for i in range(NDSEM):
                if dcount[q][i] > 0:
                    final.append((dsems[q][i], dcount[q][i] * 16))

        def run_engine(e, name):
            for op in per_eng[name]:
                for (sem, val) in waits[op.id]:
                    e.wait_ge(sem, val)
                ins = op.fn(e)
                if op.is_dma:
                    q, i = op.dsem
                    ins.then_inc(dsems[q][i], 16)
                elif op.inc:
                    ins.then_inc(esem[name], 1)
            if name == "sp":
                for (sem, val) in final:
                    e.wait_ge(sem, val)

        with nc.Block() as block:
            @block.sync
            def _(e):
                run_engine(e, "sp")

            @block.tensor
            def _(e):
                run_engine(e, "pe")

            @block.scalar
            def _(e):
                run_engine(e, "act")

            @block.vector
            def _(e):
                run_engine(e, "dve")

            @block.gpsimd
            def _(e):
                run_engine(e, "pool")
        for cm in reversed(semctx):
            cm.__exit__(None, None, None)
        while self.scopes:
            for cm in reversed(self.scopes.pop()):
                cm.__exit__(None, None, None)


def seg_of_tile(t):
    return 2 if t == 0 else (0 if t <= 4 else 1)


def modcol(i, grp, kc, seg):
    return (i * 48 + grp * 8 + kc) * 3 + seg


class Ctx:
    pass


def phase_consts(P, C, IN):
    C.ones = P.sb("ones", [128, 128], F32)
    C.ident = P.sb("ident", [128, 128], F32)
    C.identb = P.sb("identb", [128, 128], BF16)
    C.mod = P.sb("mod", [128, DEPTH * 48 * 3], F32)
    C.lng = P.sb("lng", [128, DEPTH * 2 * 8], F32)
    C.lnb = P.sb("lnb", [128, DEPTH * 2 * 8], F32)
    C.rw = P.sb("rw", [128, 8, NE], F32)
    C.rb = P.sb("rb", [128, NE], F32)
    C.selE = P.sb("selE", [NE, NE * 128], F32)
    C.eps = P.sb("epsc", [128, 1], F32)
    P.memset("dve", C.ones[:], 1.0)
    P.memset("dve", C.eps[:], LN_EPS)
    P.dma("sp", C.ident[:], IN("ident")[:, :])
    P.copy("dve", C.identb[:], C.ident[:])
    P.dma("sp", C.lng[:], IN("lng")[:, :])
    P.dma("sp", C.lnb[:], IN("lnb")[:, :])
    P.dma("sp", C.rw[:], IN("router_w").t.rearrange("(kc p) e -> p kc e", p=128))
    P.dma("sp", C.rb[:], IN("rb")[:, :])
    P.dma("sp", C.selE[:], IN("selE")[:, :])


def phase_mod(P, C, IN, layers):
    P.push()
    cT = P.sb("cT", [128, 8, 3], F32)
    cv = P.sb("cv", [128, 8, 3], F32)
    adab = P.sb("adab", [128, DEPTH * 48 * 3], F32)
    aw = [P.sb("aw%d" % j, [128, 8, 512], F32) for j in range(2)]
    pm = [P.ps("pm%d" % j) for j in range(2)]
    P.dma("sp", cT[:], IN("cT")[:, :, :])
    P.dma("sp", adab[:], IN("adab")[:, :])
    P.act(cv[:], cT[:], AF.Silu)
    n = 0
    for i in layers:
        awv = IN("ada_w_%d" % i, [D, 6 * D]).t.rearrange("(kc p) n -> p kc n", p=128)
        for p in range(12):
            a = aw[n % 2]
            ps = pm[n % 2]
            n += 1
            P.dma("sp" if n % 2 else "pool", a[:], awv[:, :, p * 512:(p + 1) * 512])
            for f in range(4):
                for kc in range(8):
                    P.mm(ps[:, f * 3:(f + 1) * 3], a[:, kc, f * 128:(f + 1) * 128], cv[:, kc, :], start=(kc == 0), stop=(kc == 7))
            c0 = (i * 48 + p * 4) * 3
            P.tt("dve", C.mod[:, c0:c0 + 12], ps[:, 0:12], adab[:, c0:c0 + 12], ALU.add)
        for grp in (1, 4):
            c0 = (i * 48 + grp * 8) * 3
            P.ts("dve", C.mod[:, c0:c0 + 24], C.mod[:, c0:c0 + 24], 1.0, None, op0=ALU.add)
    P.pop()


def ln_tile(P, C, L, y, out, i, which, ps_a, ps_b):
    mean, rstd, sq = L.mean, L.rstd, L.sq
    for kc in range(8):
        P.mm(ps_a[:], C.ones[:], y[:, kc, :], start=(kc == 0), stop=(kc == 7))
    P.act(mean[:], ps_a[:], AF.Identity, scale=1.0 / D)
    for kc in range(8):
        P.tt("pool" if kc % 2 else "dve", y[:, kc, :], y[:, kc, :], mean[:], ALU.subtract)
        P.act(sq[:, kc, :], y[:, kc, :], AF.Square)
    for kc in range(8):
        P.mm(ps_b[:], C.ones[:], sq[:, kc, :], start=(kc == 0), stop=(kc == 7))
    P.act(rstd[:], ps_b[:], AF.Sqrt, bias=C.eps[:], scale=1.0 / D)
    P.recip(rstd[:], rstd[:])
    for kc in range(8):
        col = (i * 2 + which) * 8 + kc
        P.stt("dve", sq[:, kc, :], y[:, kc, :], C.lng[:, col:col + 1], rstd[:], ALU.mult, ALU.mult)
        P.act(out[:, kc, :], sq[:, kc, :], AF.Identity, bias=C.lnb[:, col:col + 1])


def phase_wprep(P, C, IN, i):
    P.push()
    st = [P.sb("wp_f%d" % j, [128, 8, 512], F32) for j in range(3)]
    sb = [P.sb("wp_b%d" % j, [128, 8, 512], BF16) for j in range(3)]
    n = 0
    engs = ("pool", "act", "dve")
    for e in range(NE):
        for (src, dst, pat, isw2) in ((IN("moe_w1_%d" % i, [NE, D, DE]), C.W1B, "(kc p) n -> p kc n", False),
                                      (IN("moe_w3_%d" % i, [NE, D, DE]), C.W3B, "(kc p) n -> p kc n", False),
                                      (IN("moe_w2_%d" % i, [NE, DE, D]), C.W2B, "", True)):
            j = n % 3
            n += 1
            if isw2:
                sv = src.t[e].rearrange("(kc p) (h n) -> p kc h n", p=128, h=2)
                stv = View(st[j].t[:].rearrange("p (kc h) n -> p kc h n", h=2), st[j][:].key)
                P.dma("sp", stv, sv)
                P.copy(engs[j], sb[j][:], st[j][:])
                for half in range(2):
                    dv = dst.t[e, half].rearrange("p (kc n) -> p kc n", kc=4)
                    sbv = View(sb[j].t[:].rearrange("p (kc h) n -> p kc h n", h=2)[:, :, half, :], sb[j][:].key)
                    P.dma("pool", View(dv, (dst.name, e)), sbv)
            else:
                sv = src.t[e].rearrange(pat, p=128)
                dv = dst.t[e].rearrange("p (kc n) -> p kc n", kc=8)
                P.dma("sp", st[j][:], sv)
                P.copy(engs[j], sb[j][:], st[j][:])
                P.dma("pool", View(dv, (dst.name, e)), sb[j][:])
    P.pop()


def phase_moe(P, C, IN, i, tiles):
    P.push()
    xts = [P.sb("m_x%d" % j, [128, 8, TT], F32) for j in range(2)]
    h2fs = [P.sb("m_h2f%d" % j, [128, 8, TT], F32) for j in range(2)]
    h2bs = [P.sb("m_h2b%d" % j, [128, 8, TT], BF16) for j in range(2)]
    combTs = [P.sb("m_combT%d" % j, [NE, TT], F32) for j in range(2)]
    hid = P.sb("m_hid", [128, NE * 4, TT], BF16)
    wsl = [P.sb("m_w%d" % j, [128, 8, 512], BF16) for j in range(3)]
    w2st = [P.sb("m_w2%d" % j, [128, 4, 512], BF16) for j in range(2)]
    Ls = []
    mean_ = P.sb("m_mean", [128, TT], F32)
    rstd_ = P.sb("m_rstd", [128, TT], F32)
    for j in range(2):
        L = Ctx()
        L.mean = mean_
        L.rstd = rstd_
        L.sq = h2fs[j]
        Ls.append(L)
    cbs = [P.sb("m_cb%d" % j, [128, TT], F32) for j in range(1)]
    gS = [P.sb("m_g%d" % j, [128, TT], F32) for j in range(2)]
    nws = [0]
    R = [dict((nm, P.sb("r_%s%d" % (nm, j), [128, w], F32)) for nm, w in
              (("lg", 16), ("e", 16), ("pr", 16), ("sel", 16), ("eq", 16), ("s2", 16), ("msk", 16), ("pw", 16), ("cmb", 16),
               ("mx", 1), ("se", 1), ("m1", 4), ("m2", 4), ("gs", 4), ("gm", 1), ("ing", 4), ("sw", 1))) for j in range(2)]
    pb = [P.ps("m_ps%d" % j) for j in range(8)]
    rr = [0]

    def prologue(t, pj):
        ops = []
        xt, h2f, h2b, combT = xts[pj], h2fs[pj], h2bs[pj], combTs[pj]
        seg = seg_of_tile(t)
        tok = slice(t * TT, (t + 1) * TT)
        ops.append(lambda: P.dma("sp", xt[:], View(C.XS.t[:, :, tok].rearrange("c p n -> p c n"), ("XS", t))))
        for kc in range(8):
            def f(kc=kc):
                c_sc = modcol(i, 4, kc, seg)
                c_sh = modcol(i, 3, kc, seg)
                P.ts("dve" if kc % 2 else "pool", h2f[:, kc, :], xt[:, kc, :], C.mod[:, c_sc:c_sc + 1], C.mod[:, c_sh:c_sh + 1], op0=ALU.mult, op1=ALU.add)
                P.copy("act", h2b[:, kc, :], h2f[:, kc, :])
            ops.append(f)
        for s in range(4):
            r = R[0]
            rr[0] += 1
            pl = pb[6 + (s % 2)]

            def f1(s=s, r=r, pl=pl):
                for kc in range(8):
                    P.mm(pl[:, 0:16], h2f[:, kc, s * 128:(s + 1) * 128], C.rw[:, kc, :], start=(kc == 0), stop=(kc == 7))
                P.copy("act", r["lg"][:], pl[:, 0:16])
            ops.append(f1)
            seq = [
                lambda r=r: P.reduce("dve", r["mx"][:], r["lg"][:], ALU.max),
                lambda r=r: P.ts("dve", r["mx"][:], r["mx"][:], -1.0, None, op0=ALU.mult),
                lambda r=r: P.act(r["e"][:], r["lg"][:], AF.Exp, bias=r["mx"][:], accum_out=r["se"][:]),
                lambda r=r: P.recip(r["se"][:], r["se"][:]),
                lambda r=r: P.ts("dve", r["pr"][:], r["e"][:], r["se"][:], None, op0=ALU.mult),
                lambda r=r: P.tt("dve", r["sel"][:], r["pr"][:], C.rb[:], ALU.add),
                lambda r=r: P.reduce("dve", r["m1"][:], View(r["sel"].t[:].rearrange("p (g k) -> p g k", k=4), r["sel"][:].key), ALU.max),
            ]
            for g in range(4):
                seq.append(lambda r=r, g=g: P.ts("dve", r["eq"][:, g * 4:(g + 1) * 4], r["sel"][:, g * 4:(g + 1) * 4], r["m1"][:, g:g + 1], None, op0=ALU.is_equal))
            seq += [
                lambda r=r: P.stt("dve", r["s2"][:], r["eq"][:], -1e9, r["sel"][:], ALU.mult, ALU.add),
                lambda r=r: P.reduce("dve", r["m2"][:], View(r["s2"].t[:].rearrange("p (g k) -> p g k", k=4), r["s2"][:].key), ALU.max),
                lambda r=r: P.tt("dve", r["gs"][:], r["m1"][:], r["m2"][:], ALU.add),
                lambda r=r: P.reduce("dve", r["gm"][:], r["gs"][:], ALU.max),
                lambda r=r: P.ts("dve", r["ing"][:], r["gs"][:], r["gm"][:], None, op0=ALU.is_equal),
            ]
            for g in range(4):
                seq.append(lambda r=r, g=g: P.ts("dve", r["msk"][:, g * 4:(g + 1) * 4], r["sel"][:, g * 4:(g + 1) * 4], r["m2"][:, g:g + 1], r["ing"][:, g:g + 1],
                                                 op0=ALU.is_ge, op1=ALU.mult))
            seq += [
                lambda r=r: P.tt("dve", r["pw"][:], r["pr"][:], r["msk"][:], ALU.mult),
                lambda r=r: P.reduce("dve", r["sw"][:], r["pw"][:], ALU.add),
                lambda r=r: P.recip(r["sw"][:], r["sw"][:]),
                lambda r=r: P.ts("dve", r["cmb"][:], r["pw"][:], r["sw"][:], None, op0=ALU.mult),
            ]
            ops += seq

            def f2(s=s, r=r, pl=pl):
                P.transpose(pl[0:16, 128:256], r["cmb"][:], C.ident[:])
                P.copy("act", combT[:, s * 128:(s + 1) * 128], pl[0:16, 128:256])
            ops += [(lambda: None)] * 10
            ops.append(f2)
        return ops

    def epilogue(t, pj):
        ops = []
        xt, L = xts[pj], Ls[pj]
        tok = slice(t * TT, (t + 1) * TT)
        y = xt
        mean, rstd, sq = L.mean, L.rstd, L.sq
        ps_a, ps_b = pb[6], pb[7]

        def s1():
            for kc in range(8):
                P.mm(ps_a[:], C.ones[:], y[:, kc, :], start=(kc == 0), stop=(kc == 7))
            P.act(mean[:], ps_a[:], AF.Identity, scale=1.0 / D)
        ops.append(s1)
        for kc in range(8):
            def f(kc=kc):
                P.tt("pool" if kc % 2 else "dve", y[:, kc, :], y[:, kc, :], mean[:], ALU.subtract)
                P.act(sq[:, kc, :], y[:, kc, :], AF.Square)
            ops.append(f)

        def s2():
            for kc in range(8):
                P.mm(ps_b[:], C.ones[:], sq[:, kc, :], start=(kc == 0), stop=(kc == 7))
            P.act(rstd[:], ps_b[:], AF.Sqrt, bias=C.eps[:], scale=1.0 / D)
            P.recip(rstd[:], rstd[:])
        ops += [(lambda: None)] * 6
        ops.append(s2)
        for kc in range(8):
            def f(kc=kc):
                col = (i * 2 + 1) * 8 + kc
                P.stt("dve", sq[:, kc, :], y[:, kc, :], C.lng[:, col:col + 1], rstd[:], ALU.mult, ALU.mult)
                P.act(xt[:, kc, :], sq[:, kc, :], AF.Identity, bias=C.lnb[:, col:col + 1])
            ops.append(f)
        ops.append(lambda: P.dma("sp", View(C.XS.t[:, :, tok].rearrange("c p n -> p c n"), ("XS", t)), xt[:]))
        return ops

    def drain(q, k):
        for _ in range(k):
            if q:
                q.pop(0)()

    for f in prologue(tiles[0], 0):
        f()
    pend_epi = []
    for ti, t in enumerate(tiles):
        pj = ti % 2
        xt, h2f, h2b, combT = xts[pj], h2fs[pj], h2bs[pj], combTs[pj]
        seg = seg_of_tile(t)
        pend_pro = prologue(tiles[ti + 1], 1 - pj) if ti + 1 < len(tiles) else []
        k_epi = (len(pend_epi) + 5) // 6
        k_pro = (len(pend_pro) + 13) // 14
        n = 0
        for e in range(NE):
            wa = wsl[nws[0] % 3]
            wb_ = wsl[(nws[0] + 1) % 3]
            nws[0] += 2
            P.dma("sp", wa[:], View(C.W1B.t[e].rearrange("p (kc n) -> p kc n", kc=8), ("W1B", e)))
            P.dma("sp", wb_[:], View(C.W3B.t[e].rearrange("p (kc n) -> p kc n", kc=8), ("W3B", e)))
            cb = cbs[0]
            pc = pb[6 + (e % 2)]
            P.mm(pc[:], C.selE[:, e * 128:(e + 1) * 128], combT[:], start=True, stop=True)
            P.copy("act", cb[:], pc[:])
            for fc in range(4):
                p1 = pb[(n % 3) * 2]
                p3 = pb[(n % 3) * 2 + 1]
                g = gS[n % 2]
                u = g
                n += 1
                for kc in range(8):
                    P.mm(p1[:], wa[:, kc, fc * 128:(fc + 1) * 128], h2b[:, kc, :], start=(kc == 0), stop=(kc == 7))
                for kc in range(8):
                    P.mm(p3[:], wb_[:, kc, fc * 128:(fc + 1) * 128], h2b[:, kc, :], start=(kc == 0), stop=(kc == 7))
                P.act(g[:], p1[:], AF.Silu)
                P.tt("dve", u[:], g[:], p3[:], ALU.mult)
                P.tt("pool", hid[:, e * 4 + fc, :], u[:], cb[:], ALU.mult)
            if pend_epi:
                drain(pend_epi, k_epi)
            else:
                drain(pend_pro, k_pro)
        drain(pend_epi, len(pend_epi))
        n = 0
        for half in range(2):
            for e in range(NE):
                w2 = w2st[n % 2]
                n += 1
                P.dma("sp", w2[:], View(C.W2B.t[e, half].rearrange("p (fc n) -> p fc n", fc=4), ("W2B", e)))
                for fc in range(4):
                    for o in range(4):
                        P.mm(pb[half * 4 + o][:], w2[:, fc, o * 128:(o + 1) * 128], hid[:, e * 4 + fc, :],
                             start=(e == 0 and fc == 0), stop=(e == NE - 1 and fc == 3))
                if half == 0:
                    drain(pend_pro, k_pro)
            for o in range(4):
                oc = half * 4 + o
                cg = modcol(i, 5, oc, seg)
                P.act(h2f[:, oc, :], pb[half * 4 + o][:], AF.Identity, scale=C.mod[:, cg:cg + 1])
                P.stt("dve", xt[:, oc, :], xt[:, oc, :], ALPHA, h2f[:, oc, :], ALU.mult, ALU.add)
        drain(pend_pro, len(pend_pro))
        pend_epi = epilogue(t, pj)
    drain(pend_epi, len(pend_epi))
    P.pop()


CH = 64


def chunk_plan(b, d):
    ctx0 = 256 * b
    lat0 = 512 + 2048 * b
    blocks = [(ctx0, 4)] + [(lat0 + 512 * k, 8) for k in range(4)]
    if d == 0:
        return [(t0, list(range(n))) for (t0, n) in blocks]
    return [(blocks[0][0], [3, 2, 1, 0])] + [(t0, list(range(n - 1, -1, -1))) for (t0, n) in reversed(blocks[1:])]


def load_w_bf16(P, dst, src_ap, ncols, stg, engs=("pool", "act", "dve"), ctr=[0]):
    sv = src_ap.rearrange("(kc p) n -> p kc n", p=128)
    c0 = 0
    while c0 < ncols:
        w = min(512, ncols - c0)
        j = ctr[0] % len(stg)
        ctr[0] += 1
        P.dma("sp" if j % 2 else "pool", stg[j][:, :, 0:w], sv[:, :, c0:c0 + w])
        P.copy(engs[j % 3], dst[:, :, c0:c0 + w], stg[j][:, :, 0:w])
        c0 += w


def mixer_consts(P, C, IN):
    C.cm = P.sb("cm", [64, 4 * 64], F32)
    P.dma("sp", C.cm[:], IN("cmats", [64, 256])[:, :])
    C.ones64 = P.sb("ones64", [64, 128], F32)
    P.memset("dve", C.ones64[:], 1.0)


def cmat(C, name, d):
    idx = {"U": 0, "UT": 1, "S": 2, "ST": 3}[name]
    if d == 1:
        idx = idx ^ 1
    return C.cm[:, idx * 64:(idx + 1) * 64]


def load_xh(P, C, i, t, xt, hb, halo=False):
    seg = seg_of_tile(t)
    tok = slice(t * TT, (t + 1) * TT)
    P.dma("sp", xt[:], View(C.XS.t[:, :, tok].rearrange("c p n -> p c n"), ("XS", t)))
    for kc in range(8):
        c_sc = modcol(i, 1, kc, seg)
        c_sh = modcol(i, 0, kc, seg)
        P.ts("dve" if kc % 2 else "pool", hb[:, kc, :], xt[:, kc, :], C.mod[:, c_sc:c_sc + 1], C.mod[:, c_sh:c_sh + 1], op0=ALU.mult, op1=ALU.add)


def out_proj_ln(P, C, L, i, t, xt, yT, Wo, pb):
    seg = seg_of_tile(t)
    tok = slice(t * TT, (t + 1) * TT)
    for oc in range(8):
        ps = pb[2 + oc % 4]
        for kc in range(8):
            P.mm(ps[:], Wo[:, kc, oc * 128:(oc + 1) * 128], yT[:, kc, :], start=(kc == 0), stop=(kc == 7))
        cg = modcol(i, 2, oc, seg)
        P.act(L.sq[:, oc, :], ps[:], AF.Identity, scale=C.mod[:, cg:cg + 1])
        P.stt("dve", xt[:, oc, :], xt[:, oc, :], ALPHA, L.sq[:, oc, :], ALU.mult, ALU.add)
    ln_tile(P, C, L, xt, xt, i, 0, pb[0], pb[1])
    P.dma("sp", View(C.XS.t[:, :, tok].rearrange("c p n -> p c n"), ("XS", t)), xt[:])


def rope_evac(P, C, R, ps, dst, t, scale, n):
    y = R.y[n % 2]
    if t == 0:
        P.act(dst, ps[:], AF.Identity, scale=scale)
        return
    pos = ((t - 1) % 4) * TT
    P.act(y[:], ps[:], AF.Identity, scale=scale)
    pr = R.pr[n % 2]
    P.mm(pr[:], R.rotT[:], y[:], start=True, stop=True)
    y1 = R.y1[n % 2]
    P.tt("pool", y1[:], y[:], R.cos[:, pos:pos + TT], ALU.mult)
    y2 = R.y2[n % 2]
    P.tt("dve", y2[:], pr[:], R.sin[:, pos:pos + TT], ALU.mult)
    P.tt("pool", dst, y1[:], y2[:], ALU.add)


def rope_setup(P, C, IN, pbanks):
    R = Ctx()
    R.cos = P.sb("ropecos", [128, 2048], F32)
    R.sin = P.sb("ropesin", [128, 2048], F32)
    R.rotT = P.sb("rotT", [128, 128], F32)
    P.dma("sp", R.cos[:], IN("ropecos", [128, 2048])[:, :])
    P.dma("pool", R.sin[:], IN("ropesin", [128, 2048])[:, :])
    P.dma("sp", R.rotT[:], IN("rotT", [128, 128])[:, :])
    R.y = [P.sb("rp_y%d" % j, [128, TT], F32) for j in range(2)]
    R.y1 = [P.sb("rp_y1%d" % j, [128, TT], F32) for j in range(2)]
    R.y2 = [P.sb("rp_y2%d" % j, [128, TT], F32) for j in range(2)]
    R.pr = pbanks
    return R


ML_H = 4
ML_DV = 256
ML_VW = ML_DV + 1


def phase_mlstm(P, C, IN, i, tiles):
    QK = P.dram("ml_QK", [8, 128, NT], BF16)
    OG = P.dram("ml_OG", [8, 128, NT], BF16)
    KT = P.dram("ml_KT", [NT, 512], BF16)
    VT = P.dram("ml_VT", [NT, ML_H * ML_VW], BF16)
    GT = P.dram("ml_GT", [NT, 16], F32)
    OD = [P.dram("ml_OD%d" % d, [NT, 1024], F32) for d in range(2)]
    w_in = IN("mlstm_w_in", [D, 3088])
    P.push()
    pb = [P.ps("ml_ps%d" % j) for j in range(6)]
    pbt = [P.ps("ml_pt%d" % j, [128, 1024], BF16) for j in range(2)]
    W = P.sb("ml_W", [128, 8, 3088], BF16)
    stg = [P.sb("ml_stg%d" % j, [128, 8, 512], F32) for j in range(2)]
    load_w_bf16(P, W, w_in.t, 3088, stg)
    R = rope_setup(P, C, IN, pb[4:6])
    gb = P.sb("ml_gb", [128, 16], F32)
    P.dma("sp", gb[:], IN("ml_gateb", [128, 16])[:, :])
    xt = P.sb("ml_x", [128, 8, TT], F32)
    hb = P.sb("ml_hb", [128, 8, TT], BF16)
    qk = P.sb("ml_qk", [128, 8, TT], BF16)
    og = P.sb("ml_og", [128, 8, TT], BF16)
    kt = P.sb("ml_kt", [128, 4, 512], BF16)
    vt = P.sb("ml_vt", [128, 4, ML_H * ML_VW], BF16)
    gt = P.sb("ml_gt", [128, 4, 16], F32)
    ge = P.sb("ml_ge", [128, 4, 16], F32)
    P.memset("dve", vt[:], 1.0)
    n = 0
    for t in tiles:
        tok = slice(t * TT, (t + 1) * TT)
        load_xh(P, C, i, t, xt, hb)
        for oc in range(8):
            ps = pb[n % 4]
            for kc in range(8):
                P.mm(ps[:], W[:, kc, oc * 128:(oc + 1) * 128], hb[:, kc, :], start=(kc == 0), stop=(kc == 7))
            rope_evac(P, C, R, ps, qk[:, oc, :], t, (128.0 ** -0.5) if oc < 4 else 1.0, n)
            n += 1
            if oc >= 4:
                for s in range(4):
                    pt = pbt[s % 2]
                    P.transpose(pt[:, 0:128], qk[:, oc, s * 128:(s + 1) * 128], C.identb[:])
                    P.copy("act" if s % 2 else "dve", kt[:, s, (oc - 4) * 128:(oc - 3) * 128], pt[:, 0:128])
        for c in range(8):
            ps = pb[n % 4]
            n += 1
            for kc in range(8):
                P.mm(ps[:], W[:, kc, 2048 + c * 128:2048 + (c + 1) * 128], hb[:, kc, :], start=(kc == 0), stop=(kc == 7))
            P.act(og[:, c, :], ps[:], AF.Sigmoid)
        for s in range(4):
            for blk in range(2):
                ps = pb[n % 4]
                n += 1
                for kc in range(8):
                    P.mm(ps[:], hb[:, kc, s * 128:(s + 1) * 128], W[:, kc, 1024 + blk * 512:1024 + (blk + 1) * 512], start=(kc == 0), stop=(kc == 7))
                for hh in range(2):
                    h = blk * 2 + hh
                    P.copy("act" if hh else "dve", vt[:, s, h * ML_VW:h * ML_VW + ML_DV], ps[:, hh * 256:(hh + 1) * 256])
            ps = pb[n % 4]
            n += 1
            for kc in range(8):
                P.mm(ps[:, 0:16], hb[:, kc, s * 128:(s + 1) * 128], W[:, kc, 3072:3088], start=(kc == 0), stop=(kc == 7))
            P.tt("dve", gt[:, s, :], ps[:, 0:16], gb[:], ALU.add)
        P.act(ge[:], gt[:], AF.Exp, scale=-1.0)
        P.act(ge[:], ge[:], AF.Ln, bias=1.0)
        for d in range(2):
            P.ts("dve", gt[:, :, d * 8 + 4:d * 8 + 8], ge[:, :, d * 8 + 4:d * 8 + 8], -1.0, None, op0=ALU.mult)
        P.dma("sp", View(QK.t[:, :, tok].rearrange("c p n -> p c n"), ("ml_QK", t)), qk[:])
        P.dma("pool", View(OG.t[:, :, tok].rearrange("c p n -> p c n"), ("ml_OG", t)), og[:])
        P.dma("sp", View(KT.t[tok, :].rearrange("(s p) f -> p s f", p=128), ("ml_KT", t)), kt[:])
        P.dma("pool", View(VT.t[tok, :].rearrange("(s p) f -> p s f", p=128), ("ml_VT", t)), vt[:])
        P.dma("sp", View(GT.t[tok, :].rearrange("(s p) f -> p s f", p=128), ("ml_GT", t)), gt[:])
    P.pop()
    P.push()
    pb = [P.ps("m2_ps%d" % j) for j in range(8)]
    qkb = [P.sb("m2_qk%d" % j, [128, 8, TT], BF16) for j in range(2)]
    ktb = [P.sb("m2_kt%d" % j, [64, 8, 512], BF16) for j in range(2)]
    vtb = [P.sb("m2_vt%d" % j, [64, 8, ML_H * ML_VW], BF16) for j in range(2)]
    gtb = [P.sb("m2_gt%d" % j, [64, 8, 16], F32) for j in range(2)]
    S = [P.sb("m2_S%d" % j, [128, ML_H, ML_VW], F32) for j in range(4)]
    Sb = [P.sb("m2_Sb%d" % j, [128, ML_H, ML_VW], BF16) for j in range(4)]
    NB = 3
    eg = [P.sb("m2_eg%d" % j, [64, 8], F32) for j in range(NB)]
    ege = [P.sb("m2_ege%d" % j, [128, 4], F32) for j in range(NB)]
    e1la = [P.sb("m2_e1la%d" % j, [64, 64], F32) for j in range(NB)]
    gm = [P.sb("m2_gm%d" % j, [64, 64], F32) for j in range(NB)]
    ptb = [P.sb("m2_pt%d" % j, [64, 64], BF16) for j in range(NB)]
    kd = [P.sb("m2_kd%d" % j, [64, 128], BF16) for j in range(NB)]
    o1 = [P.sb("m2_o1%d" % j, [64, ML_VW], F32) for j in range(NB)]
    den = [P.sb("m2_den%d" % j, [64, 1], F32) for j in range(NB)]
    ob = [P.sb("m2_ob%d" % j, [64, 1024], F32) for j in range(2)]
    nblk = 0
    n = 0
    nch = 0
    chains = [(b, d) for b in range(2) for d in range(2)]
    plans = {bd: chunk_plan(*bd) for bd in chains}
    for ci, bd in enumerate(chains):
        P.memset("pool", S[ci][:], 0.0)
        P.memset("pool", Sb[ci][:], 0.0)
    for step in range(5):
        for ci, (b, d) in enumerate(chains):
            t0, clist = plans[(b, d)][step]
            ntok = 64 * len(clist)
            j = nblk % 2
            nblk += 1
            tk = ("blk", t0)
            P.dma("sp", qkb[j][:, :, 0:ntok], View(QK.t[:, :, t0:t0 + ntok].rearrange("c p n -> p c n"), ("ml_QK", None)))
            P.dma("pool", ktb[j][:, 0:len(clist), :], View(KT.t[t0:t0 + ntok, :].rearrange("(c p) f -> p c f", p=64), ("ml_KT", None)))
            P.dma("sp", vtb[j][:, 0:len(clist), :], View(VT.t[t0:t0 + ntok, :].rearrange("(c p) f -> p c f", p=64), ("ml_VT", None)))
            P.dma("pool", gtb[j][:, 0:len(clist), :], View(GT.t[t0:t0 + ntok, :].rearrange("(c p) f -> p c f", p=64), ("ml_GT", None)))
            for c in clist:
                cs = slice(c * 64, (c + 1) * 64)
                la = gtb[j][:, c, d * 8 + 4:d * 8 + 8]
                ip = gtb[j][:, c, d * 8:d * 8 + 4]
                m = nch % NB
                nch += 1
                pg = pb[6 + nch % 2]
                P.mm(pg[0:64, 0:4], cmat(C, "U", d), la, start=True, stop=True)
                P.mm(pg[0:64, 4:8], cmat(C, "ST", d), la, start=True, stop=True)
                P.mm(pg[:, 8:12], C.ones64[:], la, start=True, stop=True)
                P.tt("dve", eg[m][:, 4:8], pg[0:64, 4:8], ip, ALU.add)
                P.act(eg[m][:, 4:8], eg[m][:, 4:8], AF.Exp)
                P.act(eg[m][:, 0:4], pg[0:64, 0:4], AF.Exp)
                P.act(ege[m][:], pg[:, 8:12], AF.Exp)
                obuf = ob[nch % 2]
                for h in range(ML_H):
                    u = n % NB
                    n += 1
                    P.ts("dve", e1la[u][:], cmat(C, "ST", d), la[:, h:h + 1] if False else gtb[j][:, c, d * 8 + 4 + h:d * 8 + 5 + h], None, op0=ALU.mult)
                    pl = pb[(n % 3) * 2]
                    P.mm(pl[0:64, 0:64], e1la[u][:], cmat(C, "U", d), start=True, stop=True)
                    P.act(gm[u][:], pl[0:64, 0:64], AF.Exp, bias=gtb[j][:, c, d * 8 + h:d * 8 + h + 1])
                    P.tt("pool", gm[u][:], gm[u][:], cmat(C, "U", d), ALU.mult)
                    P.mm(pl[0:64, 64:128], qkb[j][:, 4 + h, cs], qkb[j][:, h, cs], start=True, stop=True)
                    P.tt("dve", ptb[u][:], pl[0:64, 64:128], gm[u][:], ALU.mult)
                    P.ts("pool", kd[u][:], ktb[j][:, c, h * 128:(h + 1) * 128], eg[m][:, 4 + h:5 + h], None, op0=ALU.mult)
                    vh = vtb[j][:, c, h * ML_VW:(h + 1) * ML_VW]
                    po = pb[(n % 3) * 2 + 1]
                    P.mm(po[0:64, 0:ML_VW], qkb[j][:, h, cs], Sb[ci][:, h, :], start=True, stop=True)
                    P.act(o1[u][:], po[0:64, 0:ML_VW], AF.Identity, scale=eg[m][:, h:h + 1])
                    P.mm(pl[0:64, 128:128 + ML_VW], ptb[u][:], vh, start=True, stop=True)
                    P.tt("dve", o1[u][:], o1[u][:], pl[0:64, 128:128 + ML_VW], ALU.add)
                    P.act(den[u][:], o1[u][:, ML_DV:ML_VW], AF.Abs)
                    P.ts("dve", den[u][:], den[u][:], 1.0, None, op0=ALU.max)
                    P.recip(den[u][:], den[u][:])
                    P.ts("pool", obuf[:, h * ML_DV:(h + 1) * ML_DV], o1[u][:, 0:ML_DV], den[u][:], None, op0=ALU.mult)
                    P.mm(po[:, 0:ML_VW], kd[u][:], vh, start=True, stop=True)
                    P.stt("dve", S[ci][:, h, :], S[ci][:, h, :], ege[m][:, h:h + 1], po[:, 0:ML_VW], ALU.mult, ALU.add)
                    P.copy("act", Sb[ci][:, h, :], S[ci][:, h, :])
                tk0 = t0 + c * 64
                P.dma("sp" if nch % 2 else "pool", View(OD[d].t[tk0:tk0 + 64, :], ("ml_OD%d" % d, None)), obuf[:])
    P.pop()
    P.push()
    pb = [P.ps("m3_ps%d" % j) for j in range(8)]
    L = Ctx()
    L.mean = P.sb("m3_mean", [128, TT], F32)
    L.rstd = P.sb("m3_rstd", [128, TT], F32)
    L.sq = P.sb("m3_sq", [128, 8, TT], F32)
    Wo = P.sb("m3_Wo", [128, 8, 1024], BF16)
    stg = [P.sb("m3_stg%d" % j, [128, 8, 512], F32) for j in range(2)]
    load_w_bf16(P, Wo, IN("mlstm_w_out", [D, D]).t, 1024, stg)
    ng = P.sb("m3_ng", [128, 8], F32)
    P.dma("sp", ng[:], IN("ml_normg", [128, 8])[:, :])
    xt = P.sb("m3_x", [128, 8, TT], F32)
    yT = P.sb("m3_yT", [128, 8, TT], BF16)
    ogt = P.sb("m3_og", [128, 8, TT], BF16)
    oa = [P.sb("m3_oa%d" % j, [128, 1024], F32) for j in range(2)]
    obb = [P.sb("m3_ob%d" % j, [128, 1024], F32) for j in range(2)]
    st = [P.sb("m3_st%d" % j, [128, 8], F32) for j in range(2)]
    n = 0
    for t in tiles:
        seg = seg_of_tile(t)
        tok = slice(t * TT, (t + 1) * TT)
        P.dma("sp", xt[:], View(C.XS.t[:, :, tok].rearrange("c p n -> p c n"), ("XS", t)))
        P.dma("pool", ogt[:], View(OG.t[:, :, tok].rearrange("c p n -> p c n"), ("ml_OG", None)))
        for s in range(4):
            a = oa[s % 2]
            bb = obb[s % 2]
            sv = st[s % 2]
            r0 = t * TT + s * 128
            P.dma("sp", a[:], View(OD[0].t[r0:r0 + 128, :], ("ml_OD0", None)))
            P.dma("pool", bb[:], View(OD[1].t[r0:r0 + 128, :], ("ml_OD1", None)))
            P.tt("pool", a[:], a[:], bb[:], ALU.add)
            P.reduce("dve", sv[:, 0:4], View(a.t[:].rearrange("p (h k) -> p h k", k=ML_DV), a[:].key), ALU.add)
            P.ts("dve", sv[:, 0:4], sv[:, 0:4], -1.0 / ML_DV, None, op0=ALU.mult)
            for h in range(ML_H):
                hs = slice(h * ML_DV, (h + 1) * ML_DV)
                P.ts("dve" if h % 2 else "pool", a[:, hs], a[:, hs], sv[:, h:h + 1], None, op0=ALU.add)
                P.act(bb[:, hs], a[:, hs], AF.Square, accum_out=sv[:, 4 + h:5 + h])
            P.ts("dve", sv[:, 4:8], sv[:, 4:8], 1.0 / ML_DV, 1e-6, op0=ALU.mult, op1=ALU.add)
            P.act(sv[:, 4:8], sv[:, 4:8], AF.Sqrt)
            P.recip(sv[:, 4:8], sv[:, 4:8])
            for h in range(ML_H):
                hs = slice(h * ML_DV, (h + 1) * ML_DV)
                P.ts("dve" if h % 2 else "pool", a[:, hs], a[:, hs], sv[:, 4 + h:5 + h], None, op0=ALU.mult)
            for c in range(8):
                pt = pb[2 + n % 6]
                n += 1
                P.transpose(pt[:, 0:128], a[:, c * 128:(c + 1) * 128], C.ident[:])
                P.stt("dve", yT[:, c, s * 128:(s + 1) * 128], pt[:, 0:128], ng[:, c:c + 1], ogt[:, c, s * 128:(s + 1) * 128], ALU.mult, ALU.mult)
        out_proj_ln(P, C, L, i, t, xt, yT, Wo, pb)
    P.pop()


GD_H = 8
SEQS = [(0, 256, False), (256, 256, False), (512, 2048, True), (2560, 2048, True)]


def inv_unit_lower(P, C, Bm, W, pbX, pbY, pbP):
    X = [Bm] + W.x
    Y = W.y
    Pm = W.p
    P.tt("pool", Pm[:], Y[0][:], C.ident[0:64, 0:64], ALU.add)
    for k in range(1, 6):
        cx = ((k - 1) % 2) * 64
        P.mm(pbX[0:64, cx:cx + 64], Y[k - 1][:], X[k - 1][:], start=True, stop=True)
        if k < 5:
            P.mm(pbY[0:64, cx:cx + 64], X[k - 1][:], Y[k - 1][:], start=True, stop=True)
        P.copy("act", X[k][:], pbX[0:64, cx:cx + 64])
        if k < 5:
            P.copy("dve", Y[k][:], pbY[0:64, cx:cx + 64])
        P.mm(pbP[0:64, cx:cx + 64], X[k][:], Pm[:], start=True, stop=True)
        P.tt("dve", Pm[:], Pm[:], pbP[0:64, cx:cx + 64], ALU.add)
    return Pm


def phase_gdn(P, C, IN, i, tiles):
    ZR = P.dram("gd_ZR", [24, 128, NT], F32)
    GG = P.dram("gd_GG", [8, 128, NT], BF16)
    QK = P.dram("gd_QK", [16, 128, NT], BF16)
    KT = P.dram("gd_KT", [NT, 1024], BF16)
    VT = P.dram("gd_VT", [NT, 1024], BF16)
    GT = P.dram("gd_GT", [NT, 32], F32)
    OD = [P.dram("gd_OD%d" % d, [NT, 1024], F32) for d in range(2)]
    P.push()
    pb = [P.ps("g1_ps%d" % j) for j in range(6)]
    W = P.sb("g1_W", [128, 8, 4128], BF16)
    stg = [P.sb("g1_stg%d" % j, [128, 8, 512], F32) for j in range(2)]
    load_w_bf16(P, W, IN("gdn_w_in", [D, 4128]).t, 4128, stg)
    dtb = P.sb("g1_dtb", [128, 32], F32)
    nea = P.sb("g1_nea", [128, 32], F32)
    P.dma("sp", dtb[:], IN("gd_dtb", [128, 32])[:, :])
    P.dma("sp", nea[:], IN("gd_alog", [128, 32])[:, :])
    P.act(nea[:], nea[:], AF.Exp)
    P.ts("dve", nea[:], nea[:], -1.0, None, op0=ALU.mult)
    xt = P.sb("g1_x", [128, 8, TT], F32)
    hb = P.sb("g1_hb", [128, 8, TT], BF16)
    zt = [P.sb("g1_z%d" % j, [128, 4, TT], F32) for j in range(2)]
    gg = P.sb("g1_gg", [128, 8, TT], BF16)
    gt = P.sb("g1_gt", [128, 4, 32], F32)
    ge = P.sb("g1_ge", [128, 4, 32], F32)
    n = 0
    for t in tiles:
        tok = slice(t * TT, (t + 1) * TT)
        load_xh(P, C, i, t, xt, hb)
        for g4 in range(6):
            z = zt[g4 % 2]
            for cc in range(4):
                oc = g4 * 4 + cc
                ps = pb[n % 4]
                n += 1
                for kc in range(8):
                    P.mm(ps[:], W[:, kc, oc * 128:(oc + 1) * 128], hb[:, kc, :], start=(kc == 0), stop=(kc == 7))
                P.copy("act" if cc % 2 else "dve", z[:, cc, :], ps[:])
            P.dma("sp" if g4 % 2 else "pool", View(ZR.t[g4 * 4:(g4 + 1) * 4, :, tok].rearrange("c p n -> p c n"), ("gd_ZR", t)), z[:])
        for c in range(8):
            ps = pb[n % 4]
            n += 1
            for kc in range(8):
                P.mm(ps[:], W[:, kc, 3072 + c * 128:3072 + (c + 1) * 128], hb[:, kc, :], start=(kc == 0), stop=(kc == 7))
            P.act(gg[:, c, :], ps[:], AF.Silu)
        P.dma("pool", View(GG.t[:, :, tok].rearrange("c p n -> p c n"), ("gd_GG", t)), gg[:])
        for s in range(4):
            ps = pb[4 + s % 2]
            for kc in range(8):
                P.mm(ps[:, 0:32], hb[:, kc, s * 128:(s + 1) * 128], W[:, kc, 4096:4128], start=(kc == 0), stop=(kc == 7))
            P.tt("dve", gt[:, s, :], ps[:, 0:32], dtb[:], ALU.add)
        P.act(ge[:], gt[:], AF.Exp)
        P.act(ge[:], ge[:], AF.Ln, bias=1.0)
        P.act(gt[:], gt[:], AF.Sigmoid)
        for d in range(2):
            for s in range(4):
                P.tt("dve", gt[:, s, d * 16:d * 16 + 8], ge[:, s, d * 16:d * 16 + 8], nea[:, d * 16:d * 16 + 8], ALU.mult)
        P.dma("sp", View(GT.t[tok, :].rearrange("(s p) f -> p s f", p=128), ("gd_GT", t)), gt[:])
    P.pop()
    P.push()
    pb = [P.ps("g1b_ps%d" % j) for j in range(6)]
    pbt = [P.ps("g1b_pt%d" % j, [128, 1024], BF16) for j in range(2)]
    R = rope_setup(P, C, IN, pb[4:6])
    cw = P.sb("g1b_cw", [128, 24 * 3], F32)
    P.dma("sp", cw[:], IN("gd_conv", [128, 72])[:, :])
    zr = [P.sb("g1b_z%d" % j, [128, 2048], F32) for j in range(2)]
    yy = [P.sb("g1b_y%d" % j, [128, 2048], F32) for j in range(2)]
    sq = P.sb("g1b_sq", [128, 512], F32)
    rn = P.sb("g1b_rn", [128, 512], F32)
    yb = [P.sb("g1b_yb%d" % j, [128, 2048], BF16) for j in range(2)]
    tm = [P.sb("g1b_tm%d" % j, [128, 16, 128], BF16) for j in range(2)]
    epsq = P.sb("g1b_epsq", [128, 1], F32)
    epsk = P.sb("g1b_epsk", [128, 1], F32)
    P.memset("dve", epsq[:], 1e-6 * 128.0)
    P.memset("dve", epsk[:], 1e-6)
    n = 0
    for c in range(24):
        for (t0, ns, is_lat) in SEQS:
            j = n % 2
            n += 1
            z = zr[j]
            y = yy[j]
            P.dma("sp" if n % 2 else "pool", z[:, 0:ns], View(ZR.t[c, :, t0:t0 + ns], ("gd_ZR", None)))
            P.ts("dve", y[:, 0:ns], z[:, 0:ns], cw[:, c * 3 + 1:c * 3 + 2], None, op0=ALU.mult)
            P.stt("pool", y[:, 1:ns], z[:, 0:ns - 1], cw[:, c * 3:c * 3 + 1], y[:, 1:ns], ALU.mult, ALU.add)
            P.stt("dve", y[:, 0:ns - 1], z[:, 1:ns], cw[:, c * 3 + 2:c * 3 + 3], y[:, 0:ns - 1], ALU.mult, ALU.add)
            P.act(y[:, 0:ns], y[:, 0:ns], AF.Silu)
            ybj = yb[j]
            if c < 16:
                for s0 in range(0, ns, 512):
                    w = min(512, ns - s0)
                    sl = slice(s0, s0 + w)
                    P.act(sq[:, 0:w], y[:, sl], AF.Square)
                    ps = pb[n % 2]
                    P.mm(ps[:, 0:w], C.ones[:], sq[:, 0:w], start=True, stop=True)
                    if c < 8:
                        P.act(rn[:, 0:w], ps[:, 0:w], AF.Sqrt, bias=epsq[:], scale=128.0)
                    else:
                        P.act(rn[:, 0:w], ps[:, 0:w], AF.Sqrt, bias=epsk[:], scale=1.0)
                    P.recip(rn[:, 0:w], rn[:, 0:w])
                    if is_lat:
                        P.tt("dve", y[:, sl], y[:, sl], rn[:, 0:w], ALU.mult)
                        pr = pb[2 + (s0 // 512) % 2]
                        P.mm(pr[:, 0:w], R.rotT[:], y[:, sl], start=True, stop=True)
                        P.tt("pool", sq[:, 0:w], y[:, sl], R.cos[:, sl], ALU.mult)
                        P.tt("dve", rn[:, 0:w], pr[:, 0:w], R.sin[:, sl], ALU.mult)
                        P.tt("pool", ybj[:, sl], sq[:, 0:w], rn[:, 0:w], ALU.add)
                    else:
                        P.tt("dve", ybj[:, sl], y[:, sl], rn[:, 0:w], ALU.mult)
                P.dma("sp", View(QK.t[c, :, t0:t0 + ns], ("gd_QK", None)), ybj[:, 0:ns])
            else:
                P.copy("pool", ybj[:, 0:ns], y[:, 0:ns])
            if c >= 8:
                tmj = tm[j]
                for s in range(ns // 128):
                    pt = pbt[s % 2]
                    P.transpose(pt[:, 0:128], ybj[:, s * 128:(s + 1) * 128], C.identb[:])
                    P.copy("act" if s % 2 else "dve", tmj[:, s, :], pt[:, 0:128])
                dst = KT if c < 16 else VT
                hh = (c - 8) % 8
                P.dma("pool", View(dst.t[t0:t0 + ns, hh * 128:(hh + 1) * 128].rearrange("(s p) f -> p s f", p=128), (dst.name, None)),
                      tmj[:, 0:ns // 128, :])
    P.pop()
    P.push()
    B = [P.ps("g2_ps%d" % j) for j in range(8)]
    HG = 4
    cm4 = P.sb("g2_cm4", [64, 2, 1536], F32)
    P.dma("sp", cm4[:], IN("rw_cm4", [64, 2, 1536])[:, :, :])
    qkb = [P.sb("g2_qk%d" % j, [128, 16, TT], BF16) for j in range(2)]
    ktb = [P.sb("g2_kt%d" % j, [64, 8, 1024], BF16) for j in range(2)]
    vtb = [P.sb("g2_vt%d" % j, [64, 8, 1024], BF16) for j in range(2)]
    gtb = [P.sb("g2_gt%d" % j, [64, 8, 32], F32) for j in range(2)]
    S = [P.sb("g2_S%d" % j, [128, GD_H, 128], F32) for j in range(4)]
    Sb = [P.sb("g2_Sb%d" % j, [128, GD_H, 128], BF16) for j in range(4)]
    NB = 2
    eg = [P.sb("g2_eg%d" % j, [64, 32], F32) for j in range(NB)]
    ege = [P.sb("g2_ege%d" % j, [128, 8], F32) for j in range(NB)]
    ob = [P.sb("g2_ob%d" % j, [64, 1024], F32) for j in range(2)]
    shared = {}

    def tset(j):
        T = Ctx()
        f = lambda nm, shp=(64, HG, 64), dt=F32: P.sb("g2_%s%d" % (nm, j), list(shp), dt)
        T.ula, T.sla, T.gi, T.gj, T.A = f("ula"), f("sla"), f("gi"), f("gj"), f("A")
        if j == 0:
            T.x = [f("x%d" % k) for k in range(1, 6)]
            T.y = [f("y0")] + [f("y%d" % k) for k in range(1, 5)]
            shared["x"], shared["y"] = T.x, T.y
        else:
            T.x = shared["x"]
            T.y = [f("y0")] + shared["y"][1:]
        T.p = f("p")
        T.ttb = f("ttb", dt=BF16)
        T.ptb = f("ptb", dt=BF16)
        T.bv = f("bv", (64, HG, 128), BF16)
        T.bk = f("bk", (64, HG, 128), BF16)
        T.kd = f("kd", (64, HG, 128), BF16)
        T.u0 = f("u0", (64, HG, 128))
        T.wx = f("wx", (128, HG, 64), BF16)
        T.dl = f("dl", (64, HG, 128), BF16)
        T.o1 = f("o1", (64, HG, 128))
        T.stmp = f("stmp", (128, HG, 128))
        return T

    TS = [tset(j) for j in range(2)]
    chains = [(b, d) for b in range(2) for d in range(2)]
    plans = {bd: chunk_plan(*bd) for bd in chains}
    for ci in range(4):
        P.memset("pool", S[ci][:], 0.0)
        P.memset("pool", Sb[ci][:], 0.0)
    nblk = 0
    n = 0
    nch = 0

    def f2(v3):
        return View(v3.ap.rearrange("p a b -> p (a b)"), v3.key)

    def bc(v2, w):
        return View(v2.ap.rearrange("p (h o) -> p h o", o=1).to_broadcast([v2.ap.shape[0], HG, w]), v2.key)

    for step in range(5):
        for ci, (b, d) in enumerate(chains):
            t0, clist = plans[(b, d)][step]
            ntok = 64 * len(clist)
            nc_ = len(clist)
            j = nblk % 2
            nblk += 1
            U4 = cm4[:, d, 0:256]
            ST4 = cm4[:, d, 512:768]
            I4 = cm4[:, d, 1280:1536]
            Ud, STd = cmat(C, "U", d), cmat(C, "ST", d)
            P.dma("sp", qkb[j][:, :, 0:ntok], View(QK.t[:, :, t0:t0 + ntok].rearrange("c p n -> p c n"), ("gd_QK", None)))
            P.dma("pool", ktb[j][:, 0:nc_, :], View(KT.t[t0:t0 + ntok, :].rearrange("(c p) f -> p c f", p=64), ("gd_KT", None)))
            P.dma("sp", vtb[j][:, 0:nc_, :], View(VT.t[t0:t0 + ntok, :].rearrange("(c p) f -> p c f", p=64), ("gd_VT", None)))
            P.dma("pool", gtb[j][:, 0:nc_, :], View(GT.t[t0:t0 + ntok, :].rearrange("(c p) f -> p c f", p=64), ("gd_GT", None)))
            for c in clist:
                cs = slice(c * 64, (c + 1) * 64)
                la = gtb[j][:, c, d * 16:d * 16 + 8]
                be = gtb[j][:, c, d * 16 + 8:d * 16 + 16]
                m_ = nch % NB
                nch += 1
                pg = B[7]
                P.mm(pg[0:64, 448:456], Ud, la, start=True, stop=True)
                P.mm(pg[0:64, 456:464], STd, la, start=True, stop=True)
                P.mm(pg[:, 464:472], C.ones64[:], la, start=True, stop=True)
                P.act(eg[m_][:, 0:16], pg[0:64, 448:464], AF.Exp)
                P.act(ege[m_][:], pg[:, 464:472], AF.Exp)
                P.tt("dve", eg[m_][:, 16:24], eg[m_][:, 0:8], be, ALU.mult)
                P.ts("dve", eg[m_][:, 24:32], be, -1.0, None, op0=ALU.mult)
                obuf = ob[nch % 2]
                for g in range(GD_H // HG):
                    T = TS[n % 2]
                    n += 1
                    H = range(HG)
                    h0 = g * HG
                    lag = gtb[j][:, c, d * 16 + h0:d * 16 + h0 + HG]
                    P.tt("dve", T.ula[:], View(U4.ap.rearrange("p (h k) -> p h k", h=HG), U4.key), bc(lag, 64), ALU.mult)
                    P.tt("pool", T.sla[:], View(ST4.ap.rearrange("p (h k) -> p h k", h=HG), ST4.key), bc(lag, 64), ALU.mult)
                    for hh in H:
                        h = h0 + hh
                        kT = qkb[j][:, 8 + h, cs]
                        qT = qkb[j][:, h, cs]
                        P.mm(B[0][0:64, hh * 64:(hh + 1) * 64], T.ula[:, hh, :], STd, start=True, stop=True)
                        P.mm(B[0][0:64, 256 + hh * 64:256 + (hh + 1) * 64], T.sla[:, hh, :], Ud, start=True, stop=True)
                        P.mm(B[1][0:64, hh * 64:(hh + 1) * 64], kT, kT, start=True, stop=True)
                        P.mm(B[1][0:64, 256 + hh * 64:256 + (hh + 1) * 64], kT, qT, start=True, stop=True)
                    P.act(f2(T.gi[:]), B[0][0:64, 0:256], AF.Exp)
                    P.act(f2(T.gj[:]), B[0][0:64, 256:512], AF.Exp)
                    P.tt("pool", f2(T.gi[:]), f2(T.gi[:]), ST4, ALU.mult)
                    P.tt("pool", f2(T.gj[:]), f2(T.gj[:]), U4, ALU.mult)
                    P.tt("dve", f2(T.gi[:]), B[1][0:64, 0:256], f2(T.gi[:]), ALU.mult)
                    P.tt("dve", T.A[:], T.gi[:], bc(eg[m_][:, 24 + h0:24 + h0 + HG], 64), ALU.mult)
                    P.tt("dve", f2(T.ptb[:]), B[1][0:64, 256:512], f2(T.gj[:]), ALU.mult)
                    for hh in H:
                        P.transpose(B[2][0:64, hh * 64:(hh + 1) * 64], T.A[:, hh, :], C.ident[0:64, 0:64])
                    P.copy("act", f2(T.y[0][:]), B[2][0:64, 0:256])
                    X = [T.A] + T.x
                    Y = T.y
                    Pm = T.p
                    P.tt("pool", f2(Pm[:]), f2(Y[0][:]), I4, ALU.add)
                    for k in range(1, 6):
                        cx = ((k - 1) % 2) * 256
                        for hh in H:
                            P.mm(B[4][0:64, cx + hh * 64:cx + (hh + 1) * 64], Y[k - 1][:, hh, :], X[k - 1][:, hh, :], start=True, stop=True)
                        if k < 5:
                            for hh in H:
                                P.mm(B[5][0:64, cx + hh * 64:cx + (hh + 1) * 64], X[k - 1][:, hh, :], Y[k - 1][:, hh, :], start=True, stop=True)
                        P.copy("act", f2(X[k][:]), B[4][0:64, cx:cx + 256])
                        if k < 5:
                            P.copy("dve", f2(Y[k][:]), B[5][0:64, cx:cx + 256])
                        for hh in H:
                            P.mm(B[6][0:64, cx + hh * 64:cx + (hh + 1) * 64], X[k][:, hh, :], Pm[:, hh, :], start=True, stop=True)
                        P.tt("dve", f2(Pm[:]), f2(Pm[:]), B[6][0:64, cx:cx + 256], ALU.add)
                    P.copy("act", T.ttb[:], Pm[:])
                    k3 = View(ktb[j].t[:, c, h0 * 128:(h0 + HG) * 128].rearrange("p (h k) -> p h k", h=HG), ktb[j][:].key)
                    v3 = View(vtb[j].t[:, c, h0 * 128:(h0 + HG) * 128].rearrange("p (h k) -> p h k", h=HG), vtb[j][:].key)
                    P.tt("pool", T.bv[:], v3, bc(gtb[j][:, c, d * 16 + 8 + h0:d * 16 + 8 + h0 + HG], 128), ALU.mult)
                    P.tt("pool", T.bk[:], k3, bc(eg[m_][:, 16 + h0:16 + h0 + HG], 128), ALU.mult)
                    P.tt("pool", T.kd[:], k3, bc(eg[m_][:, 8 + h0:8 + h0 + HG], 128), ALU.mult)
                    for hh in H:
                        P.mm(B[3][0:64, hh * 128:(hh + 1) * 128], T.ttb[:, hh, :], T.bv[:, hh, :], start=True, stop=True)
                        P.mm(B[7][:, hh * 64:(hh + 1) * 64], T.bk[:, hh, :], T.ttb[:, hh, :], start=True, stop=True)
                    P.copy("dve", f2(T.u0[:]), B[3][0:64, :])
                    P.act(f2(T.wx[:]), B[7][:, 0:256], AF.Identity, scale=-1.0)
                    for hh in H:
                        P.mm(B[0][0:64, hh * 128:(hh + 1) * 128], T.wx[:, hh, :], Sb[ci][:, h0 + hh, :], start=True, stop=True)
                        P.mm(B[1][0:64, hh * 128:(hh + 1) * 128], qkb[j][:, h0 + hh, cs], Sb[ci][:, h0 + hh, :], start=True, stop=True)
                    P.tt("dve", f2(T.dl[:]), B[0][0:64, :], f2(T.u0[:]), ALU.add)
                    o13 = View(B[1].t[0:64, :].rearrange("p (h k) -> p h k", h=HG), B[1][:].key)
                    P.tt("dve", T.o1[:], o13, bc(eg[m_][:, h0:h0 + HG], 128), ALU.mult)
                    for hh in H:
                        P.mm(B[2][0:64, hh * 128:(hh + 1) * 128], T.ptb[:, hh, :], T.dl[:, hh, :], start=True, stop=True)
                        P.mm(B[3][:, hh * 128:(hh + 1) * 128], T.kd[:, hh, :], T.dl[:, hh, :], start=True, stop=True)
                    P.tt("dve", obuf[:, h0 * 128:(h0 + HG) * 128], f2(T.o1[:]), B[2][0:64, :], ALU.add)
                    Sg = S[ci][:, h0:h0 + HG, :]
                    P.tt("pool", T.stmp[:], Sg, bc(ege[m_][:, h0:h0 + HG], 128), ALU.mult)
                    P.tt("dve", f2(Sg), f2(T.stmp[:]), B[3][:, :], ALU.add)
                    P.copy("act", Sb[ci][:, h0:h0 + HG, :], Sg)
                tk0 = t0 + c * 64
                P.dma("sp" if nch % 2 else "pool", View(OD[d].t[tk0:tk0 + 64, :], ("gd_OD%d" % d, None)), obuf[:])
    P.pop()
    P.push()
    pb = [P.ps("g3_ps%d" % j) for j in range(8)]
    L = Ctx()
    L.mean = P.sb("g3_mean", [128, TT], F32)
    L.rstd = P.sb("g3_rstd", [128, TT], F32)
    L.sq = P.sb("g3_sq", [128, 8, TT], F32)
    Wo = P.sb("g3_Wo", [128, 8, 1024], BF16)
    stg = [P.sb("g3_stg%d" % j, [128, 8, 512], F32) for j in range(2)]
    load_w_bf16(P, Wo, IN("gdn_w_out", [D, D]).t, 1024, stg)
    ng = P.sb("g3_ng", [128, 1], F32)
    P.dma("sp", ng[:], IN("gd_normg", [128, 1])[:, :])
    xt = P.sb("g3_x", [128, 8, TT], F32)
    yT = P.sb("g3_yT", [128, 8, TT], BF16)
    ggt = P.sb("g3_gg", [128, 8, TT], BF16)
    oa = [P.sb("g3_oa%d" % j, [128, 1024], F32) for j in range(2)]
    obb = [P.sb("g3_ob%d" % j, [128, 1024], F32) for j in range(2)]
    st = [P.sb("g3_st%d" % j, [128, 8], F32) for j in range(2)]
    n = 0
    for t in tiles:
        tok = slice(t * TT, (t + 1) * TT)
        P.dma("sp", xt[:], View(C.XS.t[:, :, tok].rearrange("c p n -> p c n"), ("XS", t)))
        P.dma("pool", ggt[:], View(GG.t[:, :, tok].rearrange("c p n -> p c n"), ("gd_GG", None)))
        for s in range(4):
            a = oa[s % 2]
            bb = obb[s % 2]
            sv = st[s % 2]
            r0 = t * TT + s * 128
            P.dma("sp", a[:], View(OD[0].t[r0:r0 + 128, :], ("gd_OD0", None)))
            P.dma("pool", bb[:], View(OD[1].t[r0:r0 + 128, :], ("gd_OD1", None)))
            P.tt("pool", a[:], a[:], bb[:], ALU.add)
            for h in range(GD_H):
                hs = slice(h * 128, (h + 1) * 128)
                P.act(bb[:, hs], a[:, hs], AF.Square, accum_out=sv[:, h:h + 1])
            P.ts("dve", sv[:], sv[:], 1.0 / 128, 1e-6, op0=ALU.mult, op1=ALU.add)
            P.act(sv[:], sv[:], AF.Sqrt)
            P.recip(sv[:], sv[:])
            for h in range(GD_H):
                hs = slice(h * 128, (h + 1) * 128)
                P.ts("dve" if h % 2 else "pool", a[:, hs], a[:, hs], sv[:, h:h + 1], None, op0=ALU.mult)
            for c in range(8):
                pt = pb[2 + n % 6]
                n += 1
                P.transpose(pt[:, 0:128], a[:, c * 128:(c + 1) * 128], C.ident[:])
                P.stt("dve", yT[:, c, s * 128:(s + 1) * 128], pt[:, 0:128], ng[:, 0:1], ggt[:, c, s * 128:(s + 1) * 128], ALU.mult, ALU.mult)
        out_proj_ln(P, C, L, i, t, xt, yT, Wo, pb)
    P.pop()


RW_H = 16
RW_DEBUG = 0
RW_CUT = 0
HT = 128


def dma_heads_out(P, X, u0, n, src):
    for half in range(2):
        dv = X.t.rearrange("(c two) p n -> two c p n", two=2)[half][:, :, u0:u0 + n].rearrange("c p n -> p c n")
        sv = View(src.ap[half * 64:(half + 1) * 64], src.key)
        P.dma("sp" if half else "pool", View(dv, (X.name, None)), sv)


def phase_rwkv(P, C, IN, i, tiles):
    RF = P.dram("rw_RF", [16, 64, NT], F32)
    KK = P.dram("rw_KK", [16, 64, NT], F32)
    KD = [P.dram("rw_KD%d" % d, [16, 64, NT], F32) for d in range(2)]
    AT = [P.dram("rw_AT%d" % d, [16, 64, NT], F32) for d in range(2)]
    LW = [P.dram("rw_LW%d" % d, [NT, 1024], F32) for d in range(2)]
    VT = P.dram("rw_VT", [NT, 1024], BF16)
    GG = P.dram("rw_GG", [8, 128, NT], BF16)
    BN = P.dram("rw_BN", [8, 128, NT], F32)
    OD = [P.dram("rw_OD%d" % d, [NT, 1024], F32) for d in range(2)]
    P.push()
    pb = [P.ps("r1_ps%d" % j) for j in range(8)]
    Wr = [P.sb("r1_W%d" % j, [128, 8, 1024], BF16) for j in range(3)]
    g1w = P.sb("r1_g1w", [128, 8, 128], BF16)
    a1w = [P.sb("r1_a1w%d" % d, [128, 8, 64], BF16) for d in range(2)]
    w1w = [P.sb("r1_w1w%d" % d, [128, 8, 64], BF16) for d in range(2)]
    g2w = P.sb("r1_g2w", [128, 1024], BF16)
    a2w = [P.sb("r1_a2w%d" % d, [64, 1024], BF16) for d in range(2)]
    w2w = [P.sb("r1_w2w%d" % d, [64, 1024], BF16) for d in range(2)]
    P.push()
    stg = [P.sb("r1_stg%d" % j, [128, 8, 512], F32) for j in range(2)]
    for j in range(3):
        load_w_bf16(P, Wr[j], IN("rwkv_w_rkv", [3, D, D]).t[j], 1024, stg)
    load_w_bf16(P, g1w, IN("rwkv_g1", [D, 128]).t, 128, stg)
    for d in range(2):
        load_w_bf16(P, a1w[d], IN("rwkv_a1", [2, D, 64]).t[d], 64, stg)
        load_w_bf16(P, w1w[d], IN("rwkv_w1", [2, D, 64]).t[d], 64, stg)
    sflat = stg[0].t[:].rearrange("p a b -> p (a b)")
    skey = stg[0][:].key
    P.dma("sp", View(sflat[:, 0:1024], skey), IN("rwkv_g2", [128, D])[:, :])
    P.copy("dve", g2w[:], View(sflat[:, 0:1024], skey))
    for d in range(2):
        P.dma("sp", View(sflat[0:64, 0:1024], skey), IN("rwkv_a2", [2, 64, D])[d])
        P.copy("dve", a2w[d][:], View(sflat[0:64, 0:1024], skey))
        P.dma("sp", View(sflat[0:64, 0:1024], skey), IN("rwkv_w2", [2, 64, D])[d])
        P.copy("dve", w2w[d][:], View(sflat[0:64, 0:1024], skey))
    P.pop()
    w0r = P.sb("r1_w0", [128, 2, 1024], F32)
    P.dma("sp", w0r[:], IN("rw_w0", [128, 2, 1024])[:, :, :])
    cst = P.sb("r1_cst", [128, 13 * 8], F32)
    P.dma("sp", cst[:], IN("rw_cst", [128, 104])[:, :])
    P.ts("dve", cst[:, 64:72], cst[:, 56:64], -1.0, 1.0, op0=ALU.mult, op1=ALU.add)
    bones = P.sb("r1_bones", [128, 128], F32)
    P.dma("sp", bones[:], IN("rw_bones", [128, 128])[:, :])
    eps6 = P.sb("r1_eps6", [128, 1], F32)
    P.memset("dve", eps6[:], 1e-6)
    mhalf = P.sb("r1_mhalf", [128, 1], F32)
    P.memset("dve", mhalf[:], -0.5)
    HL = 64
    HW = HT + 2 * HL
    hx = P.sb("r1_hx", [128, 8, HW], F32)
    dx = P.sb("r1_dx", [128, 8, HT], F32)
    xm = [P.sb("r1_xm%d" % j, [128, 8, HT], BF16) for j in range(2)]
    kf = P.sb("r1_kf", [128, 8, HT], F32)
    vf = P.sb("r1_vf", [128, 8, HT], F32)
    rr = P.sb("r1_rr", [128, 8, HT], F32)
    kkf = P.sb("r1_kkf", [128, 8, HT], F32)
    of = [P.sb("r1_of%d" % j, [128, 8, HT], F32) for j in range(2)]
    ogb = P.sb("r1_ogb", [128, 8, HT], BF16)
    vtt = P.sb("r1_vtt", [128, HT // 128, 1024], BF16)
    lwt = [P.sb("r1_lwt%d" % j, [128, max(HT // 128, 1), 1024], F32) for j in range(2)]
    tmp = [P.sb("r1_tmp%d" % j, [128, 512], F32) for j in range(4)]
    tl = [P.sb("r1_tl%d" % j, [128, HT], BF16) for j in range(2)]
    n = 0
    nm = 0

    def mix(j):
        nonlocal nm
        x = xm[nm % 2]
        nm += 1
        for kc in range(8):
            P.stt("dve", x[:, kc, :], dx[:, kc, :], cst[:, j * 8 + kc:j * 8 + kc + 1], hx[:, kc, HL:HL + HT], ALU.mult, ALU.add)
        return x

    def proj_fm(x, Wt, oc, ncols=128, krows=128, kchunks=8):
        nonlocal n
        ps = pb[n % 6]
        n += 1
        for kc in range(kchunks):
            P.mm(ps[0:ncols, 0:HT], Wt[:, kc, oc * 128:oc * 128 + ncols], x[:, kc, :], start=(kc == 0), stop=(kc == kchunks - 1))
        return ps

    units = []
    for t in tiles:
        units += [(t, hf) for hf in range(TT // HT)]
    for (t, hf) in units:
        seg = seg_of_tile(t)
        u0 = t * TT + hf * HT
        sq0, sqn = [(a, b) for (a, b, _) in SEQS if a <= u0 < a + b][0]
        has_l, has_r = u0 > sq0, u0 + HT < sq0 + sqn
        P.dma("sp", hx[:, :, HL:HL + HT], View(C.XS.t[:, :, u0:u0 + HT].rearrange("c p n -> p c n"), ("XS", t)))
        if has_l:
            P.dma("pool", hx[:, :, 0:HL], View(C.XS.t[:, :, u0 - HL:u0].rearrange("c p n -> p c n"), ("XS", (u0 - 1) // TT)))
        if has_r:
            P.dma("pool", hx[:, :, HL + HT:HW], View(C.XS.t[:, :, u0 + HT:u0 + HT + HL].rearrange("c p n -> p c n"), ("XS", (u0 + HT) // TT)))
        for kc in range(8):
            c_sc = modcol(i, 1, kc, seg)
            c_sh = modcol(i, 0, kc, seg)
            P.ts("dve" if kc % 2 else "pool", hx[:, kc, HL - 1:HL + HT + 1], hx[:, kc, HL - 1:HL + HT + 1], C.mod[:, c_sc:c_sc + 1], C.mod[:, c_sh:c_sh + 1], op0=ALU.mult, op1=ALU.add)
        if not has_l:
            P.memset("pool", hx[:, :, HL - 1:HL], 0.0)
        if not has_r:
            P.memset("pool", hx[:, :, HL + HT:HL + HT + 1], 0.0)
        for kc in range(8):
            P.tt("pool", dx[:, kc, :], hx[:, kc, HL - 1:HL - 1 + HT], hx[:, kc, HL + 1:HL + 1 + HT], ALU.add)
            P.stt("dve", dx[:, kc, :], dx[:, kc, :], 0.5, hx[:, kc, HL:HL + HT], ALU.mult, ALU.subtract)
        x = mix(0)
        o = of[0]
        for c in range(8):
            ps = proj_fm(x, Wr[0], c)
            P.copy("act", o[:, c, :], ps[:, 0:HT])
            P.ts("pool", rr[:, c, :], o[:, c, :], cst[:, 72 + c:73 + c], None, op0=ALU.mult)
        dma_heads_out(P, RF, u0, HT, o[:])
        x = mix(2)
        o = of[1]
        for c in range(8):
            ps = proj_fm(x, Wr[1], c)
            P.copy("act", kf[:, c, :], ps[:, 0:HT])
            t1 = tmp[c % 2]
            P.ts("pool", t1[:, 0:HT], kf[:, c, :], cst[:, 48 + c:49 + c], None, op0=ALU.mult)
            t2 = tmp[2 + c % 2]
            P.act(t2[:, 0:HT], t1[:, 0:HT], AF.Square)
            pq = pb[6 + c % 2]
            P.mm(pq[:, 0:HT], bones[:], t2[:, 0:HT], start=True, stop=True)
            P.act(t2[:, 0:HT], pq[:, 0:HT], AF.Sqrt, bias=eps6[:])
            P.recip(t2[:, 0:HT], t2[:, 0:HT])
            P.tt("pool", kkf[:, c, :], t1[:, 0:HT], t2[:, 0:HT], ALU.mult)
        dma_heads_out(P, KK, u0, HT, kkf[:])
        x = mix(3)
        for c in range(8):
            ps = proj_fm(x, Wr[2], c)
            P.copy("act", vf[:, c, :], ps[:, 0:HT])
        for s in range(HT // 128):
            for blk in range(2):
                ps = pb[n % 6]
                n += 1
                for kc in range(8):
                    P.mm(ps[:], x[:, kc, s * 128:(s + 1) * 128], Wr[2][:, kc, blk * 512:(blk + 1) * 512], start=(kc == 0), stop=(kc == 7))
                P.copy("act" if blk else "dve", vtt[:, s, blk * 512:(blk + 1) * 512], ps[:])
        P.dma("sp", View(VT.t[u0:u0 + HT, :].rearrange("(s p) f -> p s f", p=128), ("rw_VT", None)), vtt[:])
        x = mix(5)
        ps = proj_fm(x, g1w, 0)
        P.act(tl[0][:], ps[:, 0:HT], AF.Sigmoid)
        for c in range(8):
            ps = pb[n % 6]
            n += 1
            P.mm(ps[:, 0:HT], g2w[:, c * 128:(c + 1) * 128], tl[0][:], start=True, stop=True)
            P.copy("act", ogb[:, c, :], ps[:, 0:HT])
        P.dma("pool", View(GG.t[:, :, u0:u0 + HT].rearrange("c p n -> p c n"), ("rw_GG", None)), ogb[:])
        xa = mix(4)
        kdo = [of[0], of[1]]
        for d in range(2):
            ps = proj_fm(xa, a1w[d], 0, ncols=64)
            P.copy("act", tl[d][0:64, :], ps[0:64, 0:HT])
        ato = lwt
        atv = [View(lwt[d].t[:].rearrange("p a b -> p (a b)")[:, 0:8 * HT].rearrange("p (c n) -> p c n", c=8), lwt[d][:].key) for d in range(2)]
        for c in range(8):
            pbn = pb[6 + c % 2]
            for d in range(2):
                ps = pb[n % 6]
                n += 1
                P.mm(ps[:, 0:HT], a2w[d][:, c * 128:(c + 1) * 128], tl[d][0:64, :], start=True, stop=True)
                af = tmp[d]
                P.act(af[:, 0:HT], ps[:, 0:HT], AF.Sigmoid, bias=cst[:, 80 + d * 8 + c:81 + d * 8 + c])
                P.tt("pool", View(atv[d].ap[:, c, :], atv[d].key), af[:, 0:HT], kkf[:, c, :], ALU.mult)
                P.ts("dve", af[:, 0:HT], af[:, 0:HT], cst[:, 56 + c:57 + c], cst[:, 64 + c:65 + c], op0=ALU.mult, op1=ALU.add)
                P.tt("dve", kdo[d][:, c, :], kf[:, c, :], af[:, 0:HT], ALU.mult)
                t2 = tmp[2 + d]
                P.tt("pool", t2[:, 0:HT], rr[:, c, :], kdo[d][:, c, :], ALU.mult)
                P.mm(pbn[:, 0:HT], bones[:], t2[:, 0:HT], start=(d == 0), stop=(d == 1))
            P.tt("dve", vf[:, c, :], vf[:, c, :], pbn[:, 0:HT], ALU.mult)
        for d in range(2):
            dma_heads_out(P, KD[d], u0, HT, kdo[d][:])
            dma_heads_out(P, AT[d], u0, HT, atv[d])
        P.dma("sp", View(BN.t[:, :, u0:u0 + HT].rearrange("c p n -> p c n"), ("rw_BN", None)), vf[:])
        xw = mix(1)
        for d in range(2):
            ps = proj_fm(xw, w1w[d], 0, ncols=64)
            P.act(tl[d][0:64, :], ps[0:64, 0:HT], AF.Tanh)
        for d in range(2):
            lo = lwt[d]
            for s in range(HT // 128):
                for blk in range(2):
                    ps = pb[n % 6]
                    n += 1
                    P.mm(ps[:], tl[d][0:64, s * 128:(s + 1) * 128], w2w[d][:, blk * 512:(blk + 1) * 512], start=True, stop=True)
                    sl = slice(blk * 512, (blk + 1) * 512)
                    tq = tmp[(s * 2 + blk) % 4]
                    P.tt("dve", tq[:], ps[:], w0r[:, d, sl], ALU.add)
                    P.act(tq[:], tq[:], AF.Exp, scale=-1.0)
                    P.act(tq[:], tq[:], AF.Ln, bias=1.0)
                    P.act(tq[:], tq[:], AF.Exp, scale=-1.0, bias=mhalf[:])
                    P.ts("pool", lo[:, s, sl], tq[:], -1.0, None, op0=ALU.mult)
            P.dma("sp" if d else "pool", View(LW[d].t[u0:u0 + HT, :].rearrange("(s p) f -> p s f", p=128), ("rw_LW%d" % d, None)), lo[:])
    P.pop()
    P.push()
    B = [P.ps("r2_ps%d" % j) for j in range(8)]
    HG = 4
    HW4 = HG * 64
    cm4 = P.sb("r2_cm4", [64, 2, 1536], F32)
    P.dma("sp", cm4[:], IN("rw_cm4", [64, 2, 1536])[:, :, :])
    fmb = [[P.sb("r2_fm%d_%d" % (k, j), [64, HG, TT], F32) for k in range(4)] for j in range(2)]
    lwb = [P.sb("r2_lw%d" % j, [64, 8, HW4], F32) for j in range(2)]
    vtb = [P.sb("r2_vt%d" % j, [64, 8, HW4], BF16) for j in range(2)]
    S = P.sb("r2_S", [64, 64, 64], F32)
    Sb = P.sb("r2_Sb", [64, 64, 64], BF16)
    for q in range(8):
        P.memset("pool", S[:, q * 8:(q + 1) * 8, :], 0.0)
        P.memset("dve", Sb[:, q * 8:(q + 1) * 8, :], 0.0)
    ob = [P.sb("r2_ob%d" % j, [64, 8, HW4], F32) for j in range(2)]

    shared = {}

    def tset(j):
        T = Ctx()
        f = lambda nm, shp=(64, HG, 64), dt=F32: P.sb("r2_%s%d" % (nm, j), list(shp), dt)
        T.cls, T.ecl, T.ece, T.encl, T.dec, T.A = f("cls"), f("ecl"), f("ece"), f("encl"), f("dec"), f("A")
        T.br = f("br", (64, HG, 2, 64), BF16)
        T.sk = f("sk", dt=BF16)
        T.sa = f("sa", dt=BF16)
        T.kdec = f("kdec")
        T.adec = f("adec")
        T.kdT = f("kdT", dt=BF16)
        T.nadT = f("nadT", dt=BF16)
        T.g23 = f("g23", (64, HG, 2, 64), BF16)
        T.ng4t = f("ng4t", dt=BF16)
        if j == 0:
            T.x = [f("x%d" % k) for k in range(1, 6)]
            T.y = [f("y0")] + [f("y%d" % k) for k in range(1, 5)]
            shared["x"], shared["y"] = T.x, T.y
        else:
            T.x = shared["x"]
            T.y = [f("y0")] + shared["y"][1:]
        T.p = f("p")
        T.ttb = f("ttb", dt=BF16)
        T.g2v = f("g2v")
        T.zz = f("zz", dt=BF16)
        T.ub = f("ub", dt=BF16)
        T.stmp = f("stmp")
        return T

    TS = [tset(j) for j in range(2)]
    chains = [(b, d) for b in range(2) for d in range(2)]
    plans = {bd: chunk_plan(*bd) for bd in chains}
    nblk = 0
    n = 0

    def f2(v3):
        return View(v3.ap.rearrange("p a b -> p (a b)"), v3.key)

    for step in range(5):
        for ci, (b, d) in enumerate(chains):
            t0, clist = plans[(b, d)][step]
            ntok = 64 * len(clist)
            nc_ = len(clist)
            last = 63 if d == 0 else 0
            U4 = cm4[:, d, 0:256]
            S4 = cm4[:, d, 256:512]
            ST4 = cm4[:, d, 512:768]
            SU4 = cm4[:, d, 768:1280]
            I4 = cm4[:, d, 1280:1536]
            Ud, Sd = cmat(C, "U", d), cmat(C, "S", d)
            for hg in range(RW_H // HG):
                j = nblk % 2
                nblk += 1
                hs = slice(hg * HG, (hg + 1) * HG)
                for k, src in enumerate((RF, KK, KD[d], AT[d])):
                    P.dma("sp" if k % 2 else "pool", fmb[j][k][:, :, 0:ntok], View(src.t[hs, :, t0:t0 + ntok].rearrange("h p n -> p h n"), (src.name, None)))
                P.dma("sp", lwb[j][:, 0:nc_, :], View(LW[d].t[t0:t0 + ntok, hg * HW4:(hg + 1) * HW4].rearrange("(c p) f -> p c f", p=64), ("rw_LW%d" % d, None)))
                P.dma("pool", vtb[j][:, 0:nc_, :], View(VT.t[t0:t0 + ntok, hg * HW4:(hg + 1) * HW4].rearrange("(c p) f -> p c f", p=64), ("rw_VT", None)))
                obuf = ob[j]
                si0 = ci * 16 + hg * HG
                for c in clist:
                    cs = slice(c * 64, (c + 1) * 64)
                    T = TS[n % 2]
                    n += 1
                    H = range(HG)
                    c64 = lambda hh: slice(hh * 64, (hh + 1) * 64)
                    rF, kkF, kdF, atF = [fmb[j][k][:, :, cs] for k in range(4)]
                    for hh in H:
                        lw = lwb[j][:, c, c64(hh)]
                        P.mm(B[0][0:64, hh * 64:(hh + 1) * 64], lw, Ud, start=True, stop=True)
                        P.mm(B[0][0:64, 256 + hh * 64:256 + (hh + 1) * 64], lw, Sd, start=True, stop=True)
                    P.copy("dve", f2(T.cls[:]), B[0][0:64, 0:256])
                    P.act(f2(T.ecl[:]), B[0][0:64, 0:256], AF.Exp)
                    P.act(f2(T.ece[:]), B[0][0:64, 256:512], AF.Exp)
                    P.act(f2(T.encl[:]), B[0][0:64, 0:256], AF.Exp, scale=-1.0)
                    lam = View(T.ecl.t[:, :, last:last + 1].to_broadcast([64, HG, 64]), T.ecl[:].key)
                    P.tt("dve", T.dec[:], T.encl[:], lam, ALU.mult)
                    P.tt("pool", T.br[:, :, 0, :], kkF, T.ece[:], ALU.mult)
                    P.tt("dve", T.br[:, :, 1, :], rF, T.ecl[:], ALU.mult)
                    P.tt("pool", T.sk[:], kdF, T.encl[:], ALU.mult)
                    P.tt("dve", T.sa[:], atF, T.encl[:], ALU.mult)
                    P.tt("pool", T.kdec[:], kdF, T.dec[:], ALU.mult)
                    P.tt("pool", T.adec[:], atF, T.dec[:], ALU.mult)
                    for hh in H:
                        P.transpose(B[7][0:64, hh * 64:(hh + 1) * 64], T.kdec[:, hh, :], C.ident[0:64, 0:64])
                        P.transpose(B[7][0:64, 256 + hh * 64:256 + (hh + 1) * 64], T.adec[:, hh, :], C.ident[0:64, 0:64])
                    P.copy("act", f2(T.kdT[:]), B[7][0:64, 0:256])
                    P.act(f2(T.nadT[:]), B[7][0:64, 256:512], AF.Identity, scale=-1.0)
                    for hh in H:
                        bt = T.br[:, hh, 0, :]
                        brh = View(T.br.t[:, hh].rearrange("p a b -> p (a b)"), T.br[:].key)
                        P.mm(B[1][0:64, hh * 64:(hh + 1) * 64], bt, T.sa[:, hh, :], start=True, stop=True)
                        P.mm(B[2][0:64, hh * 128:(hh + 1) * 128], T.sk[:, hh, :], brh, start=True, stop=True)
                        P.mm(B[3][0:64, hh * 128:(hh + 1) * 128], T.sa[:, hh, :], brh, start=True, stop=True)
                    b3v = B[3].t[0:64, :].rearrange("p (h t k) -> p h t k", h=HG, t=2)
                    P.stt("dve", f2(T.A[:]), B[1][0:64, 0:256], -1.0, ST4, ALU.mult, ALU.mult)
                    P.stt("dve", T.y[0][:], View(b3v[:, :, 0, :], B[3][:].key), -1.0, View(S4.ap.rearrange("p (h k) -> p h k", h=HG), S4.key), ALU.mult, ALU.mult)
                    P.tt("dve", View(T.g23.t[:].rearrange("p a b c -> p (a b c)"), T.g23[:].key), B[2][0:64, :], SU4, ALU.mult)
                    P.stt("dve", T.ng4t[:], View(b3v[:, :, 1, :], B[3][:].key), -1.0, View(U4.ap.rearrange("p (h k) -> p h k", h=HG), U4.key), ALU.mult, ALU.mult)
                    X = [T.A] + T.x
                    Y = T.y
                    Pm = T.p
                    P.tt("pool", f2(Pm[:]), f2(Y[0][:]), I4, ALU.add)
                    for k in range(1, 6):
                        cx = ((k - 1) % 2) * 256
                        for hh in H:
                            P.mm(B[4][0:64, cx + hh * 64:cx + (hh + 1) * 64], Y[k - 1][:, hh, :], X[k - 1][:, hh, :], start=True, stop=True)
                        if k < 5:
                            for hh in H:
                                P.mm(B[5][0:64, cx + hh * 64:cx + (hh + 1) * 64], X[k - 1][:, hh, :], Y[k - 1][:, hh, :], start=True, stop=True)
                        P.copy("act", f2(X[k][:]), B[4][0:64, cx:cx + 256])
                        if k < 5:
                            P.copy("dve", f2(Y[k][:]), B[5][0:64, cx:cx + 256])
                        for hh in H:
                            P.mm(B[6][0:64, cx + hh * 64:cx + (hh + 1) * 64], X[k][:, hh, :], Pm[:, hh, :], start=True, stop=True)
                        P.tt("dve", f2(Pm[:]), f2(Pm[:]), B[6][0:64, cx:cx + 256], ALU.add)
                    P.copy("act", T.ttb[:], Pm[:])
                    for hh in H:
                        P.mm(B[1][0:64, 256 + hh * 64:256 + (hh + 1) * 64], T.g23[:, hh, 0, :], vtb[j][:, c, c64(hh)], start=True, stop=True)
                    P.copy("act", f2(T.g2v[:]), B[1][0:64, 256:512])
                    for hh in H:
                        P.mm(B[0][0:64, hh * 64:(hh + 1) * 64], T.br[:, hh, 0, :], Sb[:, si0 + hh, :], start=True, stop=True)
                    P.tt("dve", f2(T.zz[:]), B[0][0:64, 0:256], f2(T.g2v[:]), ALU.add)
                    for hh in H:
                        P.mm(B[0][0:64, 256 + hh * 64:256 + (hh + 1) * 64], T.ttb[:, hh, :], T.zz[:, hh, :], start=True, stop=True)
                    P.copy("act", f2(T.ub[:]), B[0][0:64, 256:512])
                    for hh in H:
                        o_ = B[2][0:64, hh * 64:(hh + 1) * 64]
                        vh = vtb[j][:, c, c64(hh)]
                        P.mm(o_, T.br[:, hh, 1, :], Sb[:, si0 + hh, :], start=True, stop=False)
                        P.mm(o_, T.g23[:, hh, 1, :], vh, start=False, stop=False)
                        P.mm(o_, T.ng4t[:, hh, :], T.ub[:, hh, :], start=False, stop=True)
                    P.copy("act", obuf[:, c, :], B[2][0:64, 0:256])
                    for hh in H:
                        s_ = B[3][0:64, hh * 64:(hh + 1) * 64]
                        vh = vtb[j][:, c, c64(hh)]
                        P.mm(s_, T.kdT[:, hh, :], vh, start=True, stop=False)
                        P.mm(s_, T.nadT[:, hh, :], T.ub[:, hh, :], start=False, stop=True)
                    Sg = S[:, si0:si0 + HG, :]
                    P.tt("pool", T.stmp[:], Sg, lam, ALU.mult)
                    P.tt("dve", f2(Sg), f2(T.stmp[:]), B[3][0:64, 0:256], ALU.add)
                    P.copy("act", Sb[:, si0:si0 + HG, :], Sg)
                P.dma("sp", View(OD[d].t[t0:t0 + ntok, hg * HW4:(hg + 1) * HW4].rearrange("(c p) f -> p c f", p=64), ("rw_OD%d" % d, None)),
                      obuf[:, 0:nc_, :])
    P.pop()
    if RW_DEBUG in (2, 4):
        return
    P.push()
    pb = [P.ps("r3_ps%d" % j) for j in range(8)]
    L = Ctx()
    L.mean = P.sb("r3_mean", [128, TT], F32)
    L.rstd = P.sb("r3_rstd", [128, TT], F32)
    L.sq = P.sb("r3_sq", [128, 8, TT], F32)
    Wo = P.sb("r3_Wo", [128, 8, 1024], BF16)
    stg = [P.sb("r3_stg%d" % j, [128, 8, 512], F32) for j in range(2)]
    load_w_bf16(P, Wo, IN("rwkv_w_out", [D, D]).t, 1024, stg)
    lx = P.sb("r3_lx", [128, 16], F32)
    P.dma("sp", lx[:], IN("rw_lnx", [128, 16])[:, :])
    xt = P.sb("r3_x", [128, 8, TT], F32)
    yT = P.sb("r3_yT", [128, 8, TT], BF16)
    ggt = P.sb("r3_gg", [128, 8, TT], BF16)
    bnt = P.sb("r3_bn", [128, 8, TT], F32)
    oa = [P.sb("r3_oa%d" % j, [128, 1024], F32) for j in range(2)]
    obb = [P.sb("r3_ob%d" % j, [128, 1024], F32) for j in range(2)]
    st = [P.sb("r3_st%d" % j, [128, 32], F32) for j in range(2)]
    yf = [P.sb("r3_yf%d" % j, [128, 128], F32) for j in range(2)]
    n = 0
    for t in tiles:
        tok = slice(t * TT, (t + 1) * TT)
        P.dma("sp", xt[:], View(C.XS.t[:, :, tok].rearrange("c p n -> p c n"), ("XS", t)))
        P.dma("pool", ggt[:], View(GG.t[:, :, tok].rearrange("c p n -> p c n"), ("rw_GG", None)))
        P.dma("sp", bnt[:], View(BN.t[:, :, tok].rearrange("c p n -> p c n"), ("rw_BN", None)))
        for s in range(4):
            a = oa[s % 2]
            bb = obb[s % 2]
            sv = st[s % 2]
            r0 = t * TT + s * 128
            P.dma("sp", a[:], View(OD[0].t[r0:r0 + 128, :], ("rw_OD0", None)))
            P.dma("pool", bb[:], View(OD[1].t[r0:r0 + 128, :], ("rw_OD1", None)))
            P.tt("pool", a[:], a[:], bb[:], ALU.add)
            P.reduce("dve", sv[:, 0:16], View(a.t[:].rearrange("p (h k) -> p h k", k=64), a[:].key), ALU.add)
            P.ts("dve", sv[:, 0:16], sv[:, 0:16], -1.0 / 64, None, op0=ALU.mult)
            for h in range(RW_H):
                hs = slice(h * 64, (h + 1) * 64)
                P.ts("dve" if h % 2 else "pool", a[:, hs], a[:, hs], sv[:, h:h + 1], None, op0=ALU.add)
                P.act(bb[:, hs], a[:, hs], AF.Square, accum_out=sv[:, 16 + h:17 + h])
            P.ts("dve", sv[:, 16:32], sv[:, 16:32], 1.0 / 64, 64e-5, op0=ALU.mult, op1=ALU.add)
            P.act(sv[:, 16:32], sv[:, 16:32], AF.Sqrt)
            P.recip(sv[:, 16:32], sv[:, 16:32])
            for h in range(RW_H):
                hs = slice(h * 64, (h + 1) * 64)
                P.ts("dve" if h % 2 else "pool", a[:, hs], a[:, hs], sv[:, 16 + h:17 + h], None, op0=ALU.mult)
            for c in range(8):
                pt = pb[2 + n % 6]
                y = yf[n % 2]
                n += 1
                sl = slice(s * 128, (s + 1) * 128)
                P.transpose(pt[:, 0:128], a[:, c * 128:(c + 1) * 128], C.ident[:])
                P.act(y[:], pt[:, 0:128], AF.Identity, scale=lx[:, c:c + 1], bias=lx[:, 8 + c:9 + c])
                P.tt("pool", y[:], y[:], bnt[:, c, sl], ALU.add)
                P.tt("dve", yT[:, c, sl], y[:], ggt[:, c, sl], ALU.mult)
        out_proj_ln(P, C, L, i, t, xt, yT, Wo, pb)
    P.pop()


NA_H = 16


def na_geom(r):
    R0 = min(max(r - 4, 0), 23)
    ty = 0 if r == 0 else 1 if r == 2 else 3 if r == 28 else 4 if r == 30 else 2
    return ty, R0


def dma_heads_out_b(P, X, u0, n, src):
    for half in range(2):
        dv = X.t.rearrange("(c two) p n -> two c p n", two=2)[half][:, :, u0:u0 + n].rearrange("c p n -> p c n")
        sv = View(src.ap[half * 64:(half + 1) * 64], src.key)
        P.dma("sp" if half else "pool", View(dv, (X.name, None)), sv)


def phase_na(P, C, IN, i, tiles):
    QF = P.dram("na_QF", [16, 64, NT], BF16)
    KF = P.dram("na_KF", [16, 64, NT], BF16)
    VT = P.dram("na_VT", [NT, 1024], BF16)
    OT = P.dram("na_OT", [NT, 1024], F32)
    P.push()
    pb = [P.ps("n1_ps%d" % j) for j in range(6)]
    W = P.sb("n1_W", [128, 8, 3072], BF16)
    P.push()
    stg = [P.sb("n1_stg%d" % j, [128, 8, 512], F32) for j in range(2)]
    load_w_bf16(P, W, IN("na_w_in", [D, 3072]).t, 3072, stg)
    P.pop()
    xt = P.sb("n1_x", [128, 8, TT], F32)
    hb = P.sb("n1_hb", [128, 8, TT], BF16)
    qf = P.sb("n1_qf", [128, 8, TT], BF16)
    kfb = P.sb("n1_kf", [128, 8, TT], BF16)
    vt = P.sb("n1_vt", [128, 4, 1024], BF16)
    n = 0
    for t in tiles:
        tok = slice(t * TT, (t + 1) * TT)
        load_xh(P, C, i, t, xt, hb)
        for c in range(16):
            if c < 8 and t == 0:
                continue
            ps = pb[n % 6]
            n += 1
            for kc in range(8):
                P.mm(ps[:], W[:, kc, c * 128:(c + 1) * 128], hb[:, kc, :], start=(kc == 0), stop=(kc == 7))
            if c < 8:
                P.act(qf[:, c, :], ps[:], AF.Identity, scale=0.125)
            else:
                P.copy("dve", kfb[:, c - 8, :], ps[:])
        if t > 0:
            dma_heads_out_b(P, QF, t * TT, TT, qf[:])
        dma_heads_out_b(P, KF, t * TT, TT, kfb[:])
        for s in range(4):
            for blk in range(2):
                ps = pb[n % 6]
                n += 1
                for kc in range(8):
                    P.mm(ps[:], hb[:, kc, s * 128:(s + 1) * 128], W[:, kc, 2048 + blk * 512:2048 + (blk + 1) * 512], start=(kc == 0), stop=(kc == 7))
                P.copy("act" if blk else "dve", vt[:, s, blk * 512:(blk + 1) * 512], ps[:])
        P.dma("sp", View(VT.t[tok, :].rearrange("(s p) f -> p s f", p=128), ("na_VT", None)), vt[:])
    P.pop()
    P.push()
    ps1 = [P.ps("n2_s1%d" % j) for j in range(2)]
    ps2 = [P.ps("n2_s2%d" % j) for j in range(2)]
    pst = [P.ps("n2_pt%d" % j, [128, 1024], BF16) for j in range(2)]
    pso = [P.ps("n2_po%d" % j) for j in range(2)]
    bias = [P.sb("n2_bias%d" % j, [128, 5, 576], F32) for j in range(2)]
    qT = [P.sb("n2_q%d" % j, [64, 2048], BF16) for j in range(2)]
    kT = [P.sb("n2_k%d" % j, [64, 2304], BF16) for j in range(2)]
    va = [P.sb("n2_va%d" % j, [128, 16, 64], BF16) for j in range(2)]
    vb = [P.sb("n2_vb%d" % j, [128, 16, 64], BF16) for j in range(2)]
    vc = [P.sb("n2_vc%d" % j, [128, 2, 64], BF16) for j in range(2)]
    Sm = [P.sb("n2_S%d" % j, [128, 832], F32) for j in range(2)]
    Pm = [P.sb("n2_P%d" % j, [128, 896], BF16) for j in range(2)]
    PT = [P.sb("n2_PT%d" % j, [128, 7, 128], BF16) for j in range(2)]
    sm = [P.sb("n2_sm%d" % j, [128, 4], F32) for j in range(2)]
    oo = [P.sb("n2_o%d" % j, [128, 64], F32) for j in range(2)]
    for j in range(2):
        P.memset("pool", vb[j][:], 0.0)
    bsrc = IN("na_bias", [NA_H, 128, 5, 576])
    n = 0
    nhb = 0
    for h in range(NA_H):
        bj = bias[h % 2]
        P.dma("sp", bj[:], bsrc[h])
        for b in range(2):
            j = nhb % 2
            nhb += 1
            lat0 = 512 + 2048 * b
            ctx0 = 256 * b
            P.dma("sp", qT[j][:], View(QF.t[h, :, lat0:lat0 + 2048], ("na_QF", None)))
            P.dma("pool", kT[j][:, 0:2048], View(KF.t[h, :, lat0:lat0 + 2048], ("na_KF", None)))
            P.dma("pool", kT[j][:, 2048:2304], View(KF.t[h, :, ctx0:ctx0 + 256], ("na_KF", None)))
            P.dma("sp", va[j][:], View(VT.t[lat0:lat0 + 2048, h * 64:(h + 1) * 64].rearrange("(t p) f -> p t f", p=128), ("na_VT", None)))
            P.dma("pool", vb[j][:, 0:15, :], View(VT.t[lat0 + 64:lat0 + 64 + 1920, h * 64:(h + 1) * 64].rearrange("(t p) f -> p t f", p=128), ("na_VT", None)))
            P.dma("sp", vb[j][0:64, 15, :], View(VT.t[lat0 + 1984:lat0 + 2048, h * 64:(h + 1) * 64], ("na_VT", None)))
            P.dma("sp", vc[j][:], View(VT.t[ctx0:ctx0 + 256, h * 64:(h + 1) * 64].rearrange("(t p) f -> p t f", p=128), ("na_VT", None)))
            for r in range(0, 32, 2):
                ty, R0 = na_geom(r)
                u = n % 2
                n += 1
                q = qT[j][:, r * 64:r * 64 + 128]
                k0 = R0 * 64
                p1, p2 = ps1[u], ps2[u]
                P.mm(p1[:], q, kT[j][:, k0:k0 + 512], start=True, stop=True)
                P.mm(p2[:, 0:64], q, kT[j][:, k0 + 512:k0 + 576], start=True, stop=True)
                P.mm(p2[:, 64:320], q, kT[j][:, 2048:2304], start=True, stop=True)
                S = Sm[u]
                P.copy("act", S[:, 0:256], p2[:, 64:320])
                P.tt("dve", S[:, 256:768], p1[:], bj[:, ty, 0:512], ALU.add)
                P.tt("dve", S[:, 768:832], p2[:, 0:64], bj[:, ty, 512:576], ALU.add)
                st = sm[u]
                P.reduce("dve", st[:, 0:1], S[:], ALU.max)
                P.ts("dve", st[:, 0:1], st[:, 0:1], -1.0, None, op0=ALU.mult)
                Pb = Pm[u]
                P.act(Pb[:, 0:832], S[:], AF.Exp, bias=st[:, 0:1], accum_out=st[:, 1:2])
                P.recip(st[:, 2:3], st[:, 1:2])
                pt = pst[u]
                ptr = PT[u]
                for kt in range(7):
                    w = 128 if kt < 6 else 64
                    P.transpose(pt[0:w, kt * 128:(kt + 1) * 128], Pb[:, kt * 128:kt * 128 + w], C.identb[:])
                P.copy("act", ptr[:, 0:3, :], View(pt.t[:, 0:384].rearrange("p (a b) -> p a b", b=128), pt[:].key))
                P.copy("dve", ptr[:, 3:6, :], View(pt.t[:, 384:768].rearrange("p (a b) -> p a b", b=128), pt[:].key))
                P.copy("act", ptr[0:64, 6, :], pt[0:64, 768:896])
                po = pso[u]
                vx = va[j] if R0 % 2 == 0 else vb[j]
                t0 = R0 // 2
                P.mm(po[:, 0:64], ptr[:, 0, :], vc[j][:, 0, :], start=True, stop=False)
                P.mm(po[:, 0:64], ptr[:, 1, :], vc[j][:, 1, :], start=False, stop=False)
                for kt in range(4):
                    P.mm(po[:, 0:64], ptr[:, 2 + kt, :], vx[:, t0 + kt, :], start=False, stop=False)
                P.mm(po[:, 0:64], ptr[0:64, 6, :], vx[0:64, t0 + 4, :], start=False, stop=True)
                o = oo[u]
                P.act(o[:], po[:, 0:64], AF.Identity, scale=st[:, 2:3])
                q0 = lat0 + r * 64
                P.dma("sp" if n % 2 else "pool", View(OT.t[q0:q0 + 128, h * 64:(h + 1) * 64], ("na_OT", None)), o[:])
    P.pop()
    P.push()
    pb = [P.ps("n3_ps%d" % j) for j in range(8)]
    L = Ctx()
    L.mean = P.sb("n3_mean", [128, TT], F32)
    L.rstd = P.sb("n3_rstd", [128, TT], F32)
    L.sq = P.sb("n3_sq", [128, 8, TT], F32)
    Wo = P.sb("n3_Wo", [128, 8, 1024], BF16)
    stg = [P.sb("n3_stg%d" % j, [128, 8, 512], F32) for j in range(2)]
    load_w_bf16(P, Wo, IN("na_w_out", [D, D]).t, 1024, stg)
    xt = P.sb("n3_x", [128, 8, TT], F32)
    yT = P.sb("n3_yT", [128, 8, TT], BF16)
    oa = [P.sb("n3_oa%d" % j, [128, 1024], F32) for j in range(2)]
    n = 0
    for t in tiles:
        if t == 0:
            continue
        tok = slice(t * TT, (t + 1) * TT)
        P.dma("sp", xt[:], View(C.XS.t[:, :, tok].rearrange("c p n -> p c n"), ("XS", t)))
        for s in range(4):
            a = oa[s % 2]
            r0 = t * TT + s * 128
            P.dma("sp" if s % 2 else "pool", a[:], View(OT.t[r0:r0 + 128, :], ("na_OT", None)))
            for c in range(8):
                pt = pb[2 + n % 6]
                n += 1
                P.transpose(pt[:, 0:128], a[:, c * 128:(c + 1) * 128], C.ident[:])
                P.copy("act" if c % 2 else "dve", yT[:, c, s * 128:(s + 1) * 128], pt[:, 0:128])
        out_proj_ln(P, C, L, i, t, xt, yT, Wo, pb)
    P.pop()


class Inputs:
    def __init__(self, P):
        self.P = P
        self.d = {}

    def __call__(self, name, shape=None, dt=F32):
        if name not in self.d:
            self.d[name] = self.P.dram(name, SHAPES[name] if shape is None else shape, dt, kind="ExternalInput")
        return self.d[name]


SHAPES = {
    "xT": [8, 128, NT], "cT": [128, 8, 3], "adab": [128, DEPTH * 48 * 3], "lng": [128, DEPTH * 2 * 8], "lnb": [128, DEPTH * 2 * 8],
    "ident": [128, 128], "selE": [NE, NE * 128], "rb": [128, NE], "router_w": [D, NE],
}


def build(stages, tiles=None):
    nc = bass.Bass("TRN2", target_bir_lowering=False)
    P = Prog(nc)
    C = Ctx()
    IN = Inputs(P)
    OUT = P.dram("out", [8, 128, NT], F32, kind="ExternalOutput")
    C.XS = P.dram("XS", [8, 128, NT], F32)
    C.W1B = P.dram("W1B", [NE, 128, 8 * DE], BF16)
    C.W3B = P.dram("W3B", [NE, 128, 8 * DE], BF16)
    C.W2B = P.dram("W2B", [NE, 2, 128, 4 * 512], BF16)
    tiles = list(range(NTILE)) if tiles is None else tiles
    for c in range(8):
        P.dma("sp" if c % 2 else "pool", C.XS[c], IN("xT")[c])
    phase_consts(P, C, IN)
    mixer_consts(P, C, IN)
    layers = sorted(set(i for (_, i) in stages))
    phase_mod(P, C, IN, layers)
    for (kind, i) in stages:
        if kind == "moe":
            phase_wprep(P, C, IN, i)
            phase_moe(P, C, IN, i, tiles if i < DEPTH - 1 else [t for t in tiles if t > 0])
        elif kind == "mixer":
            if i % 4 == 0:
                phase_gdn(P, C, IN, i, tiles)
            elif i % 4 == 1:
                phase_mlstm(P, C, IN, i, tiles)
            elif i % 4 == 2:
                phase_rwkv(P, C, IN, i, tiles)
            else:
                phase_na(P, C, IN, i, tiles)
    P.pop()
    P.push()
    for c in range(8):
        P.dma("sp" if c % 2 else "pool", OUT[c], C.XS[c])
    P.emit()
    return nc, P, IN


def host_inputs(inputs, core):
    f = np.float32
    b0, b1 = 2 * core, 2 * core + 1
    x, ctx = inputs["x"], inputs["ctx"]
    tok = np.concatenate([ctx[b0], ctx[b1], x[b0], x[b1]], axis=0)
    m = {}
    m["xT"] = np.ascontiguousarray(tok.T.reshape(8, 128, NT)).astype(f)
    c3 = np.stack([inputs["c"][b0], inputs["c"][b1], inputs["c_ctx"]], axis=0)
    m["cT"] = np.ascontiguousarray(c3.reshape(3, 8, 128).transpose(2, 1, 0)).astype(f)
    return m


def rowrep(v, n=128):
    v = np.asarray(v, np.float32).reshape(1, -1)
    return np.ascontiguousarray(np.broadcast_to(v, (n, v.shape[1])))


def fm(v):
    v = np.asarray(v, np.float32)
    return np.ascontiguousarray(v.reshape(-1, 128).T)


def host_shared(inputs, names):
    f = np.float32
    m = {}
    ab = inputs["ada_b"].reshape(DEPTH, 48, 128).transpose(2, 0, 1)
    m["adab"] = np.ascontiguousarray(np.repeat(ab[..., None], 3, axis=-1).reshape(128, -1)).astype(f)
    m["lng"] = np.ascontiguousarray(inputs["ln_g"].reshape(DEPTH, 2, 8, 128).transpose(3, 0, 1, 2).reshape(128, -1)).astype(f)
    m["lnb"] = np.ascontiguousarray(inputs["ln_b"].reshape(DEPTH, 2, 8, 128).transpose(3, 0, 1, 2).reshape(128, -1)).astype(f)
    m["ident"] = np.eye(128, dtype=f)
    sel = np.zeros((NE, NE, 128), f)
    for e in range(NE):
        sel[e, e, :] = 1.0
    m["selE"] = sel.reshape(NE, NE * 128)
    m["rb"] = rowrep(inputs["router_b"])
    m["router_w"] = inputs["router_w"]
    for i in range(DEPTH):
        m["ada_w_%d" % i] = inputs["ada_w"][i]
        m["moe_w1_%d" % i] = inputs["moe_w1"][i]
        m["moe_w3_%d" % i] = inputs["moe_w3"][i]
        m["moe_w2_%d" % i] = inputs["moe_w2"][i]
    a = np.arange(64)
    Uf = (a[:, None] <= a[None, :]).astype(f)
    Sf = (a[:, None] < a[None, :]).astype(f)
    m["cmats"] = np.concatenate([Uf, Uf.T, Sf, Sf.T], axis=1)
    p = np.arange(128)
    inv = (10000.0 ** (-(p % 32).astype(np.float64) / 32.0))
    t = np.arange(2048)
    posv = np.where((p // 64)[:, None] == 0, (t // 64)[None, :], (t % 64)[None, :]).astype(np.float64)
    ang = posv * inv[:, None]
    m["ropecos"] = np.cos(ang).astype(f)
    m["ropesin"] = np.sin(ang).astype(f)
    R = np.zeros((128, 128), f)
    for mm_ in range(128):
        if (mm_ % 64) < 32:
            R[mm_, mm_ + 32] = -1.0
        else:
            R[mm_, mm_ - 32] = 1.0
    m["rotT"] = np.ascontiguousarray(R.T)
    m["mlstm_w_in"] = inputs["mlstm_w_in"]
    m["mlstm_w_out"] = inputs["mlstm_w_out"]
    m["ml_gateb"] = rowrep(inputs["mlstm_gate_b"].reshape(-1))
    m["ml_normg"] = fm(inputs["mlstm_norm_g"])
    m["gdn_w_in"] = inputs["gdn_w_in"]
    m["gdn_w_out"] = inputs["gdn_w_out"]
    dtb = np.zeros((2, 16), f)
    dtb[:, 0:8] = inputs["gdn_dt_bias"]
    m["gd_dtb"] = rowrep(dtb.reshape(-1))
    al = np.zeros((2, 16), f)
    al[:, 0:8] = inputs["gdn_a_log"]
    m["gd_alog"] = rowrep(al.reshape(-1))
    m["gd_conv"] = np.ascontiguousarray(inputs["gdn_conv"].T.reshape(24, 128, 3).transpose(1, 0, 2).reshape(128, 72))
    m["gd_normg"] = np.asarray(inputs["gdn_norm_g"], f).reshape(128, 1)
    for k in ("rwkv_w_rkv", "rwkv_g1", "rwkv_a1", "rwkv_w1", "rwkv_g2", "rwkv_a2", "rwkv_w2", "rwkv_w_out"):
        m[k] = inputs[k]
    m["rw_w0"] = np.ascontiguousarray(np.broadcast_to(np.asarray(inputs["rwkv_w0"], f)[None], (128, 2, 1024)))
    cst = [fm(inputs["rwkv_mu"][j]) for j in range(6)]
    cst += [fm(inputs["rwkv_k_k"]), fm(inputs["rwkv_k_a"]), np.zeros((128, 8), f), fm(inputs["rwkv_r_k"].reshape(-1))]
    cst += [fm(inputs["rwkv_a0"][0]), fm(inputs["rwkv_a0"][1]), np.zeros((128, 8), f)]
    m["rw_cst"] = np.concatenate(cst, axis=1)
    bo = np.zeros((128, 128), f)
    bo[0:64, 0:64] = 1.0
    bo[64:128, 64:128] = 1.0
    m["rw_bones"] = bo
    m["rw_lnx"] = np.concatenate([fm(inputs["rwkv_lnx_g"]), fm(inputs["rwkv_lnx_b"])], axis=1)
    cm4 = np.zeros((64, 2, 1536), f)
    for d_ in range(2):
        U_ = Uf if d_ == 0 else Uf.T
        S_ = Sf if d_ == 0 else Sf.T
        cm4[:, d_, 0:256] = np.tile(U_, (1, 4))
        cm4[:, d_, 256:512] = np.tile(S_, (1, 4))
        cm4[:, d_, 512:768] = np.tile(S_.T, (1, 4))
        cm4[:, d_, 768:1280] = np.tile(np.concatenate([S_, U_], axis=1), (1, 4))
        cm4[:, d_, 1280:1536] = np.tile(np.eye(64, dtype=f), (1, 4))
    m["rw_cm4"] = cm4
    m["na_w_in"] = inputs["na_w_in"]
    m["na_w_out"] = inputs["na_w_out"]
    if "na_bias" in names:
        rpb = np.asarray(inputs["na_rpb"], f)
        bt = np.full((NA_H, 128, 5, 576), -30000.0, f)
        w = np.arange(64)
        c0 = np.clip(w - 8, 0, 48)
        for ty, (r, R0, r00, r01) in enumerate([(0, 0, 0, 0), (2, 0, 0, 0), (6, 2, 2, 3), (28, 23, 24, 24), (30, 23, 24, 24)]):
            for dq, r0q in ((0, r00), (1, r01)):
                qrow = r + dq
                for kr in range(9):
                    krow = R0 + kr
                    if not (r0q <= krow < r0q + 8):
                        continue
                    dr = krow - qrow + 7
                    wp = np.arange(64)
                    valid = (wp[None, :] >= c0[:, None]) & (wp[None, :] < c0[:, None] + 16)
                    dc = np.clip(wp[None, :] - w[:, None] + 15, 0, 30)
                    vals = rpb[:, dr, :][:, dc]
                    blk = np.where(valid[None], vals, f(-30000.0))
                    bt[:, dq * 64:(dq + 1) * 64, ty, kr * 64:(kr + 1) * 64] = blk
        m["na_bias"] = bt
    return {k: np.ascontiguousarray(np.asarray(v, f)) for k, v in m.items() if k in names}


def unpack_out(res_core):
    o = res_core["out"].reshape(D, NT).T
    return o[512:512 + 2048], o[512 + 2048:], o[0:256], o[256:512]


ALL_STAGES = [(k, i) for i in range(DEPTH) for k in ("mixer", "moe")]


def kernel(**inputs):
    inputs = {k: np.asarray(v) for k, v in inputs.items()}
    nc, _, IN = build(ALL_STAGES)
    shared = host_shared(inputs, set(IN.d.keys()))
    in_maps = []
    for core in range(NCORES):
        m = dict(shared)
        m.update(host_inputs(inputs, core))
        in_maps.append(m)
    res = run_bass_kernel_spmd(nc, in_maps, core_ids=list(range(NCORES)))
    out = np.zeros((16, 2048, D), np.float32)
    for core in range(NCORES):
        l0, l1, _, _ = unpack_out(res.results[core])
        out[2 * core] = l0
        out[2 * core + 1] = l1
    return out
```
